# Optimizing a Trainium2 kernel written in Bass

```python
import jax
import jax.numpy as jnp
from jax import lax
import numpy as np


D_MODEL = 1024
BATCH = 8
SEQ = 4096
DEPTH = 2

CHUNK = 64
NORM_EPS = 1e-6
GLA_HEADS = 4
GLA_DK = 64
GLA_DV = 128
GLA_GATE_RANK = 16
GLA_GATE_TAU = 16.0
GDN_HEADS = 4
GDN_DK = 128
GDN_DV = 128
GDN_CONV = 4
SSD_HEADS = 8
SSD_HEAD_DIM = 64
SSD_GROUPS = 2
SSD_STATE = 64
SSD_CONV = 4
SSD_INNER = SSD_HEADS * SSD_HEAD_DIM
N_BRANCH = 3
BRANCH_WIDTH = 512
N_EXPERTS = 32
TOP_K = 4
D_FF_EXPERT = D_MODEL
SWIGLU_LIMIT = 7.0
SWIGLU_ALPHA = 1.702
MOE_BLOCK = 256

IN_SPLITS = (
    GLA_HEADS * GLA_DK,
    GLA_HEADS * GLA_DK,
    GLA_HEADS * GLA_DV,
    GLA_GATE_RANK,
    GLA_HEADS * GLA_DV,
    GDN_HEADS * (2 * GDN_DK + GDN_DV),
    GDN_HEADS,
    GDN_HEADS,
    GDN_HEADS * GDN_DV,
    SSD_INNER,
    SSD_INNER + 2 * SSD_GROUPS * SSD_STATE,
    SSD_HEADS,
    N_BRANCH * D_MODEL,
)
IN_COLS = sum(IN_SPLITS)

kernel_name = 'hybrid_gla_gdn_ssd_moe_adaln'


def _split_points():
    pts, acc = [], 0
    for s in IN_SPLITS[:-1]:
        acc += s
        pts.append(acc)
    return pts


def _rms(x):
    xf = x.astype(jnp.float32)
    return xf * lax.rsqrt(jnp.mean(xf * xf, axis=-1, keepdims=True) + NORM_EPS)


def rmsnorm(x, g):
    return (_rms(x) * g.astype(jnp.float32)).astype(x.dtype)


def l2norm(x):
    return x * lax.rsqrt(jnp.sum(x * x, axis=-1, keepdims=True) + 1e-6)


def causal_dwconv(u, w, b=None):
    k = w.shape[0]
    ch = u.shape[-1]
    out = lax.conv_general_dilated(
        u, w.astype(u.dtype)[:, None, :], window_strides=(1,), padding=[(k - 1, 0)],
        dimension_numbers=('NWC', 'WIO', 'NWC'), feature_group_count=ch)
    if b is not None:
        out = out + b.astype(u.dtype)
    return out


def to_heads(u, n_heads):
    bsz, seq, width = u.shape
    return u.reshape(bsz, seq // CHUNK, CHUNK, n_heads, width // n_heads).transpose(0, 3, 1, 2, 4)


def heads_scalar(u):
    bsz, seq, nh = u.shape
    return u.reshape(bsz, seq // CHUNK, CHUNK, nh).transpose(0, 3, 1, 2)


def from_heads(o):
    bsz, nh, nc, cl, d = o.shape
    return o.transpose(0, 2, 3, 1, 4).reshape(bsz, nc * cl, nh * d)


def chunk_masks():
    ones = jnp.ones((CHUNK, CHUNK), dtype=bool)
    return jnp.tril(ones), jnp.tril(ones, k=-1)


def masked_decay(cum, mask):
    diff = cum[..., :, None] - cum[..., None, :]
    return jnp.where(mask, jnp.exp(jnp.where(mask, diff, 0.0)), 0.0)


def inter_chunk_scan(decay, update, axis):
    d = jnp.moveaxis(decay, axis, 0)
    u = jnp.moveaxis(update, axis, 0)

    def step(s, du):
        dn, un = du
        return dn * s + un, s

    _, s_in = lax.scan(step, jnp.zeros_like(u[0]), (d, u))
    return jnp.moveaxis(s_in, 0, axis)


def gla_mixer(q, k, v, lr, r, w_gate2, b_gate2, norm_g):
    incl, _ = chunk_masks()
    gk = jax.nn.log_sigmoid(lr @ w_gate2.astype(jnp.float32) + b_gate2.astype(jnp.float32)) / GLA_GATE_TAU
    q = to_heads(q, GLA_HEADS) * (GLA_DK ** -0.5)
    k = to_heads(k, GLA_HEADS)
    v = to_heads(v, GLA_HEADS)
    gk = to_heads(gk, GLA_HEADS)
    b = jnp.cumsum(gk, axis=3)
    b_last = b[..., -1:, :]
    q_dec = q * jnp.exp(b)
    att = jnp.einsum('bhnld,bhnmd->bhnlm', q_dec, k * jnp.exp(-b))
    o = jnp.einsum('bhnlm,bhnme->bhnle', jnp.where(incl, att, 0.0), v)
    upd = jnp.einsum('bhnld,bhnle->bhnde', k * jnp.exp(b_last - b), v)
    s_in = inter_chunk_scan(jnp.exp(b_last[..., 0, :])[..., None], upd, axis=2)
    o = o + jnp.einsum('bhnld,bhnde->bhnle', q_dec, s_in)
    o = _rms(o) * norm_g.astype(jnp.float32)
    return from_heads(o) * jax.nn.silu(r)


def gdn_mixer(qkv, a_raw, b_raw, g_raw, conv_w, a_log, dt_bias, norm_g):
    incl, strict = chunk_masks()
    qkv = jax.nn.silu(causal_dwconv(qkv, conv_w))
    q, k, v = jnp.split(qkv, [GDN_HEADS * GDN_DK, 2 * GDN_HEADS * GDN_DK], axis=-1)
    q = l2norm(to_heads(q, GDN_HEADS)) * (GDN_DK ** -0.5)
    k = l2norm(to_heads(k, GDN_HEADS))
    v = to_heads(v, GDN_HEADS)
    beta = heads_scalar(jax.nn.sigmoid(b_raw))
    g = heads_scalar(-jnp.exp(a_log.astype(jnp.float32)) * jax.nn.softplus(a_raw + dt_bias.astype(jnp.float32)))
    cum = jnp.cumsum(g, axis=-1)
    gam = masked_decay(cum, incl)
    a_mat = jnp.where(strict, beta[..., None] * jnp.einsum('bhnld,bhnmd->bhnlm', k, k) * gam, 0.0)
    rhs = jnp.concatenate([beta[..., None] * v, (beta * jnp.exp(cum))[..., None] * k], axis=-1)
    sol = lax.linalg.triangular_solve(a_mat + jnp.eye(CHUNK, dtype=jnp.float32), rhs,
                                      left_side=True, lower=True, unit_diagonal=True)
    u_pre, w_mix = sol[..., :GDN_DV], sol[..., GDN_DV:]
    p_mat = jnp.einsum('bhnld,bhnmd->bhnlm', q, k) * gam
    q_dec = q * jnp.exp(cum)[..., None]
    k_dec = k * jnp.exp(cum[..., -1:] - cum)[..., None]
    chunk_decay = jnp.exp(cum[..., -1])

    def step(m, inp):
        u_pre_n, w_n, p_n, q_n, k_n, d_n = inp
        u_n = u_pre_n - jnp.einsum('bhld,bhde->bhle', w_n, m)
        o_n = jnp.einsum('bhld,bhde->bhle', q_n, m) + jnp.einsum('bhlm,bhme->bhle', p_n, u_n)
        m = d_n[..., None, None] * m + jnp.einsum('bhld,bhle->bhde', k_n, u_n)
        return m, o_n

    xs = tuple(jnp.moveaxis(t, 2, 0) for t in (u_pre, w_mix, p_mat, q_dec, k_dec, chunk_decay))
    m0 = jnp.zeros((qkv.shape[0], GDN_HEADS, GDN_DK, GDN_DV), jnp.float32)
    _, o = lax.scan(step, m0, xs)
    o = jnp.moveaxis(o, 0, 2)
    o = _rms(o) * norm_g.astype(jnp.float32)
    return from_heads(o) * jax.nn.silu(g_raw)


def ssd_mixer(z, xbc, dt_raw, conv_w, conv_b, a_log, dt_bias, d_skip, norm_g):
    incl, _ = chunk_masks()
    bsz, seq, _ = z.shape
    nc = seq // CHUNK
    hg = SSD_HEADS // SSD_GROUPS
    xbc = jax.nn.silu(causal_dwconv(xbc, conv_w, conv_b))
    xs, bm, cm = jnp.split(xbc, [SSD_INNER, SSD_INNER + SSD_GROUPS * SSD_STATE], axis=-1)
    x = xs.reshape(bsz, nc, CHUNK, SSD_GROUPS, hg, SSD_HEAD_DIM)
    bm = bm.reshape(bsz, nc, CHUNK, SSD_GROUPS, SSD_STATE)
    cm = cm.reshape(bsz, nc, CHUNK, SSD_GROUPS, SSD_STATE)
    dt = jax.nn.softplus(dt_raw + dt_bias.astype(jnp.float32)).reshape(bsz, nc, CHUNK, SSD_GROUPS, hg)
    a_head = -jnp.exp(a_log.astype(jnp.float32)).reshape(SSD_GROUPS, hg)
    cum = jnp.cumsum((dt * a_head).transpose(0, 1, 3, 4, 2), axis=-1)
    seg = masked_decay(cum, incl)
    xdt = x * dt[..., None]
    cb = jnp.einsum('bclgn,bcsgn->bcgls', cm, bm)
    y = jnp.einsum('bcgls,bcghls,bcsghp->bclghp', cb, seg, xdt)
    states = jnp.einsum('bcsgn,bcghs,bcsghp->bcghnp', bm, jnp.exp(cum[..., -1:] - cum), xdt)
    s_in = inter_chunk_scan(jnp.exp(cum[..., -1])[..., None, None], states, axis=1)
    y = y + jnp.einsum('bclgn,bcghnp,bcghl->bclghp', cm, s_in, jnp.exp(cum))
    y = y + d_skip.astype(jnp.float32).reshape(SSD_GROUPS, hg)[:, :, None] * x
    y = y.reshape(bsz, seq, SSD_INNER) * jax.nn.silu(z)
    y = _rms(y.reshape(bsz, seq, SSD_GROUPS, SSD_INNER // SSD_GROUPS)).reshape(bsz, seq, SSD_INNER)
    return y * norm_g.astype(jnp.float32)


def hybrid_mixer(h, w_in, gla_w_gate2, gla_b_gate2, gla_norm, gdn_conv_w, gdn_a_log, gdn_dt_bias, gdn_norm,
                 ssd_conv_w, ssd_conv_b, ssd_a_log, ssd_dt_bias, ssd_d, ssd_norm,
                 w_branch_gla, w_branch_gdn, w_branch_ssd, b_merge, w_out):
    proj = (h @ w_in).astype(jnp.float32)
    (gla_q, gla_k, gla_v, gla_lr, gla_r, gdn_qkv, gdn_a, gdn_b, gdn_g,
     ssd_z, ssd_xbc, ssd_dt, merge_raw) = jnp.split(proj, _split_points(), axis=-1)
    y_gla = gla_mixer(gla_q, gla_k, gla_v, gla_lr, gla_r, gla_w_gate2, gla_b_gate2, gla_norm).astype(h.dtype) @ w_branch_gla
    y_gdn = gdn_mixer(gdn_qkv, gdn_a, gdn_b, gdn_g, gdn_conv_w, gdn_a_log, gdn_dt_bias, gdn_norm).astype(h.dtype) @ w_branch_gdn
    y_ssd = ssd_mixer(ssd_z, ssd_xbc, ssd_dt, ssd_conv_w, ssd_conv_b, ssd_a_log, ssd_dt_bias, ssd_d, ssd_norm).astype(h.dtype) @ w_branch_ssd
    g_gla, g_gdn, g_ssd = jnp.split(jax.nn.sigmoid(merge_raw + b_merge.astype(jnp.float32)), N_BRANCH, axis=-1)
    merged = g_gla * y_gla + g_gdn * y_gdn + g_ssd * y_ssd
    return merged.astype(h.dtype) @ w_out


def moe_ffn(h, w_router, b_router, w_gate_up, b_gate_up, w_down, b_down):
    bsz, seq, dm = h.shape
    t = h.reshape(-1, dm)
    n_tok = t.shape[0]
    logits = (t @ w_router).astype(jnp.float32) + b_router.astype(jnp.float32)
    top_val, top_idx = lax.top_k(logits, TOP_K)
    gates = jax.nn.softmax(top_val, axis=-1)
    n_assign = n_tok * TOP_K
    flat_e = top_idx.reshape(-1)
    flat_tok = jnp.arange(n_assign, dtype=jnp.int32) // TOP_K
    order = jnp.argsort(flat_e)
    se = flat_e[order]
    counts = jnp.bincount(flat_e, length=N_EXPERTS)
    padded = ((counts + MOE_BLOCK - 1) // MOE_BLOCK) * MOE_BLOCK
    pad_end = jnp.cumsum(padded)
    pad_start = pad_end - padded
    start = jnp.cumsum(counts) - counts
    pos = jnp.arange(n_assign, dtype=jnp.int32) - start[se] + pad_start[se]
    n_blocks = -(-n_assign // MOE_BLOCK) + N_EXPERTS
    n_rows = n_blocks * MOE_BLOCK
    row_tok = jnp.zeros((n_rows,), jnp.int32).at[pos].set(flat_tok[order])
    row_gate = jnp.zeros((n_rows,), jnp.float32).at[pos].set(gates.reshape(-1)[order])
    block_e = jnp.minimum(jnp.searchsorted(pad_end, jnp.arange(n_blocks) * MOE_BLOCK, side='right'), N_EXPERTS - 1)
    xs = t[row_tok].reshape(n_blocks, MOE_BLOCK, dm)

    def expert_block(args):
        xb, e = args
        gu = xb @ w_gate_up[e] + b_gate_up[e]
        gate = jnp.minimum(gu[..., 0::2], SWIGLU_LIMIT)
        up = jnp.clip(gu[..., 1::2], -SWIGLU_LIMIT, SWIGLU_LIMIT)
        act = (up + 1.0) * gate * jax.nn.sigmoid(SWIGLU_ALPHA * gate)
        return act @ w_down[e] + b_down[e]

    ys = lax.map(expert_block, (xs, block_e)).reshape(n_rows, dm)
    y = jax.ops.segment_sum(ys.astype(jnp.float32) * row_gate[:, None], row_tok, num_segments=n_tok)
    return y.reshape(bsz, seq, dm).astype(h.dtype)


def setup_inputs(seed: int = 0) -> dict:
    key = jax.random.key(seed)
    keys = iter(jax.random.split(key, 48))

    def nrm(shape, scale):
        return jax.random.normal(next(keys), shape, jnp.float32) * scale

    def gain(shape):
        return 1.0 + nrm(shape, 0.02)

    def dt_bias_init(n):
        lo, hi = np.log(1e-3), np.log(1e-1)
        dt = jnp.exp(jax.random.uniform(next(keys), (DEPTH, n), jnp.float32, minval=lo, maxval=hi))
        return dt + jnp.log(-jnp.expm1(-dt))

    def a_log_init(n):
        return jnp.log(jax.random.uniform(next(keys), (DEPTH, n), jnp.float32, minval=1.0, maxval=16.0))

    gdn_ch = GDN_HEADS * (2 * GDN_DK + GDN_DV)
    ssd_ch = SSD_INNER + 2 * SSD_GROUPS * SSD_STATE
    return {
        'x': nrm((BATCH, SEQ, D_MODEL), 1.0),
        'c': nrm((BATCH, D_MODEL), 1.0),
        'w_mod': nrm((DEPTH, D_MODEL, 6 * D_MODEL), 0.5 * D_MODEL ** -0.5),
        'b_mod': nrm((DEPTH, 6 * D_MODEL), 0.01),
        'norm_mix': gain((DEPTH, D_MODEL)),
        'norm_ffn': gain((DEPTH, D_MODEL)),
        'norm_final': gain((D_MODEL,)),
        'w_in': nrm((DEPTH, D_MODEL, IN_COLS), D_MODEL ** -0.5),
        'gla_w_gate2': nrm((DEPTH, GLA_GATE_RANK, GLA_HEADS * GLA_DK), GLA_GATE_RANK ** -0.5),
        'gla_b_gate2': nrm((DEPTH, GLA_HEADS * GLA_DK), 0.1),
        'gla_norm': gain((DEPTH, GLA_DV)),
        'gdn_conv_w': nrm((DEPTH, GDN_CONV, gdn_ch), GDN_CONV ** -0.5),
        'gdn_a_log': a_log_init(GDN_HEADS),
        'gdn_dt_bias': dt_bias_init(GDN_HEADS),
        'gdn_norm': gain((DEPTH, GDN_DV)),
        'ssd_conv_w': nrm((DEPTH, SSD_CONV, ssd_ch), SSD_CONV ** -0.5),
        'ssd_conv_b': nrm((DEPTH, ssd_ch), 0.01),
        'ssd_a_log': a_log_init(SSD_HEADS),
        'ssd_dt_bias': dt_bias_init(SSD_HEADS),
        'ssd_d': 1.0 + nrm((DEPTH, SSD_HEADS), 0.1),
        'ssd_norm': gain((DEPTH, SSD_INNER)),
        'w_branch_gla': nrm((DEPTH, GLA_HEADS * GLA_DV, D_MODEL), (GLA_HEADS * GLA_DV) ** -0.5),
        'w_branch_gdn': nrm((DEPTH, GDN_HEADS * GDN_DV, D_MODEL), (GDN_HEADS * GDN_DV) ** -0.5),
        'w_branch_ssd': nrm((DEPTH, SSD_INNER, D_MODEL), SSD_INNER ** -0.5),
        'b_merge': nrm((DEPTH, N_BRANCH * D_MODEL), 0.01),
        'w_out': nrm((DEPTH, D_MODEL, D_MODEL), D_MODEL ** -0.5),
        'w_router': nrm((DEPTH, D_MODEL, N_EXPERTS), D_MODEL ** -0.5),
        'b_router': nrm((DEPTH, N_EXPERTS), 0.01),
        'w_gate_up': nrm((DEPTH, N_EXPERTS, D_MODEL, 2 * D_FF_EXPERT), D_MODEL ** -0.5),
        'b_gate_up': nrm((DEPTH, N_EXPERTS, 2 * D_FF_EXPERT), 0.01),
        'w_down': nrm((DEPTH, N_EXPERTS, D_FF_EXPERT, D_MODEL), D_FF_EXPERT ** -0.5),
        'b_down': nrm((DEPTH, N_EXPERTS, D_MODEL), 0.01),
    }


def reference(x, c, w_mod, b_mod, norm_mix, norm_ffn, norm_final, w_in, gla_w_gate2, gla_b_gate2, gla_norm,
              gdn_conv_w, gdn_a_log, gdn_dt_bias, gdn_norm, ssd_conv_w, ssd_conv_b, ssd_a_log, ssd_dt_bias,
              ssd_d, ssd_norm, w_branch_gla, w_branch_gdn, w_branch_ssd, b_merge, w_out,
              w_router, b_router, w_gate_up, b_gate_up, w_down, b_down):
    c_act = jax.nn.silu(c)
    for l in range(DEPTH):
        mod = (c_act @ w_mod[l] + b_mod[l])[:, None, :]
        sh_m, sc_m, g_m, sh_f, sc_f, g_f = jnp.split(mod, 6, axis=-1)
        h = rmsnorm(x, norm_mix[l]) * (1.0 + sc_m) + sh_m
        mix = hybrid_mixer(h, w_in[l], gla_w_gate2[l], gla_b_gate2[l], gla_norm[l],
                           gdn_conv_w[l], gdn_a_log[l], gdn_dt_bias[l], gdn_norm[l],
                           ssd_conv_w[l], ssd_conv_b[l], ssd_a_log[l], ssd_dt_bias[l], ssd_d[l], ssd_norm[l],
                           w_branch_gla[l], w_branch_gdn[l], w_branch_ssd[l], b_merge[l], w_out[l])
        x = x + (g_m * mix).astype(x.dtype)
        h = rmsnorm(x, norm_ffn[l]) * (1.0 + sc_f) + sh_f
        ffn = moe_ffn(h, w_router[l], b_router[l], w_gate_up[l], b_gate_up[l], w_down[l], b_down[l])
        x = x + (g_f * ffn).astype(x.dtype)
    return rmsnorm(x, norm_final)
```

```python
import numpy as np
from contextlib import ExitStack
from concourse.bass_utils import run_bass_kernel_spmd

import concourse.bass as bass
import concourse.mybir as mybir

ENGINES = ("pe", "act", "dve", "pool", "sp")


class Op:
    __slots__ = ("eng", "fn", "deps", "is_dma", "dsem", "dcount", "signal", "idx", "signo")

    def __init__(self, eng, fn):
        self.eng = eng
        self.fn = fn
        self.deps = []
        self.is_dma = False
        self.dsem = None
        self.dcount = 0
        self.signal = False
        self.idx = -1
        self.signo = 0


class Sched:
    def __init__(self, nc, same_engine_sync=True):
        self.nc = nc
        self.q = {e: [] for e in ENGINES}
        self.res_w = {}
        self.res_r = {}
        self.phys = []
        self.key2phys = {}
        self.free_phys = []
        self.same_engine_sync = same_engine_sync

    def _collect(self, op, reads, writes, my_dma_key=None):
        deps = []
        for k in reads:
            t = self.res_w.get(k)
            if t is not None:
                deps.append(t)
        for k in writes:
            t = self.res_w.get(k)
            if t is not None:
                if not (my_dma_key is not None and t[0] == 'dma' and t[1] == my_dma_key):
                    deps.append(t)
            deps.extend(self.res_r.get(k, ()))
        op.deps = deps

    def _commit(self, tok, reads, writes):
        for k in reads:
            self.res_r.setdefault(k, []).append(tok)
        for k in writes:
            self.res_w[k] = tok
            self.res_r[k] = []

    @staticmethod
    def _excl(reads, writes):
        rp = [k for k in reads if len(k) == 2 and k[0] == "P" and k[1].isdigit()]
        if not rp:
            return reads, writes
        return [k for k in reads if k not in rp], list(writes) + [k for k in rp if k not in writes]

    def op(self, eng, fn, reads=(), writes=()):
        reads, writes = self._excl(reads, writes)
        o = Op(eng, fn)
        self._collect(o, reads, writes)
        o.idx = len(self.q[eng])
        self.q[eng].append(o)
        self._commit(('op', o), reads, writes)
        return o

    def dma(self, eng, fn, sem_key, reads=(), writes=()):
        reads, writes = self._excl(reads, writes)
        if sem_key not in self.key2phys:
            if self.free_phys:
                p = self.free_phys.pop()
            else:
                p = len(self.phys)
                self.phys.append(0)
            self.key2phys[sem_key] = p
        p = self.key2phys[sem_key]
        o = Op(eng, fn)
        o.is_dma = True
        self._collect(o, reads, writes, my_dma_key=p)
        self.phys[p] += 1
        c = self.phys[p]
        o.dsem = p
        o.dcount = c
        o.idx = len(self.q[eng])
        self.q[eng].append(o)
        self._commit(('dma', p, c), reads, writes)
        return o

    def final_wait(self, eng, keys):
        o = Op(eng, None)
        deps = []
        for k in keys:
            t = self.res_w.get(k)
            if t is not None:
                deps.append(t)
            deps.extend(self.res_r.get(k, ()))
        o.deps = deps
        o.idx = len(self.q[eng])
        self.q[eng].append(o)

    def barrier(self):
        toks = []
        for e in ENGINES:
            for o in reversed(self.q[e]):
                if o.fn is not None and not o.is_dma:
                    toks.append(('op', o))
                    break
        for p, c in enumerate(self.phys):
            if c:
                toks.append(('dma', p, c))
        for e in ENGINES:
            o = Op(e, None)
            o.deps = list(toks)
            o.idx = len(self.q[e])
            self.q[e].append(o)
        self.free_phys = list(range(len(self.phys)))[::-1]
        self.key2phys = {}

    def emit(self, stack):
        nc = self.nc
        for e in ENGINES:
            for o in self.q[e]:
                for t in o.deps:
                    if t[0] == 'op':
                        tgt = t[1]
                        if tgt.eng == o.eng and (not self.same_engine_sync or o.eng == 'pe'):
                            continue
                        tgt.signal = True
        for e in ENGINES:
            n = 0
            for o in self.q[e]:
                if o.signal:
                    n += 1
                    o.signo = n
        esem = {e: stack.enter_context(nc.semaphore("s_" + e)) for e in ENGINES}
        dsem = {}
        for p in range(len(self.phys)):
            dsem[p] = stack.enter_context(nc.semaphore(f"d_{p}"))
        block = stack.enter_context(nc.Block())
        stats = {}

        def run(e, engobj):
            waited = {}
            nwait = 0
            for o in self.q[e]:
                need = {}
                for t in o.deps:
                    if t[0] == 'op':
                        tgt = t[1]
                        if tgt.eng == e and (not self.same_engine_sync or e == 'pe'):
                            continue
                        key = ('e', tgt.eng)
                        val = tgt.signo
                    else:
                        key = ('d', t[1])
                        val = t[2] * 16
                    if need.get(key, 0) < val:
                        need[key] = val
                for key, val in need.items():
                    if waited.get(key, 0) >= val:
                        continue
                    waited[key] = val
                    sem = esem[key[1]] if key[0] == 'e' else dsem[key[1]]
                    engobj.wait_ge(sem, val)
                    nwait += 1
                if o.fn is None:
                    continue
                ins = o.fn(engobj)
                if o.is_dma:
                    ins.then_inc(dsem[o.dsem], 16)
                elif o.signal:
                    ins.then_inc(esem[e], 1)
            stats[e] = (len(self.q[e]), nwait)

        @block.tensor
        def _(eng):
            run("pe", eng)

        @block.scalar
        def _(eng):
            run("act", eng)

        @block.vector
        def _(eng):
            run("dve", eng)

        @block.gpsimd
        def _(eng):
            run("pool", eng)

        @block.sync
        def _(eng):
            run("sp", eng)

        return stats


F32 = mybir.dt.float32
BF16 = mybir.dt.bfloat16
I32 = mybir.dt.int32
AF = mybir.ActivationFunctionType
ALU = mybir.AluOpType
AX = mybir.AxisListType

S_TOK = 4096
D = 1024
KC = 8
NT = S_TOK // 128
DEPTH = 2
EPS = 1e-6
IN_COLS = 7968
C_GLA_Q, C_GLA_K, C_GLA_V, C_GLA_LR, C_GLA_R = 0, 256, 512, 1024, 1040
C_GDN_QKV, C_GDN_A, C_GDN_B, C_GDN_G = 1552, 3088, 3092, 3096
C_SSD_Z, C_SSD_XBC, C_SSD_DT, C_MERGE = 3608, 4120, 4888, 4896


class KB:
    def __init__(self, same_engine_sync=True):
        self.nc = bass.Bass("TRN2", target_bir_lowering=False)
        self.S = Sched(self.nc, same_engine_sync=same_engine_sync)
        self.stack = ExitStack()
        self.ins = {}
        self.outs = {}
        self._n = 0
        self.scopes = []
        self._allow_p = False
        self.P = [self.stack.enter_context(self.nc.psum_tensor(f"PB{i}", [128, 512], F32)) for i in range(8)]

    @staticmethod
    def pk(i, a=0, b=512):
        return [f"P{i}"]

    def din(self, name, shape, dt=F32):
        t = self.nc.dram_tensor(name, list(shape), dt, kind="ExternalInput")
        self.ins[name] = t
        return t.ap()

    def dout(self, name, shape, dt=F32):
        t = self.nc.dram_tensor(name, list(shape), dt, kind="ExternalOutput")
        self.outs[name] = t
        return t.ap()

    def dscr(self, name, shape, dt=F32, debug=False):
        if debug:
            return self.dout(name, shape, dt)
        return self.nc.dram_tensor(name, list(shape), dt, kind="Internal").ap()

    def sb(self, name, shape, dt=F32):
        st = self.scopes[-1] if self.scopes else self.stack
        return st.enter_context(self.nc.sbuf_tensor(name, list(shape), dt))

    def sbp(self, name, shape, dt=F32):
        assert not self.scopes or self._allow_p
        return self.stack.enter_context(self.nc.sbuf_tensor(name, list(shape), dt))

    def push_scope(self):
        self.scopes.append(ExitStack())

    def pop_scope(self):
        self.S.barrier()
        self.scopes.pop().close()

    def ps(self, name, shape=(128, 512), dt=F32):
        return self.stack.enter_context(self.nc.psum_tensor(name, list(shape), dt))

    def mm(self, out, lhsT, rhs, start=True, stop=True, r=(), w=()):
        return self.S.op("pe", lambda e: e.matmul(out, lhsT, rhs, start=start, stop=stop), r, w)

    def tr(self, out, in_, ident, r=(), w=()):
        return self.S.op("pe", lambda e: e.transpose(out, in_, ident), r, w)

    def act(self, out, in_, func, bias=None, scale=None, accum_out=None, r=(), w=(), eng="act"):
        kw = {}
        if bias is not None:
            kw["bias"] = bias
        if scale is not None:
            kw["scale"] = scale
        if accum_out is not None:
            kw["accum_out"] = accum_out
        return self.S.op(eng, lambda e: e.activation(out, in_, func, **kw), r, w)

    def ts(self, eng, out, in0, s1, s2, op0, op1=None, accum_out=None, r=(), w=()):
        kw = {}
        if op1 is not None:
            kw["op1"] = op1
        if accum_out is not None:
            kw["accum_out"] = accum_out
        return self.S.op(eng, lambda e: e.tensor_scalar(out, in0, s1, s2, op0, **kw), r, w)

    def tt(self, eng, out, in0, in1, op, r=(), w=()):
        return self.S.op(eng, lambda e: e.tensor_tensor(out, in0, in1, op), r, w)

    def stt(self, eng, out, in0, scalar, in1, op0, op1, r=(), w=()):
        return self.S.op(eng, lambda e: e.scalar_tensor_tensor(out, in0, scalar, in1, op0, op1), r, w)

    def cp(self, eng, out, in_, r=(), w=()):
        if eng == "act":
            return self.S.op(eng, lambda e: e.copy(out, in_), r, w)
        return self.S.op(eng, lambda e: e.tensor_copy(out, in_), r, w)

    def dma(self, eng, out, in_, sem, r=(), w=(), **kw):
        return self.S.dma(eng, lambda e: e.dma_start(out, in_, **kw), sem, r, w)


def phase_consts(kb):
    c = {}
    cdefs = {
        "ident": (128, 128), "tri_incl": (128, 128), "tri_strict": (128, 128), "ones": (128, 128),
        "blk": (128, 128), "selA": (128, 128), "selB": (128, 128), "neg_strict": (128, 128),
        "tri_full": (128, 128), "blk_thr": (128, 64), "base_pk": (128, 8),
    }
    for name, shp in cdefs.items():
        src = kb.din("c_" + name, shp)
        t = kb.sb("cs_" + name, shp)
        kb.dma("sp", t[:], src, sem="c_" + name, w=["c_" + name])
        c[name] = t
        tb = kb.sb("cb_" + name, shp, BF16)
        kb.cp("dve", tb[:], t[:], r=["c_" + name], w=["cb_" + name])
        c[name + "_bf"] = tb
    kb.C = c


def phase_mod(kb):
    nc = kb.nc
    cT = kb.din("cT", (128, KC))
    w_mod = kb.din("w_mod", (DEPTH, D, 6 * D))
    bmodc = kb.din("bmodc", (DEPTH, 128, 48))
    bmodrow = kb.din("bmodrow", (DEPTH, 6, D))
    nmixc = kb.din("nmixc", (DEPTH, 128, KC))
    nffnc = kb.din("nffnc", (DEPTH, 128, KC))
    pers = {}
    for l in range(DEPTH):
        pers[f"modc{l}"] = kb.sbp(f"modc{l}", (128, 48))
        pers[f"modscl{l}"] = kb.sbp(f"modscl{l}", (128, 2, KC))
        for piece in (2, 5):
            pers[f"modrow{l}_{piece}"] = kb.sbp(f"modrow{l}_{piece}", (128, D))
    kb.modrow_d = [[kb.dscr(f"modrowd{l}_{j}", (128, D)) for j in range(2)] for l in range(DEPTH)]
    kb.push_scope()
    rowtmp = kb.sb("modrowtmp", (128, D))
    cact = kb.sb("cact", (128, KC))
    crep = kb.sb("crep", (128, KC, 128))
    kb.dma("sp", cact[:], cT, sem="cact", w=["cact"])
    kb.act(cact[:], cact[:], AF.Silu, r=["cact"], w=["cact"])
    for k in range(KC):
        kb.cp("dve", crep[:, k, :], cact[:, k:k + 1].to_broadcast([128, 128]), r=["cact"], w=["crep"])
    wbuf = [kb.sb(f"modw{i}", (128, KC, 1024)) for i in range(2)]
    pcol = kb.P[0]
    prow = [kb.P[1], kb.P[2]]
    kb.modc, kb.gm_row, kb.gf_row = [], [], []
    kb.sclm, kb.shm, kb.sclf, kb.shf = [], [], [], []
    it = 0
    for l in range(DEPTH):
        modc = pers[f"modc{l}"]
        bc = kb.sb(f"bmodc{l}", (128, 48))
        nm = kb.sb(f"nmixc{l}", (128, KC))
        nf = kb.sb(f"nffnc{l}", (128, KC))
        kb.dma("sp", bc[:], bmodc[l], sem=f"bmodc{l}", w=[f"bmodc{l}"])
        kb.dma("sp", nm[:], nmixc[l], sem=f"nmixc{l}", w=[f"nmixc{l}"])
        kb.dma("sp", nf[:], nffnc[l], sem=f"nffnc{l}", w=[f"nffnc{l}"])
        rows = []
        for piece in range(6):
            wb = wbuf[it % 2]
            wk = f"modw{it % 2}"
            it += 1
            src = w_mod[l, :, piece * 1024:(piece + 1) * 1024].rearrange("(k p) c -> p k c", p=128)
            for hh in range(2):
                kb.dma("sp", wb[:, hh * 4:(hh + 1) * 4, :], src[:, hh * 4:(hh + 1) * 4, :],
                       sem=wk, w=[wk])
            for jj in range(8):
                j = piece * 8 + jj
                for k in range(KC):
                    kb.mm(pcol[:, j:j + 1], wb[:, k, jj * 128:(jj + 1) * 128], cact[:, k:k + 1],
                          start=(k == 0), stop=(k == KC - 1), r=[wk, "cact"], w=kb.pk(0, 0, 128))
            if piece in (2, 3, 4, 5):
                row = pers[f"modrow{l}_{piece}"] if piece in (2, 5) else rowtmp
                if piece in (3, 4):
                    pers_key = f"modrow{l}_{piece}"
                kb.dma("sp", row[:], bmodrow[l, piece].partition_broadcast(128),
                       sem=f"modrow{l}_{piece}", w=[f"modrow{l}_{piece}"])
                for hh in range(2):
                    for k in range(KC):
                        kb.mm(prow[hh][:, :], crep[:, k, :], wb[:, k, hh * 512:(hh + 1) * 512],
                              start=(k == 0), stop=(k == KC - 1), r=[wk, "crep"], w=kb.pk(1 + hh))
                    kb.tt("dve", row[:, hh * 512:(hh + 1) * 512], prow[hh][:, :], row[:, hh * 512:(hh + 1) * 512],
                          ALU.add, r=kb.pk(1 + hh) + [f"modrow{l}_{piece}"], w=[f"modrow{l}_{piece}"])
                if piece in (2, 5):
                    rows.append((row, f"modrow{l}_{piece}"))
                else:
                    kb.dma("sp", kb.modrow_d[l][piece - 3], row[:], sem=f"modrow{l}_{piece}", r=[f"modrow{l}_{piece}"],
                           w=[f"modrowd{l}"])
        kb.tt("dve", modc[:], pcol[:, 0:48], bc[:], ALU.add, r=kb.pk(0, 0, 128) + [f"bmodc{l}"], w=[f"modc{l}"])
        scl = pers[f"modscl{l}"]
        kb.stt("dve", scl[:, 0, :], modc[:, 8:16], 1.0, nm[:], ALU.add, ALU.mult,
               r=[f"modc{l}", f"nmixc{l}"], w=[f"modc{l}"])
        kb.stt("dve", scl[:, 1, :], modc[:, 32:40], 1.0, nf[:], ALU.add, ALU.mult,
               r=[f"modc{l}", f"nffnc{l}"], w=[f"modc{l}"])
        kb.modc.append(modc)
        kb.gm_row.append(rows[0])
        kb.gf_row.append(rows[1])
        kb.sclm.append(scl[:, 0, :])
        kb.shm.append(modc[:, 0:8])
        kb.sclf.append(scl[:, 1, :])
        kb.shf.append(modc[:, 24:32])
    kb.pop_scope()


def norm_bufs(kb, tag):
    NB = 2
    b = {
        "tag": tag,
        "xt": [kb.sb(f"{tag}_x{i}", (128, D)) for i in range(NB)],
        "xn": [kb.sb(f"{tag}_xn{i}", (128, D)) for i in range(NB)],
        "junk": kb.sb(f"{tag}_junk", (128, D), BF16),
        "ss": kb.sb(f"{tag}_ss", (128, 2)),
        "rstd": kb.sb(f"{tag}_rstd", (128, 2)),
        "n": 0,
    }
    return b


def norm_tile(kb, nb, l, which, xsrc_ap, xsrc_key, dst, dst_key):
    C = kb.C
    tag = nb["tag"]
    scl = kb.sclm[l] if which == "m" else kb.sclf[l]
    sh = kb.shm[l] if which == "m" else kb.shf[l]
    mkey = f"modc{l}"
    b = nb["n"] % 2
    nb["n"] += 1
    xt, xn, junk, ss, rstd = nb["xt"][b], nb["xn"][b], nb["junk"], nb["ss"], nb["rstd"]
    xk, xnk = f"{tag}_x{b}", f"{tag}_xn{b}"
    sk, rk = f"{tag}_ss{b}", f"{tag}_rstd{b}"
    kb.dma("sp", xt[:], xsrc_ap, sem=xk, r=[xsrc_key], w=[xk])
    kb.act(junk[:], xt[:], AF.Square, accum_out=ss[:, b:b + 1], r=[xk], w=[f"{tag}_junk", sk])
    kb.ts("dve", rstd[:, b:b + 1], ss[:, b:b + 1], 1.0 / D, EPS, ALU.mult, ALU.add, r=[sk], w=[rk])
    kb.act(rstd[:, b:b + 1], rstd[:, b:b + 1], AF.Sqrt, r=[rk], w=[rk])
    kb.S.op("dve", lambda e: e.reciprocal(rstd[:, b:b + 1], rstd[:, b:b + 1]), [rk], [rk])
    kb.ts("pool", xn[:], xt[:], rstd[:, b:b + 1], None, ALU.mult, r=[xk, rk], w=[xnk])
    for half in range(2):
        pT = kb.P[half]
        pk = kb.pk(half)
        for kk in range(4):
            k = half * 4 + kk
            kb.tr(pT[:, kk * 128:(kk + 1) * 128], xn[:, k * 128:(k + 1) * 128], C["ident"][:], r=[xnk, "c_ident"], w=pk)
        for kk in range(4):
            k = half * 4 + kk
            d = dst[:, k, :]
            src = pT[:, kk * 128:(kk + 1) * 128]
            if kk % 2 == 0:
                kb.act(d, src, AF.Identity, bias=sh[:, k:k + 1], scale=scl[:, k:k + 1], r=pk + [mkey], w=[dst_key])
            else:
                kb.ts("dve", d, src, scl[:, k:k + 1], sh[:, k:k + 1], ALU.mult, ALU.add, r=pk + [mkey], w=[dst_key])


def phase_norm(kb, l, which, xsrc, xsrc_key, hT, hT_key):
    nb = norm_bufs(kb, f"n{l}{which}")
    for i in range(NT):
        norm_tile(kb, nb, l, which, xsrc[i * 128:(i + 1) * 128, :], xsrc_key(i), hT[:, :, i * 128:(i + 1) * 128], hT_key(i))


def _consts():
    i = np.arange(128)
    same = (i[:, None] // 64) == (i[None, :] // 64)
    return {
        "c_ident": np.eye(128, dtype=np.float32),
        "c_tri_incl": ((i[:, None] <= i[None, :]) & same).astype(np.float32),
        "c_tri_strict": ((i[:, None] > i[None, :]) & same).astype(np.float32),
        "c_ones": np.ones((128, 128), np.float32),
        "c_blk": same.astype(np.float32),
        "c_selA": np.repeat((i < 64).astype(np.float32)[:, None], 128, 1),
        "c_selB": np.repeat((i >= 64).astype(np.float32)[:, None], 128, 1),
        "c_neg_strict": -((i[:, None] > i[None, :]) & same).astype(np.float32),
        "c_tri_full": (i[:, None] < i[None, :]).astype(np.float32),
        "c_blk_thr": np.repeat((np.arange(64, dtype=np.float32) * 512.0)[None, :], 128, 0),
        "c_base_pk": (i[:, None] + 128 * np.arange(8)[None, :]).astype(np.float32),
    }


def host_inputs(inp, b, names):
    m = {}
    m.update(_consts())
    m["x"] = np.ascontiguousarray(inp["x"][b])
    m["cT"] = np.ascontiguousarray(inp["c"][b].reshape(KC, 128).T)
    m["w_mod"] = inp["w_mod"]
    m["bmodc"] = np.ascontiguousarray(inp["b_mod"].reshape(DEPTH, 48, 128).transpose(0, 2, 1))
    bm = inp["b_mod"].reshape(DEPTH, 6, D)
    m["bmodrow"] = np.ascontiguousarray(bm)
    m["nmixc"] = np.ascontiguousarray(inp["norm_mix"].reshape(DEPTH, KC, 128).transpose(0, 2, 1))
    m["nffnc"] = np.ascontiguousarray(inp["norm_ffn"].reshape(DEPTH, KC, 128).transpose(0, 2, 1))
    m.update(host_inputs2(inp, b, names))
    return {k: np.ascontiguousarray(m[k], dtype=m[k].dtype) for k in names}


def proj_feat(kb, out_ps, w_sb, wkey, c0, ncols, hT, hkey, t0, nt, wkeys=None):
    for k in range(KC):
        kb.mm(out_ps, w_sb[:, k, c0:c0 + ncols], hT[:, k, t0:t0 + nt], start=(k == 0), stop=(k == KC - 1),
              r=[wkey, hkey], w=wkeys)


def proj_tok(kb, out_ps, w_sb, wkey, c0, ncols, hT, hkey, t0, nt, wkeys=None):
    for k in range(KC):
        kb.mm(out_ps, hT[:, k, t0:t0 + nt], w_sb[:, k, c0:c0 + ncols], start=(k == 0), stop=(k == KC - 1),
              r=[wkey, hkey], w=wkeys)


def load_w_cast(kb, dst, dkey, src_dram_2d, c0, ncols, nk=KC, step=512):
    src = src_dram_2d.rearrange("(k p) c -> p k c", p=128)
    for k in range(nk):
        kb.dma("pool", dst[:, k, 0:ncols], src[:, k, c0:c0 + ncols], sem=dkey, w=[dkey])


def rms_rstd(kb, tag, rs, ssq, n, width):
    kb.ts("dve", rs[:, 0:n], ssq[:, 0:n], 1.0 / width, EPS, ALU.mult, ALU.add, r=[tag + "_ssq"], w=[tag + "_rs"])
    kb.act(rs[:, 0:n], rs[:, 0:n], AF.Sqrt, r=[tag + "_rs"], w=[tag + "_rs"])
    kb.S.op("dve", lambda e: e.reciprocal(rs[:, 0:n], rs[:, 0:n]), [tag + "_rs"], [tag + "_rs"])


def phase_gla(kb, l, hT, hkey_fn, obr):
    C = kb.C
    tag = f"gla{l}"
    w_in = kb.w_in
    NW = 1552
    wg = kb.sb(tag + "_w", (128, KC, NW), BF16)
    load_w_cast(kb, wg, tag + "_w", w_in[l], 0, NW)
    w2 = kb.sb(tag + "_w2", (16, 256))
    b2 = kb.sb(tag + "_b2", (1, 256))
    gn = kb.sb(tag + "_gn", (128, 512))
    kb.dma("sp", w2[:], kb.din(tag + "_w2d", (16, 256)), sem=tag + "_w2", w=[tag + "_w2"])
    kb.dma("sp", b2[:], kb.din(tag + "_b2d", (1, 256)), sem=tag + "_b2", w=[tag + "_b2"])
    kb.dma("sp", gn[:], kb.din(tag + "_gnd", (1, 512))[0].partition_broadcast(128), sem=tag + "_gn", w=[tag + "_gn"])
    lrT = kb.sb(tag + "_lrT", (16, 128))
    sp = kb.sb(tag + "_sp", (128, 256))
    e_rem = kb.sb(tag + "_erem", (128, 256))
    e_pos = kb.sb(tag + "_epos", (128, 256))
    e_neg = kb.sb(tag + "_eneg", (128, 256))
    qdT = kb.sb(tag + "_qdT", (128, 256), BF16)
    knT = kb.sb(tag + "_knT", (128, 256), BF16)
    krem = kb.sb(tag + "_krem", (128, 256), BF16)
    v_sb = kb.sb(tag + "_v", (128, 512), BF16)
    r_sb = kb.sb(tag + "_r", (128, 512))
    attT = [kb.sb(tag + f"_attT{i}", (128, 128), BF16) for i in range(2)]
    S = [kb.sb(tag + f"_S{i}", (128, 256)) for i in range(2)]
    Sb = [kb.sb(tag + f"_Sb{i}", (128, 256), BF16) for i in range(2)]
    junk = kb.sb(tag + "_junk", (128, 128), BF16)
    ssq = kb.sb(tag + "_ssq", (128, 4))
    rs = kb.sb(tag + "_rs", (128, 4))
    og = kb.sb(tag + "_og", (128, 512))
    oint = kb.sb(tag + "_oint", (128, 512))
    o_sb = kb.sb(tag + "_o", (128, 512))
    oT = kb.sb(tag + "_oT", (128, 512), BF16)
    for p in range(2):
        kb.S.op("dve", lambda e, p=p: e.memset(S[p][:], 0.0), [], [tag + f"_S{p}"])
        kb.S.op("dve", lambda e, p=p: e.memset(Sb[p][:], 0.0), [], [tag + f"_Sb{p}"])
    P0, P1, P2, P3, P4, P5, P6, P7 = kb.P
    wk = tag + "_w"
    import os
    STOP = float(os.environ.get('GLA_STOP', '9'))
    for i in range(int(os.environ.get('GLA_NT', NT))):
        t0 = i * 128
        hk = hkey_fn(i)
        for pair in range(2):
            proj_feat(kb, P0[:, pair * 128:(pair + 1) * 128], wg, wk, C_GLA_Q + pair * 128, 128, hT, hk, t0, 128, kb.pk(0, pair * 128, pair * 128 + 128))
            proj_feat(kb, P0[:, 256 + pair * 128:256 + (pair + 1) * 128], wg, wk, C_GLA_K + pair * 128, 128, hT, hk, t0, 128, kb.pk(0, 256 + pair * 128, 384 + pair * 128))
        proj_feat(kb, P1[0:16, 0:128], wg, wk, C_GLA_LR, 16, hT, hk, t0, 128, kb.pk(1, 0, 128))
        proj_tok(kb, P2[:, 0:256], wg, wk, C_GLA_K, 256, hT, hk, t0, 128, kb.pk(2, 0, 256))
        proj_tok(kb, P3[:, :], wg, wk, C_GLA_V, 512, hT, hk, t0, 128, kb.pk(3))
        proj_tok(kb, P4[:, :], wg, wk, C_GLA_R, 512, hT, hk, t0, 128, kb.pk(4))
        if STOP <= 1:
            continue
        kb.cp("dve", lrT[:, :], P1[0:16, 0:128], r=kb.pk(1, 0, 128), w=[tag + "_lrT"])
        kb.mm(P1[:, 128:384], lrT[:, :], w2[:, :], start=True, stop=False, r=[tag + "_lrT", tag + "_w2"], w=kb.pk(1, 128, 384))
        kb.mm(P1[:, 128:384], C["ones"][0:1, :], b2[0:1, :], start=False, stop=True, r=["c_ones", tag + "_b2"], w=kb.pk(1, 128, 384))
        kb.act(sp[:], P1[:, 128:384], AF.Exp, scale=-1.0, r=kb.pk(1, 128, 384), w=[tag + "_sp"])
        kb.act(sp[:], sp[:], AF.Ln, bias=1.0, r=[tag + "_sp"], w=[tag + "_sp"])
        if STOP <= 2:
            continue
        kb.cp("act", v_sb[:], P3[:, :], r=kb.pk(3), w=[tag + "_v"])
        kb.act(r_sb[:], P4[:, :], AF.Silu, r=kb.pk(4), w=[tag + "_r"])
        kb.mm(P5[:, 256:512], C["tri_strict"][:], sp[:], r=["c_tri_strict", tag + "_sp"], w=kb.pk(5, 256, 512))
        for pair in range(2):
            kb.mm(P6[:, pair * 128:(pair + 1) * 128], sp[:, pair * 128:(pair + 1) * 128], C["tri_incl"][:],
                  r=["c_tri_incl", tag + "_sp"], w=kb.pk(6, 0, 256))
        kb.act(e_rem[:], P5[:, 256:512], AF.Exp, scale=-1.0 / 16, r=kb.pk(5, 256, 512), w=[tag + "_erem"])
        kb.act(e_pos[:], P6[:, 0:256], AF.Exp, scale=-1.0 / 16, r=kb.pk(6, 0, 256), w=[tag + "_epos"])
        kb.act(e_neg[:], P6[:, 0:256], AF.Exp, scale=1.0 / 16, r=kb.pk(6, 0, 256), w=[tag + "_eneg"])
        kb.stt("dve", qdT[:], P0[:, 0:256], 0.125, e_pos[:], ALU.mult, ALU.mult, r=kb.pk(0, 0, 256) + [tag + "_epos"], w=[tag + "_qdT"])
        kb.tt("dve", knT[:], P0[:, 256:512], e_neg[:], ALU.mult, r=kb.pk(0, 256, 512) + [tag + "_eneg"], w=[tag + "_knT"])
        kb.tt("dve", krem[:], P2[:, 0:256], e_rem[:], ALU.mult, r=kb.pk(2, 0, 256) + [tag + "_erem"], w=[tag + "_krem"])
        if STOP <= 3:
            continue
        for h in range(4):
            pair, rows = h // 2, (h % 2) * 64
            hc = slice(h * 128, (h + 1) * 128)
            pc = slice(pair * 128, (pair + 1) * 128)
            aps = P1[:, 384:512] if h % 2 == 0 else P2[:, 256:384]
            apk = kb.pk(1, 384, 512) if h % 2 == 0 else kb.pk(2, 256, 384)
            kb.mm(aps, knT[rows:rows + 64, pc], qdT[rows:rows + 64, pc], r=[tag + "_knT", tag + "_qdT"], w=apk)
            kb.tt("dve", attT[h % 2][:], aps, C["tri_incl"][:], ALU.mult, r=apk + ["c_tri_incl"], w=[tag + f"_attT{h % 2}"])
            kb.mm(P7[:, hc], attT[h % 2][:], v_sb[:, hc], start=True, stop=True, r=[tag + f"_attT{h % 2}", tag + "_v"], w=kb.pk(7, h * 128, h * 128 + 128))
        if STOP <= 4:
            continue
        for pair in range(2):
            pc0 = pair * 128
            Sk, Sbk = tag + f"_S{pair}", tag + f"_Sb{pair}"
            for ch in range(2):
                tr_ = slice(ch * 64, (ch + 1) * 64)
                kb.mm(P5[tr_, pair * 256:(pair + 1) * 256], qdT[:, pc0 + ch * 64:pc0 + (ch + 1) * 64],
                      Sb[pair][:, :], start=True, stop=True,
                      r=[tag + "_qdT", Sbk], w=kb.pk(5, pair * 256, pair * 256 + 256))
                kb.mm(P6[:, 256:512], krem[tr_, pc0:pc0 + 128], v_sb[tr_, pair * 256:(pair + 1) * 256],
                      r=[tag + "_krem", tag + "_v"], w=kb.pk(6, 256, 512))
                for hh in range(2):
                    rr = slice(hh * 64, (hh + 1) * 64)
                    cc = slice(hh * 128, (hh + 1) * 128)
                    dec = e_pos[rr, pc0 + ch * 64 + 63:pc0 + ch * 64 + 64]
                    kb.stt("dve", S[pair][rr, cc], S[pair][rr, cc], dec, P6[rr, 256 + hh * 128:256 + (hh + 1) * 128],
                           ALU.mult, ALU.add, r=[Sk, tag + "_epos"] + kb.pk(6, 256, 512), w=[Sk])
                kb.cp("pool", Sb[pair][:], S[pair][:], r=[Sk], w=[Sbk])
        if STOP <= 5:
            continue
        kb.cp("act", oint[:], P5[:, :], r=kb.pk(5), w=[tag + "_oint"])
        kb.tt("dve", o_sb[:], P7[:, :], oint[:], ALU.add, r=kb.pk(7) + [tag + "_oint"], w=[tag + "_o"])
        for h in range(4):
            kb.act(junk[:], o_sb[:, h * 128:(h + 1) * 128], AF.Square, accum_out=ssq[:, h:h + 1], r=[tag + "_o"],
                   w=[tag + "_junk", tag + "_ssq"])
        rms_rstd(kb, tag, rs, ssq, 4, 128)
        for h in range(4):
            hc = slice(h * 128, (h + 1) * 128)
            kb.stt("dve", og[:, hc], o_sb[:, hc], rs[:, h:h + 1], gn[:, hc], ALU.mult, ALU.mult,
                   r=[tag + "_o", tag + "_rs", tag + "_gn"], w=[tag + "_og"])
        kb.tt("pool", og[:], og[:], r_sb[:], ALU.mult, r=[tag + "_og", tag + "_r"], w=[tag + "_og"])
        for c in range(4):
            kb.tr(P0[:, c * 128:(c + 1) * 128], og[:, c * 128:(c + 1) * 128], C["ident"][:], r=[tag + "_og", "c_ident"], w=kb.pk(0, c * 128, c * 128 + 128))
        kb.cp("act", oT[:], P0[:, :], r=kb.pk(0), w=[tag + "_oT"])
        kb.dma("sp", obr[i], oT[:], sem=tag + "_oT", r=[tag + "_oT"], w=[f"{tag}_obr{i}"])


def host_inputs2(inp, b, names):
    m = {}
    f = np.float32
    for l in range(DEPTH):
        m[f"gla{l}_w2d"] = inp["gla_w_gate2"][l]
        m[f"gla{l}_b2d"] = inp["gla_b_gate2"][l][None, :]
        m[f"gla{l}_gnd"] = np.tile(inp["gla_norm"][l], 4)[None, :]
    m["w_in"] = inp["w_in"]
    for l in range(DEPTH):
        cw = inp["ssd_conv_w"][l].reshape(4, 6, 128).transpose(2, 1, 0)
        cb = inp["ssd_conv_b"][l].reshape(6, 128).T[:, :, None]
        m[f"ssd{l}_cwd"] = np.concatenate([cw, cb], axis=2)
        m[f"ssd{l}_gnd"] = inp["ssd_norm"][l][None, :]
        m[f"ssd{l}_hpd"] = np.concatenate([inp["ssd_dt_bias"][l], inp["ssd_a_log"][l], inp["ssd_d"][l]])[None, :]
    for l in range(DEPTH):
        m[f"gdn{l}_cwd"] = inp["gdn_conv_w"][l].reshape(4, 12, 128).transpose(2, 1, 0)
        m[f"gdn{l}_gnd"] = np.tile(inp["gdn_norm"][l], 4)[None, :]
        m[f"gdn{l}_hpd"] = np.concatenate([inp["gdn_dt_bias"][l], inp["gdn_a_log"][l]])[None, :]
    return m


def phase_gdn(kb, l, hT, hkey_fn, obr):
    import os
    C = kb.C
    tag = f"gdn{l}"
    w_in = kb.w_in
    NW = 2056
    wk = tag + "_w"
    wg = kb.sb(wk, (128, KC, NW), BF16)
    load_w_cast(kb, wg, wk, w_in[l], C_GDN_QKV, NW)
    O_QKV, O_AB, O_G = 0, 1536, 1544
    convw = kb.sb(tag + "_cw", (128, 12, 4))
    kb.dma("sp", convw[:], kb.din(tag + "_cwd", (128, 12, 4)), sem=tag + "_cw", w=[tag + "_cw"])
    gn = kb.sb(tag + "_gn", (128, 512))
    kb.dma("sp", gn[:], kb.din(tag + "_gnd", (1, 512))[0].partition_broadcast(128), sem=tag + "_gn", w=[tag + "_gn"])
    hp = kb.sb(tag + "_hp", (128, 8))
    kb.dma("sp", hp[:], kb.din(tag + "_hpd", (1, 8))[0].partition_broadcast(128), sem=tag + "_hp", w=[tag + "_hp"])
    negA = kb.sb(tag + "_negA", (128, 4))
    kb.act(negA[:], hp[:, 4:8], AF.Exp, r=[tag + "_hp"], w=[tag + "_negA"])
    kb.ts("dve", negA[:], negA[:], -1.0, None, ALU.mult, r=[tag + "_negA"], w=[tag + "_negA"])
    ubuf = kb.sb(tag + "_ubuf", (128, 12, 131))
    kb.S.op("pool", lambda e: e.memset(ubuf[:], 0.0), [], [tag + "_ubuf"])
    cacc = kb.sb(tag + "_cacc", (128, 12, 128))
    ctmp = kb.sb(tag + "_ctmp", (128, 12, 128))
    qkv = kb.sb(tag + "_qkv", (128, 12, 128))
    sq = kb.sb(tag + "_sq", (128, 8, 128))
    rinv = kb.sb(tag + "_rinv", (128, 8, 128))
    qkn = kb.sb(tag + "_qkn", (128, 8, 128))
    gsb = kb.sb(tag + "_gsb", (128, 512))
    sm = kb.sb(tag + "_sm", (128, 40))
    beta, gg, cum, ecum, erem, bec = sm[:, 0:4], sm[:, 4:8], sm[:, 8:12], sm[:, 12:16], sm[:, 16:20], sm[:, 20:24]
    dA, dB, ytmp = sm[:, 24:28], sm[:, 28:32], sm[:, 32:36]
    Gs = kb.sb(tag + "_Gs", (128, 128))
    E = kb.sb(tag + "_E", (128, 128))
    ET = kb.sb(tag + "_ET", (128, 128))
    Bm = [kb.sb(tag + f"_B{i}", (128, 128)) for i in range(2)]
    Cm = [kb.sb(tag + f"_C{i}", (128, 128)) for i in range(2)]
    PT = [kb.sb(tag + f"_PT{i}", (128, 128)) for i in range(2)]
    PmT = kb.sb(tag + "_PmT", (128, 128))
    ecb = kb.sb(tag + "_ecb", (128, 128))
    qdT = kb.sb(tag + "_qdT", (128, 128))
    V0 = kb.sb(tag + "_V0", (128, 128))
    W0 = kb.sb(tag + "_W0", (128, 128))
    kdec = kb.sb(tag + "_kdec", (128, 128))
    upre = kb.sb(tag + "_upre", (128, 128))
    wT = kb.sb(tag + "_wT", (128, 128))
    u_sb = kb.sb(tag + "_u", (128, 128))
    kb.S.op("pool", lambda e: e.memset(u_sb[:], 0.0), [], [tag + "_u"])
    M = [kb.sb(tag + f"_M{h}", (128, 128)) for h in range(4)]
    for h in range(4):
        kb.S.op("pool", lambda e, h=h: e.memset(M[h][:], 0.0), [], [tag + f"_M{h}"])
    oint = kb.sb(tag + "_oint", (128, 512))
    o_sb = kb.sb(tag + "_o", (128, 512))
    og = kb.sb(tag + "_og", (128, 512))
    oT = kb.sb(tag + "_oT", (128, 512), BF16)
    junk = kb.sb(tag + "_junk", (128, 128), BF16)
    ssq = kb.sb(tag + "_ssq", (128, 4))
    rs = kb.sb(tag + "_rs", (128, 4))
    P0, P1, P2, P3, P4, P5, P6, P7 = kb.P
    ident = C["ident"]
    STOP = float(os.environ.get('GDN_STOP', '9'))
    for i in range(int(os.environ.get('GDN_NT', NT))):
        t0 = i * 128
        hk = hkey_fn(i)
        for c in range(12):
            bank = kb.P[c // 4]
            cc = (c % 4) * 128
            proj_feat(kb, bank[:, cc:cc + 128], wg, wk, O_QKV + c * 128, 128, hT, hk, t0, 128, kb.pk(c // 4, cc, cc + 128))
        proj_tok(kb, P3[:, :], wg, wk, O_G, 512, hT, hk, t0, 128, kb.pk(3))
        proj_tok(kb, P4[:, 0:8], wg, wk, O_AB, 8, hT, hk, t0, 128, kb.pk(4, 0, 128))
        kb.act(gsb[:], P3[:, :], AF.Silu, r=kb.pk(3), w=[tag + "_gsb"])
        for b3 in range(3):
            kb.cp("act", ubuf[:, b3 * 4:(b3 + 1) * 4, 3:131], kb.P[b3][:, :].rearrange("p (c t) -> p c t", c=4),
                  r=kb.pk(b3), w=[tag + "_ubuf"])
        for j in range(4):
            wj = convw[:, :, j:j + 1].to_broadcast([128, 12, 128])
            if j == 0:
                kb.tt("dve", cacc[:], ubuf[:, :, 0:128], wj, ALU.mult, r=[tag + "_ubuf", tag + "_cw"], w=[tag + "_cacc"])
            else:
                kb.tt("pool", ctmp[:], ubuf[:, :, j:j + 128], wj, ALU.mult, r=[tag + "_ubuf", tag + "_cw"], w=[tag + "_ctmp"])
                kb.tt("dve", cacc[:], cacc[:], ctmp[:], ALU.add, r=[tag + "_cacc", tag + "_ctmp"], w=[tag + "_cacc"])
        kb.act(qkv[:], cacc[:], AF.Silu, r=[tag + "_cacc"], w=[tag + "_qkv"])
        kb.cp("pool", ubuf[:, :, 0:3], ubuf[:, :, 128:131], r=[tag + "_ubuf"], w=[tag + "_ubuf"])
        if STOP <= 1:
            continue
        kb.tt("pool", sq[:], qkv[:, 0:8, :], qkv[:, 0:8, :], ALU.mult, r=[tag + "_qkv"], w=[tag + "_sq"])
        for half in range(2):
            kb.mm(kb.P[half][:, :], C["ones"][:], sq[:, half * 4:(half + 1) * 4, :], r=["c_ones", tag + "_sq"], w=kb.pk(half))
            kb.ts("dve", rinv[:, half * 4:(half + 1) * 4, :], kb.P[half][:, :].rearrange("p (c t) -> p c t", c=4),
                  1e-6, None, ALU.add, r=kb.pk(half), w=[tag + "_rinv"])
        kb.act(rinv[:], rinv[:], AF.Sqrt, r=[tag + "_rinv"], w=[tag + "_rinv"])
        kb.S.op("dve", lambda e: e.reciprocal(rinv[:], rinv[:]), [tag + "_rinv"], [tag + "_rinv"])
        kb.stt("dve", qkn[:, 0:4, :], qkv[:, 0:4, :], 128.0 ** -0.5, rinv[:, 0:4, :], ALU.mult, ALU.mult,
               r=[tag + "_qkv", tag + "_rinv"], w=[tag + "_qkn"])
        kb.tt("pool", qkn[:, 4:8, :], qkv[:, 4:8, :], rinv[:, 4:8, :], ALU.mult, r=[tag + "_qkv", tag + "_rinv"], w=[tag + "_qkn"])
        kb.act(beta, P4[:, 4:8], AF.Sigmoid, r=kb.pk(4, 0, 128), w=[tag + "_sm"])
        kb.tt("dve", ytmp, P4[:, 0:4], hp[:, 0:4], ALU.add, r=kb.pk(4, 0, 128) + [tag + "_hp"], w=[tag + "_sm"])
        kb.act(ytmp, ytmp, AF.Exp, r=[tag + "_sm"], w=[tag + "_sm"])
        kb.act(ytmp, ytmp, AF.Ln, bias=1.0, r=[tag + "_sm"], w=[tag + "_sm"])
        kb.tt("dve", gg, ytmp, negA[:], ALU.mult, r=[tag + "_sm", tag + "_negA"], w=[tag + "_sm"])
        kb.mm(P4[:, 8:12], C["tri_incl"][:], gg, r=["c_tri_incl", tag + "_sm"], w=kb.pk(4, 0, 128))
        kb.mm(P4[:, 12:16], C["blk"][:], gg, r=["c_blk", tag + "_sm"], w=kb.pk(4, 0, 128))
        kb.mm(P4[:, 16:20], C["selA"][:], gg, r=["c_selA", tag + "_sm"], w=kb.pk(4, 0, 128))
        kb.mm(P4[:, 20:24], C["selB"][:], gg, r=["c_selB", tag + "_sm"], w=kb.pk(4, 0, 128))
        kb.cp("dve", cum, P4[:, 8:12], r=kb.pk(4, 0, 128), w=[tag + "_sm"])
        kb.act(ecum, P4[:, 8:12], AF.Exp, r=kb.pk(4, 0, 128), w=[tag + "_sm"])
        kb.tt("dve", erem, P4[:, 12:16], cum, ALU.subtract, r=kb.pk(4, 0, 128) + [tag + "_sm"], w=[tag + "_sm"])
        kb.act(erem, erem, AF.Exp, r=[tag + "_sm"], w=[tag + "_sm"])
        kb.act(dA, P4[:, 16:20], AF.Exp, r=kb.pk(4, 0, 128), w=[tag + "_sm"])
        kb.act(dB, P4[:, 20:24], AF.Exp, r=kb.pk(4, 0, 128), w=[tag + "_sm"])
        kb.tt("dve", bec, beta, ecum, ALU.mult, r=[tag + "_sm"], w=[tag + "_sm"])
        if STOP <= 2:
            continue
        for h in range(4):
            qT = qkn[:, h, :]
            kT = qkn[:, 4 + h, :]
            vT = qkv[:, 8 + h, :]
            hc = slice(h * 128, (h + 1) * 128)
            kb.mm(P5[:, 0:128], kT, kT, r=[tag + "_qkn"], w=kb.pk(5, 0, 128))
            kb.mm(P5[:, 128:256], kT, qT, r=[tag + "_qkn"], w=kb.pk(5, 128, 256))
            kb.ts("dve", Gs[:], C["tri_incl"][:], gg[:, h:h + 1], None, ALU.mult, r=["c_tri_incl", tag + "_sm"], w=[tag + "_Gs"])
            kb.mm(P5[:, 256:384], Gs[:], C["tri_strict"][:], r=[tag + "_Gs", "c_tri_strict"], w=kb.pk(5, 256, 384))
            kb.mm(P5[:, 384:512], C["tri_strict"][:], Gs[:], r=[tag + "_Gs", "c_tri_strict"], w=kb.pk(5, 384, 512))
            kb.mm(P6[:, 0:128], C["ones"][:], Gs[:], r=[tag + "_Gs", "c_ones"], w=kb.pk(6, 0, 128))
            if STOP <= 2.2:
                continue
            if os.environ.get("GDN_X", "0") == "3":
                kb.cp("dve", E[:], P5[:, 0:128], r=kb.pk(5, 0, 128), w=[tag + "_E"])
            elif os.environ.get("GDN_X", "0") == "4":
                kb.cp("dve", E[:], P5[:, 384:512], r=kb.pk(5, 384, 512), w=[tag + "_E"])
            elif os.environ.get("GDN_X", "0") == "1":
                kb.cp("dve", E[:], P5[:, 256:384], r=kb.pk(5, 256, 384), w=[tag + "_E"])
            elif os.environ.get("GDN_X", "0") == "2":
                kb.act(E[:], P5[:, 256:384], AF.Identity, r=kb.pk(5, 256, 384), w=[tag + "_E"])
            else:
                kb.act(E[:], P5[:, 256:384], AF.Exp, r=kb.pk(5, 256, 384), w=[tag + "_E"])
            if STOP <= 2.21:
                continue
            kb.act(ET[:], P5[:, 384:512], AF.Exp, r=kb.pk(5, 384, 512), w=[tag + "_ET"])
            if STOP <= 2.23:
                continue
            kb.act(ecb[:], P6[:, 0:128], AF.Exp, r=kb.pk(6, 0, 128), w=[tag + "_ecb"])
            if STOP <= 2.25:
                continue
            kb.tt("pool", E[:], E[:], C["neg_strict"][:], ALU.mult, r=[tag + "_E", "c_neg_strict"], w=[tag + "_E"])
            kb.tt("pool", ET[:], ET[:], C["tri_incl"][:], ALU.mult, r=[tag + "_ET", "c_tri_incl"], w=[tag + "_ET"])
            if STOP <= 2.3:
                continue
            kb.stt("dve", Bm[0][:], P5[:, 0:128], beta[:, h:h + 1], E[:], ALU.mult, ALU.mult,
                   r=kb.pk(5, 0, 128) + [tag + "_sm", tag + "_E"], w=[tag + "_B0"])
            if STOP <= 2.35:
                continue
            kb.tt("dve", PmT[:], P5[:, 128:256], ET[:], ALU.mult, r=kb.pk(5, 128, 256) + [tag + "_ET"], w=[tag + "_PmT"])
            kb.tt("pool", qdT[:], qT, ecb[:], ALU.mult, r=[tag + "_qkn", tag + "_ecb"], w=[tag + "_qdT"])
            if STOP <= 2.4:
                continue
            kb.tr(P6[:, 128:256], Bm[0][:], ident[:], r=[tag + "_B0", "c_ident"], w=kb.pk(6, 128, 256))
            if STOP <= 2.42:
                continue
            kb.cp("act", Cm[0][:], P6[:, 128:256], r=kb.pk(6, 128, 256), w=[tag + "_C0"])
            if STOP <= 2.44:
                continue
            kb.tt("dve", PT[0][:], P6[:, 128:256], ident[:], ALU.add, r=kb.pk(6, 128, 256) + ["c_ident"], w=[tag + "_PT0"])
            if STOP <= 2.46:
                continue
            kb.tr(P6[:, 256:384], vT, ident[:], r=[tag + "_qkv", "c_ident"], w=kb.pk(6, 256, 384))
            kb.tr(P6[:, 384:512], kT, ident[:], r=[tag + "_qkn", "c_ident"], w=kb.pk(6, 384, 512))
            if STOP <= 2.5:
                continue
            kb.ts("dve", V0[:], P6[:, 256:384], beta[:, h:h + 1], None, ALU.mult, r=kb.pk(6, 256, 384) + [tag + "_sm"], w=[tag + "_V0"])
            if STOP <= 2.52:
                continue
            kb.act(W0[:], P6[:, 384:512], AF.Identity, scale=bec[:, h:h + 1], r=kb.pk(6, 384, 512) + [tag + "_sm"], w=[tag + "_W0"])
            if STOP <= 2.54:
                continue
            kb.ts("dve", kdec[:], P6[:, 384:512], erem[:, h:h + 1], None, ALU.mult, r=kb.pk(6, 384, 512) + [tag + "_sm"], w=[tag + "_kdec"])
            if STOP <= 2.6:
                continue
            cur = 0
            for j in range(1, 6):
                nxt = 1 - cur
                Bk, Ck, PTk = tag + f"_B{cur}", tag + f"_C{cur}", tag + f"_PT{cur}"
                Bn, Cn, PTn = tag + f"_B{nxt}", tag + f"_C{nxt}", tag + f"_PT{nxt}"
                kb.mm(P7[:, 0:128], Cm[cur][:], Bm[cur][:], r=[Bk, Ck], w=kb.pk(7, 0, 128))
                if j < 5:
                    kb.mm(P7[:, 128:256], Bm[cur][:], Cm[cur][:], r=[Bk, Ck], w=kb.pk(7, 128, 256))
                kb.cp("dve", Bm[nxt][:], P7[:, 0:128], r=kb.pk(7, 0, 128), w=[Bn])
                if j < 5:
                    kb.cp("act", Cm[nxt][:], P7[:, 128:256], r=kb.pk(7, 128, 256), w=[Cn])
                kb.mm(P7[:, 256:384], Bm[nxt][:], PT[cur][:], r=[Bn, PTk], w=kb.pk(7, 256, 384))
                kb.tt("dve", PT[nxt][:], P7[:, 256:384], PT[cur][:], ALU.add, r=kb.pk(7, 256, 384) + [PTk], w=[PTn])
                cur = nxt
            if STOP <= 2.8:
                continue
            PTf, PTfk = PT[cur], tag + f"_PT{cur}"
            kb.mm(P7[:, 384:512], PTf[:], V0[:], r=[PTfk, tag + "_V0"], w=kb.pk(7, 384, 512))
            kb.mm(P2[:, 0:128], W0[:], PTf[:], r=[PTfk, tag + "_W0"], w=kb.pk(2, 0, 128))
            kb.cp("act", upre[:], P7[:, 384:512], r=kb.pk(7, 384, 512), w=[tag + "_upre"])
            kb.cp("dve", wT[:], P2[:, 0:128], r=kb.pk(2, 0, 128), w=[tag + "_wT"])
            if STOP <= 3:
                continue
            Mk = tag + f"_M{h}"
            for ch in range(2):
                tr_ = slice(ch * 64, (ch + 1) * 64)
                dch = dA if ch == 0 else dB
                kb.mm(P2[tr_, 128:256], wT[:, tr_], M[h][:], r=[tag + "_wT", Mk], w=kb.pk(2, 128, 256))
                kb.mm(P1[tr_, hc], qdT[:, tr_], M[h][:], r=[tag + "_qdT", Mk], w=kb.pk(1, h * 128, h * 128 + 128))
                kb.tt("dve", u_sb[tr_, :], upre[tr_, :], P2[tr_, 128:256], ALU.subtract,
                      r=[tag + "_upre"] + kb.pk(2, 128, 256), w=[tag + "_u"])
                kb.mm(P0[tr_, hc], PmT[:, tr_], u_sb[:, :], r=[tag + "_PmT", tag + "_u"], w=kb.pk(0, h * 128, h * 128 + 128))
                kb.mm(P2[:, 256:384], kdec[tr_, :], u_sb[tr_, :], r=[tag + "_kdec", tag + "_u"], w=kb.pk(2, 256, 384))
                kb.stt("dve", M[h][:], M[h][:], dch[:, h:h + 1], P2[:, 256:384], ALU.mult, ALU.add,
                       r=[Mk, tag + "_sm"] + kb.pk(2, 256, 384), w=[Mk])
        if STOP <= 4:
            continue
        kb.cp("act", oint[:], P1[:, :], r=kb.pk(1), w=[tag + "_oint"])
        kb.tt("dve", o_sb[:], P0[:, :], oint[:], ALU.add, r=kb.pk(0) + [tag + "_oint"], w=[tag + "_o"])
        for h in range(4):
            kb.act(junk[:], o_sb[:, h * 128:(h + 1) * 128], AF.Square, accum_out=ssq[:, h:h + 1], r=[tag + "_o"],
                   w=[tag + "_junk", tag + "_ssq"])
        rms_rstd(kb, tag, rs, ssq, 4, 128)
        for h in range(4):
            hc = slice(h * 128, (h + 1) * 128)
            kb.stt("dve", og[:, hc], o_sb[:, hc], rs[:, h:h + 1], gn[:, hc], ALU.mult, ALU.mult,
                   r=[tag + "_o", tag + "_rs", tag + "_gn"], w=[tag + "_og"])
        kb.tt("pool", og[:], og[:], gsb[:], ALU.mult, r=[tag + "_og", tag + "_gsb"], w=[tag + "_og"])
        for c in range(4):
            kb.tr(P3[:, c * 128:(c + 1) * 128], og[:, c * 128:(c + 1) * 128], ident[:], r=[tag + "_og", "c_ident"],
                  w=kb.pk(3, c * 128, c * 128 + 128))
        kb.cp("act", oT[:], P3[:, :], r=kb.pk(3), w=[tag + "_oT"])
        kb.dma("sp", obr[i], oT[:], sem=tag + "_oT", r=[tag + "_oT"], w=[f"{tag}_obr{i}"])


def phase_ssd(kb, l, hT, hkey_fn, obr):
    import os
    C = kb.C
    tag = f"ssd{l}"
    w_in = kb.w_in
    NW = 1288
    wk = tag + "_w"
    wg = kb.sb(wk, (128, KC, NW), BF16)
    load_w_cast(kb, wg, wk, w_in[l], C_SSD_Z, NW)
    O_Z, O_XBC, O_DT = 0, 512, 1280
    convw = kb.sb(tag + "_cw", (128, 6, 5))
    kb.dma("sp", convw[:], kb.din(tag + "_cwd", (128, 6, 5)), sem=tag + "_cw", w=[tag + "_cw"])
    gn = kb.sb(tag + "_gn", (128, 512))
    kb.dma("sp", gn[:], kb.din(tag + "_gnd", (1, 512))[0].partition_broadcast(128), sem=tag + "_gn", w=[tag + "_gn"])
    hp = kb.sb(tag + "_hp", (128, 24))
    kb.dma("sp", hp[:], kb.din(tag + "_hpd", (1, 24))[0].partition_broadcast(128), sem=tag + "_hp", w=[tag + "_hp"])
    negA = kb.sb(tag + "_negA", (128, 8))
    kb.act(negA[:], hp[:, 8:16], AF.Exp, r=[tag + "_hp"], w=[tag + "_negA"])
    kb.ts("dve", negA[:], negA[:], -1.0, None, ALU.mult, r=[tag + "_negA"], w=[tag + "_negA"])
    ubuf = kb.sb(tag + "_ubuf", (128, 6, 131))
    kb.S.op("pool", lambda e: e.memset(ubuf[:], 0.0), [], [tag + "_ubuf"])
    cacc = kb.sb(tag + "_cacc", (128, 6, 128))
    ctmp = kb.sb(tag + "_ctmp", (128, 6, 128))
    xbc = kb.sb(tag + "_xbc", (128, 6, 128))
    zs = kb.sb(tag + "_zs", (128, 512))
    sm = kb.sb(tag + "_sm", (128, 72))
    dt, gg, cum, ecum, erem = sm[:, 0:8], sm[:, 8:16], sm[:, 16:24], sm[:, 24:32], sm[:, 32:40]
    dA, dB, ytmp = sm[:, 40:48], sm[:, 48:56], sm[:, 56:64]
    x_tok = kb.sb(tag + "_xtok", (128, 512))
    xdt = kb.sb(tag + "_xdt", (128, 512))
    xdte = kb.sb(tag + "_xdte", (128, 512))
    xd = kb.sb(tag + "_xd", (128, 512))
    B_tok = kb.sb(tag + "_Btok", (128, 128))
    CBT = [kb.sb(tag + f"_CBT{g}", (128, 128)) for g in range(2)]
    Gs = kb.sb(tag + "_Gs", (128, 128))
    LT = kb.sb(tag + "_LT", (128, 128))
    Sbd = kb.sb(tag + "_Sbd", (128, 512))
    kb.S.op("pool", lambda e: e.memset(Sbd[:], 0.0), [], [tag + "_Sbd"])
    yint = kb.sb(tag + "_yint", (128, 512))
    y_sb = kb.sb(tag + "_y", (128, 512))
    oT = kb.sb(tag + "_oT", (128, 512), BF16)
    junk = kb.sb(tag + "_junk", (128, 256), BF16)
    ssq = kb.sb(tag + "_ssq", (128, 2))
    rs = kb.sb(tag + "_rs", (128, 2))
    P0, P1, P2, P3, P4, P5, P6, P7 = kb.P
    ident = C["ident"]
    STOP = float(os.environ.get('SSD_STOP', '9'))
    for i in range(int(os.environ.get('SSD_NT', NT))):
        t0 = i * 128
        hk = hkey_fn(i)
        for c in range(6):
            bank = kb.P[c // 4]
            cc = (c % 4) * 128
            proj_feat(kb, bank[:, cc:cc + 128], wg, wk, O_XBC + c * 128, 128, hT, hk, t0, 128, kb.pk(c // 4))
        proj_tok(kb, P2[:, :], wg, wk, O_Z, 512, hT, hk, t0, 128, kb.pk(2))
        proj_tok(kb, P3[:, 0:8], wg, wk, O_DT, 8, hT, hk, t0, 128, kb.pk(3))
        kb.act(zs[:], P2[:, :], AF.Silu, r=kb.pk(2), w=[tag + "_zs"])
        kb.cp("act", ubuf[:, 0:4, 3:131], P0[:, :].rearrange("p (c t) -> p c t", c=4), r=kb.pk(0), w=[tag + "_ubuf"])
        kb.cp("act", ubuf[:, 4:6, 3:131], P1[:, 0:256].rearrange("p (c t) -> p c t", c=2), r=kb.pk(1), w=[tag + "_ubuf"])
        for j in range(4):
            wj = convw[:, :, j:j + 1].to_broadcast([128, 6, 128])
            if j == 0:
                kb.tt("dve", cacc[:], ubuf[:, :, 0:128], wj, ALU.mult, r=[tag + "_ubuf", tag + "_cw"], w=[tag + "_cacc"])
                kb.tt("dve", cacc[:], cacc[:], convw[:, :, 4:5].to_broadcast([128, 6, 128]), ALU.add,
                      r=[tag + "_cacc", tag + "_cw"], w=[tag + "_cacc"])
            else:
                kb.tt("pool", ctmp[:], ubuf[:, :, j:j + 128], wj, ALU.mult, r=[tag + "_ubuf", tag + "_cw"], w=[tag + "_ctmp"])
                kb.tt("dve", cacc[:], cacc[:], ctmp[:], ALU.add, r=[tag + "_cacc", tag + "_ctmp"], w=[tag + "_cacc"])
        kb.act(xbc[:], cacc[:], AF.Silu, r=[tag + "_cacc"], w=[tag + "_xbc"])
        kb.cp("pool", ubuf[:, :, 0:3], ubuf[:, :, 128:131], r=[tag + "_ubuf"], w=[tag + "_ubuf"])
        kb.tt("dve", ytmp, P3[:, 0:8], hp[:, 0:8], ALU.add, r=kb.pk(3) + [tag + "_hp"], w=[tag + "_sm"])
        kb.act(ytmp, ytmp, AF.Exp, r=[tag + "_sm"], w=[tag + "_sm"])
        kb.act(dt, ytmp, AF.Ln, bias=1.0, r=[tag + "_sm"], w=[tag + "_sm"])
        kb.tt("dve", gg, dt, negA[:], ALU.mult, r=[tag + "_sm", tag + "_negA"], w=[tag + "_sm"])
        kb.mm(P3[:, 8:16], C["tri_incl"][:], gg, r=["c_tri_incl", tag + "_sm"], w=kb.pk(3))
        kb.mm(P3[:, 16:24], C["blk"][:], gg, r=["c_blk", tag + "_sm"], w=kb.pk(3))
        kb.mm(P3[:, 24:32], C["selA"][:], gg, r=["c_selA", tag + "_sm"], w=kb.pk(3))
        kb.mm(P3[:, 32:40], C["selB"][:], gg, r=["c_selB", tag + "_sm"], w=kb.pk(3))
        kb.cp("dve", cum, P3[:, 8:16], r=kb.pk(3), w=[tag + "_sm"])
        kb.tt("dve", erem, P3[:, 16:24], cum, ALU.subtract, r=kb.pk(3) + [tag + "_sm"], w=[tag + "_sm"])
        kb.act(dA, P3[:, 24:32], AF.Exp, r=kb.pk(3), w=[tag + "_sm"])
        kb.act(dB, P3[:, 32:40], AF.Exp, r=kb.pk(3), w=[tag + "_sm"])
        kb.act(ecum, cum, AF.Exp, r=[tag + "_sm"], w=[tag + "_sm"])
        kb.act(erem, erem, AF.Exp, r=[tag + "_sm"], w=[tag + "_sm"])
        if STOP <= 1:
            continue
        for c in range(4):
            kb.tr(P4[:, c * 128:(c + 1) * 128], xbc[:, c, :], ident[:], r=[tag + "_xbc", "c_ident"], w=kb.pk(4))
        kb.tr(P5[:, 0:128], xbc[:, 4, :], ident[:], r=[tag + "_xbc", "c_ident"], w=kb.pk(5))
        kb.cp("act", x_tok[:], P4[:, :], r=kb.pk(4), w=[tag + "_xtok"])
        kb.cp("act", B_tok[:], P5[:, 0:128], r=kb.pk(5), w=[tag + "_Btok"])
        v3 = lambda t: t[:, :].rearrange("p (h d) -> p h d", h=8)
        bc8 = lambda a: a.unsqueeze(2).to_broadcast([128, 8, 64])
        kb.tt("dve", v3(xdt), v3(x_tok), bc8(dt), ALU.mult, r=[tag + "_xtok", tag + "_sm"], w=[tag + "_xdt"])
        kb.tt("pool", v3(xd), v3(x_tok), bc8(hp[:, 16:24]), ALU.mult, r=[tag + "_xtok", tag + "_hp"], w=[tag + "_xd"])
        kb.tt("pool", v3(xdte), v3(xdt), bc8(erem), ALU.mult, r=[tag + "_xdt", tag + "_sm"], w=[tag + "_xdte"])
        for g in range(2):
            rows = slice(g * 64, (g + 1) * 64)
            bank = P5 if g == 0 else P6
            kb.mm(bank[:, 128:256], xbc[rows, 4, :], xbc[rows, 5, :], r=[tag + "_xbc"], w=kb.pk(5 + g))
            kb.cp("act", CBT[g][:], bank[:, 128:256], r=kb.pk(5 + g), w=[tag + f"_CBT{g}"])
        if STOP <= 2:
            continue
        for h in range(8):
            g = h // 4
            kb.ts("dve", Gs[:], C["tri_incl"][:], gg[:, h:h + 1], None, ALU.mult, r=["c_tri_incl", tag + "_sm"], w=[tag + "_Gs"])
            kb.mm(P6[:, 256:384], C["tri_strict"][:], Gs[:], r=[tag + "_Gs", "c_tri_strict"], w=kb.pk(6))
            kb.act(LT[:], P6[:, 256:384], AF.Exp, r=kb.pk(6), w=[tag + "_LT"])
            kb.tt("pool", LT[:], LT[:], C["tri_incl"][:], ALU.mult, r=[tag + "_LT", "c_tri_incl"], w=[tag + "_LT"])
            kb.tt("dve", LT[:], LT[:], CBT[g][:], ALU.mult, r=[tag + "_LT", tag + f"_CBT{g}"], w=[tag + "_LT"])
            kb.mm(P7[:, h * 64:(h + 1) * 64], LT[:], xdt[:, h * 64:(h + 1) * 64], r=[tag + "_LT", tag + "_xdt"], w=kb.pk(7))
        if STOP <= 3:
            continue
        for ch in range(2):
            tr_ = slice(ch * 64, (ch + 1) * 64)
            dch = dA if ch == 0 else dB
            kb.mm(P0[tr_, :], xbc[:, 5, tr_], Sbd[:, :], r=[tag + "_xbc", tag + "_Sbd"], w=kb.pk(0))
            kb.mm(P1[:, :], B_tok[tr_, :], xdte[tr_, :], r=[tag + "_Btok", tag + "_xdte"], w=kb.pk(1))
            for g in range(2):
                rr = slice(g * 64, (g + 1) * 64)
                cc = slice(g * 256, (g + 1) * 256)
                s3 = Sbd[rr, cc].rearrange("p (h d) -> p h d", h=4)
                kb.tt("dve", s3, s3, dch[rr, g * 4:(g + 1) * 4].unsqueeze(2).to_broadcast([64, 4, 64]), ALU.mult,
                      r=[tag + "_Sbd", tag + "_sm"], w=[tag + "_Sbd"])
                kb.tt("dve", Sbd[rr, cc], Sbd[rr, cc], P1[rr, cc], ALU.add, r=[tag + "_Sbd"] + kb.pk(1), w=[tag + "_Sbd"])
        kb.cp("act", yint[:], P0[:, :], r=kb.pk(0), w=[tag + "_yint"])
        kb.tt("pool", v3(yint), v3(yint), bc8(ecum), ALU.mult, r=[tag + "_yint", tag + "_sm"], w=[tag + "_yint"])
        kb.tt("dve", y_sb[:], P7[:, :], yint[:], ALU.add, r=kb.pk(7) + [tag + "_yint"], w=[tag + "_y"])
        kb.tt("pool", y_sb[:], y_sb[:], xd[:], ALU.add, r=[tag + "_y", tag + "_xd"], w=[tag + "_y"])
        kb.tt("pool", y_sb[:], y_sb[:], zs[:], ALU.mult, r=[tag + "_y", tag + "_zs"], w=[tag + "_y"])
        for g in range(2):
            kb.act(junk[:], y_sb[:, g * 256:(g + 1) * 256], AF.Square, accum_out=ssq[:, g:g + 1], r=[tag + "_y"],
                   w=[tag + "_junk", tag + "_ssq"])
        rms_rstd(kb, tag, rs, ssq, 2, 256)
        for g in range(2):
            gc = slice(g * 256, (g + 1) * 256)
            kb.stt("dve", y_sb[:, gc], y_sb[:, gc], rs[:, g:g + 1], gn[:, gc], ALU.mult, ALU.mult,
                   r=[tag + "_y", tag + "_rs", tag + "_gn"], w=[tag + "_y"])
        for c in range(4):
            kb.tr(P4[:, c * 128:(c + 1) * 128], y_sb[:, c * 128:(c + 1) * 128], ident[:], r=[tag + "_y", "c_ident"], w=kb.pk(4))
        kb.cp("act", oT[:], P4[:, :], r=kb.pk(4), w=[tag + "_oT"])
        kb.dma("sp", obr[i], oT[:], sem=tag + "_oT", r=[tag + "_oT"], w=[f"{tag}_obr{i}"])


def phase_merge(kb, l, hT, hkey_fn, obrs, obr_keys, xsrc, xsrc_key, xdst, xdst_key):
    C = kb.C
    tag = f"mrg{l}"
    wm = kb.sb(tag + "_wm", (128, KC, 3072), BF16)
    load_w_cast(kb, wm, tag + "_wm", kb.w_in[l], C_MERGE, 3072)
    wbr = []
    for b, nm in enumerate(("w_branch_gla", "w_branch_gdn", "w_branch_ssd")):
        t = kb.sb(tag + f"_wb{b}", (128, 4, D), BF16)
        load_w_cast(kb, t, tag + f"_wb{b}", kb.dins[nm][l], 0, D, nk=4)
        wbr.append(t)
    wo = kb.sb(tag + "_wo", (128, KC, D), BF16)
    load_w_cast(kb, wo, tag + "_wo", kb.dins["w_out"][l], 0, D)
    bmb = kb.sb(tag + "_bmb", (1, 3072), BF16)
    kb.dma("pool", bmb[:], kb.din(tag + "_bmd", (1, 3072)), sem=tag + "_bmb", w=[tag + "_bmb"])
    gm_row, gm_key = kb.gm_row[l]
    ob = [kb.sb(tag + f"_ob{b}", (128, 512), BF16) for b in range(3)]
    sig = kb.sb(tag + "_sig", (128, 512))
    acc = kb.sb(tag + "_acc", (128, 512))
    tmp = kb.sb(tag + "_tmp", (128, 512))
    mT = kb.sb(tag + "_mT", (128, KC, 128), BF16)
    xt = kb.sb(tag + "_xt", (128, D))
    xo = kb.sb(tag + "_xo", (128, D))
    P = kb.P
    for i in range(NT):
        t0 = i * 128
        hk = hkey_fn(i)
        for b in range(3):
            kb.dma("sp", ob[b][:], obrs[b][i], sem=tag + f"_ob{b}", r=[obr_keys[b](i)], w=[tag + f"_ob{b}"])
        kb.dma("sp", xt[:], xsrc[t0:t0 + 128, :], sem=tag + "_xt", r=[xsrc_key(i)], w=[tag + "_xt"])
        for half in range(2):
            for b in range(3):
                PG, PY = P[(b % 2) * 2], P[(b % 2) * 2 + 1]
                kg, ky = kb.pk((b % 2) * 2), kb.pk((b % 2) * 2 + 1)
                for jj in range(4):
                    j = half * 4 + jj
                    col = b * D + j * 128
                    zone = slice(jj * 128, (jj + 1) * 128)
                    for k in range(KC):
                        kb.mm(PG[:, zone], wm[:, k, col:col + 128], hT[:, k, t0:t0 + 128], start=(k == 0), stop=False,
                              r=[tag + "_wm", hk], w=kg)
                    kb.mm(PG[:, zone], bmb[0:1, col:col + 128], C["ones_bf"][0:1, :], start=False, stop=True,
                          r=[tag + "_bmb", "cb_ones"], w=kg)
                    for c in range(4):
                        kb.mm(PY[:, zone], wbr[b][:, c, j * 128:(j + 1) * 128], ob[b][:, c * 128:(c + 1) * 128],
                              start=(c == 0), stop=(c == 3), r=[tag + f"_wb{b}", tag + f"_ob{b}"], w=ky)
                kb.act(sig[:], PG[:, :], AF.Sigmoid, r=kg, w=[tag + "_sig"])
                if b == 0:
                    kb.tt("dve", acc[:], PY[:, :], sig[:], ALU.mult, r=ky + [tag + "_sig"], w=[tag + "_acc"])
                else:
                    kb.tt("dve", tmp[:], PY[:, :], sig[:], ALU.mult, r=ky + [tag + "_sig"], w=[tag + "_tmp"])
                    kb.tt("pool", acc[:], acc[:], tmp[:], ALU.add, r=[tag + "_acc", tag + "_tmp"], w=[tag + "_acc"])
            kb.cp("act", mT[:, half * 4:(half + 1) * 4, :], acc[:, :].rearrange("p (j t) -> p j t", j=4),
                  r=[tag + "_acc"], w=[tag + "_mT"])
        for half in range(2):
            PO, ko = P[4 + half], kb.pk(4 + half)
            for j in range(KC):
                kb.mm(PO[:, :], mT[:, j, :], wo[:, j, half * 512:(half + 1) * 512], start=(j == 0), stop=(j == KC - 1),
                      r=[tag + "_mT", tag + "_wo"], w=ko)
            hs = slice(half * 512, (half + 1) * 512)
            kb.tt("dve", xo[:, hs], PO[:, :], gm_row[:, hs], ALU.mult, r=ko + [gm_key], w=[tag + "_xo"])
            kb.tt("pool", xo[:, hs], xo[:, hs], xt[:, hs], ALU.add, r=[tag + "_xo", tag + "_xt"], w=[tag + "_xo"])
        kb.dma("sp", xdst[t0:t0 + 128, :], xo[:], sem=tag + "_xo", r=[tag + "_xo"], w=[xdst_key(i)])


def phase_moe(kb, l, xsrc, xsrc_key, xdst, xdst_key, final=None):
    import os
    C = kb.C
    tag = f"moe{l}"
    P = kb.P
    TS = 512
    NSUP = S_TOK // TS
    NE = int(os.environ.get("MOE_NE", 32))
    wr = kb.sb(tag + "_wr", (128, KC, 32), BF16)
    load_w_cast(kb, wr, tag + "_wr", kb.dins["w_router"][l], 0, 32)
    brb = kb.sb(tag + "_brb", (1, 32), BF16)
    kb.dma("pool", brb[:], kb.din(tag + "_brd", (1, 32)), sem=tag + "_brb", w=[tag + "_brb"])
    ones5 = kb.sb(tag + "_ones5", (1, 512), BF16)
    kb.S.op("dve", lambda e: e.memset(ones5[:], 1.0), [], [tag + "_ones5"])
    gf_row, gf_key = kb.gf_row[l]
    nb = norm_bufs(kb, tag + "_n")
    hTs = kb.sb(tag + "_hT", (128, KC, TS), BF16)
    G = kb.sb(tag + "_G", (128, 4, 32))
    lg = kb.sb(tag + "_lg", (128, 32))
    v8 = kb.sb(tag + "_v8", (128, 8))
    msk = kb.sb(tag + "_msk", (128, 32))
    sml = kb.sb(tag + "_sml", (128, 4))
    wgu = [kb.sb(tag + f"_wgu{i}", (128, KC, 2048), BF16) for i in range(2)]
    wd = [kb.sb(tag + f"_wd{i}", (128, KC, D), BF16) for i in range(2)]
    bgu = [kb.sb(tag + f"_bgu{i}", (1, 2048), BF16) for i in range(2)]
    bd = [kb.sb(tag + f"_bd{i}", (1, D), BF16) for i in range(2)]
    yacc = kb.sb(tag + "_yacc", (128, 4, D))
    actT = kb.sb(tag + "_actT", (128, KC, TS), BF16)
    g7 = kb.sb(tag + "_g7", (128, TS))
    sg = kb.sb(tag + "_sg", (128, TS))
    u7 = kb.sb(tag + "_u7", (128, TS))
    xt = kb.sb(tag + "_xt", (128, D))
    xo = kb.sb(tag + "_xo", (128, D))
    if final is not None:
        nfr = kb.sb(tag + "_nfr", (128, D))
        kb.dma("sp", nfr[:], final["nf"].partition_broadcast(128), sem=tag + "_nfr", w=[tag + "_nfr"])
        fj = kb.sb(tag + "_fj", (128, D), BF16)
        fs = kb.sb(tag + "_fs", (128, 2))
    w_gu_d, w_d_d = kb.dins["w_gate_up"][l], kb.dins["w_down"][l]
    b_gu_d, b_d_d = kb.dins["b_gate_up"][l], kb.dins["b_down"][l]

    def load_expert(e, slot):
        srcg = w_gu_d[e].rearrange("(k p) c -> p k c", p=128)
        srcd = w_d_d[e].rearrange("(k p) c -> p k c", p=128)
        for k in range(KC):
            kb.dma("pool", wgu[slot][:, k, :], srcg[:, k, :], sem=tag + f"_wgu{slot}", w=[tag + f"_wgu{slot}"])
        for k in range(KC):
            kb.dma("pool", wd[slot][:, k, :], srcd[:, k, :], sem=tag + f"_wd{slot}", w=[tag + f"_wd{slot}"])
        kb.dma("pool", bgu[slot][:], b_gu_d[e:e + 1, :], sem=tag + f"_bgu{slot}", w=[tag + f"_bgu{slot}"])
        kb.dma("pool", bd[slot][:], b_d_d[e:e + 1, :], sem=tag + f"_bd{slot}", w=[tag + f"_bd{slot}"])

    it = 0
    for T in range(int(os.environ.get("MOE_NSUP", NSUP))):
        for tt in range(4):
            i = T * 4 + tt
            norm_tile(kb, nb, l, "f", xsrc[i * 128:(i + 1) * 128, :], xsrc_key(i), hTs[:, :, tt * 128:(tt + 1) * 128], tag + "_hT")
        for tt in range(4):
            for k in range(KC):
                kb.mm(P[7][:, 0:32], hTs[:, k, tt * 128:(tt + 1) * 128], wr[:, k, :], start=(k == 0), stop=False,
                      r=[tag + "_hT", tag + "_wr"], w=kb.pk(7))
            kb.mm(P[7][:, 0:32], ones5[0:1, 0:128], brb[0:1, :], start=False, stop=True, r=[tag + "_ones5", tag + "_brb"], w=kb.pk(7))
            kb.cp("dve", lg[:], P[7][:, 0:32], r=kb.pk(7), w=[tag + "_lg"])
            kb.S.op("dve", lambda e: e.max(out=v8[:], in_=lg[:]), [tag + "_lg"], [tag + "_v8"])
            kb.ts("dve", msk[:], lg[:], v8[:, 3:4], None, ALU.is_ge, r=[tag + "_lg", tag + "_v8"], w=[tag + "_msk"])
            kb.ts("dve", sml[:, 0:1], v8[:, 0:1], -1.0, None, ALU.mult, r=[tag + "_v8"], w=[tag + "_sml"])
            kb.act(lg[:], lg[:], AF.Exp, bias=sml[:, 0:1], r=[tag + "_lg", tag + "_sml"], w=[tag + "_lg"])
            kb.tt("dve", lg[:], lg[:], msk[:], ALU.mult, r=[tag + "_lg", tag + "_msk"], w=[tag + "_lg"])
            kb.S.op("dve", lambda e: e.reduce_sum(sml[:, 1:2], lg[:], AX.X), [tag + "_lg"], [tag + "_sml"])
            kb.S.op("dve", lambda e: e.reciprocal(sml[:, 1:2], sml[:, 1:2]), [tag + "_sml"], [tag + "_sml"])
            kb.ts("dve", G[:, tt, :], lg[:], sml[:, 1:2], None, ALU.mult, r=[tag + "_lg", tag + "_sml"], w=[tag + "_G"])
        kb.S.op("pool", lambda e: e.memset(yacc[:], 0.0), [], [tag + "_yacc"])
        for e_ in range(NE):
            slot = it % 2
            if it == 0:
                load_expert(e_, slot)
            nxt = (e_ + 1) % NE
            if not (T == NSUP - 1 and e_ == NE - 1):
                load_expert(nxt, 1 - slot)
            it += 1
            wk, dk, bgk, bdk = tag + f"_wgu{slot}", tag + f"_wd{slot}", tag + f"_bgu{slot}", tag + f"_bd{slot}"
            for jc in range(KC):
                pb = (jc % 2) * 2
                PG, PU = P[pb], P[pb + 1]
                for which, PX in ((0, PG), (1, PU)):
                    cols = slice(jc * 256 + which, (jc + 1) * 256, 2)
                    for k in range(KC):
                        kb.mm(PX[:, :], wgu[slot][:, k, cols], hTs[:, k, :], start=(k == 0), stop=False,
                              r=[wk, tag + "_hT"], w=kb.pk(pb + which))
                    kb.mm(PX[:, :], bgu[slot][0:1, cols], ones5[0:1, :], start=False, stop=True,
                          r=[bgk, tag + "_ones5"], w=kb.pk(pb + which))
                kb.ts("dve", g7[:], PG[:, :], SW_LIMIT, None, ALU.min, r=kb.pk(pb), w=[tag + "_g7"])
                kb.ts("dve", u7[:], PU[:, :], -SW_LIMIT, SW_LIMIT, ALU.max, ALU.min, r=kb.pk(pb + 1), w=[tag + "_u7"])
                kb.act(sg[:], g7[:], AF.Sigmoid, scale=SW_ALPHA, r=[tag + "_g7"], w=[tag + "_sg"])
                kb.ts("pool", u7[:], u7[:], 1.0, None, ALU.add, r=[tag + "_u7"], w=[tag + "_u7"])
                kb.tt("pool", u7[:], u7[:], g7[:], ALU.mult, r=[tag + "_u7", tag + "_g7"], w=[tag + "_u7"])
                kb.tt("pool", actT[:, jc, :], u7[:], sg[:], ALU.mult, r=[tag + "_u7", tag + "_sg"], w=[tag + "_actT"])
            for tt in range(4):
                for half in range(2):
                    pi = 4 + (tt % 2) * 2 + half
                    PO = P[pi]
                    hs = slice(half * 512, (half + 1) * 512)
                    for jc in range(KC):
                        kb.mm(PO[:, :], actT[:, jc, tt * 128:(tt + 1) * 128], wd[slot][:, jc, hs], start=(jc == 0), stop=False,
                              r=[tag + "_actT", dk], w=kb.pk(pi))
                    kb.mm(PO[:, :], ones5[0:1, 0:128], bd[slot][0:1, hs], start=False, stop=True,
                          r=[tag + "_ones5", bdk], w=kb.pk(pi))
                    kb.stt("dve", yacc[:, tt, hs], PO[:, :], G[:, tt, e_:e_ + 1], yacc[:, tt, hs], ALU.mult, ALU.add,
                           r=kb.pk(pi) + [tag + "_G", tag + "_yacc"], w=[tag + "_yacc"])
        for tt in range(4):
            i = T * 4 + tt
            kb.dma("sp", xt[:], xsrc[i * 128:(i + 1) * 128, :], sem=tag + "_xt", r=[xsrc_key(i)], w=[tag + "_xt"])
            kb.tt("dve", xo[:], yacc[:, tt, :], gf_row[:], ALU.mult, r=[tag + "_yacc", gf_key], w=[tag + "_xo"])
            kb.tt("pool", xo[:], xo[:], xt[:], ALU.add, r=[tag + "_xo", tag + "_xt"], w=[tag + "_xo"])
            if final is None:
                kb.dma("sp", xdst[i * 128:(i + 1) * 128, :], xo[:], sem=tag + "_xo", r=[tag + "_xo"], w=[xdst_key(i)])
            else:
                kb.act(fj[:], xo[:], AF.Square, accum_out=fs[:, 0:1], r=[tag + "_xo"], w=[tag + "_fj", tag + "_fs"])
                kb.ts("dve", fs[:, 1:2], fs[:, 0:1], 1.0 / D, EPS, ALU.mult, ALU.add, r=[tag + "_fs"], w=[tag + "_fs"])
                kb.act(fs[:, 1:2], fs[:, 1:2], AF.Sqrt, r=[tag + "_fs"], w=[tag + "_fs"])
                kb.S.op("dve", lambda e: e.reciprocal(fs[:, 1:2], fs[:, 1:2]), [tag + "_fs"], [tag + "_fs"])
                kb.stt("dve", xo[:], xo[:], fs[:, 1:2], nfr[:], ALU.mult, ALU.mult, r=[tag + "_xo", tag + "_fs", tag + "_nfr"], w=[tag + "_xo"])
                kb.dma("sp", final["out"][i * 128:(i + 1) * 128, :], xo[:], sem=tag + "_xo", r=[tag + "_xo"], w=[f"out{i}"])


SW_LIMIT = 7.0
SW_ALPHA = 1.702


W_SHAPES = {
    "w_branch_gla": (DEPTH, 512, D), "w_branch_gdn": (DEPTH, 512, D), "w_branch_ssd": (DEPTH, 512, D),
    "w_out": (DEPTH, D, D), "w_router": (DEPTH, D, 32),
    "w_gate_up": (DEPTH, 32, D, 2 * D), "b_gate_up": (DEPTH, 32, 2 * D),
    "w_down": (DEPTH, 32, D, D), "b_down": (DEPTH, 32, D),
}


def build_program(layers=(0, 1), do_mix=True, do_moe=True, dbg=None, same_engine_sync=True):
    kb = KB(same_engine_sync=same_engine_sync)
    phase_consts(kb)
    x = kb.din("x", (S_TOK, D))
    kb.w_in = kb.din("w_in", (DEPTH, D, IN_COLS))
    kb.dins = {nm: kb.din(nm, shp) for nm, shp in W_SHAPES.items()}
    nf = kb.din("norm_final", (D,))
    out = kb.dout("out", (S_TOK, D))
    phase_mod(kb)
    xin, xin_key = x, (lambda i: "x_in")
    last = layers[-1]
    for l in layers:
        xmid = kb.dscr(f"xmid{l}", (S_TOK, D), debug=(dbg == "xmid" and l == layers[0]))
        xmid_key = (lambda i, l=l: f"xmid{l}_{i}")
        if do_mix:
            obr = [kb.dscr(f"obr{l}_{b}", (NT, 128, 512), BF16) for b in range(3)]
            kb.push_scope()
            hT = kb.sb(f"hT{l}", (128, KC, S_TOK), BF16)
            hk = (lambda i, l=l: f"hT{l}_{i}")
            kb.push_scope(); phase_norm(kb, l, "m", xin, xin_key, hT, hk); kb.pop_scope()
            kb.push_scope(); phase_gla(kb, l, hT, hk, obr[0]); kb.pop_scope()
            kb.push_scope(); phase_gdn(kb, l, hT, hk, obr[1]); kb.pop_scope()
            kb.push_scope(); phase_ssd(kb, l, hT, hk, obr[2]); kb.pop_scope()
            keys = [(lambda i, l=l, t=t: f"{t}{l}_obr{i}") for t in ("gla", "gdn", "ssd")]
            kb.push_scope(); phase_merge(kb, l, hT, hk, obr, keys, xin, xin_key, xmid, xmid_key); kb.pop_scope()
            kb.pop_scope()
            msrc, msrc_key = xmid, xmid_key
        else:
            msrc, msrc_key = xin, xin_key
        if dbg == "xmid":
            kb.S.final_wait("sp", [xmid_key(i) for i in range(NT)])
            break
        if do_moe:
            xnext = kb.dscr(f"xres{l}", (S_TOK, D))
            xnext_key = (lambda i, l=l: f"xres{l}_{i}")
            kb.push_scope()
            moe_fn = phase_moe_sorted if MOE_SORTED else phase_moe
            moe_fn(kb, l, msrc, msrc_key, xnext, xnext_key, final=(dict(out=out, nf=nf) if l == last else None))
            kb.pop_scope()
            xin, xin_key = xnext, xnext_key
    kb.S.final_wait("sp", [f"out{i}" for i in range(NT)])
    kb.stats = kb.S.emit(kb.stack)
    return kb


def host_all(inputs, b, names):
    m = {}
    for nm in W_SHAPES:
        m[nm] = inputs[nm]
    m["norm_final"] = inputs["norm_final"]
    for l in range(DEPTH):
        m[f"mrg{l}_bmd"] = inputs["b_merge"][l][None, :]
        m[f"moe{l}_brd"] = inputs["b_router"][l][None, :]
        m[f"moe{l}_nffn"] = inputs["norm_ffn"][l][None, :]
    base = host_inputs(inputs, b, [n for n in names if n not in m])
    for n in names:
        if n in m:
            base[n] = np.ascontiguousarray(m[n])
    return base


_PROG = {}


def kernel(**inputs):
    inputs = {k: np.asarray(v) for k, v in inputs.items()}
    if "kb" not in _PROG:
        _PROG["kb"] = build_program()
    kb = _PROG["kb"]
    names = list(kb.ins.keys())
    in_maps = [host_all(inputs, b, names) for b in range(8)]
    res = run_bass_kernel_spmd(kb.nc, in_maps, core_ids=list(range(8)))
    return np.stack([np.asarray(r["out"]) for r in res.results], axis=0).astype(np.float32)


MOE_SORTED = True
MOE_BLK = 512
MOE_NB = (S_TOK * 4) // MOE_BLK + 32
MOE_ROWS = MOE_NB * MOE_BLK


def phase_moe_sorted(kb, l, xsrc, xsrc_key, xdst, xdst_key, final=None):
    import os
    C = kb.C
    tag = f"moe{l}"
    P = kb.P
    BLK, NB = MOE_BLK, MOE_NB
    NTB = BLK // 128
    IOA = bass.IndirectOffsetOnAxis
    wr = kb.sb(tag + "_wr", (128, KC, 32), BF16)
    load_w_cast(kb, wr, tag + "_wr", kb.dins["w_router"][l], 0, 32)
    brb = kb.sb(tag + "_brb", (1, 32), BF16)
    kb.dma("pool", brb[:], kb.din(tag + "_brd", (1, 32)), sem=tag + "_brb", w=[tag + "_brb"])
    ones5 = kb.sb(tag + "_ones5", (1, 512), BF16)
    kb.S.op("dve", lambda e: e.memset(ones5[:], 1.0), [], [tag + "_ones5"])
    hTs = kb.sb(tag + "_hT", (128, KC, BLK), BF16)
    lg_all = kb.sb(tag + "_lg", (128, NT, 32))
    msk_all = kb.sb(tag + "_msk", (128, NT, 32))
    R_all = kb.sb(tag + "_R", (128, NT, 32))
    v8_all = kb.sb(tag + "_v8", (128, NT, 8))
    gk_all = kb.sb(tag + "_gk", (128, NT, 4))
    sml = kb.sb(tag + "_sml", (128, 4))
    cnt = kb.sb(tag + "_cnt", (128, 32))
    kb.S.op("pool", lambda e: e.memset(cnt[:], 0.0), [], [tag + "_cnt"])
    padded = kb.sb(tag + "_padded", (128, 32))
    pstart = kb.sb(tag + "_pstart", (128, 32))
    pend = kb.sb(tag + "_pend", (128, 32))
    pcol = kb.sb(tag + "_pcol", (32, 1))
    pcb = kb.sb(tag + "_pcb", (32, 128))
    dg = kb.sb(tag + "_dg", (32, 32))
    posf = kb.sb(tag + "_posf", (128, NT, 4))
    posi = kb.sb(tag + "_posi", (128, NT, 4), I32)
    eb = kb.sb(tag + "_eb", (128, NB))
    offi = kb.sb(tag + "_offi", (128, NB, KC), I32)
    oh = kb.sb(tag + "_oh", (32, NB), BF16)
    kb.push_scope()
    gf_row, gf_key = kb.gf_row[l]
    scf = kb.sb(tag + "_scf", (128, D))
    shf = kb.sb(tag + "_shf", (128, D))
    nfrow = kb.sb(tag + "_nfrow", (128, D))
    kb.dma("sp", shf[:], kb.modrow_d[l][0], sem=tag + "_shf", r=[f"modrowd{l}"], w=[tag + "_shf"])
    kb.dma("sp", scf[:], kb.modrow_d[l][1], sem=tag + "_scf", r=[f"modrowd{l}"], w=[tag + "_scf"])
    kb.dma("sp", nfrow[:], kb.din(tag + "_nffn", (1, D))[0].partition_broadcast(128), sem=tag + "_nfrow", w=[tag + "_nfrow"])
    kb.stt("dve", scf[:], scf[:], 1.0, nfrow[:], ALU.add, ALU.mult, r=[tag + "_scf", tag + "_nfrow"], w=[tag + "_scf"])
    xs_d = kb.dscr(f"moe_xs{l}", (MOE_ROWS, D))
    ys_d = kb.dscr(f"moe_ys{l}", (MOE_ROWS, D))
    h2_d = kb.dscr(f"moe_h2{l}", (S_TOK, D))
    zt = kb.sb(tag + "_zt", (128, D))
    kb.S.op("pool", lambda e: e.memset(zt[:], 0.0), [], [tag + "_zt"])
    xs_v = xs_d.rearrange("(n p) c -> n p c", p=128)
    for n in range(MOE_ROWS // 128):
        kb.dma("sp", xs_v[n], zt[:], sem=tag + "_zt", r=[tag + "_zt"], w=[tag + "_xs"])
    nb = norm_bufs(kb, tag + "_n")
    h2 = kb.sb(tag + "_h2", (128, D))
    eq = kb.sb(tag + "_eq", (128, NT, 32))
    cmp3 = kb.sb(tag + "_cmp3", (128, NB, 32))
    offf = kb.sb(tag + "_offf", (128, NB, KC))
    for i in range(NT):
        hv = hTs[:, :, 0:128]
        norm_tile(kb, nb, l, "f", xsrc[i * 128:(i + 1) * 128, :], xsrc_key(i), hv, tag + "_hT")
        b_ = (nb["n"] - 1) % 2
        xn, xnk = nb["xn"][b_], f"{tag}_n_xn{b_}"
        kb.tt("pool", h2[:], xn[:], scf[:], ALU.mult, r=[xnk, tag + "_scf"], w=[tag + "_h2"])
        kb.tt("pool", h2[:], h2[:], shf[:], ALU.add, r=[tag + "_h2", tag + "_shf"], w=[tag + "_h2"])
        kb.dma("sp", h2_d[i * 128:(i + 1) * 128, :], h2[:], sem=tag + "_h2", r=[tag + "_h2"], w=[f"{tag}_h2d{i}"])
        for k in range(KC):
            kb.mm(P[7][:, 0:32], hTs[:, k, 0:128], wr[:, k, :], start=(k == 0), stop=False, r=[tag + "_hT", tag + "_wr"], w=kb.pk(7))
        kb.mm(P[7][:, 0:32], ones5[0:1, 0:128], brb[0:1, :], start=False, stop=True, r=[tag + "_ones5", tag + "_brb"], w=kb.pk(7))
        lg, v8, msk = lg_all[:, i, :], v8_all[:, i, :], msk_all[:, i, :]
        kb.cp("dve", lg, P[7][:, 0:32], r=kb.pk(7), w=[tag + "_lg"])
        kb.S.op("dve", lambda e, v8=v8, lg=lg: e.max(out=v8, in_=lg), [tag + "_lg"], [tag + "_v8"])
        kb.ts("dve", msk, lg, v8[:, 3:4], None, ALU.is_ge, r=[tag + "_lg", tag + "_v8"], w=[tag + "_msk"])
        kb.ts("dve", sml[:, 0:1], v8[:, 0:1], -1.0, None, ALU.mult, r=[tag + "_v8"], w=[tag + "_sml"])
        kb.act(gk_all[:, i, :], v8[:, 0:4], AF.Exp, bias=sml[:, 0:1], r=[tag + "_v8", tag + "_sml"], w=[tag + "_gk"])
        kb.S.op("dve", lambda e, i=i: e.reduce_sum(sml[:, 1:2], gk_all[:, i, :], AX.X), [tag + "_gk"], [tag + "_sml"])
        kb.S.op("dve", lambda e: e.reciprocal(sml[:, 1:2], sml[:, 1:2]), [tag + "_sml"], [tag + "_sml"])
        kb.ts("dve", gk_all[:, i, :], gk_all[:, i, :], sml[:, 1:2], None, ALU.mult, r=[tag + "_gk", tag + "_sml"], w=[tag + "_gk"])
        kb.mm(P[6][:, 0:32], C["tri_full"][:], msk, r=["c_tri_full", tag + "_msk"], w=kb.pk(6))
        kb.mm(P[6][:, 32:64], C["ones"][:], msk, r=["c_ones", tag + "_msk"], w=kb.pk(6))
        kb.tt("dve", R_all[:, i, :], P[6][:, 0:32], cnt[:], ALU.add, r=kb.pk(6) + [tag + "_cnt"], w=[tag + "_R"])
        kb.tt("dve", cnt[:], P[6][:, 32:64], cnt[:], ALU.add, r=kb.pk(6) + [tag + "_cnt"], w=[tag + "_cnt"])
    kb.tt("dve", eq[:, 0:8, :].rearrange("p j e -> p e j"), cnt[:].unsqueeze(2).to_broadcast([128, 32, 8]),
          C["blk_thr"][:, 0:8].unsqueeze(1).to_broadcast([128, 32, 8]), ALU.is_gt, r=[tag + "_cnt", "c_blk_thr"], w=[tag + "_eq"])
    kb.S.op("dve", lambda e: e.reduce_sum(padded[:], eq[:, 0:8, :].rearrange("p j e -> p e j"), AX.X), [tag + "_eq"], [tag + "_padded"])
    kb.ts("dve", padded[:], padded[:], float(BLK), None, ALU.mult, r=[tag + "_padded"], w=[tag + "_padded"])
    kb.tt("dve", dg[:], padded[0:32, :], C["ident"][0:32, 0:32], ALU.mult, r=[tag + "_padded", "c_ident"], w=[tag + "_dg"])
    kb.S.op("dve", lambda e: e.reduce_sum(pcol[:], dg[:], AX.X), [tag + "_dg"], [tag + "_pcol"])
    kb.cp("dve", pcb[:], pcol[:, 0:1].to_broadcast([32, 128]), r=[tag + "_pcol"], w=[tag + "_pcb"])
    kb.mm(P[6][:, 0:32], pcb[:], C["tri_full"][0:32, 0:32], r=[tag + "_pcb", "c_tri_full"], w=kb.pk(6))
    kb.cp("dve", pstart[:], P[6][:, 0:32], r=kb.pk(6), w=[tag + "_pstart"])
    kb.tt("dve", pend[:], pstart[:], padded[:], ALU.add, r=[tag + "_pstart", tag + "_padded"], w=[tag + "_pend"])
    kb.tt("dve", R_all[:], R_all[:], pstart[:].unsqueeze(1).to_broadcast([128, NT, 32]), ALU.add,
          r=[tag + "_R", tag + "_pstart"], w=[tag + "_R"])
    for k in range(4):
        kb.tt("dve", eq[:], lg_all[:], v8_all[:, :, k:k + 1].to_broadcast([128, NT, 32]), ALU.is_equal,
              r=[tag + "_lg", tag + "_v8"], w=[tag + "_eq"])
        kb.tt("dve", eq[:], eq[:], R_all[:], ALU.mult, r=[tag + "_eq", tag + "_R"], w=[tag + "_eq"])
        kb.S.op("dve", lambda e, k=k: e.reduce_sum(posf[:, :, k], eq[:], AX.X), [tag + "_eq"], [tag + "_posf"])
    kb.cp("dve", posi[:], posf[:], r=[tag + "_posf"], w=[tag + "_posi"])
    kb.tt("dve", cmp3[:], pend[:].unsqueeze(1).to_broadcast([128, NB, 32]),
          C["blk_thr"][:, 0:NB].unsqueeze(2).to_broadcast([128, NB, 32]), ALU.is_le, r=[tag + "_pend", "c_blk_thr"], w=[tag + "_cmp3"])
    kb.S.op("dve", lambda e: e.reduce_sum(eb[:], cmp3[:], AX.X), [tag + "_cmp3"], [tag + "_eb"])
    kb.ts("dve", eb[:], eb[:], 31.0, None, ALU.min, r=[tag + "_eb"], w=[tag + "_eb"])
    kb.ts("dve", offf[:], eb[:].unsqueeze(2).to_broadcast([128, NB, KC]), float(D), float(l * 32 * D), ALU.mult, ALU.add,
          r=[tag + "_eb"], w=[tag + "_offf"])
    kb.tt("dve", offf[:], offf[:], C["base_pk"][:, 0:KC].unsqueeze(1).to_broadcast([128, NB, KC]), ALU.add,
          r=[tag + "_offf", "c_base_pk"], w=[tag + "_offf"])
    kb.cp("dve", offi[:], offf[:], r=[tag + "_offf"], w=[tag + "_offi"])
    kb.ts("dve", oh[:], eb[0:32, :], C["base_pk"][0:32, 0:1], None, ALU.is_equal, r=[tag + "_eb", "c_base_pk"], w=[tag + "_oh"])
    for i in range(NT):
        kb.dma("sp", h2[:], h2_d[i * 128:(i + 1) * 128, :], sem=tag + "_h2", r=[f"{tag}_h2d{i}"], w=[tag + "_h2"])
        for k in range(4):
            kb.S.dma("pool", lambda e, i=i, k=k: e.indirect_dma_start(
                out=xs_d, out_offset=IOA(ap=posi[:, i, k:k + 1], axis=0), in_=h2[:], in_offset=None),
                tag + "_h2", [tag + "_h2", tag + "_posi"], [tag + "_xs"])
    kb.pop_scope()
    kb.push_scope()
    bgu_sb = kb.sb(tag + "_bgu", (32, 2048), BF16)
    bd_sb = kb.sb(tag + "_bd", (32, D), BF16)
    kb.dma("pool", bgu_sb[:], kb.dins["b_gate_up"][l], sem=tag + "_bgu", w=[tag + "_bgu"])
    kb.dma("pool", bd_sb[:], kb.dins["b_down"][l], sem=tag + "_bd", w=[tag + "_bd"])
    wgu = [kb.sb(tag + f"_wgu{i}", (128, KC, 2048), BF16) for i in range(2)]
    wd = [kb.sb(tag + f"_wd{i}", (128, KC, D), BF16) for i in range(2)]
    ohb = kb.sb(tag + "_ohb", (32, BLK), BF16)
    xr = [kb.sb(tag + f"_xr{i}", (128, D)) for i in range(2)]
    actT = kb.sb(tag + "_actT", (128, KC, BLK), BF16)
    g7 = kb.sb(tag + "_g7", (128, BLK))
    sg = kb.sb(tag + "_sg", (128, BLK))
    u7 = kb.sb(tag + "_u7", (128, BLK))
    yb = [kb.sb(tag + f"_yb{i}", (128, D)) for i in range(2)]
    wgu_flat = kb.dins["w_gate_up"].rearrange("l e r c -> (l e r) c")
    wd_flat = kb.dins["w_down"].rearrange("l e r c -> (l e r) c")

    def load_block_w(b, slot):
        for k in range(KC):
            kb.S.dma("pool", lambda e, b=b, k=k, slot=slot: e.indirect_dma_start(
                out=wgu[slot][:, k, :], out_offset=None, in_=wgu_flat, in_offset=IOA(ap=offi[:, b, k:k + 1], axis=0)),
                tag + f"_wgu{slot}", [tag + "_offi"], [tag + f"_wgu{slot}"])
        for k in range(KC):
            kb.S.dma("pool", lambda e, b=b, k=k, slot=slot: e.indirect_dma_start(
                out=wd[slot][:, k, :], out_offset=None, in_=wd_flat, in_offset=IOA(ap=offi[:, b, k:k + 1], axis=0)),
                tag + f"_wd{slot}", [tag + "_offi"], [tag + f"_wd{slot}"])

    NBR = int(os.environ.get("MOE_NBLK", NB))
    load_block_w(0, 0)
    nx = 0
    for b in range(NBR):
        slot = b % 2
        if b + 1 < NBR:
            load_block_w(b + 1, 1 - slot)
        wk, dk = tag + f"_wgu{slot}", tag + f"_wd{slot}"
        for tt in range(NTB):
            xb = xr[nx % 2]
            xbk = tag + f"_xr{nx % 2}"
            nx += 1
            r0 = b * BLK + tt * 128
            kb.dma("sp", xb[:], xs_d[r0:r0 + 128, :], sem=xbk, r=[tag + "_xs"], w=[xbk])
            for half in range(2):
                pT, pk = P[half], kb.pk(half)
                for kk in range(4):
                    k = half * 4 + kk
                    kb.tr(pT[:, kk * 128:(kk + 1) * 128], xb[:, k * 128:(k + 1) * 128], C["ident"][:], r=[xbk, "c_ident"], w=pk)
                kb.cp("act" if half == 0 else "dve", hTs[:, half * 4:(half + 1) * 4, tt * 128:(tt + 1) * 128],
                      pT[:, :].rearrange("p (k t) -> p k t", k=4), r=pk, w=[tag + "_hT"])
        kb.cp("dve", ohb[:], oh[:, b:b + 1].to_broadcast([32, BLK]), r=[tag + "_oh"], w=[tag + "_ohb"])
        for jc in range(KC):
            pb = 2 + (jc % 2) * 2
            PG, PU = P[pb], P[pb + 1]
            for which, PX in ((0, PG), (1, PU)):
                cols = slice(jc * 256 + which, (jc + 1) * 256, 2)
                for k in range(KC):
                    kb.mm(PX[:, :], wgu[slot][:, k, cols], hTs[:, k, :], start=(k == 0), stop=False,
                          r=[wk, tag + "_hT"], w=kb.pk(pb + which))
                kb.mm(PX[:, :], bgu_sb[:, cols], ohb[:, :], start=False, stop=True, r=[tag + "_bgu", tag + "_ohb"], w=kb.pk(pb + which))
            kb.ts("dve", g7[:], PG[:, :], SW_LIMIT, None, ALU.min, r=kb.pk(pb), w=[tag + "_g7"])
            kb.ts("dve", u7[:], PU[:, :], -SW_LIMIT, SW_LIMIT, ALU.max, ALU.min, r=kb.pk(pb + 1), w=[tag + "_u7"])
            kb.act(sg[:], g7[:], AF.Sigmoid, scale=SW_ALPHA, r=[tag + "_g7"], w=[tag + "_sg"])
            kb.ts("pool", u7[:], u7[:], 1.0, None, ALU.add, r=[tag + "_u7"], w=[tag + "_u7"])
            kb.tt("pool", u7[:], u7[:], g7[:], ALU.mult, r=[tag + "_u7", tag + "_g7"], w=[tag + "_u7"])
            kb.tt("pool", actT[:, jc, :], u7[:], sg[:], ALU.mult, r=[tag + "_u7", tag + "_sg"], w=[tag + "_actT"])
        for tt in range(NTB):
            ybt, ybk = yb[tt % 2], tag + f"_yb{tt % 2}"
            for half in range(2):
                pi = 6 + half
                PO = P[pi]
                hs = slice(half * 512, (half + 1) * 512)
                for jc in range(KC):
                    kb.mm(PO[:, :], actT[:, jc, tt * 128:(tt + 1) * 128], wd[slot][:, jc, hs], start=(jc == 0), stop=False,
                          r=[tag + "_actT", dk], w=kb.pk(pi))
                kb.mm(PO[:, :], ohb[:, 0:128], bd_sb[:, hs], start=False, stop=True, r=[tag + "_ohb", tag + "_bd"], w=kb.pk(pi))
                kb.cp("act" if half == 0 else "dve", ybt[:, hs], PO[:, :], r=kb.pk(pi), w=[ybk])
            r0 = b * BLK + tt * 128
            kb.dma("sp", ys_d[r0:r0 + 128, :], ybt[:], sem=ybk, r=[ybk], w=[tag + "_ys"])
    kb.pop_scope()
    kb.push_scope()
    yk = [kb.sb(tag + f"_yk{i}", (128, D)) for i in range(2)]
    acc = kb.sb(tag + "_acc", (128, D))
    xt = kb.sb(tag + "_xt", (128, D))
    if final is not None:
        nfr = kb.sb(tag + "_nfr", (128, D))
        kb.dma("sp", nfr[:], final["nf"].partition_broadcast(128), sem=tag + "_nfr", w=[tag + "_nfr"])
        fj = kb.sb(tag + "_fj", (128, D), BF16)
        fs = kb.sb(tag + "_fs", (128, 2))
    ng = 0
    for i in range(NT):
        kb.dma("sp", xt[:], xsrc[i * 128:(i + 1) * 128, :], sem=tag + "_xt", r=[xsrc_key(i)], w=[tag + "_xt"])
        for k in range(4):
            yt, ytk = yk[ng % 2], tag + f"_yk{ng % 2}"
            ng += 1
            kb.S.dma("pool", lambda e, i=i, k=k, yt=yt: e.indirect_dma_start(
                out=yt[:], out_offset=None, in_=ys_d, in_offset=IOA(ap=posi[:, i, k:k + 1], axis=0)),
                ytk, [tag + "_ys", tag + "_posi"], [ytk])
            if k == 0:
                kb.ts("dve", acc[:], yt[:], gk_all[:, i, k:k + 1], None, ALU.mult, r=[ytk, tag + "_gk"], w=[tag + "_acc"])
            else:
                kb.stt("dve", acc[:], yt[:], gk_all[:, i, k:k + 1], acc[:], ALU.mult, ALU.add, r=[ytk, tag + "_gk", tag + "_acc"], w=[tag + "_acc"])
        kb.tt("pool", acc[:], acc[:], gf_row[:], ALU.mult, r=[tag + "_acc", gf_key], w=[tag + "_acc"])
        kb.tt("pool", acc[:], acc[:], xt[:], ALU.add, r=[tag + "_acc", tag + "_xt"], w=[tag + "_acc"])
        if final is None:
            kb.dma("sp", xdst[i * 128:(i + 1) * 128, :], acc[:], sem=tag + "_acc", r=[tag + "_acc"], w=[xdst_key(i)])
        else:
            kb.act(fj[:], acc[:], AF.Square, accum_out=fs[:, 0:1], r=[tag + "_acc"], w=[tag + "_fj", tag + "_fs"])
            kb.ts("dve", fs[:, 1:2], fs[:, 0:1], 1.0 / D, EPS, ALU.mult, ALU.add, r=[tag + "_fs"], w=[tag + "_fs"])
            kb.act(fs[:, 1:2], fs[:, 1:2], AF.Sqrt, r=[tag + "_fs"], w=[tag + "_fs"])
            kb.S.op("dve", lambda e: e.reciprocal(fs[:, 1:2], fs[:, 1:2]), [tag + "_fs"], [tag + "_fs"])
            kb.stt("dve", acc[:], acc[:], fs[:, 1:2], nfr[:], ALU.mult, ALU.mult, r=[tag + "_acc", tag + "_fs", tag + "_nfr"], w=[tag + "_acc"])
            kb.dma("sp", final["out"][i * 128:(i + 1) * 128, :], acc[:], sem=tag + "_acc", r=[tag + "_acc"], w=[f"out{i}"])
    kb.pop_scope()
```

```python
import numpy as np
from contextlib import ExitStack
from concourse.bass_utils import run_bass_kernel_spmd

import concourse.bass as bass
import concourse.mybir as mybir

ENGINES = ("pe", "act", "dve", "pool", "sp")


class Op:
    __slots__ = ("eng", "fn", "deps", "is_dma", "dsem", "dcount", "signal", "idx", "signo")

    def __init__(self, eng, fn):
        self.eng = eng
        self.fn = fn
        self.deps = []
        self.is_dma = False
        self.dsem = None
        self.dcount = 0
        self.signal = False
        self.idx = -1
        self.signo = 0


class Sched:
    def __init__(self, nc, same_engine_sync=True):
        self.nc = nc
        self.q = {e: [] for e in ENGINES}
        self.res_w = {}
        self.res_r = {}
        self.phys = []
        self.key2phys = {}
        self.free_phys = []
        self.same_engine_sync = same_engine_sync

    def _collect(self, op, reads, writes, my_dma_key=None):
        deps = []
        for k in reads:
            t = self.res_w.get(k)
            if t is not None:
                deps.append(t)
        for k in writes:
            t = self.res_w.get(k)
            if t is not None:
                if not (my_dma_key is not None and t[0] == 'dma' and t[1] == my_dma_key):
                    deps.append(t)
            deps.extend(self.res_r.get(k, ()))
        op.deps = deps

    def _commit(self, tok, reads, writes):
        for k in reads:
            self.res_r.setdefault(k, []).append(tok)
        for k in writes:
            self.res_w[k] = tok
            self.res_r[k] = []

    @staticmethod
    def _excl(reads, writes):
        rp = [k for k in reads if len(k) == 2 and k[0] == "P" and k[1].isdigit()]
        if not rp:
            return reads, writes
        return [k for k in reads if k not in rp], list(writes) + [k for k in rp if k not in writes]

    def op(self, eng, fn, reads=(), writes=()):
        reads, writes = self._excl(reads, writes)
        o = Op(eng, fn)
        self._collect(o, reads, writes)
        o.idx = len(self.q[eng])
        self.q[eng].append(o)
        self._commit(('op', o), reads, writes)
        return o

    def dma(self, eng, fn, sem_key, reads=(), writes=()):
        reads, writes = self._excl(reads, writes)
        if sem_key not in self.key2phys:
            if self.free_phys:
                p = self.free_phys.pop()
            else:
                p = len(self.phys)
                self.phys.append(0)
            self.key2phys[sem_key] = p
        p = self.key2phys[sem_key]
        o = Op(eng, fn)
        o.is_dma = True
        self._collect(o, reads, writes, my_dma_key=p)
        self.phys[p] += 1
        c = self.phys[p]
        o.dsem = p
        o.dcount = c
        o.idx = len(self.q[eng])
        self.q[eng].append(o)
        self._commit(('dma', p, c), reads, writes)
        return o

    def final_wait(self, eng, keys):
        o = Op(eng, None)
        deps = []
        for k in keys:
            t = self.res_w.get(k)
            if t is not None:
                deps.append(t)
            deps.extend(self.res_r.get(k, ()))
        o.deps = deps
        o.idx = len(self.q[eng])
        self.q[eng].append(o)

    def barrier(self):
        toks = []
        for e in ENGINES:
            for o in reversed(self.q[e]):
                if o.fn is not None and not o.is_dma:
                    toks.append(('op', o))
                    break
        for p, c in enumerate(self.phys):
            if c:
                toks.append(('dma', p, c))
        for e in ENGINES:
            o = Op(e, None)
            o.deps = list(toks)
            o.idx = len(self.q[e])
            self.q[e].append(o)
        self.free_phys = list(range(len(self.phys)))[::-1]
        self.key2phys = {}

    def emit(self, stack):
        nc = self.nc
        for e in ENGINES:
            for o in self.q[e]:
                for t in o.deps:
                    if t[0] == 'op':
                        tgt = t[1]
                        if tgt.eng == o.eng and (not self.same_engine_sync or o.eng == 'pe'):
                            continue
                        tgt.signal = True
        for e in ENGINES:
            n = 0
            for o in self.q[e]:
                if o.signal:
                    n += 1
                    o.signo = n
        esem = {e: stack.enter_context(nc.semaphore("s_" + e)) for e in ENGINES}
        dsem = {}
        for p in range(len(self.phys)):
            dsem[p] = stack.enter_context(nc.semaphore(f"d_{p}"))
        block = stack.enter_context(nc.Block())
        stats = {}

        def run(e, engobj):
            waited = {}
            nwait = 0
            for o in self.q[e]:
                need = {}
                for t in o.deps:
                    if t[0] == 'op':
                        tgt = t[1]
                        if tgt.eng == e and (not self.same_engine_sync or e == 'pe'):
                            continue
                        key = ('e', tgt.eng)
                        val = tgt.signo
                    else:
                        key = ('d', t[1])
                        val = t[2] * 16
                    if need.get(key, 0) < val:
                        need[key] = val
                for key, val in need.items():
                    if waited.get(key, 0) >= val:
                        continue
                    waited[key] = val
                    sem = esem[key[1]] if key[0] == 'e' else dsem[key[1]]
                    engobj.wait_ge(sem, val)
                    nwait += 1
                if o.fn is None:
                    continue
                ins = o.fn(engobj)
                if o.is_dma:
                    ins.then_inc(dsem[o.dsem], 16)
                elif o.signal:
                    ins.then_inc(esem[e], 1)
            stats[e] = (len(self.q[e]), nwait)

        @block.tensor
        def _(eng):
            run("pe", eng)

        @block.scalar
        def _(eng):
            run("act", eng)

        @block.vector
        def _(eng):
            run("dve", eng)

        @block.gpsimd
        def _(eng):
            run("pool", eng)

        @block.sync
        def _(eng):
            run("sp", eng)

        return stats


F32 = mybir.dt.float32
BF16 = mybir.dt.bfloat16
I32 = mybir.dt.int32
AF = mybir.ActivationFunctionType
ALU = mybir.AluOpType
AX = mybir.AxisListType

S_TOK = 4096
D = 1024
KC = 8
NT = S_TOK // 128
DEPTH = 2
EPS = 1e-6
IN_COLS = 7968
C_GLA_Q, C_GLA_K, C_GLA_V, C_GLA_LR, C_GLA_R = 0, 256, 512, 1024, 1040
C_GDN_QKV, C_GDN_A, C_GDN_B, C_GDN_G = 1552, 3088, 3092, 3096
C_SSD_Z, C_SSD_XBC, C_SSD_DT, C_MERGE = 3608, 4120, 4888, 4896


class KB:
    def __init__(self, same_engine_sync=True):
        self.nc = bass.Bass("TRN2", target_bir_lowering=False)
        self.S = Sched(self.nc, same_engine_sync=same_engine_sync)
        self.stack = ExitStack()
        self.ins = {}
        self.outs = {}
        self._n = 0
        self.scopes = []
        self._allow_p = False
        self.P = [self.stack.enter_context(self.nc.psum_tensor(f"PB{i}", [128, 512], F32)) for i in range(8)]

    @staticmethod
    def pk(i, a=0, b=512):
        return [f"P{i}"]

    def din(self, name, shape, dt=F32):
        t = self.nc.dram_tensor(name, list(shape), dt, kind="ExternalInput")
        self.ins[name] = t
        return t.ap()

    def dout(self, name, shape, dt=F32):
        t = self.nc.dram_tensor(name, list(shape), dt, kind="ExternalOutput")
        self.outs[name] = t
        return t.ap()

    def dscr(self, name, shape, dt=F32, debug=False):
        if debug:
            return self.dout(name, shape, dt)
        return self.nc.dram_tensor(name, list(shape), dt, kind="Internal").ap()

    def sb(self, name, shape, dt=F32):
        st = self.scopes[-1] if self.scopes else self.stack
        return st.enter_context(self.nc.sbuf_tensor(name, list(shape), dt))

    def sbp(self, name, shape, dt=F32):
        assert not self.scopes or self._allow_p
        return self.stack.enter_context(self.nc.sbuf_tensor(name, list(shape), dt))

    def push_scope(self):
        self.scopes.append(ExitStack())

    def pop_scope(self):
        self.S.barrier()
        self.scopes.pop().close()

    def ps(self, name, shape=(128, 512), dt=F32):
        return self.stack.enter_context(self.nc.psum_tensor(name, list(shape), dt))

    def mm(self, out, lhsT, rhs, start=True, stop=True, r=(), w=()):
        return self.S.op("pe", lambda e: e.matmul(out, lhsT, rhs, start=start, stop=stop), r, w)

    def tr(self, out, in_, ident, r=(), w=()):
        return self.S.op("pe", lambda e: e.transpose(out, in_, ident), r, w)

    def act(self, out, in_, func, bias=None, scale=None, accum_out=None, r=(), w=(), eng="act"):
        kw = {}
        if bias is not None:
            kw["bias"] = bias
        if scale is not None:
            kw["scale"] = scale
        if accum_out is not None:
            kw["accum_out"] = accum_out
        return self.S.op(eng, lambda e: e.activation(out, in_, func, **kw), r, w)

    def ts(self, eng, out, in0, s1, s2, op0, op1=None, accum_out=None, r=(), w=()):
        kw = {}
        if op1 is not None:
            kw["op1"] = op1
        if accum_out is not None:
            kw["accum_out"] = accum_out
        return self.S.op(eng, lambda e: e.tensor_scalar(out, in0, s1, s2, op0, **kw), r, w)

    def tt(self, eng, out, in0, in1, op, r=(), w=()):
        return self.S.op(eng, lambda e: e.tensor_tensor(out, in0, in1, op), r, w)

    def stt(self, eng, out, in0, scalar, in1, op0, op1, r=(), w=()):
        return self.S.op(eng, lambda e: e.scalar_tensor_tensor(out, in0, scalar, in1, op0, op1), r, w)

    def cp(self, eng, out, in_, r=(), w=()):
        if eng == "act":
            return self.S.op(eng, lambda e: e.copy(out, in_), r, w)
        return self.S.op(eng, lambda e: e.tensor_copy(out, in_), r, w)

    def dma(self, eng, out, in_, sem, r=(), w=(), **kw):
        return self.S.dma(eng, lambda e: e.dma_start(out, in_, **kw), sem, r, w)


def phase_consts(kb):
    c = {}
    cdefs = {
        "ident": (128, 128), "tri_incl": (128, 128), "tri_strict": (128, 128), "ones": (128, 128),
        "blk": (128, 128), "selA": (128, 128), "selB": (128, 128), "neg_strict": (128, 128),
        "tri_full": (128, 128), "blk_thr": (128, 64), "base_pk": (128, 8),
    }
    for name, shp in cdefs.items():
        src = kb.din("c_" + name, shp)
        t = kb.sb("cs_" + name, shp)
        kb.dma("sp", t[:], src, sem="c_" + name, w=["c_" + name])
        c[name] = t
        tb = kb.sb("cb_" + name, shp, BF16)
        kb.cp("dve", tb[:], t[:], r=["c_" + name], w=["cb_" + name])
        c[name + "_bf"] = tb
    kb.C = c


def phase_mod(kb):
    nc = kb.nc
    cT = kb.din("cT", (128, KC))
    w_mod = kb.din("w_mod", (DEPTH, D, 6 * D))
    bmodc = kb.din("bmodc", (DEPTH, 128, 48))
    bmodrow = kb.din("bmodrow", (DEPTH, 6, D))
    nmixc = kb.din("nmixc", (DEPTH, 128, KC))
    nffnc = kb.din("nffnc", (DEPTH, 128, KC))
    pers = {}
    for l in range(DEPTH):
        pers[f"modc{l}"] = kb.sbp(f"modc{l}", (128, 48))
        pers[f"modscl{l}"] = kb.sbp(f"modscl{l}", (128, 2, KC))
        for piece in (2, 5):
            pers[f"modrow{l}_{piece}"] = kb.sbp(f"modrow{l}_{piece}", (128, D))
    kb.modrow_d = [[kb.dscr(f"modrowd{l}_{j}", (128, D)) for j in range(2)] for l in range(DEPTH)]
    kb.push_scope()
    rowtmp = kb.sb("modrowtmp", (128, D))
    cact = kb.sb("cact", (128, KC))
    crep = kb.sb("crep", (128, KC, 128))
    kb.dma("sp", cact[:], cT, sem="cact", w=["cact"])
    kb.act(cact[:], cact[:], AF.Silu, r=["cact"], w=["cact"])
    for k in range(KC):
        kb.cp("dve", crep[:, k, :], cact[:, k:k + 1].to_broadcast([128, 128]), r=["cact"], w=["crep"])
    wbuf = [kb.sb(f"modw{i}", (128, KC, 1024)) for i in range(2)]
    pcol = kb.P[0]
    prow = [kb.P[1], kb.P[2]]
    kb.modc, kb.gm_row, kb.gf_row = [], [], []
    kb.sclm, kb.shm, kb.sclf, kb.shf = [], [], [], []
    it = 0
    for l in range(DEPTH):
        modc = pers[f"modc{l}"]
        bc = kb.sb(f"bmodc{l}", (128, 48))
        nm = kb.sb(f"nmixc{l}", (128, KC))
        nf = kb.sb(f"nffnc{l}", (128, KC))
        kb.dma("sp", bc[:], bmodc[l], sem=f"bmodc{l}", w=[f"bmodc{l}"])
        kb.dma("sp", nm[:], nmixc[l], sem=f"nmixc{l}", w=[f"nmixc{l}"])
        kb.dma("sp", nf[:], nffnc[l], sem=f"nffnc{l}", w=[f"nffnc{l}"])
        rows = []
        for piece in range(6):
            wb = wbuf[it % 2]
            wk = f"modw{it % 2}"
            it += 1
            src = w_mod[l, :, piece * 1024:(piece + 1) * 1024].rearrange("(k p) c -> p k c", p=128)
            for hh in range(2):
                kb.dma("sp", wb[:, hh * 4:(hh + 1) * 4, :], src[:, hh * 4:(hh + 1) * 4, :],
                       sem=wk, w=[wk])
            for jj in range(8):
                j = piece * 8 + jj
                for k in range(KC):
                    kb.mm(pcol[:, j:j + 1], wb[:, k, jj * 128:(jj + 1) * 128], cact[:, k:k + 1],
                          start=(k == 0), stop=(k == KC - 1), r=[wk, "cact"], w=kb.pk(0, 0, 128))
            if piece in (2, 3, 4, 5):
                row = pers[f"modrow{l}_{piece}"] if piece in (2, 5) else rowtmp
                if piece in (3, 4):
                    pers_key = f"modrow{l}_{piece}"
                kb.dma("sp", row[:], bmodrow[l, piece].partition_broadcast(128),
                       sem=f"modrow{l}_{piece}", w=[f"modrow{l}_{piece}"])
                for hh in range(2):
                    for k in range(KC):
                        kb.mm(prow[hh][:, :], crep[:, k, :], wb[:, k, hh * 512:(hh + 1) * 512],
                              start=(k == 0), stop=(k == KC - 1), r=[wk, "crep"], w=kb.pk(1 + hh))
                    kb.tt("dve", row[:, hh * 512:(hh + 1) * 512], prow[hh][:, :], row[:, hh * 512:(hh + 1) * 512],
                          ALU.add, r=kb.pk(1 + hh) + [f"modrow{l}_{piece}"], w=[f"modrow{l}_{piece}"])
                if piece in (2, 5):
                    rows.append((row, f"modrow{l}_{piece}"))
                else:
                    kb.dma("sp", kb.modrow_d[l][piece - 3], row[:], sem=f"modrow{l}_{piece}", r=[f"modrow{l}_{piece}"],
                           w=[f"modrowd{l}"])
        kb.tt("dve", modc[:], pcol[:, 0:48], bc[:], ALU.add, r=kb.pk(0, 0, 128) + [f"bmodc{l}"], w=[f"modc{l}"])
        scl = pers[f"modscl{l}"]
        kb.stt("dve", scl[:, 0, :], modc[:, 8:16], 1.0, nm[:], ALU.add, ALU.mult,
               r=[f"modc{l}", f"nmixc{l}"], w=[f"modc{l}"])
        kb.stt("dve", scl[:, 1, :], modc[:, 32:40], 1.0, nf[:], ALU.add, ALU.mult,
               r=[f"modc{l}", f"nffnc{l}"], w=[f"modc{l}"])
        kb.modc.append(modc)
        kb.gm_row.append(rows[0])
        kb.gf_row.append(rows[1])
        kb.sclm.append(scl[:, 0, :])
        kb.shm.append(modc[:, 0:8])
        kb.sclf.append(scl[:, 1, :])
        kb.shf.append(modc[:, 24:32])
    kb.pop_scope()


def norm_bufs(kb, tag):
    NB = 2
    b = {
        "tag": tag,
        "xt": [kb.sb(f"{tag}_x{i}", (128, D)) for i in range(NB)],
        "xn": [kb.sb(f"{tag}_xn{i}", (128, D)) for i in range(NB)],
        "junk": kb.sb(f"{tag}_junk", (128, D), BF16),
        "ss": kb.sb(f"{tag}_ss", (128, 2)),
        "rstd": kb.sb(f"{tag}_rstd", (128, 2)),
        "n": 0,
    }
    return b


def norm_tile(kb, nb, l, which, xsrc_ap, xsrc_key, dst, dst_key):
    C = kb.C
    tag = nb["tag"]
    scl = kb.sclm[l] if which == "m" else kb.sclf[l]
    sh = kb.shm[l] if which == "m" else kb.shf[l]
    mkey = f"modc{l}"
    b = nb["n"] % 2
    nb["n"] += 1
    xt, xn, junk, ss, rstd = nb["xt"][b], nb["xn"][b], nb["junk"], nb["ss"], nb["rstd"]
    xk, xnk = f"{tag}_x{b}", f"{tag}_xn{b}"
    sk, rk = f"{tag}_ss{b}", f"{tag}_rstd{b}"
    kb.dma("sp", xt[:], xsrc_ap, sem=xk, r=[xsrc_key], w=[xk])
    kb.act(junk[:], xt[:], AF.Square, accum_out=ss[:, b:b + 1], r=[xk], w=[f"{tag}_junk", sk])
    kb.ts("dve", rstd[:, b:b + 1], ss[:, b:b + 1], 1.0 / D, EPS, ALU.mult, ALU.add, r=[sk], w=[rk])
    kb.act(rstd[:, b:b + 1], rstd[:, b:b + 1], AF.Sqrt, r=[rk], w=[rk])
    kb.S.op("dve", lambda e: e.reciprocal(rstd[:, b:b + 1], rstd[:, b:b + 1]), [rk], [rk])
    kb.ts("pool", xn[:], xt[:], rstd[:, b:b + 1], None, ALU.mult, r=[xk, rk], w=[xnk])
    for half in range(2):
        pT = kb.P[half]
        pk = kb.pk(half)
        for kk in range(4):
            k = half * 4 + kk
            kb.tr(pT[:, kk * 128:(kk + 1) * 128], xn[:, k * 128:(k + 1) * 128], C["ident"][:], r=[xnk, "c_ident"], w=pk)
        for kk in range(4):
            k = half * 4 + kk
            d = dst[:, k, :]
            src = pT[:, kk * 128:(kk + 1) * 128]
            if kk % 2 == 0:
                kb.act(d, src, AF.Identity, bias=sh[:, k:k + 1], scale=scl[:, k:k + 1], r=pk + [mkey], w=[dst_key])
            else:
                kb.ts("dve", d, src, scl[:, k:k + 1], sh[:, k:k + 1], ALU.mult, ALU.add, r=pk + [mkey], w=[dst_key])


def phase_norm(kb, l, which, xsrc, xsrc_key, hT, hT_key):
    nb = norm_bufs(kb, f"n{l}{which}")
    for i in range(NT):
        norm_tile(kb, nb, l, which, xsrc[i * 128:(i + 1) * 128, :], xsrc_key(i), hT[:, :, i * 128:(i + 1) * 128], hT_key(i))


def _consts():
    i = np.arange(128)
    same = (i[:, None] // 64) == (i[None, :] // 64)
    return {
        "c_ident": np.eye(128, dtype=np.float32),
        "c_tri_incl": ((i[:, None] <= i[None, :]) & same).astype(np.float32),
        "c_tri_strict": ((i[:, None] > i[None, :]) & same).astype(np.float32),
        "c_ones": np.ones((128, 128), np.float32),
        "c_blk": same.astype(np.float32),
        "c_selA": np.repeat((i < 64).astype(np.float32)[:, None], 128, 1),
        "c_selB": np.repeat((i >= 64).astype(np.float32)[:, None], 128, 1),
        "c_neg_strict": -((i[:, None] > i[None, :]) & same).astype(np.float32),
        "c_tri_full": (i[:, None] < i[None, :]).astype(np.float32),
        "c_blk_thr": np.repeat((np.arange(64, dtype=np.float32) * 512.0)[None, :], 128, 0),
        "c_base_pk": (i[:, None] + 128 * np.arange(8)[None, :]).astype(np.float32),
    }


def host_inputs(inp, b, names):
    m = {}
    m.update(_consts())
    m["x"] = np.ascontiguousarray(inp["x"][b])
    m["cT"] = np.ascontiguousarray(inp["c"][b].reshape(KC, 128).T)
    m["w_mod"] = inp["w_mod"]
    m["bmodc"] = np.ascontiguousarray(inp["b_mod"].reshape(DEPTH, 48, 128).transpose(0, 2, 1))
    bm = inp["b_mod"].reshape(DEPTH, 6, D)
    m["bmodrow"] = np.ascontiguousarray(bm)
    m["nmixc"] = np.ascontiguousarray(inp["norm_mix"].reshape(DEPTH, KC, 128).transpose(0, 2, 1))
    m["nffnc"] = np.ascontiguousarray(inp["norm_ffn"].reshape(DEPTH, KC, 128).transpose(0, 2, 1))
    m.update(host_inputs2(inp, b, names))
    return {k: np.ascontiguousarray(m[k], dtype=m[k].dtype) for k in names}


def proj_feat(kb, out_ps, w_sb, wkey, c0, ncols, hT, hkey, t0, nt, wkeys=None):
    for k in range(KC):
        kb.mm(out_ps, w_sb[:, k, c0:c0 + ncols], hT[:, k, t0:t0 + nt], start=(k == 0), stop=(k == KC - 1),
              r=[wkey, hkey], w=wkeys)


def proj_tok(kb, out_ps, w_sb, wkey, c0, ncols, hT, hkey, t0, nt, wkeys=None):
    for k in range(KC):
        kb.mm(out_ps, hT[:, k, t0:t0 + nt], w_sb[:, k, c0:c0 + ncols], start=(k == 0), stop=(k == KC - 1),
              r=[wkey, hkey], w=wkeys)


def load_w_cast(kb, dst, dkey, src_dram_2d, c0, ncols, nk=KC, step=512):
    src = src_dram_2d.rearrange("(k p) c -> p k c", p=128)
    for k in range(nk):
        kb.dma("pool", dst[:, k, 0:ncols], src[:, k, c0:c0 + ncols], sem=dkey, w=[dkey])


def rms_rstd(kb, tag, rs, ssq, n, width):
    kb.ts("dve", rs[:, 0:n], ssq[:, 0:n], 1.0 / width, EPS, ALU.mult, ALU.add, r=[tag + "_ssq"], w=[tag + "_rs"])
    kb.act(rs[:, 0:n], rs[:, 0:n], AF.Sqrt, r=[tag + "_rs"], w=[tag + "_rs"])
    kb.S.op("dve", lambda e: e.reciprocal(rs[:, 0:n], rs[:, 0:n]), [tag + "_rs"], [tag + "_rs"])


def phase_gla(kb, l, hT, hkey_fn, obr):
    C = kb.C
    tag = f"gla{l}"
    w_in = kb.w_in
    NW = 1552
    wg = kb.sb(tag + "_w", (128, KC, NW), BF16)
    load_w_cast(kb, wg, tag + "_w", w_in[l], 0, NW)
    w2 = kb.sb(tag + "_w2", (16, 256))
    b2 = kb.sb(tag + "_b2", (1, 256))
    gn = kb.sb(tag + "_gn", (128, 512))
    kb.dma("sp", w2[:], kb.din(tag + "_w2d", (16, 256)), sem=tag + "_w2", w=[tag + "_w2"])
    kb.dma("sp", b2[:], kb.din(tag + "_b2d", (1, 256)), sem=tag + "_b2", w=[tag + "_b2"])
    kb.dma("sp", gn[:], kb.din(tag + "_gnd", (1, 512))[0].partition_broadcast(128), sem=tag + "_gn", w=[tag + "_gn"])
    lrT = kb.sb(tag + "_lrT", (16, 128))
    sp = kb.sb(tag + "_sp", (128, 256))
    e_rem = kb.sb(tag + "_erem", (128, 256))
    e_pos = kb.sb(tag + "_epos", (128, 256))
    e_neg = kb.sb(tag + "_eneg", (128, 256))
    qdT = kb.sb(tag + "_qdT", (128, 256), BF16)
    knT = kb.sb(tag + "_knT", (128, 256), BF16)
    krem = kb.sb(tag + "_krem", (128, 256), BF16)
    v_sb = kb.sb(tag + "_v", (128, 512), BF16)
    r_sb = kb.sb(tag + "_r", (128, 512))
    attT = [kb.sb(tag + f"_attT{i}", (128, 128), BF16) for i in range(2)]
    S = [kb.sb(tag + f"_S{i}", (128, 256)) for i in range(2)]
    Sb = [kb.sb(tag + f"_Sb{i}", (128, 256), BF16) for i in range(2)]
    junk = kb.sb(tag + "_junk", (128, 128), BF16)
    ssq = kb.sb(tag + "_ssq", (128, 4))
    rs = kb.sb(tag + "_rs", (128, 4))
    og = kb.sb(tag + "_og", (128, 512))
    oint = kb.sb(tag + "_oint", (128, 512))
    o_sb = kb.sb(tag + "_o", (128, 512))
    oT = kb.sb(tag + "_oT", (128, 512), BF16)
    for p in range(2):
        kb.S.op("dve", lambda e, p=p: e.memset(S[p][:], 0.0), [], [tag + f"_S{p}"])
        kb.S.op("dve", lambda e, p=p: e.memset(Sb[p][:], 0.0), [], [tag + f"_Sb{p}"])
    P0, P1, P2, P3, P4, P5, P6, P7 = kb.P
    wk = tag + "_w"
    import os
    STOP = float(os.environ.get('GLA_STOP', '9'))
    for i in range(int(os.environ.get('GLA_NT', NT))):
        t0 = i * 128
        hk = hkey_fn(i)
        for pair in range(2):
            proj_feat(kb, P0[:, pair * 128:(pair + 1) * 128], wg, wk, C_GLA_Q + pair * 128, 128, hT, hk, t0, 128, kb.pk(0, pair * 128, pair * 128 + 128))
            proj_feat(kb, P0[:, 256 + pair * 128:256 + (pair + 1) * 128], wg, wk, C_GLA_K + pair * 128, 128, hT, hk, t0, 128, kb.pk(0, 256 + pair * 128, 384 + pair * 128))
        proj_feat(kb, P1[0:16, 0:128], wg, wk, C_GLA_LR, 16, hT, hk, t0, 128, kb.pk(1, 0, 128))
        proj_tok(kb, P2[:, 0:256], wg, wk, C_GLA_K, 256, hT, hk, t0, 128, kb.pk(2, 0, 256))
        proj_tok(kb, P3[:, :], wg, wk, C_GLA_V, 512, hT, hk, t0, 128, kb.pk(3))
        proj_tok(kb, P4[:, :], wg, wk, C_GLA_R, 512, hT, hk, t0, 128, kb.pk(4))
        if STOP <= 1:
            continue
        kb.cp("dve", lrT[:, :], P1[0:16, 0:128], r=kb.pk(1, 0, 128), w=[tag + "_lrT"])
        kb.mm(P1[:, 128:384], lrT[:, :], w2[:, :], start=True, stop=False, r=[tag + "_lrT", tag + "_w2"], w=kb.pk(1, 128, 384))
        kb.mm(P1[:, 128:384], C["ones"][0:1, :], b2[0:1, :], start=False, stop=True, r=["c_ones", tag + "_b2"], w=kb.pk(1, 128, 384))
        kb.act(sp[:], P1[:, 128:384], AF.Exp, scale=-1.0, r=kb.pk(1, 128, 384), w=[tag + "_sp"])
        kb.act(sp[:], sp[:], AF.Ln, bias=1.0, r=[tag + "_sp"], w=[tag + "_sp"])
        if STOP <= 2:
            continue
        kb.cp("act", v_sb[:], P3[:, :], r=kb.pk(3), w=[tag + "_v"])
        kb.act(r_sb[:], P4[:, :], AF.Silu, r=kb.pk(4), w=[tag + "_r"])
        kb.mm(P5[:, 256:512], C["tri_strict"][:], sp[:], r=["c_tri_strict", tag + "_sp"], w=kb.pk(5, 256, 512))
        for pair in range(2):
            kb.mm(P6[:, pair * 128:(pair + 1) * 128], sp[:, pair * 128:(pair + 1) * 128], C["tri_incl"][:],
                  r=["c_tri_incl", tag + "_sp"], w=kb.pk(6, 0, 256))
        kb.act(e_rem[:], P5[:, 256:512], AF.Exp, scale=-1.0 / 16, r=kb.pk(5, 256, 512), w=[tag + "_erem"])
        kb.act(e_pos[:], P6[:, 0:256], AF.Exp, scale=-1.0 / 16, r=kb.pk(6, 0, 256), w=[tag + "_epos"])
        kb.act(e_neg[:], P6[:, 0:256], AF.Exp, scale=1.0 / 16, r=kb.pk(6, 0, 256), w=[tag + "_eneg"])
        kb.stt("dve", qdT[:], P0[:, 0:256], 0.125, e_pos[:], ALU.mult, ALU.mult, r=kb.pk(0, 0, 256) + [tag + "_epos"], w=[tag + "_qdT"])
        kb.tt("dve", knT[:], P0[:, 256:512], e_neg[:], ALU.mult, r=kb.pk(0, 256, 512) + [tag + "_eneg"], w=[tag + "_knT"])
        kb.tt("dve", krem[:], P2[:, 0:256], e_rem[:], ALU.mult, r=kb.pk(2, 0, 256) + [tag + "_erem"], w=[tag + "_krem"])
        if STOP <= 3:
            continue
        for h in range(4):
            pair, rows = h // 2, (h % 2) * 64
            hc = slice(h * 128, (h + 1) * 128)
            pc = slice(pair * 128, (pair + 1) * 128)
            aps = P1[:, 384:512] if h % 2 == 0 else P2[:, 256:384]
            apk = kb.pk(1, 384, 512) if h % 2 == 0 else kb.pk(2, 256, 384)
            kb.mm(aps, knT[rows:rows + 64, pc], qdT[rows:rows + 64, pc], r=[tag + "_knT", tag + "_qdT"], w=apk)
            kb.tt("dve", attT[h % 2][:], aps, C["tri_incl"][:], ALU.mult, r=apk + ["c_tri_incl"], w=[tag + f"_attT{h % 2}"])
            kb.mm(P7[:, hc], attT[h % 2][:], v_sb[:, hc], start=True, stop=True, r=[tag + f"_attT{h % 2}", tag + "_v"], w=kb.pk(7, h * 128, h * 128 + 128))
        if STOP <= 4:
            continue
        for pair in range(2):
            pc0 = pair * 128
            Sk, Sbk = tag + f"_S{pair}", tag + f"_Sb{pair}"
            for ch in range(2):
                tr_ = slice(ch * 64, (ch + 1) * 64)
                kb.mm(P5[tr_, pair * 256:(pair + 1) * 256], qdT[:, pc0 + ch * 64:pc0 + (ch + 1) * 64],
                      Sb[pair][:, :], start=True, stop=True,
                      r=[tag + "_qdT", Sbk], w=kb.pk(5, pair * 256, pair * 256 + 256))
                kb.mm(P6[:, 256:512], krem[tr_, pc0:pc0 + 128], v_sb[tr_, pair * 256:(pair + 1) * 256],
                      r=[tag + "_krem", tag + "_v"], w=kb.pk(6, 256, 512))
                for hh in range(2):
                    rr = slice(hh * 64, (hh + 1) * 64)
                    cc = slice(hh * 128, (hh + 1) * 128)
                    dec = e_pos[rr, pc0 + ch * 64 + 63:pc0 + ch * 64 + 64]
                    kb.stt("dve", S[pair][rr, cc], S[pair][rr, cc], dec, P6[rr, 256 + hh * 128:256 + (hh + 1) * 128],
                           ALU.mult, ALU.add, r=[Sk, tag + "_epos"] + kb.pk(6, 256, 512), w=[Sk])
                kb.cp("pool", Sb[pair][:], S[pair][:], r=[Sk], w=[Sbk])
        if STOP <= 5:
            continue
        kb.cp("act", oint[:], P5[:, :], r=kb.pk(5), w=[tag + "_oint"])
        kb.tt("dve", o_sb[:], P7[:, :], oint[:], ALU.add, r=kb.pk(7) + [tag + "_oint"], w=[tag + "_o"])
        for h in range(4):
            kb.act(junk[:], o_sb[:, h * 128:(h + 1) * 128], AF.Square, accum_out=ssq[:, h:h + 1], r=[tag + "_o"],
                   w=[tag + "_junk", tag + "_ssq"])
        rms_rstd(kb, tag, rs, ssq, 4, 128)
        for h in range(4):
            hc = slice(h * 128, (h + 1) * 128)
            kb.stt("dve", og[:, hc], o_sb[:, hc], rs[:, h:h + 1], gn[:, hc], ALU.mult, ALU.mult,
                   r=[tag + "_o", tag + "_rs", tag + "_gn"], w=[tag + "_og"])
        kb.tt("pool", og[:], og[:], r_sb[:], ALU.mult, r=[tag + "_og", tag + "_r"], w=[tag + "_og"])
        for c in range(4):
            kb.tr(P0[:, c * 128:(c + 1) * 128], og[:, c * 128:(c + 1) * 128], C["ident"][:], r=[tag + "_og", "c_ident"], w=kb.pk(0, c * 128, c * 128 + 128))
        kb.cp("act", oT[:], P0[:, :], r=kb.pk(0), w=[tag + "_oT"])
        kb.dma("sp", obr[i], oT[:], sem=tag + "_oT", r=[tag + "_oT"], w=[f"{tag}_obr{i}"])


def host_inputs2(inp, b, names):
    m = {}
    f = np.float32
    for l in range(DEPTH):
        m[f"gla{l}_w2d"] = inp["gla_w_gate2"][l]
        m[f"gla{l}_b2d"] = inp["gla_b_gate2"][l][None, :]
        m[f"gla{l}_gnd"] = np.tile(inp["gla_norm"][l], 4)[None, :]
    m["w_in"] = inp["w_in"]
    for l in range(DEPTH):
        cw = inp["ssd_conv_w"][l].reshape(4, 6, 128).transpose(2, 1, 0)
        cb = inp["ssd_conv_b"][l].reshape(6, 128).T[:, :, None]
        m[f"ssd{l}_cwd"] = np.concatenate([cw, cb], axis=2)
        m[f"ssd{l}_gnd"] = inp["ssd_norm"][l][None, :]
        m[f"ssd{l}_hpd"] = np.concatenate([inp["ssd_dt_bias"][l], inp["ssd_a_log"][l], inp["ssd_d"][l]])[None, :]
    for l in range(DEPTH):
        m[f"gdn{l}_cwd"] = inp["gdn_conv_w"][l].reshape(4, 12, 128).transpose(2, 1, 0)
        m[f"gdn{l}_gnd"] = np.tile(inp["gdn_norm"][l], 4)[None, :]
        m[f"gdn{l}_hpd"] = np.concatenate([inp["gdn_dt_bias"][l], inp["gdn_a_log"][l]])[None, :]
    return m


def phase_gdn(kb, l, hT, hkey_fn, obr):
    import os
    C = kb.C
    tag = f"gdn{l}"
    w_in = kb.w_in
    NW = 2056
    wk = tag + "_w"
    wg = kb.sb(wk, (128, KC, NW), BF16)
    load_w_cast(kb, wg, wk, w_in[l], C_GDN_QKV, NW)
    O_QKV, O_AB, O_G = 0, 1536, 1544
    convw = kb.sb(tag + "_cw", (128, 12, 4))
    kb.dma("sp", convw[:], kb.din(tag + "_cwd", (128, 12, 4)), sem=tag + "_cw", w=[tag + "_cw"])
    gn = kb.sb(tag + "_gn", (128, 512))
    kb.dma("sp", gn[:], kb.din(tag + "_gnd", (1, 512))[0].partition_broadcast(128), sem=tag + "_gn", w=[tag + "_gn"])
    hp = kb.sb(tag + "_hp", (128, 8))
    kb.dma("sp", hp[:], kb.din(tag + "_hpd", (1, 8))[0].partition_broadcast(128), sem=tag + "_hp", w=[tag + "_hp"])
    negA = kb.sb(tag + "_negA", (128, 4))
    kb.act(negA[:], hp[:, 4:8], AF.Exp, r=[tag + "_hp"], w=[tag + "_negA"])
    kb.ts("dve", negA[:], negA[:], -1.0, None, ALU.mult, r=[tag + "_negA"], w=[tag + "_negA"])
    ubuf = kb.sb(tag + "_ubuf", (128, 12, 131))
    kb.S.op("pool", lambda e: e.memset(ubuf[:], 0.0), [], [tag + "_ubuf"])
    cacc = kb.sb(tag + "_cacc", (128, 12, 128))
    ctmp = kb.sb(tag + "_ctmp", (128, 12, 128))
    qkv = kb.sb(tag + "_qkv", (128, 12, 128))
    sq = kb.sb(tag + "_sq", (128, 8, 128))
    rinv = kb.sb(tag + "_rinv", (128, 8, 128))
    qkn = kb.sb(tag + "_qkn", (128, 8, 128))
    gsb = kb.sb(tag + "_gsb", (128, 512))
    sm = kb.sb(tag + "_sm", (128, 40))
    beta, gg, cum, ecum, erem, bec = sm[:, 0:4], sm[:, 4:8], sm[:, 8:12], sm[:, 12:16], sm[:, 16:20], sm[:, 20:24]
    dA, dB, ytmp = sm[:, 24:28], sm[:, 28:32], sm[:, 32:36]
    HB = []
    for s_ in range(2):
        d_ = {}
        for nm in ("Gs", "E", "ET", "B0", "B1", "C0", "C1", "PT0", "PT1", "PmT", "ecb", "qdT", "V0", "W0", "kdec", "upre", "wT", "u"):
            d_[nm] = kb.sb(tag + f"_{nm}_{s_}", (128, 128))
        kb.S.op("pool", lambda e, t=d_["u"]: e.memset(t[:], 0.0), [], [tag + f"_u_{s_}"])
        HB.append(d_)
    M = [kb.sb(tag + f"_M{h}", (128, 128)) for h in range(4)]
    for h in range(4):
        kb.S.op("pool", lambda e, h=h: e.memset(M[h][:], 0.0), [], [tag + f"_M{h}"])
    oint = kb.sb(tag + "_oint", (128, 512))
    o_sb = kb.sb(tag + "_o", (128, 512))
    og = kb.sb(tag + "_og", (128, 512))
    oT = kb.sb(tag + "_oT", (128, 512), BF16)
    junk = kb.sb(tag + "_junk", (128, 128), BF16)
    ssq = kb.sb(tag + "_ssq", (128, 4))
    rs = kb.sb(tag + "_rs", (128, 4))
    P0, P1, P2, P3, P4, P5, P6, P7 = kb.P
    ident = C["ident"]
    STOP = float(os.environ.get('GDN_STOP', '9'))
    for i in range(int(os.environ.get('GDN_NT', NT))):
        t0 = i * 128
        hk = hkey_fn(i)
        for c in range(12):
            bank = kb.P[c // 4]
            cc = (c % 4) * 128
            proj_feat(kb, bank[:, cc:cc + 128], wg, wk, O_QKV + c * 128, 128, hT, hk, t0, 128, kb.pk(c // 4, cc, cc + 128))
        proj_tok(kb, P3[:, :], wg, wk, O_G, 512, hT, hk, t0, 128, kb.pk(3))
        proj_tok(kb, P4[:, 0:8], wg, wk, O_AB, 8, hT, hk, t0, 128, kb.pk(4, 0, 128))
        kb.act(gsb[:], P3[:, :], AF.Silu, r=kb.pk(3), w=[tag + "_gsb"])
        for b3 in range(3):
            kb.cp("act", ubuf[:, b3 * 4:(b3 + 1) * 4, 3:131], kb.P[b3][:, :].rearrange("p (c t) -> p c t", c=4),
                  r=kb.pk(b3), w=[tag + "_ubuf"])
        for j in range(4):
            wj = convw[:, :, j:j + 1].to_broadcast([128, 12, 128])
            if j == 0:
                kb.tt("dve", cacc[:], ubuf[:, :, 0:128], wj, ALU.mult, r=[tag + "_ubuf", tag + "_cw"], w=[tag + "_cacc"])
            else:
                kb.tt("pool", ctmp[:], ubuf[:, :, j:j + 128], wj, ALU.mult, r=[tag + "_ubuf", tag + "_cw"], w=[tag + "_ctmp"])
                kb.tt("dve", cacc[:], cacc[:], ctmp[:], ALU.add, r=[tag + "_cacc", tag + "_ctmp"], w=[tag + "_cacc"])
        kb.act(qkv[:], cacc[:], AF.Silu, r=[tag + "_cacc"], w=[tag + "_qkv"])
        kb.cp("pool", ubuf[:, :, 0:3], ubuf[:, :, 128:131], r=[tag + "_ubuf"], w=[tag + "_ubuf"])
        if STOP <= 1:
            continue
        kb.tt("pool", sq[:], qkv[:, 0:8, :], qkv[:, 0:8, :], ALU.mult, r=[tag + "_qkv"], w=[tag + "_sq"])
        for half in range(2):
            kb.mm(kb.P[half][:, :], C["ones"][:], sq[:, half * 4:(half + 1) * 4, :], r=["c_ones", tag + "_sq"], w=kb.pk(half))
            kb.ts("dve", rinv[:, half * 4:(half + 1) * 4, :], kb.P[half][:, :].rearrange("p (c t) -> p c t", c=4),
                  1e-6, None, ALU.add, r=kb.pk(half), w=[tag + "_rinv"])
        kb.act(rinv[:], rinv[:], AF.Sqrt, r=[tag + "_rinv"], w=[tag + "_rinv"])
        kb.S.op("dve", lambda e: e.reciprocal(rinv[:], rinv[:]), [tag + "_rinv"], [tag + "_rinv"])
        kb.stt("dve", qkn[:, 0:4, :], qkv[:, 0:4, :], 128.0 ** -0.5, rinv[:, 0:4, :], ALU.mult, ALU.mult,
               r=[tag + "_qkv", tag + "_rinv"], w=[tag + "_qkn"])
        kb.tt("pool", qkn[:, 4:8, :], qkv[:, 4:8, :], rinv[:, 4:8, :], ALU.mult, r=[tag + "_qkv", tag + "_rinv"], w=[tag + "_qkn"])
        kb.act(beta, P4[:, 4:8], AF.Sigmoid, r=kb.pk(4, 0, 128), w=[tag + "_sm"])
        kb.tt("dve", ytmp, P4[:, 0:4], hp[:, 0:4], ALU.add, r=kb.pk(4, 0, 128) + [tag + "_hp"], w=[tag + "_sm"])
        kb.act(ytmp, ytmp, AF.Exp, r=[tag + "_sm"], w=[tag + "_sm"])
        kb.act(ytmp, ytmp, AF.Ln, bias=1.0, r=[tag + "_sm"], w=[tag + "_sm"])
        kb.tt("dve", gg, ytmp, negA[:], ALU.mult, r=[tag + "_sm", tag + "_negA"], w=[tag + "_sm"])
        kb.mm(P4[:, 8:12], C["tri_incl"][:], gg, r=["c_tri_incl", tag + "_sm"], w=kb.pk(4, 0, 128))
        kb.mm(P4[:, 12:16], C["blk"][:], gg, r=["c_blk", tag + "_sm"], w=kb.pk(4, 0, 128))
        kb.mm(P4[:, 16:20], C["selA"][:], gg, r=["c_selA", tag + "_sm"], w=kb.pk(4, 0, 128))
        kb.mm(P4[:, 20:24], C["selB"][:], gg, r=["c_selB", tag + "_sm"], w=kb.pk(4, 0, 128))
        kb.cp("dve", cum, P4[:, 8:12], r=kb.pk(4, 0, 128), w=[tag + "_sm"])
        kb.act(ecum, P4[:, 8:12], AF.Exp, r=kb.pk(4, 0, 128), w=[tag + "_sm"])
        kb.tt("dve", erem, P4[:, 12:16], cum, ALU.subtract, r=kb.pk(4, 0, 128) + [tag + "_sm"], w=[tag + "_sm"])
        kb.act(erem, erem, AF.Exp, r=[tag + "_sm"], w=[tag + "_sm"])
        kb.act(dA, P4[:, 16:20], AF.Exp, r=kb.pk(4, 0, 128), w=[tag + "_sm"])
        kb.act(dB, P4[:, 20:24], AF.Exp, r=kb.pk(4, 0, 128), w=[tag + "_sm"])
        kb.tt("dve", bec, beta, ecum, ALU.mult, r=[tag + "_sm"], w=[tag + "_sm"])
        if STOP <= 2:
            continue
        def head(h, s_):
            hb = HB[s_]
            X1, X2, X3 = (P2, P5, P6) if s_ == 0 else (P3, P4, P7)
            k1, k2, k3 = (kb.pk(2), kb.pk(5), kb.pk(6)) if s_ == 0 else (kb.pk(3), kb.pk(4), kb.pk(7))
            K = lambda nm: tag + f"_{nm}_{s_}"
            Gs, E, ET, PmT, ecb, qdT = hb["Gs"], hb["E"], hb["ET"], hb["PmT"], hb["ecb"], hb["qdT"]
            V0, W0, kdec, upre, wT, u_sb = hb["V0"], hb["W0"], hb["kdec"], hb["upre"], hb["wT"], hb["u"]
            Bm, Cm, PT = [hb["B0"], hb["B1"]], [hb["C0"], hb["C1"]], [hb["PT0"], hb["PT1"]]
            qT = qkn[:, h, :]
            kT = qkn[:, 4 + h, :]
            vT = qkv[:, 8 + h, :]
            hc = slice(h * 128, (h + 1) * 128)
            kb.ts("dve", Gs[:], C["tri_incl"][:], gg[:, h:h + 1], None, ALU.mult, r=["c_tri_incl", tag + "_sm"], w=[K("Gs")])
            kb.mm(X1[:, 0:128], kT, kT, r=[tag + "_qkn"], w=k1)
            kb.mm(X1[:, 128:256], kT, qT, r=[tag + "_qkn"], w=k1)
            yield
            kb.mm(X1[:, 256:384], Gs[:], C["tri_strict"][:], r=[K("Gs"), "c_tri_strict"], w=k1)
            kb.mm(X1[:, 384:512], C["tri_strict"][:], Gs[:], r=[K("Gs"), "c_tri_strict"], w=k1)
            kb.mm(X2[:, 0:128], C["ones"][:], Gs[:], r=[K("Gs"), "c_ones"], w=k2)
            yield
            kb.act(E[:], X1[:, 256:384], AF.Exp, r=k1, w=[K("E")])
            kb.act(ET[:], X1[:, 384:512], AF.Exp, r=k1, w=[K("ET")])
            kb.act(ecb[:], X2[:, 0:128], AF.Exp, r=k2, w=[K("ecb")])
            yield
            kb.tt("pool", E[:], E[:], C["neg_strict"][:], ALU.mult, r=[K("E"), "c_neg_strict"], w=[K("E")])
            kb.tt("pool", ET[:], ET[:], C["tri_incl"][:], ALU.mult, r=[K("ET"), "c_tri_incl"], w=[K("ET")])
            kb.tt("pool", qdT[:], qT, ecb[:], ALU.mult, r=[tag + "_qkn", K("ecb")], w=[K("qdT")])
            yield
            kb.stt("dve", Bm[0][:], X1[:, 0:128], beta[:, h:h + 1], E[:], ALU.mult, ALU.mult,
                   r=k1 + [tag + "_sm", K("E")], w=[K("B0")])
            kb.tt("dve", PmT[:], X1[:, 128:256], ET[:], ALU.mult, r=k1 + [K("ET")], w=[K("PmT")])
            yield
            kb.tr(X2[:, 128:256], Bm[0][:], ident[:], r=[K("B0"), "c_ident"], w=k2)
            kb.tr(X2[:, 256:384], vT, ident[:], r=[tag + "_qkv", "c_ident"], w=k2)
            kb.tr(X2[:, 384:512], kT, ident[:], r=[tag + "_qkn", "c_ident"], w=k2)
            yield
            kb.cp("act", Cm[0][:], X2[:, 128:256], r=k2, w=[K("C0")])
            kb.tt("dve", PT[0][:], X2[:, 128:256], ident[:], ALU.add, r=k2 + ["c_ident"], w=[K("PT0")])
            kb.ts("dve", V0[:], X2[:, 256:384], beta[:, h:h + 1], None, ALU.mult, r=k2 + [tag + "_sm"], w=[K("V0")])
            kb.act(W0[:], X2[:, 384:512], AF.Identity, scale=bec[:, h:h + 1], r=k2 + [tag + "_sm"], w=[K("W0")])
            kb.ts("dve", kdec[:], X2[:, 384:512], erem[:, h:h + 1], None, ALU.mult, r=k2 + [tag + "_sm"], w=[K("kdec")])
            yield
            cur = 0
            for j in range(1, 6):
                nxt = 1 - cur
                Bk, Ck, PTk = K(f"B{cur}"), K(f"C{cur}"), K(f"PT{cur}")
                Bn, Cn, PTn = K(f"B{nxt}"), K(f"C{nxt}"), K(f"PT{nxt}")
                kb.mm(X3[:, 0:128], Cm[cur][:], Bm[cur][:], r=[Bk, Ck], w=k3)
                if j < 5:
                    kb.mm(X3[:, 128:256], Bm[cur][:], Cm[cur][:], r=[Bk, Ck], w=k3)
                yield
                kb.cp("dve", Bm[nxt][:], X3[:, 0:128], r=k3, w=[Bn])
                if j < 5:
                    kb.cp("act", Cm[nxt][:], X3[:, 128:256], r=k3, w=[Cn])
                yield
                kb.mm(X3[:, 256:384], Bm[nxt][:], PT[cur][:], r=[Bn, PTk], w=k3)
                yield
                kb.tt("dve", PT[nxt][:], X3[:, 256:384], PT[cur][:], ALU.add, r=k3 + [PTk], w=[PTn])
                yield
                cur = nxt
            PTf, PTfk = PT[cur], K(f"PT{cur}")
            kb.mm(X3[:, 384:512], PTf[:], V0[:], r=[PTfk, K("V0")], w=k3)
            kb.mm(X1[:, 0:128], W0[:], PTf[:], r=[PTfk, K("W0")], w=k1)
            yield
            kb.cp("act", upre[:], X3[:, 384:512], r=k3, w=[K("upre")])
            kb.cp("dve", wT[:], X1[:, 0:128], r=k1, w=[K("wT")])
            yield
            Mk = tag + f"_M{h}"
            for ch in range(2):
                tr_ = slice(ch * 64, (ch + 1) * 64)
                dch = dA if ch == 0 else dB
                kb.mm(X1[tr_, 128:256], wT[:, tr_], M[h][:], r=[K("wT"), Mk], w=k1)
                kb.mm(P1[tr_, hc], qdT[:, tr_], M[h][:], r=[K("qdT"), Mk], w=kb.pk(1))
                yield
                kb.tt("dve", u_sb[tr_, :], upre[tr_, :], X1[tr_, 128:256], ALU.subtract, r=[K("upre")] + k1, w=[K("u")])
                yield
                kb.mm(P0[tr_, hc], PmT[:, tr_], u_sb[:, :], r=[K("PmT"), K("u")], w=kb.pk(0))
                kb.mm(X1[:, 256:384], kdec[tr_, :], u_sb[tr_, :], r=[K("kdec"), K("u")], w=k1)
                yield
                kb.stt("dve", M[h][:], M[h][:], dch[:, h:h + 1], X1[:, 256:384], ALU.mult, ALU.add,
                       r=[Mk, tag + "_sm"] + k1, w=[Mk])
                yield

        for grp in ((0, 1), (2, 3)):
            gens = [head(h, s_) for s_, h in enumerate(grp)]
            while gens:
                for gen in list(gens):
                    try:
                        next(gen)
                    except StopIteration:
                        gens.remove(gen)
        if STOP <= 4:
            continue
        kb.cp("act", oint[:], P1[:, :], r=kb.pk(1), w=[tag + "_oint"])
        kb.tt("dve", o_sb[:], P0[:, :], oint[:], ALU.add, r=kb.pk(0) + [tag + "_oint"], w=[tag + "_o"])
        for h in range(4):
            kb.act(junk[:], o_sb[:, h * 128:(h + 1) * 128], AF.Square, accum_out=ssq[:, h:h + 1], r=[tag + "_o"],
                   w=[tag + "_junk", tag + "_ssq"])
        rms_rstd(kb, tag, rs, ssq, 4, 128)
        for h in range(4):
            hc = slice(h * 128, (h + 1) * 128)
            kb.stt("dve", og[:, hc], o_sb[:, hc], rs[:, h:h + 1], gn[:, hc], ALU.mult, ALU.mult,
                   r=[tag + "_o", tag + "_rs", tag + "_gn"], w=[tag + "_og"])
        kb.tt("pool", og[:], og[:], gsb[:], ALU.mult, r=[tag + "_og", tag + "_gsb"], w=[tag + "_og"])
        for c in range(4):
            kb.tr(P3[:, c * 128:(c + 1) * 128], og[:, c * 128:(c + 1) * 128], ident[:], r=[tag + "_og", "c_ident"],
                  w=kb.pk(3, c * 128, c * 128 + 128))
        kb.cp("act", oT[:], P3[:, :], r=kb.pk(3), w=[tag + "_oT"])
        kb.dma("sp", obr[i], oT[:], sem=tag + "_oT", r=[tag + "_oT"], w=[f"{tag}_obr{i}"])


def phase_ssd(kb, l, hT, hkey_fn, obr):
    import os
    C = kb.C
    tag = f"ssd{l}"
    w_in = kb.w_in
    NW = 1288
    wk = tag + "_w"
    wg = kb.sb(wk, (128, KC, NW), BF16)
    load_w_cast(kb, wg, wk, w_in[l], C_SSD_Z, NW)
    O_Z, O_XBC, O_DT = 0, 512, 1280
    convw = kb.sb(tag + "_cw", (128, 6, 5))
    kb.dma("sp", convw[:], kb.din(tag + "_cwd", (128, 6, 5)), sem=tag + "_cw", w=[tag + "_cw"])
    gn = kb.sb(tag + "_gn", (128, 512))
    kb.dma("sp", gn[:], kb.din(tag + "_gnd", (1, 512))[0].partition_broadcast(128), sem=tag + "_gn", w=[tag + "_gn"])
    hp = kb.sb(tag + "_hp", (128, 24))
    kb.dma("sp", hp[:], kb.din(tag + "_hpd", (1, 24))[0].partition_broadcast(128), sem=tag + "_hp", w=[tag + "_hp"])
    negA = kb.sb(tag + "_negA", (128, 8))
    kb.act(negA[:], hp[:, 8:16], AF.Exp, r=[tag + "_hp"], w=[tag + "_negA"])
    kb.ts("dve", negA[:], negA[:], -1.0, None, ALU.mult, r=[tag + "_negA"], w=[tag + "_negA"])
    ubuf = kb.sb(tag + "_ubuf", (128, 6, 131))
    kb.S.op("pool", lambda e: e.memset(ubuf[:], 0.0), [], [tag + "_ubuf"])
    cacc = kb.sb(tag + "_cacc", (128, 6, 128))
    ctmp = kb.sb(tag + "_ctmp", (128, 6, 128))
    xbc = kb.sb(tag + "_xbc", (128, 6, 128))
    zs = kb.sb(tag + "_zs", (128, 512))
    sm = kb.sb(tag + "_sm", (128, 72))
    dt, gg, cum, ecum, erem = sm[:, 0:8], sm[:, 8:16], sm[:, 16:24], sm[:, 24:32], sm[:, 32:40]
    dA, dB, ytmp = sm[:, 40:48], sm[:, 48:56], sm[:, 56:64]
    x_tok = kb.sb(tag + "_xtok", (128, 512))
    xdt = kb.sb(tag + "_xdt", (128, 512))
    xdte = kb.sb(tag + "_xdte", (128, 512))
    xd = kb.sb(tag + "_xd", (128, 512))
    B_tok = kb.sb(tag + "_Btok", (128, 128))
    CBT = [kb.sb(tag + f"_CBT{g}", (128, 128)) for g in range(2)]
    GsL = [kb.sb(tag + f"_Gs{i}", (128, 128)) for i in range(4)]
    LTL = [kb.sb(tag + f"_LT{i}", (128, 128)) for i in range(4)]
    Sbd = kb.sb(tag + "_Sbd", (128, 512))
    kb.S.op("pool", lambda e: e.memset(Sbd[:], 0.0), [], [tag + "_Sbd"])
    yint = kb.sb(tag + "_yint", (128, 512))
    y_sb = kb.sb(tag + "_y", (128, 512))
    oT = kb.sb(tag + "_oT", (128, 512), BF16)
    junk = kb.sb(tag + "_junk", (128, 256), BF16)
    ssq = kb.sb(tag + "_ssq", (128, 2))
    rs = kb.sb(tag + "_rs", (128, 2))
    P0, P1, P2, P3, P4, P5, P6, P7 = kb.P
    ident = C["ident"]
    STOP = float(os.environ.get('SSD_STOP', '9'))
    for i in range(int(os.environ.get('SSD_NT', NT))):
        t0 = i * 128
        hk = hkey_fn(i)
        for c in range(6):
            bank = kb.P[c // 4]
            cc = (c % 4) * 128
            proj_feat(kb, bank[:, cc:cc + 128], wg, wk, O_XBC + c * 128, 128, hT, hk, t0, 128, kb.pk(c // 4))
        proj_tok(kb, P2[:, :], wg, wk, O_Z, 512, hT, hk, t0, 128, kb.pk(2))
        proj_tok(kb, P3[:, 0:8], wg, wk, O_DT, 8, hT, hk, t0, 128, kb.pk(3))
        kb.act(zs[:], P2[:, :], AF.Silu, r=kb.pk(2), w=[tag + "_zs"])
        kb.cp("act", ubuf[:, 0:4, 3:131], P0[:, :].rearrange("p (c t) -> p c t", c=4), r=kb.pk(0), w=[tag + "_ubuf"])
        kb.cp("act", ubuf[:, 4:6, 3:131], P1[:, 0:256].rearrange("p (c t) -> p c t", c=2), r=kb.pk(1), w=[tag + "_ubuf"])
        for j in range(4):
            wj = convw[:, :, j:j + 1].to_broadcast([128, 6, 128])
            if j == 0:
                kb.tt("dve", cacc[:], ubuf[:, :, 0:128], wj, ALU.mult, r=[tag + "_ubuf", tag + "_cw"], w=[tag + "_cacc"])
                kb.tt("dve", cacc[:], cacc[:], convw[:, :, 4:5].to_broadcast([128, 6, 128]), ALU.add,
                      r=[tag + "_cacc", tag + "_cw"], w=[tag + "_cacc"])
            else:
                kb.tt("pool", ctmp[:], ubuf[:, :, j:j + 128], wj, ALU.mult, r=[tag + "_ubuf", tag + "_cw"], w=[tag + "_ctmp"])
                kb.tt("dve", cacc[:], cacc[:], ctmp[:], ALU.add, r=[tag + "_cacc", tag + "_ctmp"], w=[tag + "_cacc"])
        kb.act(xbc[:], cacc[:], AF.Silu, r=[tag + "_cacc"], w=[tag + "_xbc"])
        kb.cp("pool", ubuf[:, :, 0:3], ubuf[:, :, 128:131], r=[tag + "_ubuf"], w=[tag + "_ubuf"])
        kb.tt("dve", ytmp, P3[:, 0:8], hp[:, 0:8], ALU.add, r=kb.pk(3) + [tag + "_hp"], w=[tag + "_sm"])
        kb.act(ytmp, ytmp, AF.Exp, r=[tag + "_sm"], w=[tag + "_sm"])
        kb.act(dt, ytmp, AF.Ln, bias=1.0, r=[tag + "_sm"], w=[tag + "_sm"])
        kb.tt("dve", gg, dt, negA[:], ALU.mult, r=[tag + "_sm", tag + "_negA"], w=[tag + "_sm"])
        kb.mm(P3[:, 8:16], C["tri_incl"][:], gg, r=["c_tri_incl", tag + "_sm"], w=kb.pk(3))
        kb.mm(P3[:, 16:24], C["blk"][:], gg, r=["c_blk", tag + "_sm"], w=kb.pk(3))
        kb.mm(P3[:, 24:32], C["selA"][:], gg, r=["c_selA", tag + "_sm"], w=kb.pk(3))
        kb.mm(P3[:, 32:40], C["selB"][:], gg, r=["c_selB", tag + "_sm"], w=kb.pk(3))
        kb.cp("dve", cum, P3[:, 8:16], r=kb.pk(3), w=[tag + "_sm"])
        kb.tt("dve", erem, P3[:, 16:24], cum, ALU.subtract, r=kb.pk(3) + [tag + "_sm"], w=[tag + "_sm"])
        kb.act(dA, P3[:, 24:32], AF.Exp, r=kb.pk(3), w=[tag + "_sm"])
        kb.act(dB, P3[:, 32:40], AF.Exp, r=kb.pk(3), w=[tag + "_sm"])
        kb.act(ecum, cum, AF.Exp, r=[tag + "_sm"], w=[tag + "_sm"])
        kb.act(erem, erem, AF.Exp, r=[tag + "_sm"], w=[tag + "_sm"])
        if STOP <= 1:
            continue
        for c in range(4):
            kb.tr(P4[:, c * 128:(c + 1) * 128], xbc[:, c, :], ident[:], r=[tag + "_xbc", "c_ident"], w=kb.pk(4))
        kb.tr(P5[:, 0:128], xbc[:, 4, :], ident[:], r=[tag + "_xbc", "c_ident"], w=kb.pk(5))
        kb.cp("act", x_tok[:], P4[:, :], r=kb.pk(4), w=[tag + "_xtok"])
        kb.cp("act", B_tok[:], P5[:, 0:128], r=kb.pk(5), w=[tag + "_Btok"])
        v3 = lambda t: t[:, :].rearrange("p (h d) -> p h d", h=8)
        bc8 = lambda a: a.unsqueeze(2).to_broadcast([128, 8, 64])
        kb.tt("dve", v3(xdt), v3(x_tok), bc8(dt), ALU.mult, r=[tag + "_xtok", tag + "_sm"], w=[tag + "_xdt"])
        kb.tt("pool", v3(xd), v3(x_tok), bc8(hp[:, 16:24]), ALU.mult, r=[tag + "_xtok", tag + "_hp"], w=[tag + "_xd"])
        kb.tt("pool", v3(xdte), v3(xdt), bc8(erem), ALU.mult, r=[tag + "_xdt", tag + "_sm"], w=[tag + "_xdte"])
        for g in range(2):
            rows = slice(g * 64, (g + 1) * 64)
            bank = P5 if g == 0 else P6
            kb.mm(bank[:, 128:256], xbc[rows, 4, :], xbc[rows, 5, :], r=[tag + "_xbc"], w=kb.pk(5 + g))
            kb.cp("act", CBT[g][:], bank[:, 128:256], r=kb.pk(5 + g), w=[tag + f"_CBT{g}"])
        if STOP <= 2:
            continue
        def head(h, s_):
            g = h // 4
            Gs, LT = GsL[s_], LTL[s_]
            gk_, lk_ = tag + f"_Gs{s_}", tag + f"_LT{s_}"
            bi = (2, 3, 5, 6)[s_]
            X, kx = kb.P[bi], kb.pk(bi)
            kb.ts("dve", Gs[:], C["tri_incl"][:], gg[:, h:h + 1], None, ALU.mult, r=["c_tri_incl", tag + "_sm"], w=[gk_])
            yield
            kb.mm(X[:, 256:384], C["tri_strict"][:], Gs[:], r=[gk_, "c_tri_strict"], w=kx)
            yield
            kb.act(LT[:], X[:, 256:384], AF.Exp, r=kx, w=[lk_])
            yield
            kb.tt("pool", LT[:], LT[:], C["tri_incl"][:], ALU.mult, r=[lk_, "c_tri_incl"], w=[lk_])
            yield
            kb.tt("dve", LT[:], LT[:], CBT[g][:], ALU.mult, r=[lk_, tag + f"_CBT{g}"], w=[lk_])
            yield
            kb.mm(P7[:, h * 64:(h + 1) * 64], LT[:], xdt[:, h * 64:(h + 1) * 64], r=[lk_, tag + "_xdt"], w=kb.pk(7))
            yield

        for grp in ((0, 1, 2, 3), (4, 5, 6, 7)):
            gens = [head(h, s_) for s_, h in enumerate(grp)]
            while gens:
                for gen in list(gens):
                    try:
                        next(gen)
                    except StopIteration:
                        gens.remove(gen)
        if STOP <= 3:
            continue
        for ch in range(2):
            tr_ = slice(ch * 64, (ch + 1) * 64)
            dch = dA if ch == 0 else dB
            kb.mm(P0[tr_, :], xbc[:, 5, tr_], Sbd[:, :], r=[tag + "_xbc", tag + "_Sbd"], w=kb.pk(0))
            kb.mm(P1[:, :], B_tok[tr_, :], xdte[tr_, :], r=[tag + "_Btok", tag + "_xdte"], w=kb.pk(1))
            for g in range(2):
                rr = slice(g * 64, (g + 1) * 64)
                cc = slice(g * 256, (g + 1) * 256)
                s3 = Sbd[rr, cc].rearrange("p (h d) -> p h d", h=4)
                kb.tt("dve", s3, s3, dch[rr, g * 4:(g + 1) * 4].unsqueeze(2).to_broadcast([64, 4, 64]), ALU.mult,
                      r=[tag + "_Sbd", tag + "_sm"], w=[tag + "_Sbd"])
                kb.tt("dve", Sbd[rr, cc], Sbd[rr, cc], P1[rr, cc], ALU.add, r=[tag + "_Sbd"] + kb.pk(1), w=[tag + "_Sbd"])
        kb.cp("act", yint[:], P0[:, :], r=kb.pk(0), w=[tag + "_yint"])
        kb.tt("pool", v3(yint), v3(yint), bc8(ecum), ALU.mult, r=[tag + "_yint", tag + "_sm"], w=[tag + "_yint"])
        kb.tt("dve", y_sb[:], P7[:, :], yint[:], ALU.add, r=kb.pk(7) + [tag + "_yint"], w=[tag + "_y"])
        kb.tt("pool", y_sb[:], y_sb[:], xd[:], ALU.add, r=[tag + "_y", tag + "_xd"], w=[tag + "_y"])
        kb.tt("pool", y_sb[:], y_sb[:], zs[:], ALU.mult, r=[tag + "_y", tag + "_zs"], w=[tag + "_y"])
        for g in range(2):
            kb.act(junk[:], y_sb[:, g * 256:(g + 1) * 256], AF.Square, accum_out=ssq[:, g:g + 1], r=[tag + "_y"],
                   w=[tag + "_junk", tag + "_ssq"])
        rms_rstd(kb, tag, rs, ssq, 2, 256)
        for g in range(2):
            gc = slice(g * 256, (g + 1) * 256)
            kb.stt("dve", y_sb[:, gc], y_sb[:, gc], rs[:, g:g + 1], gn[:, gc], ALU.mult, ALU.mult,
                   r=[tag + "_y", tag + "_rs", tag + "_gn"], w=[tag + "_y"])
        for c in range(4):
            kb.tr(P4[:, c * 128:(c + 1) * 128], y_sb[:, c * 128:(c + 1) * 128], ident[:], r=[tag + "_y", "c_ident"], w=kb.pk(4))
        kb.cp("act", oT[:], P4[:, :], r=kb.pk(4), w=[tag + "_oT"])
        kb.dma("sp", obr[i], oT[:], sem=tag + "_oT", r=[tag + "_oT"], w=[f"{tag}_obr{i}"])


def phase_merge(kb, l, hT, hkey_fn, obrs, obr_keys, xsrc, xsrc_key, xdst, xdst_key):
    C = kb.C
    tag = f"mrg{l}"
    wm = kb.sb(tag + "_wm", (128, KC, 3072), BF16)
    load_w_cast(kb, wm, tag + "_wm", kb.w_in[l], C_MERGE, 3072)
    wbr = []
    for b, nm in enumerate(("w_branch_gla", "w_branch_gdn", "w_branch_ssd")):
        t = kb.sb(tag + f"_wb{b}", (128, 4, D), BF16)
        load_w_cast(kb, t, tag + f"_wb{b}", kb.dins[nm][l], 0, D, nk=4)
        wbr.append(t)
    wo = kb.sb(tag + "_wo", (128, KC, D), BF16)
    load_w_cast(kb, wo, tag + "_wo", kb.dins["w_out"][l], 0, D)
    bmb = kb.sb(tag + "_bmb", (1, 3072), BF16)
    kb.dma("pool", bmb[:], kb.din(tag + "_bmd", (1, 3072)), sem=tag + "_bmb", w=[tag + "_bmb"])
    gm_row, gm_key = kb.gm_row[l]
    ob = [kb.sb(tag + f"_ob{b}", (128, 512), BF16) for b in range(3)]
    sig = kb.sb(tag + "_sig", (128, 512))
    acc = kb.sb(tag + "_acc", (128, 512))
    tmp = kb.sb(tag + "_tmp", (128, 512))
    mT = kb.sb(tag + "_mT", (128, KC, 128), BF16)
    xt = kb.sb(tag + "_xt", (128, D))
    xo = kb.sb(tag + "_xo", (128, D))
    P = kb.P
    for i in range(NT):
        t0 = i * 128
        hk = hkey_fn(i)
        for b in range(3):
            kb.dma("sp", ob[b][:], obrs[b][i], sem=tag + f"_ob{b}", r=[obr_keys[b](i)], w=[tag + f"_ob{b}"])
        kb.dma("sp", xt[:], xsrc[t0:t0 + 128, :], sem=tag + "_xt", r=[xsrc_key(i)], w=[tag + "_xt"])
        for half in range(2):
            for b in range(3):
                PG, PY = P[(b % 2) * 2], P[(b % 2) * 2 + 1]
                kg, ky = kb.pk((b % 2) * 2), kb.pk((b % 2) * 2 + 1)
                for jj in range(4):
                    j = half * 4 + jj
                    col = b * D + j * 128
                    zone = slice(jj * 128, (jj + 1) * 128)
                    for k in range(KC):
                        kb.mm(PG[:, zone], wm[:, k, col:col + 128], hT[:, k, t0:t0 + 128], start=(k == 0), stop=False,
                              r=[tag + "_wm", hk], w=kg)
                    kb.mm(PG[:, zone], bmb[0:1, col:col + 128], C["ones_bf"][0:1, :], start=False, stop=True,
                          r=[tag + "_bmb", "cb_ones"], w=kg)
                    for c in range(4):
                        kb.mm(PY[:, zone], wbr[b][:, c, j * 128:(j + 1) * 128], ob[b][:, c * 128:(c + 1) * 128],
                              start=(c == 0), stop=(c == 3), r=[tag + f"_wb{b}", tag + f"_ob{b}"], w=ky)
                kb.act(sig[:], PG[:, :], AF.Sigmoid, r=kg, w=[tag + "_sig"])
                if b == 0:
                    kb.tt("dve", acc[:], PY[:, :], sig[:], ALU.mult, r=ky + [tag + "_sig"], w=[tag + "_acc"])
                else:
                    kb.tt("dve", tmp[:], PY[:, :], sig[:], ALU.mult, r=ky + [tag + "_sig"], w=[tag + "_tmp"])
                    kb.tt("pool", acc[:], acc[:], tmp[:], ALU.add, r=[tag + "_acc", tag + "_tmp"], w=[tag + "_acc"])
            kb.cp("act", mT[:, half * 4:(half + 1) * 4, :], acc[:, :].rearrange("p (j t) -> p j t", j=4),
                  r=[tag + "_acc"], w=[tag + "_mT"])
        for half in range(2):
            PO, ko = P[4 + half], kb.pk(4 + half)
            for j in range(KC):
                kb.mm(PO[:, :], mT[:, j, :], wo[:, j, half * 512:(half + 1) * 512], start=(j == 0), stop=(j == KC - 1),
                      r=[tag + "_mT", tag + "_wo"], w=ko)
            hs = slice(half * 512, (half + 1) * 512)
            kb.tt("dve", xo[:, hs], PO[:, :], gm_row[:, hs], ALU.mult, r=ko + [gm_key], w=[tag + "_xo"])
            kb.tt("pool", xo[:, hs], xo[:, hs], xt[:, hs], ALU.add, r=[tag + "_xo", tag + "_xt"], w=[tag + "_xo"])
        kb.dma("sp", xdst[t0:t0 + 128, :], xo[:], sem=tag + "_xo", r=[tag + "_xo"], w=[xdst_key(i)])


def phase_moe(kb, l, xsrc, xsrc_key, xdst, xdst_key, final=None):
    import os
    C = kb.C
    tag = f"moe{l}"
    P = kb.P
    TS = 512
    NSUP = S_TOK // TS
    NE = int(os.environ.get("MOE_NE", 32))
    wr = kb.sb(tag + "_wr", (128, KC, 32), BF16)
    load_w_cast(kb, wr, tag + "_wr", kb.dins["w_router"][l], 0, 32)
    brb = kb.sb(tag + "_brb", (1, 32), BF16)
    kb.dma("pool", brb[:], kb.din(tag + "_brd", (1, 32)), sem=tag + "_brb", w=[tag + "_brb"])
    ones5 = kb.sb(tag + "_ones5", (1, 512), BF16)
    kb.S.op("dve", lambda e: e.memset(ones5[:], 1.0), [], [tag + "_ones5"])
    gf_row, gf_key = kb.gf_row[l]
    nb = norm_bufs(kb, tag + "_n")
    hTs = kb.sb(tag + "_hT", (128, KC, TS), BF16)
    G = kb.sb(tag + "_G", (128, 4, 32))
    lg = kb.sb(tag + "_lg", (128, 32))
    v8 = kb.sb(tag + "_v8", (128, 8))
    msk = kb.sb(tag + "_msk", (128, 32))
    sml = kb.sb(tag + "_sml", (128, 4))
    wgu = [kb.sb(tag + f"_wgu{i}", (128, KC, 2048), BF16) for i in range(2)]
    wd = [kb.sb(tag + f"_wd{i}", (128, KC, D), BF16) for i in range(2)]
    bgu = [kb.sb(tag + f"_bgu{i}", (1, 2048), BF16) for i in range(2)]
    bd = [kb.sb(tag + f"_bd{i}", (1, D), BF16) for i in range(2)]
    yacc = kb.sb(tag + "_yacc", (128, 4, D))
    actT = kb.sb(tag + "_actT", (128, KC, TS), BF16)
    g7 = kb.sb(tag + "_g7", (128, TS))
    sg = kb.sb(tag + "_sg", (128, TS))
    u7 = kb.sb(tag + "_u7", (128, TS))
    xt = kb.sb(tag + "_xt", (128, D))
    xo = kb.sb(tag + "_xo", (128, D))
    if final is not None:
        nfr = kb.sb(tag + "_nfr", (128, D))
        kb.dma("sp", nfr[:], final["nf"].partition_broadcast(128), sem=tag + "_nfr", w=[tag + "_nfr"])
        fj = kb.sb(tag + "_fj", (128, D), BF16)
        fs = kb.sb(tag + "_fs", (128, 2))
    w_gu_d, w_d_d = kb.dins["w_gate_up"][l], kb.dins["w_down"][l]
    b_gu_d, b_d_d = kb.dins["b_gate_up"][l], kb.dins["b_down"][l]

    def load_expert(e, slot):
        srcg = w_gu_d[e].rearrange("(k p) c -> p k c", p=128)
        srcd = w_d_d[e].rearrange("(k p) c -> p k c", p=128)
        for k in range(KC):
            kb.dma("pool", wgu[slot][:, k, :], srcg[:, k, :], sem=tag + f"_wgu{slot}", w=[tag + f"_wgu{slot}"])
        for k in range(KC):
            kb.dma("pool", wd[slot][:, k, :], srcd[:, k, :], sem=tag + f"_wd{slot}", w=[tag + f"_wd{slot}"])
        kb.dma("pool", bgu[slot][:], b_gu_d[e:e + 1, :], sem=tag + f"_bgu{slot}", w=[tag + f"_bgu{slot}"])
        kb.dma("pool", bd[slot][:], b_d_d[e:e + 1, :], sem=tag + f"_bd{slot}", w=[tag + f"_bd{slot}"])

    it = 0
    for T in range(int(os.environ.get("MOE_NSUP", NSUP))):
        for tt in range(4):
            i = T * 4 + tt
            norm_tile(kb, nb, l, "f", xsrc[i * 128:(i + 1) * 128, :], xsrc_key(i), hTs[:, :, tt * 128:(tt + 1) * 128], tag + "_hT")
        for tt in range(4):
            for k in range(KC):
                kb.mm(P[7][:, 0:32], hTs[:, k, tt * 128:(tt + 1) * 128], wr[:, k, :], start=(k == 0), stop=False,
                      r=[tag + "_hT", tag + "_wr"], w=kb.pk(7))
            kb.mm(P[7][:, 0:32], ones5[0:1, 0:128], brb[0:1, :], start=False, stop=True, r=[tag + "_ones5", tag + "_brb"], w=kb.pk(7))
            kb.cp("dve", lg[:], P[7][:, 0:32], r=kb.pk(7), w=[tag + "_lg"])
            kb.S.op("dve", lambda e: e.max(out=v8[:], in_=lg[:]), [tag + "_lg"], [tag + "_v8"])
            kb.ts("dve", msk[:], lg[:], v8[:, 3:4], None, ALU.is_ge, r=[tag + "_lg", tag + "_v8"], w=[tag + "_msk"])
            kb.ts("dve", sml[:, 0:1], v8[:, 0:1], -1.0, None, ALU.mult, r=[tag + "_v8"], w=[tag + "_sml"])
            kb.act(lg[:], lg[:], AF.Exp, bias=sml[:, 0:1], r=[tag + "_lg", tag + "_sml"], w=[tag + "_lg"])
            kb.tt("dve", lg[:], lg[:], msk[:], ALU.mult, r=[tag + "_lg", tag + "_msk"], w=[tag + "_lg"])
            kb.S.op("dve", lambda e: e.reduce_sum(sml[:, 1:2], lg[:], AX.X), [tag + "_lg"], [tag + "_sml"])
            kb.S.op("dve", lambda e: e.reciprocal(sml[:, 1:2], sml[:, 1:2]), [tag + "_sml"], [tag + "_sml"])
            kb.ts("dve", G[:, tt, :], lg[:], sml[:, 1:2], None, ALU.mult, r=[tag + "_lg", tag + "_sml"], w=[tag + "_G"])
        kb.S.op("pool", lambda e: e.memset(yacc[:], 0.0), [], [tag + "_yacc"])
        for e_ in range(NE):
            slot = it % 2
            if it == 0:
                load_expert(e_, slot)
            nxt = (e_ + 1) % NE
            if not (T == NSUP - 1 and e_ == NE - 1):
                load_expert(nxt, 1 - slot)
            it += 1
            wk, dk, bgk, bdk = tag + f"_wgu{slot}", tag + f"_wd{slot}", tag + f"_bgu{slot}", tag + f"_bd{slot}"
            for jc in range(KC):
                pb = (jc % 2) * 2
                PG, PU = P[pb], P[pb + 1]
                for which, PX in ((0, PG), (1, PU)):
                    cols = slice(jc * 256 + which, (jc + 1) * 256, 2)
                    for k in range(KC):
                        kb.mm(PX[:, :], wgu[slot][:, k, cols], hTs[:, k, :], start=(k == 0), stop=False,
                              r=[wk, tag + "_hT"], w=kb.pk(pb + which))
                    kb.mm(PX[:, :], bgu[slot][0:1, cols], ones5[0:1, :], start=False, stop=True,
                          r=[bgk, tag + "_ones5"], w=kb.pk(pb + which))
                kb.ts("dve", g7[:], PG[:, :], SW_LIMIT, None, ALU.min, r=kb.pk(pb), w=[tag + "_g7"])
                kb.ts("dve", u7[:], PU[:, :], -SW_LIMIT, SW_LIMIT, ALU.max, ALU.min, r=kb.pk(pb + 1), w=[tag + "_u7"])
                kb.act(sg[:], g7[:], AF.Sigmoid, scale=SW_ALPHA, r=[tag + "_g7"], w=[tag + "_sg"])
                kb.ts("pool", u7[:], u7[:], 1.0, None, ALU.add, r=[tag + "_u7"], w=[tag + "_u7"])
                kb.tt("pool", u7[:], u7[:], g7[:], ALU.mult, r=[tag + "_u7", tag + "_g7"], w=[tag + "_u7"])
                kb.tt("pool", actT[:, jc, :], u7[:], sg[:], ALU.mult, r=[tag + "_u7", tag + "_sg"], w=[tag + "_actT"])
            for tt in range(4):
                for half in range(2):
                    pi = 4 + (tt % 2) * 2 + half
                    PO = P[pi]
                    hs = slice(half * 512, (half + 1) * 512)
                    for jc in range(KC):
                        kb.mm(PO[:, :], actT[:, jc, tt * 128:(tt + 1) * 128], wd[slot][:, jc, hs], start=(jc == 0), stop=False,
                              r=[tag + "_actT", dk], w=kb.pk(pi))
                    kb.mm(PO[:, :], ones5[0:1, 0:128], bd[slot][0:1, hs], start=False, stop=True,
                          r=[tag + "_ones5", bdk], w=kb.pk(pi))
                    kb.stt("dve", yacc[:, tt, hs], PO[:, :], G[:, tt, e_:e_ + 1], yacc[:, tt, hs], ALU.mult, ALU.add,
                           r=kb.pk(pi) + [tag + "_G", tag + "_yacc"], w=[tag + "_yacc"])
        for tt in range(4):
            i = T * 4 + tt
            kb.dma("sp", xt[:], xsrc[i * 128:(i + 1) * 128, :], sem=tag + "_xt", r=[xsrc_key(i)], w=[tag + "_xt"])
            kb.tt("dve", xo[:], yacc[:, tt, :], gf_row[:], ALU.mult, r=[tag + "_yacc", gf_key], w=[tag + "_xo"])
            kb.tt("pool", xo[:], xo[:], xt[:], ALU.add, r=[tag + "_xo", tag + "_xt"], w=[tag + "_xo"])
            if final is None:
                kb.dma("sp", xdst[i * 128:(i + 1) * 128, :], xo[:], sem=tag + "_xo", r=[tag + "_xo"], w=[xdst_key(i)])
            else:
                kb.act(fj[:], xo[:], AF.Square, accum_out=fs[:, 0:1], r=[tag + "_xo"], w=[tag + "_fj", tag + "_fs"])
                kb.ts("dve", fs[:, 1:2], fs[:, 0:1], 1.0 / D, EPS, ALU.mult, ALU.add, r=[tag + "_fs"], w=[tag + "_fs"])
                kb.act(fs[:, 1:2], fs[:, 1:2], AF.Sqrt, r=[tag + "_fs"], w=[tag + "_fs"])
                kb.S.op("dve", lambda e: e.reciprocal(fs[:, 1:2], fs[:, 1:2]), [tag + "_fs"], [tag + "_fs"])
                kb.stt("dve", xo[:], xo[:], fs[:, 1:2], nfr[:], ALU.mult, ALU.mult, r=[tag + "_xo", tag + "_fs", tag + "_nfr"], w=[tag + "_xo"])
                kb.dma("sp", final["out"][i * 128:(i + 1) * 128, :], xo[:], sem=tag + "_xo", r=[tag + "_xo"], w=[f"out{i}"])


SW_LIMIT = 7.0
SW_ALPHA = 1.702


W_SHAPES = {
    "w_branch_gla": (DEPTH, 512, D), "w_branch_gdn": (DEPTH, 512, D), "w_branch_ssd": (DEPTH, 512, D),
    "w_out": (DEPTH, D, D), "w_router": (DEPTH, D, 32),
    "w_gate_up": (DEPTH, 32, D, 2 * D), "b_gate_up": (DEPTH, 32, 2 * D),
    "w_down": (DEPTH, 32, D, D), "b_down": (DEPTH, 32, D),
}


def build_program(layers=(0, 1), do_mix=True, do_moe=True, dbg=None, same_engine_sync=True):
    kb = KB(same_engine_sync=same_engine_sync)
    phase_consts(kb)
    x = kb.din("x", (S_TOK, D))
    kb.w_in = kb.din("w_in", (DEPTH, D, IN_COLS))
    kb.dins = {nm: kb.din(nm, shp) for nm, shp in W_SHAPES.items()}
    nf = kb.din("norm_final", (D,))
    out = kb.dout("out", (S_TOK, D))
    phase_mod(kb)
    xin, xin_key = x, (lambda i: "x_in")
    last = layers[-1]
    for l in layers:
        xmid = kb.dscr(f"xmid{l}", (S_TOK, D), debug=(dbg == "xmid" and l == layers[0]))
        xmid_key = (lambda i, l=l: f"xmid{l}_{i}")
        if do_mix:
            obr = [kb.dscr(f"obr{l}_{b}", (NT, 128, 512), BF16) for b in range(3)]
            kb.push_scope()
            hT = kb.sb(f"hT{l}", (128, KC, S_TOK), BF16)
            hk = (lambda i, l=l: f"hT{l}_{i}")
            kb.push_scope(); phase_norm(kb, l, "m", xin, xin_key, hT, hk); kb.pop_scope()
            kb.push_scope(); phase_gla(kb, l, hT, hk, obr[0]); kb.pop_scope()
            kb.push_scope(); phase_gdn(kb, l, hT, hk, obr[1]); kb.pop_scope()
            kb.push_scope(); phase_ssd(kb, l, hT, hk, obr[2]); kb.pop_scope()
            keys = [(lambda i, l=l, t=t: f"{t}{l}_obr{i}") for t in ("gla", "gdn", "ssd")]
            kb.push_scope(); phase_merge(kb, l, hT, hk, obr, keys, xin, xin_key, xmid, xmid_key); kb.pop_scope()
            kb.pop_scope()
            msrc, msrc_key = xmid, xmid_key
        else:
            msrc, msrc_key = xin, xin_key
        if dbg == "xmid":
            kb.S.final_wait("sp", [xmid_key(i) for i in range(NT)])
            break
        if do_moe:
            xnext = kb.dscr(f"xres{l}", (S_TOK, D))
            xnext_key = (lambda i, l=l: f"xres{l}_{i}")
            kb.push_scope()
            moe_fn = phase_moe_sorted if MOE_SORTED else phase_moe
            moe_fn(kb, l, msrc, msrc_key, xnext, xnext_key, final=(dict(out=out, nf=nf) if l == last else None))
            kb.pop_scope()
            xin, xin_key = xnext, xnext_key
    kb.S.final_wait("sp", [f"out{i}" for i in range(NT)])
    kb.stats = kb.S.emit(kb.stack)
    return kb


def host_all(inputs, b, names):
    m = {}
    for nm in W_SHAPES:
        m[nm] = inputs[nm]
    m["norm_final"] = inputs["norm_final"]
    for l in range(DEPTH):
        m[f"mrg{l}_bmd"] = inputs["b_merge"][l][None, :]
        m[f"moe{l}_brd"] = inputs["b_router"][l][None, :]
        m[f"moe{l}_nffn"] = inputs["norm_ffn"][l][None, :]
    base = host_inputs(inputs, b, [n for n in names if n not in m])
    for n in names:
        if n in m:
            base[n] = np.ascontiguousarray(m[n])
    return base


_PROG = {}


def kernel(**inputs):
    inputs = {k: np.asarray(v) for k, v in inputs.items()}
    if "kb" not in _PROG:
        _PROG["kb"] = build_program()
    kb = _PROG["kb"]
    names = list(kb.ins.keys())
    in_maps = [host_all(inputs, b, names) for b in range(8)]
    res = run_bass_kernel_spmd(kb.nc, in_maps, core_ids=list(range(8)))
    return np.stack([np.asarray(r["out"]) for r in res.results], axis=0).astype(np.float32)


MOE_SORTED = True
MOE_BLK = 512
MOE_NB = (S_TOK * 4) // MOE_BLK + 32
MOE_ROWS = MOE_NB * MOE_BLK


def phase_moe_sorted(kb, l, xsrc, xsrc_key, xdst, xdst_key, final=None):
    import os
    C = kb.C
    tag = f"moe{l}"
    P = kb.P
    BLK, NB = MOE_BLK, MOE_NB
    NTB = BLK // 128
    IOA = bass.IndirectOffsetOnAxis
    wr = kb.sb(tag + "_wr", (128, KC, 32), BF16)
    load_w_cast(kb, wr, tag + "_wr", kb.dins["w_router"][l], 0, 32)
    brb = kb.sb(tag + "_brb", (1, 32), BF16)
    kb.dma("pool", brb[:], kb.din(tag + "_brd", (1, 32)), sem=tag + "_brb", w=[tag + "_brb"])
    ones5 = kb.sb(tag + "_ones5", (1, 512), BF16)
    kb.S.op("dve", lambda e: e.memset(ones5[:], 1.0), [], [tag + "_ones5"])
    hTs = kb.sb(tag + "_hT", (128, KC, BLK), BF16)
    lg_all = kb.sb(tag + "_lg", (128, NT, 32))
    msk_all = kb.sb(tag + "_msk", (128, NT, 32))
    R_all = kb.sb(tag + "_R", (128, NT, 32))
    v8_all = kb.sb(tag + "_v8", (128, NT, 8))
    gk_all = kb.sb(tag + "_gk", (128, NT, 4))
    sml = kb.sb(tag + "_sml", (128, 4))
    cnt = kb.sb(tag + "_cnt", (128, 32))
    kb.S.op("pool", lambda e: e.memset(cnt[:], 0.0), [], [tag + "_cnt"])
    padded = kb.sb(tag + "_padded", (128, 32))
    pstart = kb.sb(tag + "_pstart", (128, 32))
    pend = kb.sb(tag + "_pend", (128, 32))
    pcol = kb.sb(tag + "_pcol", (32, 1))
    pcb = kb.sb(tag + "_pcb", (32, 128))
    dg = kb.sb(tag + "_dg", (32, 32))
    posf = kb.sb(tag + "_posf", (128, NT, 4))
    posi = kb.sb(tag + "_posi", (128, NT, 4), I32)
    eb = kb.sb(tag + "_eb", (128, NB))
    offi = kb.sb(tag + "_offi", (128, NB, KC), I32)
    oh = kb.sb(tag + "_oh", (32, NB), BF16)
    kb.push_scope()
    gf_row, gf_key = kb.gf_row[l]
    scf = kb.sb(tag + "_scf", (128, D))
    shf = kb.sb(tag + "_shf", (128, D))
    nfrow = kb.sb(tag + "_nfrow", (128, D))
    kb.dma("sp", shf[:], kb.modrow_d[l][0], sem=tag + "_shf", r=[f"modrowd{l}"], w=[tag + "_shf"])
    kb.dma("sp", scf[:], kb.modrow_d[l][1], sem=tag + "_scf", r=[f"modrowd{l}"], w=[tag + "_scf"])
    kb.dma("sp", nfrow[:], kb.din(tag + "_nffn", (1, D))[0].partition_broadcast(128), sem=tag + "_nfrow", w=[tag + "_nfrow"])
    kb.stt("dve", scf[:], scf[:], 1.0, nfrow[:], ALU.add, ALU.mult, r=[tag + "_scf", tag + "_nfrow"], w=[tag + "_scf"])
    xs_d = kb.dscr(f"moe_xs{l}", (MOE_ROWS, D))
    ys_d = kb.dscr(f"moe_ys{l}", (MOE_ROWS, D))
    h2_d = kb.dscr(f"moe_h2{l}", (S_TOK, D))
    zt = kb.sb(tag + "_zt", (128, D))
    kb.S.op("pool", lambda e: e.memset(zt[:], 0.0), [], [tag + "_zt"])
    xs_v = xs_d.rearrange("(n p) c -> n p c", p=128)
    for n in range(MOE_ROWS // 128):
        kb.dma("sp", xs_v[n], zt[:], sem=tag + "_zt", r=[tag + "_zt"], w=[tag + "_xs"])
    nb = norm_bufs(kb, tag + "_n")
    h2 = kb.sb(tag + "_h2", (128, D))
    eq = kb.sb(tag + "_eq", (128, NT, 32))
    cmp3 = kb.sb(tag + "_cmp3", (128, NB, 32))
    offf = kb.sb(tag + "_offf", (128, NB, KC))
    for i in range(NT):
        hv = hTs[:, :, 0:128]
        norm_tile(kb, nb, l, "f", xsrc[i * 128:(i + 1) * 128, :], xsrc_key(i), hv, tag + "_hT")
        b_ = (nb["n"] - 1) % 2
        xn, xnk = nb["xn"][b_], f"{tag}_n_xn{b_}"
        kb.tt("pool", h2[:], xn[:], scf[:], ALU.mult, r=[xnk, tag + "_scf"], w=[tag + "_h2"])
        kb.tt("pool", h2[:], h2[:], shf[:], ALU.add, r=[tag + "_h2", tag + "_shf"], w=[tag + "_h2"])
        kb.dma("sp", h2_d[i * 128:(i + 1) * 128, :], h2[:], sem=tag + "_h2", r=[tag + "_h2"], w=[f"{tag}_h2d{i}"])
        for k in range(KC):
            kb.mm(P[7][:, 0:32], hTs[:, k, 0:128], wr[:, k, :], start=(k == 0), stop=False, r=[tag + "_hT", tag + "_wr"], w=kb.pk(7))
        kb.mm(P[7][:, 0:32], ones5[0:1, 0:128], brb[0:1, :], start=False, stop=True, r=[tag + "_ones5", tag + "_brb"], w=kb.pk(7))
        lg, v8, msk = lg_all[:, i, :], v8_all[:, i, :], msk_all[:, i, :]
        kb.cp("dve", lg, P[7][:, 0:32], r=kb.pk(7), w=[tag + "_lg"])
        kb.S.op("dve", lambda e, v8=v8, lg=lg: e.max(out=v8, in_=lg), [tag + "_lg"], [tag + "_v8"])
        kb.ts("dve", msk, lg, v8[:, 3:4], None, ALU.is_ge, r=[tag + "_lg", tag + "_v8"], w=[tag + "_msk"])
        kb.ts("dve", sml[:, 0:1], v8[:, 0:1], -1.0, None, ALU.mult, r=[tag + "_v8"], w=[tag + "_sml"])
        kb.act(gk_all[:, i, :], v8[:, 0:4], AF.Exp, bias=sml[:, 0:1], r=[tag + "_v8", tag + "_sml"], w=[tag + "_gk"])
        kb.S.op("dve", lambda e, i=i: e.reduce_sum(sml[:, 1:2], gk_all[:, i, :], AX.X), [tag + "_gk"], [tag + "_sml"])
        kb.S.op("dve", lambda e: e.reciprocal(sml[:, 1:2], sml[:, 1:2]), [tag + "_sml"], [tag + "_sml"])
        kb.ts("dve", gk_all[:, i, :], gk_all[:, i, :], sml[:, 1:2], None, ALU.mult, r=[tag + "_gk", tag + "_sml"], w=[tag + "_gk"])
        kb.mm(P[6][:, 0:32], C["tri_full"][:], msk, r=["c_tri_full", tag + "_msk"], w=kb.pk(6))
        kb.mm(P[6][:, 32:64], C["ones"][:], msk, r=["c_ones", tag + "_msk"], w=kb.pk(6))
        kb.tt("dve", R_all[:, i, :], P[6][:, 0:32], cnt[:], ALU.add, r=kb.pk(6) + [tag + "_cnt"], w=[tag + "_R"])
        kb.tt("dve", cnt[:], P[6][:, 32:64], cnt[:], ALU.add, r=kb.pk(6) + [tag + "_cnt"], w=[tag + "_cnt"])
    kb.tt("dve", eq[:, 0:8, :].rearrange("p j e -> p e j"), cnt[:].unsqueeze(2).to_broadcast([128, 32, 8]),
          C["blk_thr"][:, 0:8].unsqueeze(1).to_broadcast([128, 32, 8]), ALU.is_gt, r=[tag + "_cnt", "c_blk_thr"], w=[tag + "_eq"])
    kb.S.op("dve", lambda e: e.reduce_sum(padded[:], eq[:, 0:8, :].rearrange("p j e -> p e j"), AX.X), [tag + "_eq"], [tag + "_padded"])
    kb.ts("dve", padded[:], padded[:], float(BLK), None, ALU.mult, r=[tag + "_padded"], w=[tag + "_padded"])
    kb.tt("dve", dg[:], padded[0:32, :], C["ident"][0:32, 0:32], ALU.mult, r=[tag + "_padded", "c_ident"], w=[tag + "_dg"])
    kb.S.op("dve", lambda e: e.reduce_sum(pcol[:], dg[:], AX.X), [tag + "_dg"], [tag + "_pcol"])
    kb.cp("dve", pcb[:], pcol[:, 0:1].to_broadcast([32, 128]), r=[tag + "_pcol"], w=[tag + "_pcb"])
    kb.mm(P[6][:, 0:32], pcb[:], C["tri_full"][0:32, 0:32], r=[tag + "_pcb", "c_tri_full"], w=kb.pk(6))
    kb.cp("dve", pstart[:], P[6][:, 0:32], r=kb.pk(6), w=[tag + "_pstart"])
    kb.tt("dve", pend[:], pstart[:], padded[:], ALU.add, r=[tag + "_pstart", tag + "_padded"], w=[tag + "_pend"])
    kb.tt("dve", R_all[:], R_all[:], pstart[:].unsqueeze(1).to_broadcast([128, NT, 32]), ALU.add,
          r=[tag + "_R", tag + "_pstart"], w=[tag + "_R"])
    for k in range(4):
        kb.tt("dve", eq[:], lg_all[:], v8_all[:, :, k:k + 1].to_broadcast([128, NT, 32]), ALU.is_equal,
              r=[tag + "_lg", tag + "_v8"], w=[tag + "_eq"])
        kb.tt("dve", eq[:], eq[:], R_all[:], ALU.mult, r=[tag + "_eq", tag + "_R"], w=[tag + "_eq"])
        kb.S.op("dve", lambda e, k=k: e.reduce_sum(posf[:, :, k], eq[:], AX.X), [tag + "_eq"], [tag + "_posf"])
    kb.cp("dve", posi[:], posf[:], r=[tag + "_posf"], w=[tag + "_posi"])
    kb.tt("dve", cmp3[:], pend[:].unsqueeze(1).to_broadcast([128, NB, 32]),
          C["blk_thr"][:, 0:NB].unsqueeze(2).to_broadcast([128, NB, 32]), ALU.is_le, r=[tag + "_pend", "c_blk_thr"], w=[tag + "_cmp3"])
    kb.S.op("dve", lambda e: e.reduce_sum(eb[:], cmp3[:], AX.X), [tag + "_cmp3"], [tag + "_eb"])
    kb.ts("dve", eb[:], eb[:], 31.0, None, ALU.min, r=[tag + "_eb"], w=[tag + "_eb"])
    kb.ts("dve", offf[:], eb[:].unsqueeze(2).to_broadcast([128, NB, KC]), float(D), float(l * 32 * D), ALU.mult, ALU.add,
          r=[tag + "_eb"], w=[tag + "_offf"])
    kb.tt("dve", offf[:], offf[:], C["base_pk"][:, 0:KC].unsqueeze(1).to_broadcast([128, NB, KC]), ALU.add,
          r=[tag + "_offf", "c_base_pk"], w=[tag + "_offf"])
    kb.cp("dve", offi[:], offf[:], r=[tag + "_offf"], w=[tag + "_offi"])
    kb.ts("dve", oh[:], eb[0:32, :], C["base_pk"][0:32, 0:1], None, ALU.is_equal, r=[tag + "_eb", "c_base_pk"], w=[tag + "_oh"])
    for i in range(NT):
        kb.dma("sp", h2[:], h2_d[i * 128:(i + 1) * 128, :], sem=tag + "_h2", r=[f"{tag}_h2d{i}"], w=[tag + "_h2"])
        for k in range(4):
            kb.S.dma("pool", lambda e, i=i, k=k: e.indirect_dma_start(
                out=xs_d, out_offset=IOA(ap=posi[:, i, k:k + 1], axis=0), in_=h2[:], in_offset=None),
                tag + "_h2", [tag + "_h2", tag + "_posi"], [tag + "_xs"])
    kb.pop_scope()
    kb.push_scope()
    bgu_sb = kb.sb(tag + "_bgu", (32, 2048), BF16)
    bd_sb = kb.sb(tag + "_bd", (32, D), BF16)
    kb.dma("pool", bgu_sb[:], kb.dins["b_gate_up"][l], sem=tag + "_bgu", w=[tag + "_bgu"])
    kb.dma("pool", bd_sb[:], kb.dins["b_down"][l], sem=tag + "_bd", w=[tag + "_bd"])
    wgu = [kb.sb(tag + f"_wgu{i}", (128, KC, 2048), BF16) for i in range(2)]
    wd = [kb.sb(tag + f"_wd{i}", (128, KC, D), BF16) for i in range(2)]
    ohb = kb.sb(tag + "_ohb", (32, BLK), BF16)
    xr = [kb.sb(tag + f"_xr{i}", (128, D)) for i in range(2)]
    actT = kb.sb(tag + "_actT", (128, KC, BLK), BF16)
    g7 = kb.sb(tag + "_g7", (128, BLK))
    sg = kb.sb(tag + "_sg", (128, BLK))
    u7 = kb.sb(tag + "_u7", (128, BLK))
    yb = [kb.sb(tag + f"_yb{i}", (128, D)) for i in range(2)]
    wgu_flat = kb.dins["w_gate_up"].rearrange("l e r c -> (l e r) c")
    wd_flat = kb.dins["w_down"].rearrange("l e r c -> (l e r) c")

    def load_block_w(b, slot):
        for k in range(KC):
            kb.S.dma("pool", lambda e, b=b, k=k, slot=slot: e.indirect_dma_start(
                out=wgu[slot][:, k, :], out_offset=None, in_=wgu_flat, in_offset=IOA(ap=offi[:, b, k:k + 1], axis=0)),
                tag + f"_wgu{slot}", [tag + "_offi"], [tag + f"_wgu{slot}"])
        for k in range(KC):
            kb.S.dma("pool", lambda e, b=b, k=k, slot=slot: e.indirect_dma_start(
                out=wd[slot][:, k, :], out_offset=None, in_=wd_flat, in_offset=IOA(ap=offi[:, b, k:k + 1], axis=0)),
                tag + f"_wd{slot}", [tag + "_offi"], [tag + f"_wd{slot}"])

    NBR = int(os.environ.get("MOE_NBLK", NB))
    load_block_w(0, 0)
    nx = 0
    for b in range(NBR):
        slot = b % 2
        if b + 1 < NBR:
            load_block_w(b + 1, 1 - slot)
        wk, dk = tag + f"_wgu{slot}", tag + f"_wd{slot}"
        for tt in range(NTB):
            xb = xr[nx % 2]
            xbk = tag + f"_xr{nx % 2}"
            nx += 1
            r0 = b * BLK + tt * 128
            kb.dma("sp", xb[:], xs_d[r0:r0 + 128, :], sem=xbk, r=[tag + "_xs"], w=[xbk])
            for half in range(2):
                pT, pk = P[half], kb.pk(half)
                for kk in range(4):
                    k = half * 4 + kk
                    kb.tr(pT[:, kk * 128:(kk + 1) * 128], xb[:, k * 128:(k + 1) * 128], C["ident"][:], r=[xbk, "c_ident"], w=pk)
                kb.cp("act" if half == 0 else "dve", hTs[:, half * 4:(half + 1) * 4, tt * 128:(tt + 1) * 128],
                      pT[:, :].rearrange("p (k t) -> p k t", k=4), r=pk, w=[tag + "_hT"])
        kb.cp("dve", ohb[:], oh[:, b:b + 1].to_broadcast([32, BLK]), r=[tag + "_oh"], w=[tag + "_ohb"])
        for jc in range(KC):
            pb = 2 + (jc % 2) * 2
            PG, PU = P[pb], P[pb + 1]
            for which, PX in ((0, PG), (1, PU)):
                cols = slice(jc * 256 + which, (jc + 1) * 256, 2)
                for k in range(KC):
                    kb.mm(PX[:, :], wgu[slot][:, k, cols], hTs[:, k, :], start=(k == 0), stop=False,
                          r=[wk, tag + "_hT"], w=kb.pk(pb + which))
                kb.mm(PX[:, :], bgu_sb[:, cols], ohb[:, :], start=False, stop=True, r=[tag + "_bgu", tag + "_ohb"], w=kb.pk(pb + which))
            kb.ts("dve", g7[:], PG[:, :], SW_LIMIT, None, ALU.min, r=kb.pk(pb), w=[tag + "_g7"])
            kb.ts("dve", u7[:], PU[:, :], -SW_LIMIT, SW_LIMIT, ALU.max, ALU.min, r=kb.pk(pb + 1), w=[tag + "_u7"])
            kb.act(sg[:], g7[:], AF.Sigmoid, scale=SW_ALPHA, r=[tag + "_g7"], w=[tag + "_sg"])
            kb.ts("pool", u7[:], u7[:], 1.0, None, ALU.add, r=[tag + "_u7"], w=[tag + "_u7"])
            kb.tt("pool", u7[:], u7[:], g7[:], ALU.mult, r=[tag + "_u7", tag + "_g7"], w=[tag + "_u7"])
            kb.tt("pool", actT[:, jc, :], u7[:], sg[:], ALU.mult, r=[tag + "_u7", tag + "_sg"], w=[tag + "_actT"])
        for tt in range(NTB):
            ybt, ybk = yb[tt % 2], tag + f"_yb{tt % 2}"
            for half in range(2):
                pi = 6 + half
                PO = P[pi]
                hs = slice(half * 512, (half + 1) * 512)
                for jc in range(KC):
                    kb.mm(PO[:, :], actT[:, jc, tt * 128:(tt + 1) * 128], wd[slot][:, jc, hs], start=(jc == 0), stop=False,
                          r=[tag + "_actT", dk], w=kb.pk(pi))
                kb.mm(PO[:, :], ohb[:, 0:128], bd_sb[:, hs], start=False, stop=True, r=[tag + "_ohb", tag + "_bd"], w=kb.pk(pi))
                kb.cp("act" if half == 0 else "dve", ybt[:, hs], PO[:, :], r=kb.pk(pi), w=[ybk])
            r0 = b * BLK + tt * 128
            kb.dma("sp", ys_d[r0:r0 + 128, :], ybt[:], sem=ybk, r=[ybk], w=[tag + "_ys"])
    kb.pop_scope()
    kb.push_scope()
    yk = [kb.sb(tag + f"_yk{i}", (128, D)) for i in range(2)]
    acc = kb.sb(tag + "_acc", (128, D))
    xt = kb.sb(tag + "_xt", (128, D))
    if final is not None:
        nfr = kb.sb(tag + "_nfr", (128, D))
        kb.dma("sp", nfr[:], final["nf"].partition_broadcast(128), sem=tag + "_nfr", w=[tag + "_nfr"])
        fj = kb.sb(tag + "_fj", (128, D), BF16)
        fs = kb.sb(tag + "_fs", (128, 2))
    ng = 0
    for i in range(NT):
        kb.dma("sp", xt[:], xsrc[i * 128:(i + 1) * 128, :], sem=tag + "_xt", r=[xsrc_key(i)], w=[tag + "_xt"])
        for k in range(4):
            yt, ytk = yk[ng % 2], tag + f"_yk{ng % 2}"
            ng += 1
            kb.S.dma("pool", lambda e, i=i, k=k, yt=yt: e.indirect_dma_start(
                out=yt[:], out_offset=None, in_=ys_d, in_offset=IOA(ap=posi[:, i, k:k + 1], axis=0)),
                ytk, [tag + "_ys", tag + "_posi"], [ytk])
            if k == 0:
                kb.ts("dve", acc[:], yt[:], gk_all[:, i, k:k + 1], None, ALU.mult, r=[ytk, tag + "_gk"], w=[tag + "_acc"])
            else:
                kb.stt("dve", acc[:], yt[:], gk_all[:, i, k:k + 1], acc[:], ALU.mult, ALU.add, r=[ytk, tag + "_gk", tag + "_acc"], w=[tag + "_acc"])
        kb.tt("pool", acc[:], acc[:], gf_row[:], ALU.mult, r=[tag + "_acc", gf_key], w=[tag + "_acc"])
        kb.tt("pool", acc[:], acc[:], xt[:], ALU.add, r=[tag + "_acc", tag + "_xt"], w=[tag + "_acc"])
        if final is None:
            kb.dma("sp", xdst[i * 128:(i + 1) * 128, :], acc[:], sem=tag + "_acc", r=[tag + "_acc"], w=[xdst_key(i)])
        else:
            kb.act(fj[:], acc[:], AF.Square, accum_out=fs[:, 0:1], r=[tag + "_acc"], w=[tag + "_fj", tag + "_fs"])
            kb.ts("dve", fs[:, 1:2], fs[:, 0:1], 1.0 / D, EPS, ALU.mult, ALU.add, r=[tag + "_fs"], w=[tag + "_fs"])
            kb.act(fs[:, 1:2], fs[:, 1:2], AF.Sqrt, r=[tag + "_fs"], w=[tag + "_fs"])
            kb.S.op("dve", lambda e: e.reciprocal(fs[:, 1:2], fs[:, 1:2]), [tag + "_fs"], [tag + "_fs"])
            kb.stt("dve", acc[:], acc[:], fs[:, 1:2], nfr[:], ALU.mult, ALU.mult, r=[tag + "_acc", tag + "_fs", tag + "_nfr"], w=[tag + "_acc"])
            kb.dma("sp", final["out"][i * 128:(i + 1) * 128, :], acc[:], sem=tag + "_acc", r=[tag + "_acc"], w=[f"out{i}"])
    kb.pop_scope()
```

```python
import numpy as np
from contextlib import ExitStack
from concourse.bass_utils import run_bass_kernel_spmd

import concourse.bass as bass
import concourse.mybir as mybir

ENGINES = ("pe", "act", "dve", "pool", "sp")


class Op:
    __slots__ = ("eng", "fn", "deps", "is_dma", "dsem", "dcount", "signal", "idx", "signo")

    def __init__(self, eng, fn):
        self.eng = eng
        self.fn = fn
        self.deps = []
        self.is_dma = False
        self.dsem = None
        self.dcount = 0
        self.signal = False
        self.idx = -1
        self.signo = 0


class Sched:
    def __init__(self, nc, same_engine_sync=True):
        self.nc = nc
        self.q = {e: [] for e in ENGINES}
        self.res_w = {}
        self.res_r = {}
        self.phys = []
        self.key2phys = {}
        self.free_phys = []
        self.same_engine_sync = same_engine_sync

    def _collect(self, op, reads, writes, my_dma_key=None):
        deps = []
        for k in reads:
            t = self.res_w.get(k)
            if t is not None:
                deps.append(t)
        for k in writes:
            t = self.res_w.get(k)
            if t is not None:
                if not (my_dma_key is not None and t[0] == 'dma' and t[1] == my_dma_key):
                    deps.append(t)
            deps.extend(self.res_r.get(k, ()))
        op.deps = deps

    def _commit(self, tok, reads, writes):
        for k in reads:
            self.res_r.setdefault(k, []).append(tok)
        for k in writes:
            self.res_w[k] = tok
            self.res_r[k] = []

    @staticmethod
    def _excl(reads, writes):
        rp = [k for k in reads if len(k) == 2 and k[0] == "P" and k[1].isdigit()]
        if not rp:
            return reads, writes
        return [k for k in reads if k not in rp], list(writes) + [k for k in rp if k not in writes]

    def op(self, eng, fn, reads=(), writes=()):
        reads, writes = self._excl(reads, writes)
        o = Op(eng, fn)
        self._collect(o, reads, writes)
        o.idx = len(self.q[eng])
        self.q[eng].append(o)
        self._commit(('op', o), reads, writes)
        return o

    def dma(self, eng, fn, sem_key, reads=(), writes=()):
        reads, writes = self._excl(reads, writes)
        if sem_key not in self.key2phys:
            if self.free_phys:
                p = self.free_phys.pop()
            else:
                p = len(self.phys)
                self.phys.append(0)
            self.key2phys[sem_key] = p
        p = self.key2phys[sem_key]
        o = Op(eng, fn)
        o.is_dma = True
        self._collect(o, reads, writes, my_dma_key=p)
        self.phys[p] += 1
        c = self.phys[p]
        o.dsem = p
        o.dcount = c
        o.idx = len(self.q[eng])
        self.q[eng].append(o)
        self._commit(('dma', p, c), reads, writes)
        return o

    def final_wait(self, eng, keys):
        o = Op(eng, None)
        deps = []
        for k in keys:
            t = self.res_w.get(k)
            if t is not None:
                deps.append(t)
            deps.extend(self.res_r.get(k, ()))
        o.deps = deps
        o.idx = len(self.q[eng])
        self.q[eng].append(o)

    def barrier(self):
        toks = []
        for e in ENGINES:
            for o in reversed(self.q[e]):
                if o.fn is not None and not o.is_dma:
                    toks.append(('op', o))
                    break
        for p, c in enumerate(self.phys):
            if c:
                toks.append(('dma', p, c))
        for e in ENGINES:
            o = Op(e, None)
            o.deps = list(toks)
            o.idx = len(self.q[e])
            self.q[e].append(o)
        self.free_phys = list(range(len(self.phys)))[::-1]
        self.key2phys = {}

    def emit(self, stack):
        nc = self.nc
        for e in ENGINES:
            for o in self.q[e]:
                for t in o.deps:
                    if t[0] == 'op':
                        tgt = t[1]
                        if tgt.eng == o.eng and (not self.same_engine_sync or o.eng == 'pe'):
                            continue
                        tgt.signal = True
        for e in ENGINES:
            n = 0
            for o in self.q[e]:
                if o.signal:
                    n += 1
                    o.signo = n
        esem = {e: stack.enter_context(nc.semaphore("s_" + e)) for e in ENGINES}
        dsem = {}
        for p in range(len(self.phys)):
            dsem[p] = stack.enter_context(nc.semaphore(f"d_{p}"))
        block = stack.enter_context(nc.Block())
        stats = {}

        def run(e, engobj):
            waited = {}
            nwait = 0
            for o in self.q[e]:
                need = {}
                for t in o.deps:
                    if t[0] == 'op':
                        tgt = t[1]
                        if tgt.eng == e and (not self.same_engine_sync or e == 'pe'):
                            continue
                        key = ('e', tgt.eng)
                        val = tgt.signo
                    else:
                        key = ('d', t[1])
                        val = t[2] * 16
                    if need.get(key, 0) < val:
                        need[key] = val
                for key, val in need.items():
                    if waited.get(key, 0) >= val:
                        continue
                    waited[key] = val
                    sem = esem[key[1]] if key[0] == 'e' else dsem[key[1]]
                    engobj.wait_ge(sem, val)
                    nwait += 1
                if o.fn is None:
                    continue
                ins = o.fn(engobj)
                if o.is_dma:
                    ins.then_inc(dsem[o.dsem], 16)
                elif o.signal:
                    ins.then_inc(esem[e], 1)
            stats[e] = (len(self.q[e]), nwait)

        @block.tensor
        def _(eng):
            run("pe", eng)

        @block.scalar
        def _(eng):
            run("act", eng)

        @block.vector
        def _(eng):
            run("dve", eng)

        @block.gpsimd
        def _(eng):
            run("pool", eng)

        @block.sync
        def _(eng):
            run("sp", eng)

        return stats


F32 = mybir.dt.float32
BF16 = mybir.dt.bfloat16
I32 = mybir.dt.int32
AF = mybir.ActivationFunctionType
ALU = mybir.AluOpType
AX = mybir.AxisListType

S_TOK = 4096
D = 1024
KC = 8
NT = S_TOK // 128
DEPTH = 2
EPS = 1e-6
IN_COLS = 7968
C_GLA_Q, C_GLA_K, C_GLA_V, C_GLA_LR, C_GLA_R = 0, 256, 512, 1024, 1040
C_GDN_QKV, C_GDN_A, C_GDN_B, C_GDN_G = 1552, 3088, 3092, 3096
C_SSD_Z, C_SSD_XBC, C_SSD_DT, C_MERGE = 3608, 4120, 4888, 4896


class KB:
    def __init__(self, same_engine_sync=True):
        self.nc = bass.Bass("TRN2", target_bir_lowering=False)
        self.S = Sched(self.nc, same_engine_sync=same_engine_sync)
        self.stack = ExitStack()
        self.ins = {}
        self.outs = {}
        self._n = 0
        self.scopes = []
        self._allow_p = False
        self.P = [self.stack.enter_context(self.nc.psum_tensor(f"PB{i}", [128, 512], F32)) for i in range(8)]

    @staticmethod
    def pk(i, a=0, b=512):
        return [f"P{i}"]

    def din(self, name, shape, dt=F32):
        t = self.nc.dram_tensor(name, list(shape), dt, kind="ExternalInput")
        self.ins[name] = t
        return t.ap()

    def dout(self, name, shape, dt=F32):
        t = self.nc.dram_tensor(name, list(shape), dt, kind="ExternalOutput")
        self.outs[name] = t
        return t.ap()

    def dscr(self, name, shape, dt=F32, debug=False):
        if debug:
            return self.dout(name, shape, dt)
        return self.nc.dram_tensor(name, list(shape), dt, kind="Internal").ap()

    def sb(self, name, shape, dt=F32):
        st = self.scopes[-1] if self.scopes else self.stack
        return st.enter_context(self.nc.sbuf_tensor(name, list(shape), dt))

    def sbp(self, name, shape, dt=F32):
        assert not self.scopes or self._allow_p
        return self.stack.enter_context(self.nc.sbuf_tensor(name, list(shape), dt))

    def push_scope(self):
        self.scopes.append(ExitStack())

    def pop_scope(self):
        self.S.barrier()
        self.scopes.pop().close()

    def ps(self, name, shape=(128, 512), dt=F32):
        return self.stack.enter_context(self.nc.psum_tensor(name, list(shape), dt))

    def mm(self, out, lhsT, rhs, start=True, stop=True, r=(), w=()):
        return self.S.op("pe", lambda e: e.matmul(out, lhsT, rhs, start=start, stop=stop), r, w)

    def tr(self, out, in_, ident, r=(), w=()):
        return self.S.op("pe", lambda e: e.transpose(out, in_, ident), r, w)

    def act(self, out, in_, func, bias=None, scale=None, accum_out=None, r=(), w=(), eng="act"):
        kw = {}
        if bias is not None:
            kw["bias"] = bias
        if scale is not None:
            kw["scale"] = scale
        if accum_out is not None:
            kw["accum_out"] = accum_out
        return self.S.op(eng, lambda e: e.activation(out, in_, func, **kw), r, w)

    def ts(self, eng, out, in0, s1, s2, op0, op1=None, accum_out=None, r=(), w=()):
        kw = {}
        if op1 is not None:
            kw["op1"] = op1
        if accum_out is not None:
            kw["accum_out"] = accum_out
        return self.S.op(eng, lambda e: e.tensor_scalar(out, in0, s1, s2, op0, **kw), r, w)

    def tt(self, eng, out, in0, in1, op, r=(), w=()):
        return self.S.op(eng, lambda e: e.tensor_tensor(out, in0, in1, op), r, w)

    def stt(self, eng, out, in0, scalar, in1, op0, op1, r=(), w=()):
        return self.S.op(eng, lambda e: e.scalar_tensor_tensor(out, in0, scalar, in1, op0, op1), r, w)

    def cp(self, eng, out, in_, r=(), w=()):
        if eng == "act":
            return self.S.op(eng, lambda e: e.copy(out, in_), r, w)
        return self.S.op(eng, lambda e: e.tensor_copy(out, in_), r, w)

    def dma(self, eng, out, in_, sem, r=(), w=(), **kw):
        return self.S.dma(eng, lambda e: e.dma_start(out, in_, **kw), sem, r, w)


def phase_consts(kb):
    c = {}
    cdefs = {
        "ident": (128, 128), "tri_incl": (128, 128), "tri_strict": (128, 128), "ones": (128, 128),
        "blk": (128, 128), "selA": (128, 128), "selB": (128, 128), "neg_strict": (128, 128),
        "tri_full": (128, 128), "blk_thr": (128, 64), "base_pk": (128, 8),
    }
    for name, shp in cdefs.items():
        src = kb.din("c_" + name, shp)
        t = kb.sb("cs_" + name, shp)
        kb.dma("sp", t[:], src, sem="c_" + name, w=["c_" + name])
        c[name] = t
        tb = kb.sb("cb_" + name, shp, BF16)
        kb.cp("dve", tb[:], t[:], r=["c_" + name], w=["cb_" + name])
        c[name + "_bf"] = tb
    kb.C = c


def phase_mod(kb):
    nc = kb.nc
    cT = kb.din("cT", (128, KC))
    w_mod = kb.din("w_mod", (DEPTH, D, 6 * D))
    bmodc = kb.din("bmodc", (DEPTH, 128, 48))
    bmodrow = kb.din("bmodrow", (DEPTH, 6, D))
    nmixc = kb.din("nmixc", (DEPTH, 128, KC))
    nffnc = kb.din("nffnc", (DEPTH, 128, KC))
    pers = {}
    for l in range(DEPTH):
        pers[f"modc{l}"] = kb.sbp(f"modc{l}", (128, 48))
        pers[f"modscl{l}"] = kb.sbp(f"modscl{l}", (128, 2, KC))
        for piece in (2, 5):
            pers[f"modrow{l}_{piece}"] = kb.sbp(f"modrow{l}_{piece}", (128, D))
    kb.modrow_d = [[kb.dscr(f"modrowd{l}_{j}", (128, D)) for j in range(2)] for l in range(DEPTH)]
    kb.push_scope()
    rowtmp = kb.sb("modrowtmp", (128, D))
    cact = kb.sb("cact", (128, KC))
    crep = kb.sb("crep", (128, KC, 128))
    kb.dma("sp", cact[:], cT, sem="cact", w=["cact"])
    kb.act(cact[:], cact[:], AF.Silu, r=["cact"], w=["cact"])
    for k in range(KC):
        kb.cp("dve", crep[:, k, :], cact[:, k:k + 1].to_broadcast([128, 128]), r=["cact"], w=["crep"])
    wbuf = [kb.sb(f"modw{i}", (128, KC, 1024)) for i in range(2)]
    pcol = kb.P[0]
    prow = [kb.P[1], kb.P[2]]
    kb.modc, kb.gm_row, kb.gf_row = [], [], []
    kb.sclm, kb.shm, kb.sclf, kb.shf = [], [], [], []
    it = 0
    for l in range(DEPTH):
        modc = pers[f"modc{l}"]
        bc = kb.sb(f"bmodc{l}", (128, 48))
        nm = kb.sb(f"nmixc{l}", (128, KC))
        nf = kb.sb(f"nffnc{l}", (128, KC))
        kb.dma("sp", bc[:], bmodc[l], sem=f"bmodc{l}", w=[f"bmodc{l}"])
        kb.dma("sp", nm[:], nmixc[l], sem=f"nmixc{l}", w=[f"nmixc{l}"])
        kb.dma("sp", nf[:], nffnc[l], sem=f"nffnc{l}", w=[f"nffnc{l}"])
        rows = []
        for piece in range(6):
            wb = wbuf[it % 2]
            wk = f"modw{it % 2}"
            it += 1
            src = w_mod[l, :, piece * 1024:(piece + 1) * 1024].rearrange("(k p) c -> p k c", p=128)
            for hh in range(2):
                kb.dma("sp", wb[:, hh * 4:(hh + 1) * 4, :], src[:, hh * 4:(hh + 1) * 4, :],
                       sem=wk, w=[wk])
            for jj in range(8):
                j = piece * 8 + jj
                for k in range(KC):
                    kb.mm(pcol[:, j:j + 1], wb[:, k, jj * 128:(jj + 1) * 128], cact[:, k:k + 1],
                          start=(k == 0), stop=(k == KC - 1), r=[wk, "cact"], w=kb.pk(0, 0, 128))
            if piece in (2, 3, 4, 5):
                row = pers[f"modrow{l}_{piece}"] if piece in (2, 5) else rowtmp
                if piece in (3, 4):
                    pers_key = f"modrow{l}_{piece}"
                kb.dma("sp", row[:], bmodrow[l, piece].partition_broadcast(128),
                       sem=f"modrow{l}_{piece}", w=[f"modrow{l}_{piece}"])
                for hh in range(2):
                    for k in range(KC):
                        kb.mm(prow[hh][:, :], crep[:, k, :], wb[:, k, hh * 512:(hh + 1) * 512],
                              start=(k == 0), stop=(k == KC - 1), r=[wk, "crep"], w=kb.pk(1 + hh))
                    kb.tt("dve", row[:, hh * 512:(hh + 1) * 512], prow[hh][:, :], row[:, hh * 512:(hh + 1) * 512],
                          ALU.add, r=kb.pk(1 + hh) + [f"modrow{l}_{piece}"], w=[f"modrow{l}_{piece}"])
                if piece in (2, 5):
                    rows.append((row, f"modrow{l}_{piece}"))
                else:
                    kb.dma("sp", kb.modrow_d[l][piece - 3], row[:], sem=f"modrow{l}_{piece}", r=[f"modrow{l}_{piece}"],
                           w=[f"modrowd{l}"])
        kb.tt("dve", modc[:], pcol[:, 0:48], bc[:], ALU.add, r=kb.pk(0, 0, 128) + [f"bmodc{l}"], w=[f"modc{l}"])
        scl = pers[f"modscl{l}"]
        kb.stt("dve", scl[:, 0, :], modc[:, 8:16], 1.0, nm[:], ALU.add, ALU.mult,
               r=[f"modc{l}", f"nmixc{l}"], w=[f"modc{l}"])
        kb.stt("dve", scl[:, 1, :], modc[:, 32:40], 1.0, nf[:], ALU.add, ALU.mult,
               r=[f"modc{l}", f"nffnc{l}"], w=[f"modc{l}"])
        kb.modc.append(modc)
        kb.gm_row.append(rows[0])
        kb.gf_row.append(rows[1])
        kb.sclm.append(scl[:, 0, :])
        kb.shm.append(modc[:, 0:8])
        kb.sclf.append(scl[:, 1, :])
        kb.shf.append(modc[:, 24:32])
    kb.pop_scope()


def norm_bufs(kb, tag):
    NB = 2
    b = {
        "tag": tag,
        "xt": [kb.sb(f"{tag}_x{i}", (128, D)) for i in range(NB)],
        "xn": [kb.sb(f"{tag}_xn{i}", (128, D)) for i in range(NB)],
        "junk": kb.sb(f"{tag}_junk", (128, D), BF16),
        "ss": kb.sb(f"{tag}_ss", (128, 2)),
        "rstd": kb.sb(f"{tag}_rstd", (128, 2)),
        "n": 0,
    }
    return b


def norm_tile(kb, nb, l, which, xsrc_ap, xsrc_key, dst, dst_key):
    C = kb.C
    tag = nb["tag"]
    scl = kb.sclm[l] if which == "m" else kb.sclf[l]
    sh = kb.shm[l] if which == "m" else kb.shf[l]
    mkey = f"modc{l}"
    b = nb["n"] % 2
    nb["n"] += 1
    xt, xn, junk, ss, rstd = nb["xt"][b], nb["xn"][b], nb["junk"], nb["ss"], nb["rstd"]
    xk, xnk = f"{tag}_x{b}", f"{tag}_xn{b}"
    sk, rk = f"{tag}_ss{b}", f"{tag}_rstd{b}"
    kb.dma("sp", xt[:], xsrc_ap, sem=xk, r=[xsrc_key], w=[xk])
    kb.act(junk[:], xt[:], AF.Square, accum_out=ss[:, b:b + 1], r=[xk], w=[f"{tag}_junk", sk])
    kb.ts("dve", rstd[:, b:b + 1], ss[:, b:b + 1], 1.0 / D, EPS, ALU.mult, ALU.add, r=[sk], w=[rk])
    kb.act(rstd[:, b:b + 1], rstd[:, b:b + 1], AF.Sqrt, r=[rk], w=[rk])
    kb.S.op("dve", lambda e: e.reciprocal(rstd[:, b:b + 1], rstd[:, b:b + 1]), [rk], [rk])
    kb.ts("pool", xn[:], xt[:], rstd[:, b:b + 1], None, ALU.mult, r=[xk, rk], w=[xnk])
    for half in range(2):
        pT = kb.P[half]
        pk = kb.pk(half)
        for kk in range(4):
            k = half * 4 + kk
            kb.tr(pT[:, kk * 128:(kk + 1) * 128], xn[:, k * 128:(k + 1) * 128], C["ident"][:], r=[xnk, "c_ident"], w=pk)
        for kk in range(4):
            k = half * 4 + kk
            d = dst[:, k, :]
            src = pT[:, kk * 128:(kk + 1) * 128]
            if kk % 2 == 0:
                kb.act(d, src, AF.Identity, bias=sh[:, k:k + 1], scale=scl[:, k:k + 1], r=pk + [mkey], w=[dst_key])
            else:
                kb.ts("dve", d, src, scl[:, k:k + 1], sh[:, k:k + 1], ALU.mult, ALU.add, r=pk + [mkey], w=[dst_key])


def phase_norm(kb, l, which, xsrc, xsrc_key, hT, hT_key):
    nb = norm_bufs(kb, f"n{l}{which}")
    for i in range(NT):
        norm_tile(kb, nb, l, which, xsrc[i * 128:(i + 1) * 128, :], xsrc_key(i), hT[:, :, i * 128:(i + 1) * 128], hT_key(i))


def _consts():
    i = np.arange(128)
    same = (i[:, None] // 64) == (i[None, :] // 64)
    return {
        "c_ident": np.eye(128, dtype=np.float32),
        "c_tri_incl": ((i[:, None] <= i[None, :]) & same).astype(np.float32),
        "c_tri_strict": ((i[:, None] > i[None, :]) & same).astype(np.float32),
        "c_ones": np.ones((128, 128), np.float32),
        "c_blk": same.astype(np.float32),
        "c_selA": np.repeat((i < 64).astype(np.float32)[:, None], 128, 1),
        "c_selB": np.repeat((i >= 64).astype(np.float32)[:, None], 128, 1),
        "c_neg_strict": -((i[:, None] > i[None, :]) & same).astype(np.float32),
        "c_tri_full": (i[:, None] < i[None, :]).astype(np.float32),
        "c_blk_thr": np.repeat((np.arange(64, dtype=np.float32) * 512.0)[None, :], 128, 0),
        "c_base_pk": (i[:, None] + 128 * np.arange(8)[None, :]).astype(np.float32),
    }


def host_inputs(inp, b, names):
    m = {}
    m.update(_consts())
    m["x"] = np.ascontiguousarray(inp["x"][b])
    m["cT"] = np.ascontiguousarray(inp["c"][b].reshape(KC, 128).T)
    m["w_mod"] = inp["w_mod"]
    m["bmodc"] = np.ascontiguousarray(inp["b_mod"].reshape(DEPTH, 48, 128).transpose(0, 2, 1))
    bm = inp["b_mod"].reshape(DEPTH, 6, D)
    m["bmodrow"] = np.ascontiguousarray(bm)
    m["nmixc"] = np.ascontiguousarray(inp["norm_mix"].reshape(DEPTH, KC, 128).transpose(0, 2, 1))
    m["nffnc"] = np.ascontiguousarray(inp["norm_ffn"].reshape(DEPTH, KC, 128).transpose(0, 2, 1))
    m.update(host_inputs2(inp, b, names))
    return {k: np.ascontiguousarray(m[k], dtype=m[k].dtype) for k in names}


def proj_feat(kb, out_ps, w_sb, wkey, c0, ncols, hT, hkey, t0, nt, wkeys=None):
    for k in range(KC):
        kb.mm(out_ps, w_sb[:, k, c0:c0 + ncols], hT[:, k, t0:t0 + nt], start=(k == 0), stop=(k == KC - 1),
              r=[wkey, hkey], w=wkeys)


def proj_tok(kb, out_ps, w_sb, wkey, c0, ncols, hT, hkey, t0, nt, wkeys=None):
    for k in range(KC):
        kb.mm(out_ps, hT[:, k, t0:t0 + nt], w_sb[:, k, c0:c0 + ncols], start=(k == 0), stop=(k == KC - 1),
              r=[wkey, hkey], w=wkeys)


def load_w_cast(kb, dst, dkey, src_dram_2d, c0, ncols, nk=KC, step=512):
    src = src_dram_2d.rearrange("(k p) c -> p k c", p=128)
    for k in range(nk):
        kb.dma("pool", dst[:, k, 0:ncols], src[:, k, c0:c0 + ncols], sem=dkey, w=[dkey])


def rms_rstd(kb, tag, rs, ssq, n, width):
    kb.ts("dve", rs[:, 0:n], ssq[:, 0:n], 1.0 / width, EPS, ALU.mult, ALU.add, r=[tag + "_ssq"], w=[tag + "_rs"])
    kb.act(rs[:, 0:n], rs[:, 0:n], AF.Sqrt, r=[tag + "_rs"], w=[tag + "_rs"])
    kb.S.op("dve", lambda e: e.reciprocal(rs[:, 0:n], rs[:, 0:n]), [tag + "_rs"], [tag + "_rs"])


def phase_gla(kb, l, hT, hkey_fn, obr):
    C = kb.C
    tag = f"gla{l}"
    w_in = kb.w_in
    NW = 1552
    wg = kb.sb(tag + "_w", (128, KC, NW), BF16)
    load_w_cast(kb, wg, tag + "_w", w_in[l], 0, NW)
    w2 = kb.sb(tag + "_w2", (16, 256))
    b2 = kb.sb(tag + "_b2", (1, 256))
    gn = kb.sb(tag + "_gn", (128, 512))
    kb.dma("sp", w2[:], kb.din(tag + "_w2d", (16, 256)), sem=tag + "_w2", w=[tag + "_w2"])
    kb.dma("sp", b2[:], kb.din(tag + "_b2d", (1, 256)), sem=tag + "_b2", w=[tag + "_b2"])
    kb.dma("sp", gn[:], kb.din(tag + "_gnd", (1, 512))[0].partition_broadcast(128), sem=tag + "_gn", w=[tag + "_gn"])
    lrT = kb.sb(tag + "_lrT", (16, 128))
    sp = kb.sb(tag + "_sp", (128, 256))
    e_rem = kb.sb(tag + "_erem", (128, 256))
    e_pos = kb.sb(tag + "_epos", (128, 256))
    e_neg = kb.sb(tag + "_eneg", (128, 256))
    qdT = kb.sb(tag + "_qdT", (128, 256), BF16)
    knT = kb.sb(tag + "_knT", (128, 256), BF16)
    krem = kb.sb(tag + "_krem", (128, 256), BF16)
    v_sb = kb.sb(tag + "_v", (128, 512), BF16)
    r_sb = kb.sb(tag + "_r", (128, 512))
    attT = [kb.sb(tag + f"_attT{i}", (128, 128), BF16) for i in range(4)]
    S = [kb.sb(tag + f"_S{i}", (128, 256)) for i in range(2)]
    Sb = [kb.sb(tag + f"_Sb{i}", (128, 256), BF16) for i in range(2)]
    junk = kb.sb(tag + "_junk", (128, 128), BF16)
    ssq = kb.sb(tag + "_ssq", (128, 4))
    rs = kb.sb(tag + "_rs", (128, 4))
    og = kb.sb(tag + "_og", (128, 512))
    oint = kb.sb(tag + "_oint", (128, 512))
    o_sb = kb.sb(tag + "_o", (128, 512))
    oT = kb.sb(tag + "_oT", (128, 512), BF16)
    for p in range(2):
        kb.S.op("dve", lambda e, p=p: e.memset(S[p][:], 0.0), [], [tag + f"_S{p}"])
        kb.S.op("dve", lambda e, p=p: e.memset(Sb[p][:], 0.0), [], [tag + f"_Sb{p}"])
    P0, P1, P2, P3, P4, P5, P6, P7 = kb.P
    wk = tag + "_w"
    import os
    STOP = float(os.environ.get('GLA_STOP', '9'))
    for i in range(int(os.environ.get('GLA_NT', NT))):
        t0 = i * 128
        hk = hkey_fn(i)
        for pair in range(2):
            proj_feat(kb, P0[:, pair * 128:(pair + 1) * 128], wg, wk, C_GLA_Q + pair * 128, 128, hT, hk, t0, 128, kb.pk(0, pair * 128, pair * 128 + 128))
            proj_feat(kb, P0[:, 256 + pair * 128:256 + (pair + 1) * 128], wg, wk, C_GLA_K + pair * 128, 128, hT, hk, t0, 128, kb.pk(0, 256 + pair * 128, 384 + pair * 128))
        proj_feat(kb, P1[0:16, 0:128], wg, wk, C_GLA_LR, 16, hT, hk, t0, 128, kb.pk(1, 0, 128))
        proj_tok(kb, P2[:, 0:256], wg, wk, C_GLA_K, 256, hT, hk, t0, 128, kb.pk(2, 0, 256))
        proj_tok(kb, P3[:, :], wg, wk, C_GLA_V, 512, hT, hk, t0, 128, kb.pk(3))
        proj_tok(kb, P4[:, :], wg, wk, C_GLA_R, 512, hT, hk, t0, 128, kb.pk(4))
        if STOP <= 1:
            continue
        kb.cp("dve", lrT[:, :], P1[0:16, 0:128], r=kb.pk(1, 0, 128), w=[tag + "_lrT"])
        kb.mm(P1[:, 128:384], lrT[:, :], w2[:, :], start=True, stop=False, r=[tag + "_lrT", tag + "_w2"], w=kb.pk(1, 128, 384))
        kb.mm(P1[:, 128:384], C["ones"][0:1, :], b2[0:1, :], start=False, stop=True, r=["c_ones", tag + "_b2"], w=kb.pk(1, 128, 384))
        kb.act(sp[:], P1[:, 128:384], AF.Exp, scale=-1.0, r=kb.pk(1, 128, 384), w=[tag + "_sp"])
        kb.act(sp[:], sp[:], AF.Ln, bias=1.0, r=[tag + "_sp"], w=[tag + "_sp"])
        if STOP <= 2:
            continue
        kb.cp("act", v_sb[:], P3[:, :], r=kb.pk(3), w=[tag + "_v"])
        kb.act(r_sb[:], P4[:, :], AF.Silu, r=kb.pk(4), w=[tag + "_r"])
        kb.mm(P5[:, 256:512], C["tri_strict"][:], sp[:], r=["c_tri_strict", tag + "_sp"], w=kb.pk(5, 256, 512))
        for pair in range(2):
            kb.mm(P6[:, pair * 128:(pair + 1) * 128], sp[:, pair * 128:(pair + 1) * 128], C["tri_incl"][:],
                  r=["c_tri_incl", tag + "_sp"], w=kb.pk(6, 0, 256))
        kb.act(e_rem[:], P5[:, 256:512], AF.Exp, scale=-1.0 / 16, r=kb.pk(5, 256, 512), w=[tag + "_erem"])
        kb.act(e_pos[:], P6[:, 0:256], AF.Exp, scale=-1.0 / 16, r=kb.pk(6, 0, 256), w=[tag + "_epos"])
        kb.act(e_neg[:], P6[:, 0:256], AF.Exp, scale=1.0 / 16, r=kb.pk(6, 0, 256), w=[tag + "_eneg"])
        kb.stt("dve", qdT[:], P0[:, 0:256], 0.125, e_pos[:], ALU.mult, ALU.mult, r=kb.pk(0, 0, 256) + [tag + "_epos"], w=[tag + "_qdT"])
        kb.tt("dve", knT[:], P0[:, 256:512], e_neg[:], ALU.mult, r=kb.pk(0, 256, 512) + [tag + "_eneg"], w=[tag + "_knT"])
        kb.tt("dve", krem[:], P2[:, 0:256], e_rem[:], ALU.mult, r=kb.pk(2, 0, 256) + [tag + "_erem"], w=[tag + "_krem"])
        if STOP <= 3:
            continue
        zones = [(P1[:, 384:512], kb.pk(1)), (P2[:, 256:384], kb.pk(2)), (P3[:, 0:128], kb.pk(3)), (P4[:, 0:128], kb.pk(4))]
        for h in range(4):
            pair, rows = h // 2, (h % 2) * 64
            pc = slice(pair * 128, (pair + 1) * 128)
            aps, apk = zones[h]
            kb.mm(aps, knT[rows:rows + 64, pc], qdT[rows:rows + 64, pc], r=[tag + "_knT", tag + "_qdT"], w=apk)
        for h in range(4):
            aps, apk = zones[h]
            kb.tt("dve", attT[h][:], aps, C["tri_incl"][:], ALU.mult, r=apk + ["c_tri_incl"], w=[tag + f"_attT{h}"])
        for h in range(4):
            hc = slice(h * 128, (h + 1) * 128)
            kb.mm(P7[:, hc], attT[h][:], v_sb[:, hc], start=True, stop=True, r=[tag + f"_attT{h}", tag + "_v"], w=kb.pk(7))

        def pairchain(pair):
            pc0 = pair * 128
            Sk, Sbk = tag + f"_S{pair}", tag + f"_Sb{pair}"
            PU, ku = (P6, kb.pk(6)) if pair == 0 else (P3, kb.pk(3))
            for ch in range(2):
                tr_ = slice(ch * 64, (ch + 1) * 64)
                kb.mm(P5[tr_, pair * 256:(pair + 1) * 256], qdT[:, pc0 + ch * 64:pc0 + (ch + 1) * 64],
                      Sb[pair][:, :], start=True, stop=True, r=[tag + "_qdT", Sbk], w=kb.pk(5))
                kb.mm(PU[:, 256:512], krem[tr_, pc0:pc0 + 128], v_sb[tr_, pair * 256:(pair + 1) * 256],
                      r=[tag + "_krem", tag + "_v"], w=ku)
                yield
                for hh in range(2):
                    rr = slice(hh * 64, (hh + 1) * 64)
                    cc = slice(hh * 128, (hh + 1) * 128)
                    dec = e_pos[rr, pc0 + ch * 64 + 63:pc0 + ch * 64 + 64]
                    kb.stt("dve", S[pair][rr, cc], S[pair][rr, cc], dec, PU[rr, 256 + hh * 128:256 + (hh + 1) * 128],
                           ALU.mult, ALU.add, r=[Sk, tag + "_epos"] + ku, w=[Sk])
                yield
                kb.cp("act", Sb[pair][:], S[pair][:], r=[Sk], w=[Sbk])
                yield

        gens = [pairchain(0), pairchain(1)]
        while gens:
            for gen in list(gens):
                try:
                    next(gen)
                except StopIteration:
                    gens.remove(gen)
        if STOP <= 5:
            continue
        kb.cp("act", oint[:], P5[:, :], r=kb.pk(5), w=[tag + "_oint"])
        kb.tt("dve", o_sb[:], P7[:, :], oint[:], ALU.add, r=kb.pk(7) + [tag + "_oint"], w=[tag + "_o"])
        for h in range(4):
            kb.act(junk[:], o_sb[:, h * 128:(h + 1) * 128], AF.Square, accum_out=ssq[:, h:h + 1], r=[tag + "_o"],
                   w=[tag + "_junk", tag + "_ssq"])
        rms_rstd(kb, tag, rs, ssq, 4, 128)
        for h in range(4):
            hc = slice(h * 128, (h + 1) * 128)
            kb.stt("dve", og[:, hc], o_sb[:, hc], rs[:, h:h + 1], gn[:, hc], ALU.mult, ALU.mult,
                   r=[tag + "_o", tag + "_rs", tag + "_gn"], w=[tag + "_og"])
        kb.tt("pool", og[:], og[:], r_sb[:], ALU.mult, r=[tag + "_og", tag + "_r"], w=[tag + "_og"])
        for c in range(4):
            kb.tr(P0[:, c * 128:(c + 1) * 128], og[:, c * 128:(c + 1) * 128], C["ident"][:], r=[tag + "_og", "c_ident"], w=kb.pk(0, c * 128, c * 128 + 128))
        kb.cp("act", oT[:], P0[:, :], r=kb.pk(0), w=[tag + "_oT"])
        kb.dma("sp", obr[i], oT[:], sem=tag + "_oT", r=[tag + "_oT"], w=[f"{tag}_obr{i}"])


def host_inputs2(inp, b, names):
    m = {}
    f = np.float32
    for l in range(DEPTH):
        m[f"gla{l}_w2d"] = inp["gla_w_gate2"][l]
        m[f"gla{l}_b2d"] = inp["gla_b_gate2"][l][None, :]
        m[f"gla{l}_gnd"] = np.tile(inp["gla_norm"][l], 4)[None, :]
    m["w_in"] = inp["w_in"]
    for l in range(DEPTH):
        cw = inp["ssd_conv_w"][l].reshape(4, 6, 128).transpose(2, 1, 0)
        cb = inp["ssd_conv_b"][l].reshape(6, 128).T[:, :, None]
        m[f"ssd{l}_cwd"] = np.concatenate([cw, cb], axis=2)
        m[f"ssd{l}_gnd"] = inp["ssd_norm"][l][None, :]
        m[f"ssd{l}_hpd"] = np.concatenate([inp["ssd_dt_bias"][l], inp["ssd_a_log"][l], inp["ssd_d"][l]])[None, :]
    for l in range(DEPTH):
        m[f"gdn{l}_cwd"] = inp["gdn_conv_w"][l].reshape(4, 12, 128).transpose(2, 1, 0)
        m[f"gdn{l}_gnd"] = np.tile(inp["gdn_norm"][l], 4)[None, :]
        m[f"gdn{l}_hpd"] = np.concatenate([inp["gdn_dt_bias"][l], inp["gdn_a_log"][l]])[None, :]
    return m


def phase_gdn(kb, l, hT, hkey_fn, obr):
    import os
    C = kb.C
    tag = f"gdn{l}"
    w_in = kb.w_in
    NW = 2056
    wk = tag + "_w"
    wg = kb.sb(wk, (128, KC, NW), BF16)
    load_w_cast(kb, wg, wk, w_in[l], C_GDN_QKV, NW)
    O_QKV, O_AB, O_G = 0, 1536, 1544
    convw = kb.sb(tag + "_cw", (128, 12, 4))
    kb.dma("sp", convw[:], kb.din(tag + "_cwd", (128, 12, 4)), sem=tag + "_cw", w=[tag + "_cw"])
    gn = kb.sb(tag + "_gn", (128, 512))
    kb.dma("sp", gn[:], kb.din(tag + "_gnd", (1, 512))[0].partition_broadcast(128), sem=tag + "_gn", w=[tag + "_gn"])
    hp = kb.sb(tag + "_hp", (128, 8))
    kb.dma("sp", hp[:], kb.din(tag + "_hpd", (1, 8))[0].partition_broadcast(128), sem=tag + "_hp", w=[tag + "_hp"])
    negA = kb.sb(tag + "_negA", (128, 4))
    kb.act(negA[:], hp[:, 4:8], AF.Exp, r=[tag + "_hp"], w=[tag + "_negA"])
    kb.ts("dve", negA[:], negA[:], -1.0, None, ALU.mult, r=[tag + "_negA"], w=[tag + "_negA"])
    ubuf = kb.sb(tag + "_ubuf", (128, 12, 131))
    kb.S.op("pool", lambda e: e.memset(ubuf[:], 0.0), [], [tag + "_ubuf"])
    cacc = kb.sb(tag + "_cacc", (128, 12, 128))
    ctmp = kb.sb(tag + "_ctmp", (128, 12, 128))
    qkv = kb.sb(tag + "_qkv", (128, 12, 128))
    sq = kb.sb(tag + "_sq", (128, 8, 128))
    rinv = kb.sb(tag + "_rinv", (128, 8, 128))
    qkn = kb.sb(tag + "_qkn", (128, 8, 128))
    gsb = kb.sb(tag + "_gsb", (128, 512))
    sm = kb.sb(tag + "_sm", (128, 40))
    beta, gg, cum, ecum, erem, bec = sm[:, 0:4], sm[:, 4:8], sm[:, 8:12], sm[:, 12:16], sm[:, 16:20], sm[:, 20:24]
    dA, dB, ytmp = sm[:, 24:28], sm[:, 28:32], sm[:, 32:36]
    HB = []
    for s_ in range(4):
        d_ = {}
        for nm in ("otmp", "Gs", "E", "ET", "B0", "B1", "C0", "C1", "PT0", "PT1", "PmT", "ecb", "qdT", "V0", "W0", "kdec", "upre", "wT", "u"):
            d_[nm] = kb.sb(tag + f"_{nm}_{s_}", (128, 128))
        kb.S.op("pool", lambda e, t=d_["u"]: e.memset(t[:], 0.0), [], [tag + f"_u_{s_}"])
        HB.append(d_)
    M = [kb.sb(tag + f"_M{h}", (128, 128)) for h in range(4)]
    for h in range(4):
        kb.S.op("pool", lambda e, h=h: e.memset(M[h][:], 0.0), [], [tag + f"_M{h}"])
    oint = kb.sb(tag + "_oint", (128, 512))
    o_sb = kb.sb(tag + "_o", (128, 512))
    og = kb.sb(tag + "_og", (128, 512))
    oT = kb.sb(tag + "_oT", (128, 512), BF16)
    junk = kb.sb(tag + "_junk", (128, 128), BF16)
    ssq = kb.sb(tag + "_ssq", (128, 4))
    rs = kb.sb(tag + "_rs", (128, 4))
    P0, P1, P2, P3, P4, P5, P6, P7 = kb.P
    ident = C["ident"]
    STOP = float(os.environ.get('GDN_STOP', '9'))
    for i in range(int(os.environ.get('GDN_NT', NT))):
        t0 = i * 128
        hk = hkey_fn(i)
        for c in range(12):
            bank = kb.P[c // 4]
            cc = (c % 4) * 128
            proj_feat(kb, bank[:, cc:cc + 128], wg, wk, O_QKV + c * 128, 128, hT, hk, t0, 128, kb.pk(c // 4, cc, cc + 128))
        proj_tok(kb, P3[:, :], wg, wk, O_G, 512, hT, hk, t0, 128, kb.pk(3))
        proj_tok(kb, P4[:, 0:8], wg, wk, O_AB, 8, hT, hk, t0, 128, kb.pk(4, 0, 128))
        kb.act(gsb[:], P3[:, :], AF.Silu, r=kb.pk(3), w=[tag + "_gsb"])
        for b3 in range(3):
            kb.cp("act", ubuf[:, b3 * 4:(b3 + 1) * 4, 3:131], kb.P[b3][:, :].rearrange("p (c t) -> p c t", c=4),
                  r=kb.pk(b3), w=[tag + "_ubuf"])
        for j in range(4):
            wj = convw[:, :, j:j + 1].to_broadcast([128, 12, 128])
            if j == 0:
                kb.tt("dve", cacc[:], ubuf[:, :, 0:128], wj, ALU.mult, r=[tag + "_ubuf", tag + "_cw"], w=[tag + "_cacc"])
            else:
                kb.tt("pool", ctmp[:], ubuf[:, :, j:j + 128], wj, ALU.mult, r=[tag + "_ubuf", tag + "_cw"], w=[tag + "_ctmp"])
                kb.tt("dve", cacc[:], cacc[:], ctmp[:], ALU.add, r=[tag + "_cacc", tag + "_ctmp"], w=[tag + "_cacc"])
        kb.act(qkv[:], cacc[:], AF.Silu, r=[tag + "_cacc"], w=[tag + "_qkv"])
        kb.cp("pool", ubuf[:, :, 0:3], ubuf[:, :, 128:131], r=[tag + "_ubuf"], w=[tag + "_ubuf"])
        if STOP <= 1:
            continue
        kb.tt("pool", sq[:], qkv[:, 0:8, :], qkv[:, 0:8, :], ALU.mult, r=[tag + "_qkv"], w=[tag + "_sq"])
        for half in range(2):
            kb.mm(kb.P[half][:, :], C["ones"][:], sq[:, half * 4:(half + 1) * 4, :], r=["c_ones", tag + "_sq"], w=kb.pk(half))
            kb.ts("dve", rinv[:, half * 4:(half + 1) * 4, :], kb.P[half][:, :].rearrange("p (c t) -> p c t", c=4),
                  1e-6, None, ALU.add, r=kb.pk(half), w=[tag + "_rinv"])
        kb.act(rinv[:], rinv[:], AF.Sqrt, r=[tag + "_rinv"], w=[tag + "_rinv"])
        kb.S.op("dve", lambda e: e.reciprocal(rinv[:], rinv[:]), [tag + "_rinv"], [tag + "_rinv"])
        kb.stt("dve", qkn[:, 0:4, :], qkv[:, 0:4, :], 128.0 ** -0.5, rinv[:, 0:4, :], ALU.mult, ALU.mult,
               r=[tag + "_qkv", tag + "_rinv"], w=[tag + "_qkn"])
        kb.tt("pool", qkn[:, 4:8, :], qkv[:, 4:8, :], rinv[:, 4:8, :], ALU.mult, r=[tag + "_qkv", tag + "_rinv"], w=[tag + "_qkn"])
        kb.act(beta, P4[:, 4:8], AF.Sigmoid, r=kb.pk(4, 0, 128), w=[tag + "_sm"])
        kb.tt("dve", ytmp, P4[:, 0:4], hp[:, 0:4], ALU.add, r=kb.pk(4, 0, 128) + [tag + "_hp"], w=[tag + "_sm"])
        kb.act(ytmp, ytmp, AF.Exp, r=[tag + "_sm"], w=[tag + "_sm"])
        kb.act(ytmp, ytmp, AF.Ln, bias=1.0, r=[tag + "_sm"], w=[tag + "_sm"])
        kb.tt("dve", gg, ytmp, negA[:], ALU.mult, r=[tag + "_sm", tag + "_negA"], w=[tag + "_sm"])
        kb.mm(P4[:, 8:12], C["tri_incl"][:], gg, r=["c_tri_incl", tag + "_sm"], w=kb.pk(4, 0, 128))
        kb.mm(P4[:, 12:16], C["blk"][:], gg, r=["c_blk", tag + "_sm"], w=kb.pk(4, 0, 128))
        kb.mm(P4[:, 16:20], C["selA"][:], gg, r=["c_selA", tag + "_sm"], w=kb.pk(4, 0, 128))
        kb.mm(P4[:, 20:24], C["selB"][:], gg, r=["c_selB", tag + "_sm"], w=kb.pk(4, 0, 128))
        kb.cp("dve", cum, P4[:, 8:12], r=kb.pk(4, 0, 128), w=[tag + "_sm"])
        kb.act(ecum, P4[:, 8:12], AF.Exp, r=kb.pk(4, 0, 128), w=[tag + "_sm"])
        kb.tt("dve", erem, P4[:, 12:16], cum, ALU.subtract, r=kb.pk(4, 0, 128) + [tag + "_sm"], w=[tag + "_sm"])
        kb.act(erem, erem, AF.Exp, r=[tag + "_sm"], w=[tag + "_sm"])
        kb.act(dA, P4[:, 16:20], AF.Exp, r=kb.pk(4, 0, 128), w=[tag + "_sm"])
        kb.act(dB, P4[:, 20:24], AF.Exp, r=kb.pk(4, 0, 128), w=[tag + "_sm"])
        kb.tt("dve", bec, beta, ecum, ALU.mult, r=[tag + "_sm"], w=[tag + "_sm"])
        if STOP <= 2:
            continue
        def head(h, s_):
            hb = HB[s_]
            XA, XB = kb.P[2 * s_], kb.P[2 * s_ + 1]
            ka, kbk = kb.pk(2 * s_), kb.pk(2 * s_ + 1)
            K = lambda nm: tag + f"_{nm}_{s_}"
            Gs, E, ET, PmT, ecb, qdT = hb["Gs"], hb["E"], hb["ET"], hb["PmT"], hb["ecb"], hb["qdT"]
            V0, W0, kdec, upre, wT, u_sb, otmp = hb["V0"], hb["W0"], hb["kdec"], hb["upre"], hb["wT"], hb["u"], hb["otmp"]
            Bm, Cm, PT = [hb["B0"], hb["B1"]], [hb["C0"], hb["C1"]], [hb["PT0"], hb["PT1"]]
            qT = qkn[:, h, :]
            kT = qkn[:, 4 + h, :]
            vT = qkv[:, 8 + h, :]
            hc = slice(h * 128, (h + 1) * 128)
            Z = lambda i: slice(i * 128, (i + 1) * 128)
            kb.ts("dve", Gs[:], C["tri_incl"][:], gg[:, h:h + 1], None, ALU.mult, r=["c_tri_incl", tag + "_sm"], w=[K("Gs")])
            kb.mm(XA[:, Z(0)], kT, kT, r=[tag + "_qkn"], w=ka)
            kb.mm(XA[:, Z(1)], kT, qT, r=[tag + "_qkn"], w=ka)
            yield
            kb.mm(XA[:, Z(2)], Gs[:], C["tri_strict"][:], r=[K("Gs"), "c_tri_strict"], w=ka)
            kb.mm(XA[:, Z(3)], C["tri_strict"][:], Gs[:], r=[K("Gs"), "c_tri_strict"], w=ka)
            kb.mm(XB[:, Z(0)], C["ones"][:], Gs[:], r=[K("Gs"), "c_ones"], w=kbk)
            yield
            kb.act(E[:], XA[:, Z(2)], AF.Exp, r=ka, w=[K("E")])
            kb.act(ET[:], XA[:, Z(3)], AF.Exp, r=ka, w=[K("ET")])
            kb.act(ecb[:], XB[:, Z(0)], AF.Exp, r=kbk, w=[K("ecb")])
            yield
            kb.tt("dve", E[:], E[:], C["neg_strict"][:], ALU.mult, r=[K("E"), "c_neg_strict"], w=[K("E")])
            kb.tt("dve", ET[:], ET[:], C["tri_incl"][:], ALU.mult, r=[K("ET"), "c_tri_incl"], w=[K("ET")])
            kb.tt("pool", qdT[:], qT, ecb[:], ALU.mult, r=[tag + "_qkn", K("ecb")], w=[K("qdT")])
            yield
            kb.stt("dve", Bm[0][:], XA[:, Z(0)], beta[:, h:h + 1], E[:], ALU.mult, ALU.mult,
                   r=ka + [tag + "_sm", K("E")], w=[K("B0")])
            kb.tt("dve", PmT[:], XA[:, Z(1)], ET[:], ALU.mult, r=ka + [K("ET")], w=[K("PmT")])
            yield
            kb.tr(XB[:, Z(1)], Bm[0][:], ident[:], r=[K("B0"), "c_ident"], w=kbk)
            kb.tr(XB[:, Z(2)], vT, ident[:], r=[tag + "_qkv", "c_ident"], w=kbk)
            kb.tr(XB[:, Z(3)], kT, ident[:], r=[tag + "_qkn", "c_ident"], w=kbk)
            yield
            kb.cp("act", Cm[0][:], XB[:, Z(1)], r=kbk, w=[K("C0")])
            kb.tt("dve", PT[0][:], XB[:, Z(1)], ident[:], ALU.add, r=kbk + ["c_ident"], w=[K("PT0")])
            kb.ts("dve", V0[:], XB[:, Z(2)], beta[:, h:h + 1], None, ALU.mult, r=kbk + [tag + "_sm"], w=[K("V0")])
            kb.act(W0[:], XB[:, Z(3)], AF.Identity, scale=bec[:, h:h + 1], r=kbk + [tag + "_sm"], w=[K("W0")])
            kb.ts("dve", kdec[:], XB[:, Z(3)], erem[:, h:h + 1], None, ALU.mult, r=kbk + [tag + "_sm"], w=[K("kdec")])
            yield
            cur = 0
            kb.mm(XB[:, Z(0)], Cm[0][:], Bm[0][:], r=[K("B0"), K("C0")], w=kbk)
            kb.mm(XB[:, Z(1)], Bm[0][:], Cm[0][:], r=[K("B0"), K("C0")], w=kbk)
            yield
            for j in range(1, 6):
                nxt = 1 - cur
                Bn, Cn, PTk, PTn = K(f"B{nxt}"), K(f"C{nxt}"), K(f"PT{cur}"), K(f"PT{nxt}")
                kb.cp("dve", Bm[nxt][:], XB[:, Z(0)], r=kbk, w=[Bn])
                if j < 5:
                    kb.cp("act", Cm[nxt][:], XB[:, Z(1)], r=kbk, w=[Cn])
                yield
                kb.mm(XB[:, Z(2)], Bm[nxt][:], PT[cur][:], r=[Bn, PTk], w=kbk)
                if j < 5:
                    kb.mm(XB[:, Z(0)], Cm[nxt][:], Bm[nxt][:], r=[Bn, Cn], w=kbk)
                    if j < 4:
                        kb.mm(XB[:, Z(1)], Bm[nxt][:], Cm[nxt][:], r=[Bn, Cn], w=kbk)
                yield
                kb.tt("dve", PT[nxt][:], XB[:, Z(2)], PT[cur][:], ALU.add, r=kbk + [PTk], w=[PTn])
                cur = nxt
            yield
            PTf, PTfk = PT[cur], K(f"PT{cur}")
            kb.mm(XB[:, Z(3)], PTf[:], V0[:], r=[PTfk, K("V0")], w=kbk)
            kb.mm(XA[:, Z(0)], W0[:], PTf[:], r=[PTfk, K("W0")], w=ka)
            yield
            kb.cp("act", upre[:], XB[:, Z(3)], r=kbk, w=[K("upre")])
            kb.cp("dve", wT[:], XA[:, Z(0)], r=ka, w=[K("wT")])
            yield
            Mk = tag + f"_M{h}"
            for ch in range(2):
                tr_ = slice(ch * 64, (ch + 1) * 64)
                dch = dA if ch == 0 else dB
                kb.mm(XA[tr_, Z(1)], wT[:, tr_], M[h][:], r=[K("wT"), Mk], w=ka)
                kb.mm(XA[tr_, Z(3)], qdT[:, tr_], M[h][:], r=[K("qdT"), Mk], w=ka)
                yield
                kb.tt("dve", u_sb[tr_, :], upre[tr_, :], XA[tr_, Z(1)], ALU.subtract, r=[K("upre")] + ka, w=[K("u")])
                kb.cp("act", otmp[tr_, :], XA[tr_, Z(3)], r=ka, w=[K("otmp")])
                yield
                kb.mm(XB[tr_, Z(3)], PmT[:, tr_], u_sb[:, :], r=[K("PmT"), K("u")], w=kbk)
                kb.mm(XA[:, Z(2)], kdec[tr_, :], u_sb[tr_, :], r=[K("kdec"), K("u")], w=ka)
                yield
                kb.stt("dve", M[h][:], M[h][:], dch[:, h:h + 1], XA[:, Z(2)], ALU.mult, ALU.add,
                       r=[Mk, tag + "_sm"] + ka, w=[Mk])
                kb.tt("dve", o_sb[tr_, hc], XB[tr_, Z(3)], otmp[tr_, :], ALU.add, r=kbk + [K("otmp")], w=[tag + "_o"])
                yield

        gens = [head(h, h) for h in range(4)]
        while gens:
            for gen in list(gens):
                try:
                    next(gen)
                except StopIteration:
                    gens.remove(gen)
        if STOP <= 4:
            continue
        for h in range(4):
            kb.act(junk[:], o_sb[:, h * 128:(h + 1) * 128], AF.Square, accum_out=ssq[:, h:h + 1], r=[tag + "_o"],
                   w=[tag + "_junk", tag + "_ssq"])
        rms_rstd(kb, tag, rs, ssq, 4, 128)
        for h in range(4):
            hc = slice(h * 128, (h + 1) * 128)
            kb.stt("dve", og[:, hc], o_sb[:, hc], rs[:, h:h + 1], gn[:, hc], ALU.mult, ALU.mult,
                   r=[tag + "_o", tag + "_rs", tag + "_gn"], w=[tag + "_og"])
        kb.tt("pool", og[:], og[:], gsb[:], ALU.mult, r=[tag + "_og", tag + "_gsb"], w=[tag + "_og"])
        for c in range(4):
            kb.tr(P3[:, c * 128:(c + 1) * 128], og[:, c * 128:(c + 1) * 128], ident[:], r=[tag + "_og", "c_ident"],
                  w=kb.pk(3, c * 128, c * 128 + 128))
        kb.cp("act", oT[:], P3[:, :], r=kb.pk(3), w=[tag + "_oT"])
        kb.dma("sp", obr[i], oT[:], sem=tag + "_oT", r=[tag + "_oT"], w=[f"{tag}_obr{i}"])


def phase_ssd(kb, l, hT, hkey_fn, obr):
    import os
    C = kb.C
    tag = f"ssd{l}"
    w_in = kb.w_in
    NW = 1288
    wk = tag + "_w"
    wg = kb.sb(wk, (128, KC, NW), BF16)
    load_w_cast(kb, wg, wk, w_in[l], C_SSD_Z, NW)
    O_Z, O_XBC, O_DT = 0, 512, 1280
    convw = kb.sb(tag + "_cw", (128, 6, 5))
    kb.dma("sp", convw[:], kb.din(tag + "_cwd", (128, 6, 5)), sem=tag + "_cw", w=[tag + "_cw"])
    gn = kb.sb(tag + "_gn", (128, 512))
    kb.dma("sp", gn[:], kb.din(tag + "_gnd", (1, 512))[0].partition_broadcast(128), sem=tag + "_gn", w=[tag + "_gn"])
    hp = kb.sb(tag + "_hp", (128, 24))
    kb.dma("sp", hp[:], kb.din(tag + "_hpd", (1, 24))[0].partition_broadcast(128), sem=tag + "_hp", w=[tag + "_hp"])
    negA = kb.sb(tag + "_negA", (128, 8))
    kb.act(negA[:], hp[:, 8:16], AF.Exp, r=[tag + "_hp"], w=[tag + "_negA"])
    kb.ts("dve", negA[:], negA[:], -1.0, None, ALU.mult, r=[tag + "_negA"], w=[tag + "_negA"])
    ubuf = kb.sb(tag + "_ubuf", (128, 6, 131))
    kb.S.op("pool", lambda e: e.memset(ubuf[:], 0.0), [], [tag + "_ubuf"])
    cacc = kb.sb(tag + "_cacc", (128, 6, 128))
    ctmp = kb.sb(tag + "_ctmp", (128, 6, 128))
    xbc = kb.sb(tag + "_xbc", (128, 6, 128))
    zs = kb.sb(tag + "_zs", (128, 512))
    sm = kb.sb(tag + "_sm", (128, 72))
    dt, gg, cum, ecum, erem = sm[:, 0:8], sm[:, 8:16], sm[:, 16:24], sm[:, 24:32], sm[:, 32:40]
    dA, dB, ytmp = sm[:, 40:48], sm[:, 48:56], sm[:, 56:64]
    x_tok = kb.sb(tag + "_xtok", (128, 512))
    xdt = kb.sb(tag + "_xdt", (128, 512))
    xdte = kb.sb(tag + "_xdte", (128, 512))
    xd = kb.sb(tag + "_xd", (128, 512))
    B_tok = kb.sb(tag + "_Btok", (128, 128))
    CBT = [kb.sb(tag + f"_CBT{g}", (128, 128)) for g in range(2)]
    GsL = [kb.sb(tag + f"_Gs{i}", (128, 128)) for i in range(4)]
    LTL = [kb.sb(tag + f"_LT{i}", (128, 128)) for i in range(4)]
    Sbd = kb.sb(tag + "_Sbd", (128, 512))
    kb.S.op("pool", lambda e: e.memset(Sbd[:], 0.0), [], [tag + "_Sbd"])
    yint = kb.sb(tag + "_yint", (128, 512))
    y_sb = kb.sb(tag + "_y", (128, 512))
    oT = kb.sb(tag + "_oT", (128, 512), BF16)
    junk = kb.sb(tag + "_junk", (128, 256), BF16)
    ssq = kb.sb(tag + "_ssq", (128, 2))
    rs = kb.sb(tag + "_rs", (128, 2))
    P0, P1, P2, P3, P4, P5, P6, P7 = kb.P
    ident = C["ident"]
    STOP = float(os.environ.get('SSD_STOP', '9'))
    for i in range(int(os.environ.get('SSD_NT', NT))):
        t0 = i * 128
        hk = hkey_fn(i)
        for c in range(6):
            bank = kb.P[c // 4]
            cc = (c % 4) * 128
            proj_feat(kb, bank[:, cc:cc + 128], wg, wk, O_XBC + c * 128, 128, hT, hk, t0, 128, kb.pk(c // 4))
        proj_tok(kb, P2[:, :], wg, wk, O_Z, 512, hT, hk, t0, 128, kb.pk(2))
        proj_tok(kb, P3[:, 0:8], wg, wk, O_DT, 8, hT, hk, t0, 128, kb.pk(3))
        kb.act(zs[:], P2[:, :], AF.Silu, r=kb.pk(2), w=[tag + "_zs"])
        kb.cp("act", ubuf[:, 0:4, 3:131], P0[:, :].rearrange("p (c t) -> p c t", c=4), r=kb.pk(0), w=[tag + "_ubuf"])
        kb.cp("act", ubuf[:, 4:6, 3:131], P1[:, 0:256].rearrange("p (c t) -> p c t", c=2), r=kb.pk(1), w=[tag + "_ubuf"])
        for j in range(4):
            wj = convw[:, :, j:j + 1].to_broadcast([128, 6, 128])
            if j == 0:
                kb.tt("dve", cacc[:], ubuf[:, :, 0:128], wj, ALU.mult, r=[tag + "_ubuf", tag + "_cw"], w=[tag + "_cacc"])
                kb.tt("dve", cacc[:], cacc[:], convw[:, :, 4:5].to_broadcast([128, 6, 128]), ALU.add,
                      r=[tag + "_cacc", tag + "_cw"], w=[tag + "_cacc"])
            else:
                kb.tt("pool", ctmp[:], ubuf[:, :, j:j + 128], wj, ALU.mult, r=[tag + "_ubuf", tag + "_cw"], w=[tag + "_ctmp"])
                kb.tt("dve", cacc[:], cacc[:], ctmp[:], ALU.add, r=[tag + "_cacc", tag + "_ctmp"], w=[tag + "_cacc"])
        kb.act(xbc[:], cacc[:], AF.Silu, r=[tag + "_cacc"], w=[tag + "_xbc"])
        kb.cp("pool", ubuf[:, :, 0:3], ubuf[:, :, 128:131], r=[tag + "_ubuf"], w=[tag + "_ubuf"])
        kb.tt("dve", ytmp, P3[:, 0:8], hp[:, 0:8], ALU.add, r=kb.pk(3) + [tag + "_hp"], w=[tag + "_sm"])
        kb.act(ytmp, ytmp, AF.Exp, r=[tag + "_sm"], w=[tag + "_sm"])
        kb.act(dt, ytmp, AF.Ln, bias=1.0, r=[tag + "_sm"], w=[tag + "_sm"])
        kb.tt("dve", gg, dt, negA[:], ALU.mult, r=[tag + "_sm", tag + "_negA"], w=[tag + "_sm"])
        kb.mm(P3[:, 8:16], C["tri_incl"][:], gg, r=["c_tri_incl", tag + "_sm"], w=kb.pk(3))
        kb.mm(P3[:, 16:24], C["blk"][:], gg, r=["c_blk", tag + "_sm"], w=kb.pk(3))
        kb.mm(P3[:, 24:32], C["selA"][:], gg, r=["c_selA", tag + "_sm"], w=kb.pk(3))
        kb.mm(P3[:, 32:40], C["selB"][:], gg, r=["c_selB", tag + "_sm"], w=kb.pk(3))
        kb.cp("dve", cum, P3[:, 8:16], r=kb.pk(3), w=[tag + "_sm"])
        kb.tt("dve", erem, P3[:, 16:24], cum, ALU.subtract, r=kb.pk(3) + [tag + "_sm"], w=[tag + "_sm"])
        kb.act(dA, P3[:, 24:32], AF.Exp, r=kb.pk(3), w=[tag + "_sm"])
        kb.act(dB, P3[:, 32:40], AF.Exp, r=kb.pk(3), w=[tag + "_sm"])
        kb.act(ecum, cum, AF.Exp, r=[tag + "_sm"], w=[tag + "_sm"])
        kb.act(erem, erem, AF.Exp, r=[tag + "_sm"], w=[tag + "_sm"])
        if STOP <= 1:
            continue
        for c in range(4):
            kb.tr(P4[:, c * 128:(c + 1) * 128], xbc[:, c, :], ident[:], r=[tag + "_xbc", "c_ident"], w=kb.pk(4))
        kb.tr(P5[:, 0:128], xbc[:, 4, :], ident[:], r=[tag + "_xbc", "c_ident"], w=kb.pk(5))
        kb.cp("act", x_tok[:], P4[:, :], r=kb.pk(4), w=[tag + "_xtok"])
        kb.cp("act", B_tok[:], P5[:, 0:128], r=kb.pk(5), w=[tag + "_Btok"])
        v3 = lambda t: t[:, :].rearrange("p (h d) -> p h d", h=8)
        bc8 = lambda a: a.unsqueeze(2).to_broadcast([128, 8, 64])
        kb.tt("dve", v3(xdt), v3(x_tok), bc8(dt), ALU.mult, r=[tag + "_xtok", tag + "_sm"], w=[tag + "_xdt"])
        kb.tt("pool", v3(xd), v3(x_tok), bc8(hp[:, 16:24]), ALU.mult, r=[tag + "_xtok", tag + "_hp"], w=[tag + "_xd"])
        kb.tt("pool", v3(xdte), v3(xdt), bc8(erem), ALU.mult, r=[tag + "_xdt", tag + "_sm"], w=[tag + "_xdte"])
        for g in range(2):
            rows = slice(g * 64, (g + 1) * 64)
            bank = P5 if g == 0 else P6
            kb.mm(bank[:, 128:256], xbc[rows, 4, :], xbc[rows, 5, :], r=[tag + "_xbc"], w=kb.pk(5 + g))
            kb.cp("act", CBT[g][:], bank[:, 128:256], r=kb.pk(5 + g), w=[tag + f"_CBT{g}"])
        if STOP <= 2:
            continue
        def head(h, s_):
            g = h // 4
            Gs, LT = GsL[s_], LTL[s_]
            gk_, lk_ = tag + f"_Gs{s_}", tag + f"_LT{s_}"
            bi = (2, 3, 5, 6)[s_]
            X, kx = kb.P[bi], kb.pk(bi)
            kb.ts("dve", Gs[:], C["tri_incl"][:], gg[:, h:h + 1], None, ALU.mult, r=["c_tri_incl", tag + "_sm"], w=[gk_])
            yield
            kb.mm(X[:, 256:384], C["tri_strict"][:], Gs[:], r=[gk_, "c_tri_strict"], w=kx)
            yield
            kb.act(LT[:], X[:, 256:384], AF.Exp, r=kx, w=[lk_])
            yield
            kb.tt("dve", LT[:], LT[:], C["tri_incl"][:], ALU.mult, r=[lk_, "c_tri_incl"], w=[lk_])
            yield
            kb.tt("dve", LT[:], LT[:], CBT[g][:], ALU.mult, r=[lk_, tag + f"_CBT{g}"], w=[lk_])
            yield
            kb.mm(P7[:, h * 64:(h + 1) * 64], LT[:], xdt[:, h * 64:(h + 1) * 64], r=[lk_, tag + "_xdt"], w=kb.pk(7))
            yield

        for grp in ((0, 1, 2, 3), (4, 5, 6, 7)):
            gens = [head(h, s_) for s_, h in enumerate(grp)]
            while gens:
                for gen in list(gens):
                    try:
                        next(gen)
                    except StopIteration:
                        gens.remove(gen)
        if STOP <= 3:
            continue
        for ch in range(2):
            tr_ = slice(ch * 64, (ch + 1) * 64)
            dch = dA if ch == 0 else dB
            kb.mm(P0[tr_, :], xbc[:, 5, tr_], Sbd[:, :], r=[tag + "_xbc", tag + "_Sbd"], w=kb.pk(0))
            kb.mm(P1[:, :], B_tok[tr_, :], xdte[tr_, :], r=[tag + "_Btok", tag + "_xdte"], w=kb.pk(1))
            for g in range(2):
                rr = slice(g * 64, (g + 1) * 64)
                cc = slice(g * 256, (g + 1) * 256)
                s3 = Sbd[rr, cc].rearrange("p (h d) -> p h d", h=4)
                kb.tt("dve", s3, s3, dch[rr, g * 4:(g + 1) * 4].unsqueeze(2).to_broadcast([64, 4, 64]), ALU.mult,
                      r=[tag + "_Sbd", tag + "_sm"], w=[tag + "_Sbd"])
                kb.tt("dve", Sbd[rr, cc], Sbd[rr, cc], P1[rr, cc], ALU.add, r=[tag + "_Sbd"] + kb.pk(1), w=[tag + "_Sbd"])
        kb.cp("act", yint[:], P0[:, :], r=kb.pk(0), w=[tag + "_yint"])
        kb.tt("pool", v3(yint), v3(yint), bc8(ecum), ALU.mult, r=[tag + "_yint", tag + "_sm"], w=[tag + "_yint"])
        kb.tt("dve", y_sb[:], P7[:, :], yint[:], ALU.add, r=kb.pk(7) + [tag + "_yint"], w=[tag + "_y"])
        kb.tt("pool", y_sb[:], y_sb[:], xd[:], ALU.add, r=[tag + "_y", tag + "_xd"], w=[tag + "_y"])
        kb.tt("pool", y_sb[:], y_sb[:], zs[:], ALU.mult, r=[tag + "_y", tag + "_zs"], w=[tag + "_y"])
        for g in range(2):
            kb.act(junk[:], y_sb[:, g * 256:(g + 1) * 256], AF.Square, accum_out=ssq[:, g:g + 1], r=[tag + "_y"],
                   w=[tag + "_junk", tag + "_ssq"])
        rms_rstd(kb, tag, rs, ssq, 2, 256)
        for g in range(2):
            gc = slice(g * 256, (g + 1) * 256)
            kb.stt("dve", y_sb[:, gc], y_sb[:, gc], rs[:, g:g + 1], gn[:, gc], ALU.mult, ALU.mult,
                   r=[tag + "_y", tag + "_rs", tag + "_gn"], w=[tag + "_y"])
        for c in range(4):
            kb.tr(P4[:, c * 128:(c + 1) * 128], y_sb[:, c * 128:(c + 1) * 128], ident[:], r=[tag + "_y", "c_ident"], w=kb.pk(4))
        kb.cp("act", oT[:], P4[:, :], r=kb.pk(4), w=[tag + "_oT"])
        kb.dma("sp", obr[i], oT[:], sem=tag + "_oT", r=[tag + "_oT"], w=[f"{tag}_obr{i}"])


def phase_merge(kb, l, hT, hkey_fn, obrs, obr_keys, xsrc, xsrc_key, xdst, xdst_key):
    C = kb.C
    tag = f"mrg{l}"
    wm = kb.sb(tag + "_wm", (128, KC, 3072), BF16)
    load_w_cast(kb, wm, tag + "_wm", kb.w_in[l], C_MERGE, 3072)
    wbr = []
    for b, nm in enumerate(("w_branch_gla", "w_branch_gdn", "w_branch_ssd")):
        t = kb.sb(tag + f"_wb{b}", (128, 4, D), BF16)
        load_w_cast(kb, t, tag + f"_wb{b}", kb.dins[nm][l], 0, D, nk=4)
        wbr.append(t)
    wo = kb.sb(tag + "_wo", (128, KC, D), BF16)
    load_w_cast(kb, wo, tag + "_wo", kb.dins["w_out"][l], 0, D)
    bmb = kb.sb(tag + "_bmb", (1, 3072), BF16)
    kb.dma("pool", bmb[:], kb.din(tag + "_bmd", (1, 3072)), sem=tag + "_bmb", w=[tag + "_bmb"])
    gm_row, gm_key = kb.gm_row[l]
    ob = [kb.sb(tag + f"_ob{b}", (128, 512), BF16) for b in range(3)]
    sig = kb.sb(tag + "_sig", (128, 512))
    acc = kb.sb(tag + "_acc", (128, 512))
    tmp = kb.sb(tag + "_tmp", (128, 512))
    mT = kb.sb(tag + "_mT", (128, KC, 128), BF16)
    xt = kb.sb(tag + "_xt", (128, D))
    xo = kb.sb(tag + "_xo", (128, D))
    P = kb.P
    for i in range(NT):
        t0 = i * 128
        hk = hkey_fn(i)
        for b in range(3):
            kb.dma("sp", ob[b][:], obrs[b][i], sem=tag + f"_ob{b}", r=[obr_keys[b](i)], w=[tag + f"_ob{b}"])
        kb.dma("sp", xt[:], xsrc[t0:t0 + 128, :], sem=tag + "_xt", r=[xsrc_key(i)], w=[tag + "_xt"])
        for half in range(2):
            for b in range(3):
                PG, PY = P[(b % 2) * 2], P[(b % 2) * 2 + 1]
                kg, ky = kb.pk((b % 2) * 2), kb.pk((b % 2) * 2 + 1)
                for jj in range(4):
                    j = half * 4 + jj
                    col = b * D + j * 128
                    zone = slice(jj * 128, (jj + 1) * 128)
                    for k in range(KC):
                        kb.mm(PG[:, zone], wm[:, k, col:col + 128], hT[:, k, t0:t0 + 128], start=(k == 0), stop=False,
                              r=[tag + "_wm", hk], w=kg)
                    kb.mm(PG[:, zone], bmb[0:1, col:col + 128], C["ones_bf"][0:1, :], start=False, stop=True,
                          r=[tag + "_bmb", "cb_ones"], w=kg)
                    for c in range(4):
                        kb.mm(PY[:, zone], wbr[b][:, c, j * 128:(j + 1) * 128], ob[b][:, c * 128:(c + 1) * 128],
                              start=(c == 0), stop=(c == 3), r=[tag + f"_wb{b}", tag + f"_ob{b}"], w=ky)
                kb.act(sig[:], PG[:, :], AF.Sigmoid, r=kg, w=[tag + "_sig"])
                if b == 0:
                    kb.tt("dve", acc[:], PY[:, :], sig[:], ALU.mult, r=ky + [tag + "_sig"], w=[tag + "_acc"])
                else:
                    kb.tt("dve", tmp[:], PY[:, :], sig[:], ALU.mult, r=ky + [tag + "_sig"], w=[tag + "_tmp"])
                    kb.tt("pool", acc[:], acc[:], tmp[:], ALU.add, r=[tag + "_acc", tag + "_tmp"], w=[tag + "_acc"])
            kb.cp("act", mT[:, half * 4:(half + 1) * 4, :], acc[:, :].rearrange("p (j t) -> p j t", j=4),
                  r=[tag + "_acc"], w=[tag + "_mT"])
        for half in range(2):
            PO, ko = P[4 + half], kb.pk(4 + half)
            for j in range(KC):
                kb.mm(PO[:, :], mT[:, j, :], wo[:, j, half * 512:(half + 1) * 512], start=(j == 0), stop=(j == KC - 1),
                      r=[tag + "_mT", tag + "_wo"], w=ko)
            hs = slice(half * 512, (half + 1) * 512)
            kb.tt("dve", xo[:, hs], PO[:, :], gm_row[:, hs], ALU.mult, r=ko + [gm_key], w=[tag + "_xo"])
            kb.tt("pool", xo[:, hs], xo[:, hs], xt[:, hs], ALU.add, r=[tag + "_xo", tag + "_xt"], w=[tag + "_xo"])
        kb.dma("sp", xdst[t0:t0 + 128, :], xo[:], sem=tag + "_xo", r=[tag + "_xo"], w=[xdst_key(i)])


def phase_moe(kb, l, xsrc, xsrc_key, xdst, xdst_key, final=None):
    import os
    C = kb.C
    tag = f"moe{l}"
    P = kb.P
    TS = 512
    NSUP = S_TOK // TS
    NE = int(os.environ.get("MOE_NE", 32))
    wr = kb.sb(tag + "_wr", (128, KC, 32), BF16)
    load_w_cast(kb, wr, tag + "_wr", kb.dins["w_router"][l], 0, 32)
    brb = kb.sb(tag + "_brb", (1, 32), BF16)
    kb.dma("pool", brb[:], kb.din(tag + "_brd", (1, 32)), sem=tag + "_brb", w=[tag + "_brb"])
    ones5 = kb.sb(tag + "_ones5", (1, 512), BF16)
    kb.S.op("dve", lambda e: e.memset(ones5[:], 1.0), [], [tag + "_ones5"])
    gf_row, gf_key = kb.gf_row[l]
    nb = norm_bufs(kb, tag + "_n")
    hTs = kb.sb(tag + "_hT", (128, KC, TS), BF16)
    G = kb.sb(tag + "_G", (128, 4, 32))
    lg = kb.sb(tag + "_lg", (128, 32))
    v8 = kb.sb(tag + "_v8", (128, 8))
    msk = kb.sb(tag + "_msk", (128, 32))
    sml = kb.sb(tag + "_sml", (128, 4))
    wgu = [kb.sb(tag + f"_wgu{i}", (128, KC, 2048), BF16) for i in range(2)]
    wd = [kb.sb(tag + f"_wd{i}", (128, KC, D), BF16) for i in range(2)]
    bgu = [kb.sb(tag + f"_bgu{i}", (1, 2048), BF16) for i in range(2)]
    bd = [kb.sb(tag + f"_bd{i}", (1, D), BF16) for i in range(2)]
    yacc = kb.sb(tag + "_yacc", (128, 4, D))
    actT = kb.sb(tag + "_actT", (128, KC, TS), BF16)
    g7 = kb.sb(tag + "_g7", (128, TS))
    sg = kb.sb(tag + "_sg", (128, TS))
    u7 = kb.sb(tag + "_u7", (128, TS))
    xt = kb.sb(tag + "_xt", (128, D))
    xo = kb.sb(tag + "_xo", (128, D))
    if final is not None:
        nfr = kb.sb(tag + "_nfr", (128, D))
        kb.dma("sp", nfr[:], final["nf"].partition_broadcast(128), sem=tag + "_nfr", w=[tag + "_nfr"])
        fj = kb.sb(tag + "_fj", (128, D), BF16)
        fs = kb.sb(tag + "_fs", (128, 2))
    w_gu_d, w_d_d = kb.dins["w_gate_up"][l], kb.dins["w_down"][l]
    b_gu_d, b_d_d = kb.dins["b_gate_up"][l], kb.dins["b_down"][l]

    def load_expert(e, slot):
        srcg = w_gu_d[e].rearrange("(k p) c -> p k c", p=128)
        srcd = w_d_d[e].rearrange("(k p) c -> p k c", p=128)
        for k in range(KC):
            kb.dma("pool", wgu[slot][:, k, :], srcg[:, k, :], sem=tag + f"_wgu{slot}", w=[tag + f"_wgu{slot}"])
        for k in range(KC):
            kb.dma("pool", wd[slot][:, k, :], srcd[:, k, :], sem=tag + f"_wd{slot}", w=[tag + f"_wd{slot}"])
        kb.dma("pool", bgu[slot][:], b_gu_d[e:e + 1, :], sem=tag + f"_bgu{slot}", w=[tag + f"_bgu{slot}"])
        kb.dma("pool", bd[slot][:], b_d_d[e:e + 1, :], sem=tag + f"_bd{slot}", w=[tag + f"_bd{slot}"])

    it = 0
    for T in range(int(os.environ.get("MOE_NSUP", NSUP))):
        for tt in range(4):
            i = T * 4 + tt
            norm_tile(kb, nb, l, "f", xsrc[i * 128:(i + 1) * 128, :], xsrc_key(i), hTs[:, :, tt * 128:(tt + 1) * 128], tag + "_hT")
        for tt in range(4):
            for k in range(KC):
                kb.mm(P[7][:, 0:32], hTs[:, k, tt * 128:(tt + 1) * 128], wr[:, k, :], start=(k == 0), stop=False,
                      r=[tag + "_hT", tag + "_wr"], w=kb.pk(7))
            kb.mm(P[7][:, 0:32], ones5[0:1, 0:128], brb[0:1, :], start=False, stop=True, r=[tag + "_ones5", tag + "_brb"], w=kb.pk(7))
            kb.cp("dve", lg[:], P[7][:, 0:32], r=kb.pk(7), w=[tag + "_lg"])
            kb.S.op("dve", lambda e: e.max(out=v8[:], in_=lg[:]), [tag + "_lg"], [tag + "_v8"])
            kb.ts("dve", msk[:], lg[:], v8[:, 3:4], None, ALU.is_ge, r=[tag + "_lg", tag + "_v8"], w=[tag + "_msk"])
            kb.ts("dve", sml[:, 0:1], v8[:, 0:1], -1.0, None, ALU.mult, r=[tag + "_v8"], w=[tag + "_sml"])
            kb.act(lg[:], lg[:], AF.Exp, bias=sml[:, 0:1], r=[tag + "_lg", tag + "_sml"], w=[tag + "_lg"])
            kb.tt("dve", lg[:], lg[:], msk[:], ALU.mult, r=[tag + "_lg", tag + "_msk"], w=[tag + "_lg"])
            kb.S.op("dve", lambda e: e.reduce_sum(sml[:, 1:2], lg[:], AX.X), [tag + "_lg"], [tag + "_sml"])
            kb.S.op("dve", lambda e: e.reciprocal(sml[:, 1:2], sml[:, 1:2]), [tag + "_sml"], [tag + "_sml"])
            kb.ts("dve", G[:, tt, :], lg[:], sml[:, 1:2], None, ALU.mult, r=[tag + "_lg", tag + "_sml"], w=[tag + "_G"])
        kb.S.op("pool", lambda e: e.memset(yacc[:], 0.0), [], [tag + "_yacc"])
        for e_ in range(NE):
            slot = it % 2
            if it == 0:
                load_expert(e_, slot)
            nxt = (e_ + 1) % NE
            if not (T == NSUP - 1 and e_ == NE - 1):
                load_expert(nxt, 1 - slot)
            it += 1
            wk, dk, bgk, bdk = tag + f"_wgu{slot}", tag + f"_wd{slot}", tag + f"_bgu{slot}", tag + f"_bd{slot}"
            for jc in range(KC):
                pb = (jc % 2) * 2
                PG, PU = P[pb], P[pb + 1]
                for which, PX in ((0, PG), (1, PU)):
                    cols = slice(jc * 256 + which, (jc + 1) * 256, 2)
                    for k in range(KC):
                        kb.mm(PX[:, :], wgu[slot][:, k, cols], hTs[:, k, :], start=(k == 0), stop=False,
                              r=[wk, tag + "_hT"], w=kb.pk(pb + which))
                    kb.mm(PX[:, :], bgu[slot][0:1, cols], ones5[0:1, :], start=False, stop=True,
                          r=[bgk, tag + "_ones5"], w=kb.pk(pb + which))
                kb.ts("dve", g7[:], PG[:, :], SW_LIMIT, None, ALU.min, r=kb.pk(pb), w=[tag + "_g7"])
                kb.ts("dve", u7[:], PU[:, :], -SW_LIMIT, SW_LIMIT, ALU.max, ALU.min, r=kb.pk(pb + 1), w=[tag + "_u7"])
                kb.act(sg[:], g7[:], AF.Sigmoid, scale=SW_ALPHA, r=[tag + "_g7"], w=[tag + "_sg"])
                kb.ts("pool", u7[:], u7[:], 1.0, None, ALU.add, r=[tag + "_u7"], w=[tag + "_u7"])
                kb.tt("pool", u7[:], u7[:], g7[:], ALU.mult, r=[tag + "_u7", tag + "_g7"], w=[tag + "_u7"])
                kb.tt("pool", actT[:, jc, :], u7[:], sg[:], ALU.mult, r=[tag + "_u7", tag + "_sg"], w=[tag + "_actT"])
            for tt in range(4):
                for half in range(2):
                    pi = 4 + (tt % 2) * 2 + half
                    PO = P[pi]
                    hs = slice(half * 512, (half + 1) * 512)
                    for jc in range(KC):
                        kb.mm(PO[:, :], actT[:, jc, tt * 128:(tt + 1) * 128], wd[slot][:, jc, hs], start=(jc == 0), stop=False,
                              r=[tag + "_actT", dk], w=kb.pk(pi))
                    kb.mm(PO[:, :], ones5[0:1, 0:128], bd[slot][0:1, hs], start=False, stop=True,
                          r=[tag + "_ones5", bdk], w=kb.pk(pi))
                    kb.stt("dve", yacc[:, tt, hs], PO[:, :], G[:, tt, e_:e_ + 1], yacc[:, tt, hs], ALU.mult, ALU.add,
                           r=kb.pk(pi) + [tag + "_G", tag + "_yacc"], w=[tag + "_yacc"])
        for tt in range(4):
            i = T * 4 + tt
            kb.dma("sp", xt[:], xsrc[i * 128:(i + 1) * 128, :], sem=tag + "_xt", r=[xsrc_key(i)], w=[tag + "_xt"])
            kb.tt("dve", xo[:], yacc[:, tt, :], gf_row[:], ALU.mult, r=[tag + "_yacc", gf_key], w=[tag + "_xo"])
            kb.tt("pool", xo[:], xo[:], xt[:], ALU.add, r=[tag + "_xo", tag + "_xt"], w=[tag + "_xo"])
            if final is None:
                kb.dma("sp", xdst[i * 128:(i + 1) * 128, :], xo[:], sem=tag + "_xo", r=[tag + "_xo"], w=[xdst_key(i)])
            else:
                kb.act(fj[:], xo[:], AF.Square, accum_out=fs[:, 0:1], r=[tag + "_xo"], w=[tag + "_fj", tag + "_fs"])
                kb.ts("dve", fs[:, 1:2], fs[:, 0:1], 1.0 / D, EPS, ALU.mult, ALU.add, r=[tag + "_fs"], w=[tag + "_fs"])
                kb.act(fs[:, 1:2], fs[:, 1:2], AF.Sqrt, r=[tag + "_fs"], w=[tag + "_fs"])
                kb.S.op("dve", lambda e: e.reciprocal(fs[:, 1:2], fs[:, 1:2]), [tag + "_fs"], [tag + "_fs"])
                kb.stt("dve", xo[:], xo[:], fs[:, 1:2], nfr[:], ALU.mult, ALU.mult, r=[tag + "_xo", tag + "_fs", tag + "_nfr"], w=[tag + "_xo"])
                kb.dma("sp", final["out"][i * 128:(i + 1) * 128, :], xo[:], sem=tag + "_xo", r=[tag + "_xo"], w=[f"out{i}"])


SW_LIMIT = 7.0
SW_ALPHA = 1.702


W_SHAPES = {
    "w_branch_gla": (DEPTH, 512, D), "w_branch_gdn": (DEPTH, 512, D), "w_branch_ssd": (DEPTH, 512, D),
    "w_out": (DEPTH, D, D), "w_router": (DEPTH, D, 32),
    "w_gate_up": (DEPTH, 32, D, 2 * D), "b_gate_up": (DEPTH, 32, 2 * D),
    "w_down": (DEPTH, 32, D, D), "b_down": (DEPTH, 32, D),
}


def build_program(layers=(0, 1), do_mix=True, do_moe=True, dbg=None, same_engine_sync=True):
    kb = KB(same_engine_sync=same_engine_sync)
    phase_consts(kb)
    x = kb.din("x", (S_TOK, D))
    kb.w_in = kb.din("w_in", (DEPTH, D, IN_COLS))
    kb.dins = {nm: kb.din(nm, shp) for nm, shp in W_SHAPES.items()}
    nf = kb.din("norm_final", (D,))
    out = kb.dout("out", (S_TOK, D))
    phase_mod(kb)
    xin, xin_key = x, (lambda i: "x_in")
    last = layers[-1]
    for l in layers:
        xmid = kb.dscr(f"xmid{l}", (S_TOK, D), debug=(dbg == "xmid" and l == layers[0]))
        xmid_key = (lambda i, l=l: f"xmid{l}_{i}")
        if do_mix:
            obr = [kb.dscr(f"obr{l}_{b}", (NT, 128, 512), BF16) for b in range(3)]
            kb.push_scope()
            hT = kb.sb(f"hT{l}", (128, KC, S_TOK), BF16)
            hk = (lambda i, l=l: f"hT{l}_{i}")
            kb.push_scope(); phase_norm(kb, l, "m", xin, xin_key, hT, hk); kb.pop_scope()
            kb.push_scope(); phase_gla(kb, l, hT, hk, obr[0]); kb.pop_scope()
            kb.push_scope(); phase_gdn(kb, l, hT, hk, obr[1]); kb.pop_scope()
            kb.push_scope(); phase_ssd(kb, l, hT, hk, obr[2]); kb.pop_scope()
            keys = [(lambda i, l=l, t=t: f"{t}{l}_obr{i}") for t in ("gla", "gdn", "ssd")]
            kb.push_scope(); phase_merge(kb, l, hT, hk, obr, keys, xin, xin_key, xmid, xmid_key); kb.pop_scope()
            kb.pop_scope()
            msrc, msrc_key = xmid, xmid_key
        else:
            msrc, msrc_key = xin, xin_key
        if dbg == "xmid":
            kb.S.final_wait("sp", [xmid_key(i) for i in range(NT)])
            break
        if do_moe:
            xnext = kb.dscr(f"xres{l}", (S_TOK, D))
            xnext_key = (lambda i, l=l: f"xres{l}_{i}")
            kb.push_scope()
            moe_fn = phase_moe_sorted if MOE_SORTED else phase_moe
            moe_fn(kb, l, msrc, msrc_key, xnext, xnext_key, final=(dict(out=out, nf=nf) if l == last else None))
            kb.pop_scope()
            xin, xin_key = xnext, xnext_key
    kb.S.final_wait("sp", [f"out{i}" for i in range(NT)])
    kb.stats = kb.S.emit(kb.stack)
    return kb


def host_all(inputs, b, names):
    m = {}
    for nm in W_SHAPES:
        m[nm] = inputs[nm]
    m["norm_final"] = inputs["norm_final"]
    for l in range(DEPTH):
        m[f"mrg{l}_bmd"] = inputs["b_merge"][l][None, :]
        m[f"moe{l}_brd"] = inputs["b_router"][l][None, :]
        m[f"moe{l}_nffn"] = inputs["norm_ffn"][l][None, :]
    base = host_inputs(inputs, b, [n for n in names if n not in m])
    for n in names:
        if n in m:
            base[n] = np.ascontiguousarray(m[n])
    return base


_PROG = {}


def kernel(**inputs):
    inputs = {k: np.asarray(v) for k, v in inputs.items()}
    if "kb" not in _PROG:
        _PROG["kb"] = build_program()
    kb = _PROG["kb"]
    names = list(kb.ins.keys())
    in_maps = [host_all(inputs, b, names) for b in range(8)]
    res = run_bass_kernel_spmd(kb.nc, in_maps, core_ids=list(range(8)))
    return np.stack([np.asarray(r["out"]) for r in res.results], axis=0).astype(np.float32)


MOE_SORTED = True
MOE_BLK = 512
MOE_NB = (S_TOK * 4) // MOE_BLK + 32
MOE_ROWS = MOE_NB * MOE_BLK


def phase_moe_sorted(kb, l, xsrc, xsrc_key, xdst, xdst_key, final=None):
    import os
    C = kb.C
    tag = f"moe{l}"
    P = kb.P
    BLK, NB = MOE_BLK, MOE_NB
    NTB = BLK // 128
    IOA = bass.IndirectOffsetOnAxis
    wr = kb.sb(tag + "_wr", (128, KC, 32), BF16)
    load_w_cast(kb, wr, tag + "_wr", kb.dins["w_router"][l], 0, 32)
    brb = kb.sb(tag + "_brb", (1, 32), BF16)
    kb.dma("pool", brb[:], kb.din(tag + "_brd", (1, 32)), sem=tag + "_brb", w=[tag + "_brb"])
    ones5 = kb.sb(tag + "_ones5", (1, 512), BF16)
    kb.S.op("dve", lambda e: e.memset(ones5[:], 1.0), [], [tag + "_ones5"])
    hTb = [kb.sb(tag + f"_hT{i}", (128, KC, BLK), BF16) for i in range(2)]
    lg_all = kb.sb(tag + "_lg", (128, NT, 32))
    msk_all = kb.sb(tag + "_msk", (128, NT, 32))
    R_all = kb.sb(tag + "_R", (128, NT, 32))
    v8_all = kb.sb(tag + "_v8", (128, NT, 8))
    gk_all = kb.sb(tag + "_gk", (128, NT, 4))
    sml = kb.sb(tag + "_sml", (128, 4))
    cnt = kb.sb(tag + "_cnt", (128, 32))
    kb.S.op("pool", lambda e: e.memset(cnt[:], 0.0), [], [tag + "_cnt"])
    padded = kb.sb(tag + "_padded", (128, 32))
    pstart = kb.sb(tag + "_pstart", (128, 32))
    pend = kb.sb(tag + "_pend", (128, 32))
    pcol = kb.sb(tag + "_pcol", (32, 1))
    pcb = kb.sb(tag + "_pcb", (32, 128))
    dg = kb.sb(tag + "_dg", (32, 32))
    posf = kb.sb(tag + "_posf", (128, NT, 4))
    posi = kb.sb(tag + "_posi", (128, NT, 4), I32)
    eb = kb.sb(tag + "_eb", (128, NB))
    offi = kb.sb(tag + "_offi", (128, NB, KC), I32)
    oh = kb.sb(tag + "_oh", (32, NB), BF16)
    kb.push_scope()
    gf_row, gf_key = kb.gf_row[l]
    scf = kb.sb(tag + "_scf", (128, D))
    shf = kb.sb(tag + "_shf", (128, D))
    nfrow = kb.sb(tag + "_nfrow", (128, D))
    kb.dma("sp", shf[:], kb.modrow_d[l][0], sem=tag + "_shf", r=[f"modrowd{l}"], w=[tag + "_shf"])
    kb.dma("sp", scf[:], kb.modrow_d[l][1], sem=tag + "_scf", r=[f"modrowd{l}"], w=[tag + "_scf"])
    kb.dma("sp", nfrow[:], kb.din(tag + "_nffn", (1, D))[0].partition_broadcast(128), sem=tag + "_nfrow", w=[tag + "_nfrow"])
    kb.stt("dve", scf[:], scf[:], 1.0, nfrow[:], ALU.add, ALU.mult, r=[tag + "_scf", tag + "_nfrow"], w=[tag + "_scf"])
    xs_d = kb.dscr(f"moe_xs{l}", (MOE_ROWS, D))
    ys_d = kb.dscr(f"moe_ys{l}", (MOE_ROWS, D))
    h2_d = kb.dscr(f"moe_h2{l}", (S_TOK, D))
    zt = kb.sb(tag + "_zt", (128, D))
    kb.S.op("pool", lambda e: e.memset(zt[:], 0.0), [], [tag + "_zt"])
    xs_v = xs_d.rearrange("(n p) c -> n p c", p=128)
    for n in range(MOE_ROWS // 128):
        kb.dma("sp", xs_v[n], zt[:], sem=tag + "_zt", r=[tag + "_zt"], w=[tag + "_xs"])
    nb = norm_bufs(kb, tag + "_n")
    h2 = kb.sb(tag + "_h2", (128, D))
    eq = kb.sb(tag + "_eq", (128, NT, 32))
    cmp3 = kb.sb(tag + "_cmp3", (128, NB, 32))
    offf = kb.sb(tag + "_offf", (128, NB, KC))
    def p1_norm(i):
            hv = hTb[0][:, :, (i % 2) * 128:(i % 2 + 1) * 128]
            norm_tile(kb, nb, l, "f", xsrc[i * 128:(i + 1) * 128, :], xsrc_key(i), hv, tag + f"_hTp{i % 2}")
            b_ = (nb["n"] - 1) % 2
            xn, xnk = nb["xn"][b_], f"{tag}_n_xn{b_}"
            kb.tt("pool", h2[:], xn[:], scf[:], ALU.mult, r=[xnk, tag + "_scf"], w=[tag + "_h2"])
            kb.tt("pool", h2[:], h2[:], shf[:], ALU.add, r=[tag + "_h2", tag + "_shf"], w=[tag + "_h2"])
            kb.dma("sp", h2_d[i * 128:(i + 1) * 128, :], h2[:], sem=tag + "_h2", r=[tag + "_h2"], w=[f"{tag}_h2d{i}"])

    def p1_route(i):
            for k in range(KC):
                kb.mm(P[7][:, 0:32], hTb[0][:, k, (i % 2) * 128:(i % 2 + 1) * 128], wr[:, k, :], start=(k == 0), stop=False, r=[tag + f"_hTp{i % 2}", tag + "_wr"], w=kb.pk(7))
            kb.mm(P[7][:, 0:32], ones5[0:1, 0:128], brb[0:1, :], start=False, stop=True, r=[tag + "_ones5", tag + "_brb"], w=kb.pk(7))
            lg, v8, msk = lg_all[:, i, :], v8_all[:, i, :], msk_all[:, i, :]
            kb.cp("dve", lg, P[7][:, 0:32], r=kb.pk(7), w=[tag + "_lg"])
            kb.S.op("dve", lambda e, v8=v8, lg=lg: e.max(out=v8, in_=lg), [tag + "_lg"], [tag + "_v8"])
            kb.ts("dve", msk, lg, v8[:, 3:4], None, ALU.is_ge, r=[tag + "_lg", tag + "_v8"], w=[tag + "_msk"])
            kb.ts("dve", sml[:, 0:1], v8[:, 0:1], -1.0, None, ALU.mult, r=[tag + "_v8"], w=[tag + "_sml"])
            kb.act(gk_all[:, i, :], v8[:, 0:4], AF.Exp, bias=sml[:, 0:1], r=[tag + "_v8", tag + "_sml"], w=[tag + "_gk"])
            kb.S.op("dve", lambda e, i=i: e.reduce_sum(sml[:, 1:2], gk_all[:, i, :], AX.X), [tag + "_gk"], [tag + "_sml"])
            kb.S.op("dve", lambda e: e.reciprocal(sml[:, 1:2], sml[:, 1:2]), [tag + "_sml"], [tag + "_sml"])
            kb.ts("dve", gk_all[:, i, :], gk_all[:, i, :], sml[:, 1:2], None, ALU.mult, r=[tag + "_gk", tag + "_sml"], w=[tag + "_gk"])
            kb.mm(P[6][:, 0:32], C["tri_full"][:], msk, r=["c_tri_full", tag + "_msk"], w=kb.pk(6))
            kb.mm(P[6][:, 32:64], C["ones"][:], msk, r=["c_ones", tag + "_msk"], w=kb.pk(6))
            kb.tt("dve", R_all[:, i, :], P[6][:, 0:32], cnt[:], ALU.add, r=kb.pk(6) + [tag + "_cnt"], w=[tag + "_R"])
            kb.tt("dve", cnt[:], P[6][:, 32:64], cnt[:], ALU.add, r=kb.pk(6) + [tag + "_cnt"], w=[tag + "_cnt"])

    for i_ in range(NT + 1):
        if i_ < NT:
            p1_norm(i_)
        if i_ >= 1:
            p1_route(i_ - 1)
    kb.tt("dve", eq[:, 0:8, :].rearrange("p j e -> p e j"), cnt[:].unsqueeze(2).to_broadcast([128, 32, 8]),
          C["blk_thr"][:, 0:8].unsqueeze(1).to_broadcast([128, 32, 8]), ALU.is_gt, r=[tag + "_cnt", "c_blk_thr"], w=[tag + "_eq"])
    kb.S.op("dve", lambda e: e.reduce_sum(padded[:], eq[:, 0:8, :].rearrange("p j e -> p e j"), AX.X), [tag + "_eq"], [tag + "_padded"])
    kb.ts("dve", padded[:], padded[:], float(BLK), None, ALU.mult, r=[tag + "_padded"], w=[tag + "_padded"])
    kb.tt("dve", dg[:], padded[0:32, :], C["ident"][0:32, 0:32], ALU.mult, r=[tag + "_padded", "c_ident"], w=[tag + "_dg"])
    kb.S.op("dve", lambda e: e.reduce_sum(pcol[:], dg[:], AX.X), [tag + "_dg"], [tag + "_pcol"])
    kb.cp("dve", pcb[:], pcol[:, 0:1].to_broadcast([32, 128]), r=[tag + "_pcol"], w=[tag + "_pcb"])
    kb.mm(P[6][:, 0:32], pcb[:], C["tri_full"][0:32, 0:32], r=[tag + "_pcb", "c_tri_full"], w=kb.pk(6))
    kb.cp("dve", pstart[:], P[6][:, 0:32], r=kb.pk(6), w=[tag + "_pstart"])
    kb.tt("dve", pend[:], pstart[:], padded[:], ALU.add, r=[tag + "_pstart", tag + "_padded"], w=[tag + "_pend"])
    kb.tt("dve", R_all[:], R_all[:], pstart[:].unsqueeze(1).to_broadcast([128, NT, 32]), ALU.add,
          r=[tag + "_R", tag + "_pstart"], w=[tag + "_R"])
    for k in range(4):
        kb.tt("dve", eq[:], lg_all[:], v8_all[:, :, k:k + 1].to_broadcast([128, NT, 32]), ALU.is_equal,
              r=[tag + "_lg", tag + "_v8"], w=[tag + "_eq"])
        kb.tt("dve", eq[:], eq[:], R_all[:], ALU.mult, r=[tag + "_eq", tag + "_R"], w=[tag + "_eq"])
        kb.S.op("dve", lambda e, k=k: e.reduce_sum(posf[:, :, k], eq[:], AX.X), [tag + "_eq"], [tag + "_posf"])
    kb.cp("dve", posi[:], posf[:], r=[tag + "_posf"], w=[tag + "_posi"])
    kb.tt("dve", cmp3[:], pend[:].unsqueeze(1).to_broadcast([128, NB, 32]),
          C["blk_thr"][:, 0:NB].unsqueeze(2).to_broadcast([128, NB, 32]), ALU.is_le, r=[tag + "_pend", "c_blk_thr"], w=[tag + "_cmp3"])
    kb.S.op("dve", lambda e: e.reduce_sum(eb[:], cmp3[:], AX.X), [tag + "_cmp3"], [tag + "_eb"])
    kb.ts("dve", eb[:], eb[:], 31.0, None, ALU.min, r=[tag + "_eb"], w=[tag + "_eb"])
    kb.ts("dve", offf[:], eb[:].unsqueeze(2).to_broadcast([128, NB, KC]), float(D), float(l * 32 * D), ALU.mult, ALU.add,
          r=[tag + "_eb"], w=[tag + "_offf"])
    kb.tt("dve", offf[:], offf[:], C["base_pk"][:, 0:KC].unsqueeze(1).to_broadcast([128, NB, KC]), ALU.add,
          r=[tag + "_offf", "c_base_pk"], w=[tag + "_offf"])
    kb.cp("dve", offi[:], offf[:], r=[tag + "_offf"], w=[tag + "_offi"])
    kb.ts("dve", oh[:], eb[0:32, :], C["base_pk"][0:32, 0:1], None, ALU.is_equal, r=[tag + "_eb", "c_base_pk"], w=[tag + "_oh"])
    for i in range(NT):
        kb.dma("sp", h2[:], h2_d[i * 128:(i + 1) * 128, :], sem=tag + "_h2", r=[f"{tag}_h2d{i}"], w=[tag + "_h2"])
        for k in range(4):
            kb.S.dma("pool", lambda e, i=i, k=k: e.indirect_dma_start(
                out=xs_d, out_offset=IOA(ap=posi[:, i, k:k + 1], axis=0), in_=h2[:], in_offset=None),
                tag + "_h2", [tag + "_h2", tag + "_posi"], [tag + "_xs"])
    kb.pop_scope()
    kb.push_scope()
    bgu_sb = kb.sb(tag + "_bgu", (32, 2048), BF16)
    bd_sb = kb.sb(tag + "_bd", (32, D), BF16)
    kb.dma("pool", bgu_sb[:], kb.dins["b_gate_up"][l], sem=tag + "_bgu", w=[tag + "_bgu"])
    kb.dma("pool", bd_sb[:], kb.dins["b_down"][l], sem=tag + "_bd", w=[tag + "_bd"])
    wgu = [kb.sb(tag + f"_wgu{i}", (128, KC, 2048), BF16) for i in range(2)]
    wd = [kb.sb(tag + f"_wd{i}", (128, KC, D), BF16) for i in range(2)]
    ohbs = [kb.sb(tag + f"_ohb{i}", (32, BLK), BF16) for i in range(2)]
    xr = [kb.sb(tag + f"_xr{i}", (128, D)) for i in range(2)]
    actT = kb.sb(tag + "_actT", (128, KC, BLK), BF16)
    g7 = kb.sb(tag + "_g7", (128, BLK))
    sg = kb.sb(tag + "_sg", (128, BLK))
    u7 = kb.sb(tag + "_u7", (128, BLK))
    yb = [kb.sb(tag + f"_yb{i}", (128, D)) for i in range(2)]
    wgu_flat = kb.dins["w_gate_up"].rearrange("l e r c -> (l e r) c")
    wd_flat = kb.dins["w_down"].rearrange("l e r c -> (l e r) c")

    def load_block_w(b, slot):
        for k in range(KC):
            kb.S.dma("pool", lambda e, b=b, k=k, slot=slot: e.indirect_dma_start(
                out=wgu[slot][:, k, :], out_offset=None, in_=wgu_flat, in_offset=IOA(ap=offi[:, b, k:k + 1], axis=0)),
                tag + f"_wgu{slot}", [tag + "_offi"], [tag + f"_wgu{slot}"])
        for k in range(KC):
            kb.S.dma("pool", lambda e, b=b, k=k, slot=slot: e.indirect_dma_start(
                out=wd[slot][:, k, :], out_offset=None, in_=wd_flat, in_offset=IOA(ap=offi[:, b, k:k + 1], axis=0)),
                tag + f"_wd{slot}", [tag + "_offi"], [tag + f"_wd{slot}"])

    def prep_block(b):
        hTs, hkey, ohb_ = hTb[b % 2], tag + f"_hT{b % 2}", ohbs[b % 2]
        for tt in range(NTB):
            xb = xr[nxc[0] % 2]
            xbk = tag + f"_xr{nxc[0] % 2}"
            nxc[0] += 1
            r0 = b * BLK + tt * 128
            kb.dma("sp", xb[:], xs_d[r0:r0 + 128, :], sem=xbk, r=[tag + "_xs"], w=[xbk])
            for half in range(2):
                pT, pk = P[half], kb.pk(half)
                for kk in range(4):
                    k = half * 4 + kk
                    kb.tr(pT[:, kk * 128:(kk + 1) * 128], xb[:, k * 128:(k + 1) * 128], C["ident"][:], r=[xbk, "c_ident"], w=pk)
                kb.cp("act" if half == 0 else "dve", hTs[:, half * 4:(half + 1) * 4, tt * 128:(tt + 1) * 128],
                      pT[:, :].rearrange("p (k t) -> p k t", k=4), r=pk, w=[hkey])
        kb.cp("dve", ohb_[:], oh[:, b:b + 1].to_broadcast([32, BLK]), r=[tag + "_oh"], w=[tag + f"_ohb{b % 2}"])

    NBR = int(os.environ.get("MOE_NBLK", NB))
    load_block_w(0, 0)
    nxc = [0]
    prep_block(0)
    for b in range(NBR):
        slot = b % 2
        if b + 1 < NBR:
            load_block_w(b + 1, 1 - slot)
        wk, dk = tag + f"_wgu{slot}", tag + f"_wd{slot}"
        hTs, hkey, ohb = hTb[slot], tag + f"_hT{slot}", ohbs[slot]
        ohk = tag + f"_ohb{slot}"
        for jc in range(KC):
            pb = 2 + (jc % 2) * 2
            PG, PU = P[pb], P[pb + 1]
            for which, PX in ((0, PG), (1, PU)):
                cols = slice(jc * 256 + which, (jc + 1) * 256, 2)
                for k in range(KC):
                    kb.mm(PX[:, :], wgu[slot][:, k, cols], hTs[:, k, :], start=(k == 0), stop=False,
                          r=[wk, hkey], w=kb.pk(pb + which))
                kb.mm(PX[:, :], bgu_sb[:, cols], ohb[:, :], start=False, stop=True, r=[tag + "_bgu", ohk], w=kb.pk(pb + which))
            kb.ts("dve", g7[:], PG[:, :], SW_LIMIT, None, ALU.min, r=kb.pk(pb), w=[tag + "_g7"])
            kb.ts("dve", u7[:], PU[:, :], -SW_LIMIT, SW_LIMIT, ALU.max, ALU.min, r=kb.pk(pb + 1), w=[tag + "_u7"])
            kb.act(sg[:], g7[:], AF.Sigmoid, scale=SW_ALPHA, r=[tag + "_g7"], w=[tag + "_sg"])
            kb.stt("dve", u7[:], u7[:], 1.0, g7[:], ALU.add, ALU.mult, r=[tag + "_u7", tag + "_g7"], w=[tag + "_u7"])
            kb.tt("dve", actT[:, jc, :], u7[:], sg[:], ALU.mult, r=[tag + "_u7", tag + "_sg"], w=[tag + "_actT"])
        if b + 1 < NBR:
            prep_block(b + 1)
        for tt in range(NTB):
            ybt, ybk = yb[tt % 2], tag + f"_yb{tt % 2}"
            for half in range(2):
                pi = 6 + half
                PO = P[pi]
                hs = slice(half * 512, (half + 1) * 512)
                for jc in range(KC):
                    kb.mm(PO[:, :], actT[:, jc, tt * 128:(tt + 1) * 128], wd[slot][:, jc, hs], start=(jc == 0), stop=False,
                          r=[tag + "_actT", dk], w=kb.pk(pi))
                kb.mm(PO[:, :], ohb[:, 0:128], bd_sb[:, hs], start=False, stop=True, r=[ohk, tag + "_bd"], w=kb.pk(pi))
                kb.cp("act" if half == 0 else "dve", ybt[:, hs], PO[:, :], r=kb.pk(pi), w=[ybk])
            r0 = b * BLK + tt * 128
            kb.dma("sp", ys_d[r0:r0 + 128, :], ybt[:], sem=ybk, r=[ybk], w=[tag + "_ys"])
    kb.pop_scope()
    kb.push_scope()
    yk = [kb.sb(tag + f"_yk{i}", (128, D)) for i in range(2)]
    acc = kb.sb(tag + "_acc", (128, D))
    xt = kb.sb(tag + "_xt", (128, D))
    if final is not None:
        nfr = kb.sb(tag + "_nfr", (128, D))
        kb.dma("sp", nfr[:], final["nf"].partition_broadcast(128), sem=tag + "_nfr", w=[tag + "_nfr"])
        fj = kb.sb(tag + "_fj", (128, D), BF16)
        fs = kb.sb(tag + "_fs", (128, 2))
    ng = 0
    for i in range(NT):
        kb.dma("sp", xt[:], xsrc[i * 128:(i + 1) * 128, :], sem=tag + "_xt", r=[xsrc_key(i)], w=[tag + "_xt"])
        for k in range(4):
            yt, ytk = yk[ng % 2], tag + f"_yk{ng % 2}"
            ng += 1
            kb.S.dma("pool", lambda e, i=i, k=k, yt=yt: e.indirect_dma_start(
                out=yt[:], out_offset=None, in_=ys_d, in_offset=IOA(ap=posi[:, i, k:k + 1], axis=0)),
                ytk, [tag + "_ys", tag + "_posi"], [ytk])
            if k == 0:
                kb.ts("dve", acc[:], yt[:], gk_all[:, i, k:k + 1], None, ALU.mult, r=[ytk, tag + "_gk"], w=[tag + "_acc"])
            else:
                kb.stt("dve", acc[:], yt[:], gk_all[:, i, k:k + 1], acc[:], ALU.mult, ALU.add, r=[ytk, tag + "_gk", tag + "_acc"], w=[tag + "_acc"])
        kb.tt("pool", acc[:], acc[:], gf_row[:], ALU.mult, r=[tag + "_acc", gf_key], w=[tag + "_acc"])
        kb.tt("pool", acc[:], acc[:], xt[:], ALU.add, r=[tag + "_acc", tag + "_xt"], w=[tag + "_acc"])
        if final is None:
            kb.dma("sp", xdst[i * 128:(i + 1) * 128, :], acc[:], sem=tag + "_acc", r=[tag + "_acc"], w=[xdst_key(i)])
        else:
            kb.act(fj[:], acc[:], AF.Square, accum_out=fs[:, 0:1], r=[tag + "_acc"], w=[tag + "_fj", tag + "_fs"])
            kb.ts("dve", fs[:, 1:2], fs[:, 0:1], 1.0 / D, EPS, ALU.mult, ALU.add, r=[tag + "_fs"], w=[tag + "_fs"])
            kb.act(fs[:, 1:2], fs[:, 1:2], AF.Sqrt, r=[tag + "_fs"], w=[tag + "_fs"])
            kb.S.op("dve", lambda e: e.reciprocal(fs[:, 1:2], fs[:, 1:2]), [tag + "_fs"], [tag + "_fs"])
            kb.stt("dve", acc[:], acc[:], fs[:, 1:2], nfr[:], ALU.mult, ALU.mult, r=[tag + "_acc", tag + "_fs", tag + "_nfr"], w=[tag + "_acc"])
            kb.dma("sp", final["out"][i * 128:(i + 1) * 128, :], acc[:], sem=tag + "_acc", r=[tag + "_acc"], w=[f"out{i}"])
    kb.pop_scope()
```

```python
import numpy as np
from contextlib import ExitStack
from concourse.bass_utils import run_bass_kernel_spmd

import concourse.bass as bass
import concourse.mybir as mybir

ENGINES = ("pe", "act", "dve", "pool", "sp")


class Op:
    __slots__ = ("eng", "fn", "deps", "is_dma", "dsem", "dcount", "signal", "idx", "signo")

    def __init__(self, eng, fn):
        self.eng = eng
        self.fn = fn
        self.deps = []
        self.is_dma = False
        self.dsem = None
        self.dcount = 0
        self.signal = False
        self.idx = -1
        self.signo = 0


class Sched:
    def __init__(self, nc, same_engine_sync=True):
        self.nc = nc
        self.q = {e: [] for e in ENGINES}
        self.res_w = {}
        self.res_r = {}
        self.phys = []
        self.key2phys = {}
        self.free_phys = []
        self.same_engine_sync = same_engine_sync

    def _collect(self, op, reads, writes, my_dma_key=None):
        deps = []
        for k in reads:
            t = self.res_w.get(k)
            if t is not None:
                deps.append(t)
        for k in writes:
            t = self.res_w.get(k)
            if t is not None:
                if not (my_dma_key is not None and t[0] == 'dma' and t[1] == my_dma_key):
                    deps.append(t)
            deps.extend(self.res_r.get(k, ()))
        op.deps = deps

    def _commit(self, tok, reads, writes):
        for k in reads:
            self.res_r.setdefault(k, []).append(tok)
        for k in writes:
            self.res_w[k] = tok
            self.res_r[k] = []

    @staticmethod
    def _excl(reads, writes):
        rp = [k for k in reads if len(k) == 2 and k[0] == "P" and k[1].isdigit()]
        if not rp:
            return reads, writes
        return [k for k in reads if k not in rp], list(writes) + [k for k in rp if k not in writes]

    def op(self, eng, fn, reads=(), writes=()):
        reads, writes = self._excl(reads, writes)
        o = Op(eng, fn)
        self._collect(o, reads, writes)
        o.idx = len(self.q[eng])
        self.q[eng].append(o)
        self._commit(('op', o), reads, writes)
        return o

    def dma(self, eng, fn, sem_key, reads=(), writes=()):
        reads, writes = self._excl(reads, writes)
        if sem_key not in self.key2phys:
            if self.free_phys:
                p = self.free_phys.pop()
            else:
                p = len(self.phys)
                self.phys.append(0)
            self.key2phys[sem_key] = p
        p = self.key2phys[sem_key]
        o = Op(eng, fn)
        o.is_dma = True
        self._collect(o, reads, writes, my_dma_key=p)
        self.phys[p] += 1
        c = self.phys[p]
        o.dsem = p
        o.dcount = c
        o.idx = len(self.q[eng])
        self.q[eng].append(o)
        self._commit(('dma', p, c), reads, writes)
        return o

    def final_wait(self, eng, keys):
        o = Op(eng, None)
        deps = []
        for k in keys:
            t = self.res_w.get(k)
            if t is not None:
                deps.append(t)
            deps.extend(self.res_r.get(k, ()))
        o.deps = deps
        o.idx = len(self.q[eng])
        self.q[eng].append(o)

    def barrier(self):
        toks = []
        for e in ENGINES:
            for o in reversed(self.q[e]):
                if o.fn is not None and not o.is_dma:
                    toks.append(('op', o))
                    break
        for p, c in enumerate(self.phys):
            if c:
                toks.append(('dma', p, c))
        for e in ENGINES:
            o = Op(e, None)
            o.deps = list(toks)
            o.idx = len(self.q[e])
            self.q[e].append(o)
        self.free_phys = list(range(len(self.phys)))[::-1]
        self.key2phys = {}

    def emit(self, stack):
        nc = self.nc
        for e in ENGINES:
            for o in self.q[e]:
                for t in o.deps:
                    if t[0] == 'op':
                        tgt = t[1]
                        if tgt.eng == o.eng and (not self.same_engine_sync or o.eng == 'pe'):
                            continue
                        tgt.signal = True
        for e in ENGINES:
            n = 0
            for o in self.q[e]:
                if o.signal:
                    n += 1
                    o.signo = n
        esem = {e: stack.enter_context(nc.semaphore("s_" + e)) for e in ENGINES}
        dsem = {}
        for p in range(len(self.phys)):
            dsem[p] = stack.enter_context(nc.semaphore(f"d_{p}"))
        block = stack.enter_context(nc.Block())
        stats = {}

        def run(e, engobj):
            waited = {}
            nwait = 0
            for o in self.q[e]:
                need = {}
                for t in o.deps:
                    if t[0] == 'op':
                        tgt = t[1]
                        if tgt.eng == e and (not self.same_engine_sync or e == 'pe'):
                            continue
                        key = ('e', tgt.eng)
                        val = tgt.signo
                    else:
                        key = ('d', t[1])
                        val = t[2] * 16
                    if need.get(key, 0) < val:
                        need[key] = val
                for key, val in need.items():
                    if waited.get(key, 0) >= val:
                        continue
                    waited[key] = val
                    sem = esem[key[1]] if key[0] == 'e' else dsem[key[1]]
                    engobj.wait_ge(sem, val)
                    nwait += 1
                if o.fn is None:
                    continue
                ins = o.fn(engobj)
                if o.is_dma:
                    ins.then_inc(dsem[o.dsem], 16)
                elif o.signal:
                    ins.then_inc(esem[e], 1)
            stats[e] = (len(self.q[e]), nwait)

        @block.tensor
        def _(eng):
            run("pe", eng)

        @block.scalar
        def _(eng):
            run("act", eng)

        @block.vector
        def _(eng):
            run("dve", eng)

        @block.gpsimd
        def _(eng):
            run("pool", eng)

        @block.sync
        def _(eng):
            run("sp", eng)

        return stats


F32 = mybir.dt.float32
BF16 = mybir.dt.bfloat16
I32 = mybir.dt.int32
AF = mybir.ActivationFunctionType
ALU = mybir.AluOpType
AX = mybir.AxisListType

S_TOK = 4096
D = 1024
KC = 8
NT = S_TOK // 128
DEPTH = 2
EPS = 1e-6
IN_COLS = 7968
C_GLA_Q, C_GLA_K, C_GLA_V, C_GLA_LR, C_GLA_R = 0, 256, 512, 1024, 1040
C_GDN_QKV, C_GDN_A, C_GDN_B, C_GDN_G = 1552, 3088, 3092, 3096
C_SSD_Z, C_SSD_XBC, C_SSD_DT, C_MERGE = 3608, 4120, 4888, 4896


class KB:
    def __init__(self, same_engine_sync=True):
        self.nc = bass.Bass("TRN2", target_bir_lowering=False)
        self.S = Sched(self.nc, same_engine_sync=same_engine_sync)
        self.stack = ExitStack()
        self.ins = {}
        self.outs = {}
        self._n = 0
        self.scopes = []
        self._allow_p = False
        self.P = [self.stack.enter_context(self.nc.psum_tensor(f"PB{i}", [128, 512], F32)) for i in range(8)]

    @staticmethod
    def pk(i, a=0, b=512):
        return [f"P{i}"]

    def din(self, name, shape, dt=F32):
        t = self.nc.dram_tensor(name, list(shape), dt, kind="ExternalInput")
        self.ins[name] = t
        return t.ap()

    def dout(self, name, shape, dt=F32):
        t = self.nc.dram_tensor(name, list(shape), dt, kind="ExternalOutput")
        self.outs[name] = t
        return t.ap()

    def dscr(self, name, shape, dt=F32, debug=False):
        if debug:
            return self.dout(name, shape, dt)
        return self.nc.dram_tensor(name, list(shape), dt, kind="Internal").ap()

    def sb(self, name, shape, dt=F32):
        st = self.scopes[-1] if self.scopes else self.stack
        return st.enter_context(self.nc.sbuf_tensor(name, list(shape), dt))

    def sbp(self, name, shape, dt=F32):
        assert not self.scopes or self._allow_p
        return self.stack.enter_context(self.nc.sbuf_tensor(name, list(shape), dt))

    def push_scope(self):
        self.scopes.append(ExitStack())

    def pop_scope(self):
        self.S.barrier()
        self.scopes.pop().close()

    def ps(self, name, shape=(128, 512), dt=F32):
        return self.stack.enter_context(self.nc.psum_tensor(name, list(shape), dt))

    def mm(self, out, lhsT, rhs, start=True, stop=True, r=(), w=()):
        return self.S.op("pe", lambda e: e.matmul(out, lhsT, rhs, start=start, stop=stop), r, w)

    def tr(self, out, in_, ident, r=(), w=()):
        return self.S.op("pe", lambda e: e.transpose(out, in_, ident), r, w)

    def act(self, out, in_, func, bias=None, scale=None, accum_out=None, r=(), w=(), eng="act"):
        kw = {}
        if bias is not None:
            kw["bias"] = bias
        if scale is not None:
            kw["scale"] = scale
        if accum_out is not None:
            kw["accum_out"] = accum_out
        return self.S.op(eng, lambda e: e.activation(out, in_, func, **kw), r, w)

    def ts(self, eng, out, in0, s1, s2, op0, op1=None, accum_out=None, r=(), w=()):
        kw = {}
        if op1 is not None:
            kw["op1"] = op1
        if accum_out is not None:
            kw["accum_out"] = accum_out
        return self.S.op(eng, lambda e: e.tensor_scalar(out, in0, s1, s2, op0, **kw), r, w)

    def tt(self, eng, out, in0, in1, op, r=(), w=()):
        return self.S.op(eng, lambda e: e.tensor_tensor(out, in0, in1, op), r, w)

    def stt(self, eng, out, in0, scalar, in1, op0, op1, r=(), w=()):
        return self.S.op(eng, lambda e: e.scalar_tensor_tensor(out, in0, scalar, in1, op0, op1), r, w)

    def cp(self, eng, out, in_, r=(), w=()):
        if eng == "act":
            return self.S.op(eng, lambda e: e.copy(out, in_), r, w)
        return self.S.op(eng, lambda e: e.tensor_copy(out, in_), r, w)

    def dma(self, eng, out, in_, sem, r=(), w=(), **kw):
        return self.S.dma(eng, lambda e: e.dma_start(out, in_, **kw), sem, r, w)


def phase_consts(kb):
    c = {}
    cdefs = {
        "ident": (128, 128), "tri_incl": (128, 128), "tri_strict": (128, 128), "ones": (128, 128),
        "blk": (128, 128), "selA": (128, 128), "selB": (128, 128), "neg_strict": (128, 128),
        "tri_full": (128, 128), "blk_thr": (128, 64), "base_pk": (128, 8),
    }
    for name, shp in cdefs.items():
        src = kb.din("c_" + name, shp)
        t = kb.sb("cs_" + name, shp)
        kb.dma("sp", t[:], src, sem="c_" + name, w=["c_" + name])
        c[name] = t
        tb = kb.sb("cb_" + name, shp, BF16)
        kb.cp("dve", tb[:], t[:], r=["c_" + name], w=["cb_" + name])
        c[name + "_bf"] = tb
    kb.C = c


def phase_mod(kb):
    nc = kb.nc
    cT = kb.din("cT", (128, KC))
    w_mod = kb.din("w_mod", (DEPTH, D, 6 * D))
    bmodc = kb.din("bmodc", (DEPTH, 128, 48))
    bmodrow = kb.din("bmodrow", (DEPTH, 6, D))
    nmixc = kb.din("nmixc", (DEPTH, 128, KC))
    nffnc = kb.din("nffnc", (DEPTH, 128, KC))
    pers = {}
    for l in range(DEPTH):
        pers[f"modc{l}"] = kb.sbp(f"modc{l}", (128, 48))
        pers[f"modscl{l}"] = kb.sbp(f"modscl{l}", (128, 2, KC))
        for piece in (2, 5):
            pers[f"modrow{l}_{piece}"] = kb.sbp(f"modrow{l}_{piece}", (128, D))
    kb.modrow_d = [[kb.dscr(f"modrowd{l}_{j}", (128, D)) for j in range(2)] for l in range(DEPTH)]
    kb.push_scope()
    rowtmp = kb.sb("modrowtmp", (128, D))
    cact = kb.sb("cact", (128, KC))
    crep = kb.sb("crep", (128, KC, 128))
    kb.dma("sp", cact[:], cT, sem="cact", w=["cact"])
    kb.act(cact[:], cact[:], AF.Silu, r=["cact"], w=["cact"])
    for k in range(KC):
        kb.cp("dve", crep[:, k, :], cact[:, k:k + 1].to_broadcast([128, 128]), r=["cact"], w=["crep"])
    wbuf = [kb.sb(f"modw{i}", (128, KC, 1024)) for i in range(2)]
    pcol = kb.P[0]
    prow = [kb.P[1], kb.P[2]]
    kb.modc, kb.gm_row, kb.gf_row = [], [], []
    kb.sclm, kb.shm, kb.sclf, kb.shf = [], [], [], []
    it = 0
    for l in range(DEPTH):
        modc = pers[f"modc{l}"]
        bc = kb.sb(f"bmodc{l}", (128, 48))
        nm = kb.sb(f"nmixc{l}", (128, KC))
        nf = kb.sb(f"nffnc{l}", (128, KC))
        kb.dma("sp", bc[:], bmodc[l], sem=f"bmodc{l}", w=[f"bmodc{l}"])
        kb.dma("sp", nm[:], nmixc[l], sem=f"nmixc{l}", w=[f"nmixc{l}"])
        kb.dma("sp", nf[:], nffnc[l], sem=f"nffnc{l}", w=[f"nffnc{l}"])
        rows = []
        for piece in range(6):
            wb = wbuf[it % 2]
            wk = f"modw{it % 2}"
            it += 1
            src = w_mod[l, :, piece * 1024:(piece + 1) * 1024].rearrange("(k p) c -> p k c", p=128)
            for hh in range(2):
                kb.dma("sp", wb[:, hh * 4:(hh + 1) * 4, :], src[:, hh * 4:(hh + 1) * 4, :],
                       sem=wk, w=[wk])
            for jj in range(8):
                j = piece * 8 + jj
                for k in range(KC):
                    kb.mm(pcol[:, j:j + 1], wb[:, k, jj * 128:(jj + 1) * 128], cact[:, k:k + 1],
                          start=(k == 0), stop=(k == KC - 1), r=[wk, "cact"], w=kb.pk(0, 0, 128))
            if piece in (2, 3, 4, 5):
                row = pers[f"modrow{l}_{piece}"] if piece in (2, 5) else rowtmp
                if piece in (3, 4):
                    pers_key = f"modrow{l}_{piece}"
                kb.dma("sp", row[:], bmodrow[l, piece].partition_broadcast(128),
                       sem=f"modrow{l}_{piece}", w=[f"modrow{l}_{piece}"])
                for hh in range(2):
                    for k in range(KC):
                        kb.mm(prow[hh][:, :], crep[:, k, :], wb[:, k, hh * 512:(hh + 1) * 512],
                              start=(k == 0), stop=(k == KC - 1), r=[wk, "crep"], w=kb.pk(1 + hh))
                    kb.tt("dve", row[:, hh * 512:(hh + 1) * 512], prow[hh][:, :], row[:, hh * 512:(hh + 1) * 512],
                          ALU.add, r=kb.pk(1 + hh) + [f"modrow{l}_{piece}"], w=[f"modrow{l}_{piece}"])
                if piece in (2, 5):
                    rows.append((row, f"modrow{l}_{piece}"))
                else:
                    kb.dma("sp", kb.modrow_d[l][piece - 3], row[:], sem=f"modrow{l}_{piece}", r=[f"modrow{l}_{piece}"],
                           w=[f"modrowd{l}"])
        kb.tt("dve", modc[:], pcol[:, 0:48], bc[:], ALU.add, r=kb.pk(0, 0, 128) + [f"bmodc{l}"], w=[f"modc{l}"])
        scl = pers[f"modscl{l}"]
        kb.stt("dve", scl[:, 0, :], modc[:, 8:16], 1.0, nm[:], ALU.add, ALU.mult,
               r=[f"modc{l}", f"nmixc{l}"], w=[f"modc{l}"])
        kb.stt("dve", scl[:, 1, :], modc[:, 32:40], 1.0, nf[:], ALU.add, ALU.mult,
               r=[f"modc{l}", f"nffnc{l}"], w=[f"modc{l}"])
        kb.modc.append(modc)
        kb.gm_row.append(rows[0])
        kb.gf_row.append(rows[1])
        kb.sclm.append(scl[:, 0, :])
        kb.shm.append(modc[:, 0:8])
        kb.sclf.append(scl[:, 1, :])
        kb.shf.append(modc[:, 24:32])
    kb.pop_scope()


def norm_bufs(kb, tag):
    NB = 2
    b = {
        "tag": tag,
        "xt": [kb.sb(f"{tag}_x{i}", (128, D)) for i in range(NB)],
        "xn": [kb.sb(f"{tag}_xn{i}", (128, D)) for i in range(NB)],
        "junk": kb.sb(f"{tag}_junk", (128, D), BF16),
        "ss": kb.sb(f"{tag}_ss", (128, 2)),
        "rstd": kb.sb(f"{tag}_rstd", (128, 2)),
        "n": 0,
    }
    return b


def norm_front(kb, nb, xsrc_ap, xsrc_key):
    tag = nb["tag"]
    b = nb["n"] % 2
    nb["n"] += 1
    xt, xn, junk, ss, rstd = nb["xt"][b], nb["xn"][b], nb["junk"], nb["ss"], nb["rstd"]
    xk, xnk = f"{tag}_x{b}", f"{tag}_xn{b}"
    sk, rk = f"{tag}_ss{b}", f"{tag}_rstd{b}"
    kb.dma("sp", xt[:], xsrc_ap, sem=xk, r=[xsrc_key], w=[xk])
    kb.act(junk[:], xt[:], AF.Square, accum_out=ss[:, b:b + 1], r=[xk], w=[f"{tag}_junk", sk])
    kb.ts("dve", rstd[:, b:b + 1], ss[:, b:b + 1], 1.0 / D, EPS, ALU.mult, ALU.add, r=[sk], w=[rk])
    kb.act(rstd[:, b:b + 1], rstd[:, b:b + 1], AF.Sqrt, r=[rk], w=[rk])
    kb.S.op("dve", lambda e: e.reciprocal(rstd[:, b:b + 1], rstd[:, b:b + 1]), [rk], [rk])
    kb.ts("pool", xn[:], xt[:], rstd[:, b:b + 1], None, ALU.mult, r=[xk, rk], w=[xnk])
    return b


def norm_back(kb, nb, b, l, which, dst, dst_key):
    C = kb.C
    tag = nb["tag"]
    scl = kb.sclm[l] if which == "m" else kb.sclf[l]
    sh = kb.shm[l] if which == "m" else kb.shf[l]
    mkey = f"modc{l}"
    xn, xnk = nb["xn"][b], f"{tag}_xn{b}"
    for half in range(2):
        pT = kb.P[half]
        pk = kb.pk(half)
        for kk in range(4):
            k = half * 4 + kk
            kb.tr(pT[:, kk * 128:(kk + 1) * 128], xn[:, k * 128:(k + 1) * 128], C["ident"][:], r=[xnk, "c_ident"], w=pk)
        for kk in range(4):
            k = half * 4 + kk
            d = dst[:, k, :]
            src = pT[:, kk * 128:(kk + 1) * 128]
            if kk % 2 == 0:
                kb.act(d, src, AF.Identity, bias=sh[:, k:k + 1], scale=scl[:, k:k + 1], r=pk + [mkey], w=[dst_key])
            else:
                kb.ts("dve", d, src, scl[:, k:k + 1], sh[:, k:k + 1], ALU.mult, ALU.add, r=pk + [mkey], w=[dst_key])


def norm_tile(kb, nb, l, which, xsrc_ap, xsrc_key, dst, dst_key):
    b = norm_front(kb, nb, xsrc_ap, xsrc_key)
    norm_back(kb, nb, b, l, which, dst, dst_key)


def phase_norm(kb, l, which, xsrc, xsrc_key, hT, hT_key):
    nb = norm_bufs(kb, f"n{l}{which}")
    cur = norm_front(kb, nb, xsrc[0:128, :], xsrc_key(0))
    for i in range(NT):
        nxt = norm_front(kb, nb, xsrc[(i + 1) * 128:(i + 2) * 128, :], xsrc_key(i + 1)) if i + 1 < NT else None
        norm_back(kb, nb, cur, l, which, hT[:, :, i * 128:(i + 1) * 128], hT_key(i))
        cur = nxt


def _consts():
    i = np.arange(128)
    same = (i[:, None] // 64) == (i[None, :] // 64)
    return {
        "c_ident": np.eye(128, dtype=np.float32),
        "c_tri_incl": ((i[:, None] <= i[None, :]) & same).astype(np.float32),
        "c_tri_strict": ((i[:, None] > i[None, :]) & same).astype(np.float32),
        "c_ones": np.ones((128, 128), np.float32),
        "c_blk": same.astype(np.float32),
        "c_selA": np.repeat((i < 64).astype(np.float32)[:, None], 128, 1),
        "c_selB": np.repeat((i >= 64).astype(np.float32)[:, None], 128, 1),
        "c_neg_strict": -((i[:, None] > i[None, :]) & same).astype(np.float32),
        "c_tri_full": (i[:, None] < i[None, :]).astype(np.float32),
        "c_blk_thr": np.repeat((np.arange(64, dtype=np.float32) * 512.0)[None, :], 128, 0),
        "c_base_pk": (i[:, None] + 128 * np.arange(8)[None, :]).astype(np.float32),
    }


def host_inputs(inp, b, names):
    m = {}
    m.update(_consts())
    m["x"] = np.ascontiguousarray(inp["x"][b])
    m["cT"] = np.ascontiguousarray(inp["c"][b].reshape(KC, 128).T)
    m["w_mod"] = inp["w_mod"]
    m["bmodc"] = np.ascontiguousarray(inp["b_mod"].reshape(DEPTH, 48, 128).transpose(0, 2, 1))
    bm = inp["b_mod"].reshape(DEPTH, 6, D)
    m["bmodrow"] = np.ascontiguousarray(bm)
    m["nmixc"] = np.ascontiguousarray(inp["norm_mix"].reshape(DEPTH, KC, 128).transpose(0, 2, 1))
    m["nffnc"] = np.ascontiguousarray(inp["norm_ffn"].reshape(DEPTH, KC, 128).transpose(0, 2, 1))
    m.update(host_inputs2(inp, b, names))
    return {k: np.ascontiguousarray(m[k], dtype=m[k].dtype) for k in names}


def proj_feat(kb, out_ps, w_sb, wkey, c0, ncols, hT, hkey, t0, nt, wkeys=None):
    for k in range(KC):
        kb.mm(out_ps, w_sb[:, k, c0:c0 + ncols], hT[:, k, t0:t0 + nt], start=(k == 0), stop=(k == KC - 1),
              r=[wkey, hkey], w=wkeys)


def proj_tok(kb, out_ps, w_sb, wkey, c0, ncols, hT, hkey, t0, nt, wkeys=None):
    for k in range(KC):
        kb.mm(out_ps, hT[:, k, t0:t0 + nt], w_sb[:, k, c0:c0 + ncols], start=(k == 0), stop=(k == KC - 1),
              r=[wkey, hkey], w=wkeys)


def load_w_cast(kb, dst, dkey, src_dram_2d, c0, ncols, nk=KC, step=512):
    src = src_dram_2d.rearrange("(k p) c -> p k c", p=128)
    for k in range(nk):
        kb.dma("pool", dst[:, k, 0:ncols], src[:, k, c0:c0 + ncols], sem=dkey, w=[dkey])


def rms_rstd(kb, tag, rs, ssq, n, width):
    kb.ts("dve", rs[:, 0:n], ssq[:, 0:n], 1.0 / width, EPS, ALU.mult, ALU.add, r=[tag + "_ssq"], w=[tag + "_rs"])
    kb.act(rs[:, 0:n], rs[:, 0:n], AF.Sqrt, r=[tag + "_rs"], w=[tag + "_rs"])
    kb.S.op("dve", lambda e: e.reciprocal(rs[:, 0:n], rs[:, 0:n]), [tag + "_rs"], [tag + "_rs"])


def phase_gla(kb, l, hT, hkey_fn, obr):
    C = kb.C
    tag = f"gla{l}"
    w_in = kb.w_in
    NW = 1552
    wg = kb.sb(tag + "_w", (128, KC, NW), BF16)
    load_w_cast(kb, wg, tag + "_w", w_in[l], 0, NW)
    w2 = kb.sb(tag + "_w2", (16, 256))
    b2 = kb.sb(tag + "_b2", (1, 256))
    gn = kb.sb(tag + "_gn", (128, 512))
    kb.dma("sp", w2[:], kb.din(tag + "_w2d", (16, 256)), sem=tag + "_w2", w=[tag + "_w2"])
    kb.dma("sp", b2[:], kb.din(tag + "_b2d", (1, 256)), sem=tag + "_b2", w=[tag + "_b2"])
    kb.dma("sp", gn[:], kb.din(tag + "_gnd", (1, 512))[0].partition_broadcast(128), sem=tag + "_gn", w=[tag + "_gn"])
    lrT = kb.sb(tag + "_lrT", (16, 128))
    sp = kb.sb(tag + "_sp", (128, 256))
    e_rem = kb.sb(tag + "_erem", (128, 256))
    e_pos = kb.sb(tag + "_epos", (128, 256))
    e_neg = kb.sb(tag + "_eneg", (128, 256))
    qdT = kb.sb(tag + "_qdT", (128, 256), BF16)
    knT = kb.sb(tag + "_knT", (128, 256), BF16)
    krem = kb.sb(tag + "_krem", (128, 256), BF16)
    v_sb = kb.sb(tag + "_v", (128, 512), BF16)
    r_sb = kb.sb(tag + "_r", (128, 512))
    attT = [kb.sb(tag + f"_attT{i}", (128, 128), BF16) for i in range(4)]
    S = [kb.sb(tag + f"_S{i}", (128, 256)) for i in range(2)]
    Sb = [kb.sb(tag + f"_Sb{i}", (128, 256), BF16) for i in range(2)]
    junk = kb.sb(tag + "_junk", (128, 128), BF16)
    ssq = kb.sb(tag + "_ssq", (128, 4))
    rs = kb.sb(tag + "_rs", (128, 4))
    og = kb.sb(tag + "_og", (128, 512))
    oint = kb.sb(tag + "_oint", (128, 512))
    o_sb = kb.sb(tag + "_o", (128, 512))
    oT = kb.sb(tag + "_oT", (128, 512), BF16)
    for p in range(2):
        kb.S.op("dve", lambda e, p=p: e.memset(S[p][:], 0.0), [], [tag + f"_S{p}"])
        kb.S.op("dve", lambda e, p=p: e.memset(Sb[p][:], 0.0), [], [tag + f"_Sb{p}"])
    P0, P1, P2, P3, P4, P5, P6, P7 = kb.P
    wk = tag + "_w"
    import os
    STOP = float(os.environ.get('GLA_STOP', '9'))
    for i in range(int(os.environ.get('GLA_NT', NT))):
        t0 = i * 128
        hk = hkey_fn(i)
        for pair in range(2):
            proj_feat(kb, P0[:, pair * 128:(pair + 1) * 128], wg, wk, C_GLA_Q + pair * 128, 128, hT, hk, t0, 128, kb.pk(0, pair * 128, pair * 128 + 128))
            proj_feat(kb, P0[:, 256 + pair * 128:256 + (pair + 1) * 128], wg, wk, C_GLA_K + pair * 128, 128, hT, hk, t0, 128, kb.pk(0, 256 + pair * 128, 384 + pair * 128))
        proj_feat(kb, P1[0:16, 0:128], wg, wk, C_GLA_LR, 16, hT, hk, t0, 128, kb.pk(1, 0, 128))
        proj_tok(kb, P2[:, 0:256], wg, wk, C_GLA_K, 256, hT, hk, t0, 128, kb.pk(2, 0, 256))
        proj_tok(kb, P3[:, :], wg, wk, C_GLA_V, 512, hT, hk, t0, 128, kb.pk(3))
        proj_tok(kb, P4[:, :], wg, wk, C_GLA_R, 512, hT, hk, t0, 128, kb.pk(4))
        if STOP <= 1:
            continue
        kb.cp("dve", lrT[:, :], P1[0:16, 0:128], r=kb.pk(1, 0, 128), w=[tag + "_lrT"])
        kb.mm(P1[:, 128:384], lrT[:, :], w2[:, :], start=True, stop=False, r=[tag + "_lrT", tag + "_w2"], w=kb.pk(1, 128, 384))
        kb.mm(P1[:, 128:384], C["ones"][0:1, :], b2[0:1, :], start=False, stop=True, r=["c_ones", tag + "_b2"], w=kb.pk(1, 128, 384))
        kb.act(sp[:], P1[:, 128:384], AF.Exp, scale=-1.0, r=kb.pk(1, 128, 384), w=[tag + "_sp"])
        kb.act(sp[:], sp[:], AF.Ln, bias=1.0, r=[tag + "_sp"], w=[tag + "_sp"])
        if STOP <= 2:
            continue
        kb.cp("act", v_sb[:], P3[:, :], r=kb.pk(3), w=[tag + "_v"])
        kb.act(r_sb[:], P4[:, :], AF.Silu, r=kb.pk(4), w=[tag + "_r"])
        kb.mm(P5[:, 256:512], C["tri_strict"][:], sp[:], r=["c_tri_strict", tag + "_sp"], w=kb.pk(5, 256, 512))
        for pair in range(2):
            kb.mm(P6[:, pair * 128:(pair + 1) * 128], sp[:, pair * 128:(pair + 1) * 128], C["tri_incl"][:],
                  r=["c_tri_incl", tag + "_sp"], w=kb.pk(6, 0, 256))
        kb.act(e_rem[:], P5[:, 256:512], AF.Exp, scale=-1.0 / 16, r=kb.pk(5, 256, 512), w=[tag + "_erem"])
        kb.act(e_pos[:], P6[:, 0:256], AF.Exp, scale=-1.0 / 16, r=kb.pk(6, 0, 256), w=[tag + "_epos"])
        kb.act(e_neg[:], P6[:, 0:256], AF.Exp, scale=1.0 / 16, r=kb.pk(6, 0, 256), w=[tag + "_eneg"])
        kb.stt("dve", qdT[:], P0[:, 0:256], 0.125, e_pos[:], ALU.mult, ALU.mult, r=kb.pk(0, 0, 256) + [tag + "_epos"], w=[tag + "_qdT"])
        kb.tt("dve", knT[:], P0[:, 256:512], e_neg[:], ALU.mult, r=kb.pk(0, 256, 512) + [tag + "_eneg"], w=[tag + "_knT"])
        kb.tt("dve", krem[:], P2[:, 0:256], e_rem[:], ALU.mult, r=kb.pk(2, 0, 256) + [tag + "_erem"], w=[tag + "_krem"])
        if STOP <= 3:
            continue
        zones = [(P1[:, 384:512], kb.pk(1)), (P2[:, 256:384], kb.pk(2)), (P3[:, 0:128], kb.pk(3)), (P4[:, 0:128], kb.pk(4))]
        for h in range(4):
            pair, rows = h // 2, (h % 2) * 64
            pc = slice(pair * 128, (pair + 1) * 128)
            aps, apk = zones[h]
            kb.mm(aps, knT[rows:rows + 64, pc], qdT[rows:rows + 64, pc], r=[tag + "_knT", tag + "_qdT"], w=apk)
        for h in range(4):
            aps, apk = zones[h]
            kb.tt("dve", attT[h][:], aps, C["tri_incl"][:], ALU.mult, r=apk + ["c_tri_incl"], w=[tag + f"_attT{h}"])
        for h in range(4):
            hc = slice(h * 128, (h + 1) * 128)
            kb.mm(P7[:, hc], attT[h][:], v_sb[:, hc], start=True, stop=True, r=[tag + f"_attT{h}", tag + "_v"], w=kb.pk(7))

        def pairchain(pair):
            pc0 = pair * 128
            Sk, Sbk = tag + f"_S{pair}", tag + f"_Sb{pair}"
            PU, ku = (P6, kb.pk(6)) if pair == 0 else (P3, kb.pk(3))
            for ch in range(2):
                tr_ = slice(ch * 64, (ch + 1) * 64)
                kb.mm(P5[tr_, pair * 256:(pair + 1) * 256], qdT[:, pc0 + ch * 64:pc0 + (ch + 1) * 64],
                      Sb[pair][:, :], start=True, stop=True, r=[tag + "_qdT", Sbk], w=kb.pk(5))
                kb.mm(PU[:, 256:512], krem[tr_, pc0:pc0 + 128], v_sb[tr_, pair * 256:(pair + 1) * 256],
                      r=[tag + "_krem", tag + "_v"], w=ku)
                yield
                for hh in range(2):
                    rr = slice(hh * 64, (hh + 1) * 64)
                    cc = slice(hh * 128, (hh + 1) * 128)
                    dec = e_pos[rr, pc0 + ch * 64 + 63:pc0 + ch * 64 + 64]
                    kb.stt("dve", S[pair][rr, cc], S[pair][rr, cc], dec, PU[rr, 256 + hh * 128:256 + (hh + 1) * 128],
                           ALU.mult, ALU.add, r=[Sk, tag + "_epos"] + ku, w=[Sk])
                yield
                kb.cp("act", Sb[pair][:], S[pair][:], r=[Sk], w=[Sbk])
                yield

        gens = [pairchain(0), pairchain(1)]
        while gens:
            for gen in list(gens):
                try:
                    next(gen)
                except StopIteration:
                    gens.remove(gen)
        if STOP <= 5:
            continue
        kb.cp("act", oint[:], P5[:, :], r=kb.pk(5), w=[tag + "_oint"])
        kb.tt("dve", o_sb[:], P7[:, :], oint[:], ALU.add, r=kb.pk(7) + [tag + "_oint"], w=[tag + "_o"])
        for h in range(4):
            kb.act(junk[:], o_sb[:, h * 128:(h + 1) * 128], AF.Square, accum_out=ssq[:, h:h + 1], r=[tag + "_o"],
                   w=[tag + "_junk", tag + "_ssq"])
        rms_rstd(kb, tag, rs, ssq, 4, 128)
        for h in range(4):
            hc = slice(h * 128, (h + 1) * 128)
            kb.stt("dve", og[:, hc], o_sb[:, hc], rs[:, h:h + 1], gn[:, hc], ALU.mult, ALU.mult,
                   r=[tag + "_o", tag + "_rs", tag + "_gn"], w=[tag + "_og"])
        kb.tt("pool", og[:], og[:], r_sb[:], ALU.mult, r=[tag + "_og", tag + "_r"], w=[tag + "_og"])
        for c in range(4):
            kb.tr(P0[:, c * 128:(c + 1) * 128], og[:, c * 128:(c + 1) * 128], C["ident"][:], r=[tag + "_og", "c_ident"], w=kb.pk(0, c * 128, c * 128 + 128))
        kb.cp("act", oT[:], P0[:, :], r=kb.pk(0), w=[tag + "_oT"])
        kb.dma("sp", obr[i], oT[:], sem=tag + "_oT", r=[tag + "_oT"], w=[f"{tag}_obr{i}"])


def host_inputs2(inp, b, names):
    m = {}
    f = np.float32
    for l in range(DEPTH):
        m[f"gla{l}_w2d"] = inp["gla_w_gate2"][l]
        m[f"gla{l}_b2d"] = inp["gla_b_gate2"][l][None, :]
        m[f"gla{l}_gnd"] = np.tile(inp["gla_norm"][l], 4)[None, :]
    m["w_in"] = inp["w_in"]
    for l in range(DEPTH):
        cw = inp["ssd_conv_w"][l].reshape(4, 6, 128).transpose(2, 1, 0)
        cb = inp["ssd_conv_b"][l].reshape(6, 128).T[:, :, None]
        m[f"ssd{l}_cwd"] = np.concatenate([cw, cb], axis=2)
        m[f"ssd{l}_gnd"] = inp["ssd_norm"][l][None, :]
        m[f"ssd{l}_hpd"] = np.concatenate([inp["ssd_dt_bias"][l], inp["ssd_a_log"][l], inp["ssd_d"][l]])[None, :]
    for l in range(DEPTH):
        m[f"gdn{l}_cwd"] = inp["gdn_conv_w"][l].reshape(4, 12, 128).transpose(2, 1, 0)
        m[f"gdn{l}_gnd"] = np.tile(inp["gdn_norm"][l], 4)[None, :]
        m[f"gdn{l}_hpd"] = np.concatenate([inp["gdn_dt_bias"][l], inp["gdn_a_log"][l]])[None, :]
    return m


def phase_gdn(kb, l, hT, hkey_fn, obr):
    import os
    C = kb.C
    tag = f"gdn{l}"
    w_in = kb.w_in
    NW = 2056
    wk = tag + "_w"
    wg = kb.sb(wk, (128, KC, NW), BF16)
    load_w_cast(kb, wg, wk, w_in[l], C_GDN_QKV, NW)
    O_QKV, O_AB, O_G = 0, 1536, 1544
    convw = kb.sb(tag + "_cw", (128, 12, 4))
    kb.dma("sp", convw[:], kb.din(tag + "_cwd", (128, 12, 4)), sem=tag + "_cw", w=[tag + "_cw"])
    gn = kb.sb(tag + "_gn", (128, 512))
    kb.dma("sp", gn[:], kb.din(tag + "_gnd", (1, 512))[0].partition_broadcast(128), sem=tag + "_gn", w=[tag + "_gn"])
    hp = kb.sb(tag + "_hp", (128, 8))
    kb.dma("sp", hp[:], kb.din(tag + "_hpd", (1, 8))[0].partition_broadcast(128), sem=tag + "_hp", w=[tag + "_hp"])
    negA = kb.sb(tag + "_negA", (128, 4))
    kb.act(negA[:], hp[:, 4:8], AF.Exp, r=[tag + "_hp"], w=[tag + "_negA"])
    kb.ts("dve", negA[:], negA[:], -1.0, None, ALU.mult, r=[tag + "_negA"], w=[tag + "_negA"])
    ubuf = kb.sb(tag + "_ubuf", (128, 12, 131))
    kb.S.op("pool", lambda e: e.memset(ubuf[:], 0.0), [], [tag + "_ubuf"])
    cacc = kb.sb(tag + "_cacc", (128, 12, 128))
    ctmp = kb.sb(tag + "_ctmp", (128, 12, 128))
    qkv = kb.sb(tag + "_qkv", (128, 12, 128))
    sq = kb.sb(tag + "_sq", (128, 8, 128))
    rinv = kb.sb(tag + "_rinv", (128, 8, 128))
    qkn = kb.sb(tag + "_qkn", (128, 8, 128))
    gsb = kb.sb(tag + "_gsb", (128, 512))
    sm = kb.sb(tag + "_sm", (128, 40))
    beta, gg, cum, ecum, erem, bec = sm[:, 0:4], sm[:, 4:8], sm[:, 8:12], sm[:, 12:16], sm[:, 16:20], sm[:, 20:24]
    dA, dB, ytmp = sm[:, 24:28], sm[:, 28:32], sm[:, 32:36]
    HB = []
    for s_ in range(4):
        d_ = {}
        for nm in ("otmp", "Gs", "E", "ET", "B0", "B1", "C0", "C1", "PT0", "PT1", "PmT", "ecb", "qdT", "V0", "W0", "kdec", "upre", "wT", "u"):
            d_[nm] = kb.sb(tag + f"_{nm}_{s_}", (128, 128))
        kb.S.op("pool", lambda e, t=d_["u"]: e.memset(t[:], 0.0), [], [tag + f"_u_{s_}"])
        HB.append(d_)
    M = [kb.sb(tag + f"_M{h}", (128, 128)) for h in range(4)]
    for h in range(4):
        kb.S.op("pool", lambda e, h=h: e.memset(M[h][:], 0.0), [], [tag + f"_M{h}"])
    oint = kb.sb(tag + "_oint", (128, 512))
    o_sb = kb.sb(tag + "_o", (128, 512))
    og = kb.sb(tag + "_og", (128, 512))
    oT = kb.sb(tag + "_oT", (128, 512), BF16)
    junk = kb.sb(tag + "_junk", (128, 128), BF16)
    ssq = kb.sb(tag + "_ssq", (128, 4))
    rs = kb.sb(tag + "_rs", (128, 4))
    P0, P1, P2, P3, P4, P5, P6, P7 = kb.P
    ident = C["ident"]
    STOP = float(os.environ.get('GDN_STOP', '9'))
    for i in range(int(os.environ.get('GDN_NT', NT))):
        t0 = i * 128
        hk = hkey_fn(i)
        for c in range(12):
            bank = kb.P[c // 4]
            cc = (c % 4) * 128
            proj_feat(kb, bank[:, cc:cc + 128], wg, wk, O_QKV + c * 128, 128, hT, hk, t0, 128, kb.pk(c // 4, cc, cc + 128))
        proj_tok(kb, P3[:, :], wg, wk, O_G, 512, hT, hk, t0, 128, kb.pk(3))
        proj_tok(kb, P4[:, 0:8], wg, wk, O_AB, 8, hT, hk, t0, 128, kb.pk(4, 0, 128))
        kb.act(gsb[:], P3[:, :], AF.Silu, r=kb.pk(3), w=[tag + "_gsb"])
        for b3 in range(3):
            kb.cp("act", ubuf[:, b3 * 4:(b3 + 1) * 4, 3:131], kb.P[b3][:, :].rearrange("p (c t) -> p c t", c=4),
                  r=kb.pk(b3), w=[tag + "_ubuf"])
        for j in range(4):
            wj = convw[:, :, j:j + 1].to_broadcast([128, 12, 128])
            if j == 0:
                kb.tt("dve", cacc[:], ubuf[:, :, 0:128], wj, ALU.mult, r=[tag + "_ubuf", tag + "_cw"], w=[tag + "_cacc"])
            else:
                kb.tt("pool", ctmp[:], ubuf[:, :, j:j + 128], wj, ALU.mult, r=[tag + "_ubuf", tag + "_cw"], w=[tag + "_ctmp"])
                kb.tt("dve", cacc[:], cacc[:], ctmp[:], ALU.add, r=[tag + "_cacc", tag + "_ctmp"], w=[tag + "_cacc"])
        kb.act(qkv[:], cacc[:], AF.Silu, r=[tag + "_cacc"], w=[tag + "_qkv"])
        kb.cp("pool", ubuf[:, :, 0:3], ubuf[:, :, 128:131], r=[tag + "_ubuf"], w=[tag + "_ubuf"])
        if STOP <= 1:
            continue
        kb.tt("pool", sq[:], qkv[:, 0:8, :], qkv[:, 0:8, :], ALU.mult, r=[tag + "_qkv"], w=[tag + "_sq"])
        for half in range(2):
            kb.mm(kb.P[half][:, :], C["ones"][:], sq[:, half * 4:(half + 1) * 4, :], r=["c_ones", tag + "_sq"], w=kb.pk(half))
            kb.ts("dve", rinv[:, half * 4:(half + 1) * 4, :], kb.P[half][:, :].rearrange("p (c t) -> p c t", c=4),
                  1e-6, None, ALU.add, r=kb.pk(half), w=[tag + "_rinv"])
        kb.act(rinv[:], rinv[:], AF.Sqrt, r=[tag + "_rinv"], w=[tag + "_rinv"])
        kb.S.op("dve", lambda e: e.reciprocal(rinv[:], rinv[:]), [tag + "_rinv"], [tag + "_rinv"])
        kb.stt("dve", qkn[:, 0:4, :], qkv[:, 0:4, :], 128.0 ** -0.5, rinv[:, 0:4, :], ALU.mult, ALU.mult,
               r=[tag + "_qkv", tag + "_rinv"], w=[tag + "_qkn"])
        kb.tt("pool", qkn[:, 4:8, :], qkv[:, 4:8, :], rinv[:, 4:8, :], ALU.mult, r=[tag + "_qkv", tag + "_rinv"], w=[tag + "_qkn"])
        kb.act(beta, P4[:, 4:8], AF.Sigmoid, r=kb.pk(4, 0, 128), w=[tag + "_sm"])
        kb.tt("dve", ytmp, P4[:, 0:4], hp[:, 0:4], ALU.add, r=kb.pk(4, 0, 128) + [tag + "_hp"], w=[tag + "_sm"])
        kb.act(ytmp, ytmp, AF.Exp, r=[tag + "_sm"], w=[tag + "_sm"])
        kb.act(ytmp, ytmp, AF.Ln, bias=1.0, r=[tag + "_sm"], w=[tag + "_sm"])
        kb.tt("dve", gg, ytmp, negA[:], ALU.mult, r=[tag + "_sm", tag + "_negA"], w=[tag + "_sm"])
        kb.mm(P4[:, 8:12], C["tri_incl"][:], gg, r=["c_tri_incl", tag + "_sm"], w=kb.pk(4, 0, 128))
        kb.mm(P4[:, 12:16], C["blk"][:], gg, r=["c_blk", tag + "_sm"], w=kb.pk(4, 0, 128))
        kb.mm(P4[:, 16:20], C["selA"][:], gg, r=["c_selA", tag + "_sm"], w=kb.pk(4, 0, 128))
        kb.mm(P4[:, 20:24], C["selB"][:], gg, r=["c_selB", tag + "_sm"], w=kb.pk(4, 0, 128))
        kb.cp("dve", cum, P4[:, 8:12], r=kb.pk(4, 0, 128), w=[tag + "_sm"])
        kb.act(ecum, P4[:, 8:12], AF.Exp, r=kb.pk(4, 0, 128), w=[tag + "_sm"])
        kb.tt("dve", erem, P4[:, 12:16], cum, ALU.subtract, r=kb.pk(4, 0, 128) + [tag + "_sm"], w=[tag + "_sm"])
        kb.act(erem, erem, AF.Exp, r=[tag + "_sm"], w=[tag + "_sm"])
        kb.act(dA, P4[:, 16:20], AF.Exp, r=kb.pk(4, 0, 128), w=[tag + "_sm"])
        kb.act(dB, P4[:, 20:24], AF.Exp, r=kb.pk(4, 0, 128), w=[tag + "_sm"])
        kb.tt("dve", bec, beta, ecum, ALU.mult, r=[tag + "_sm"], w=[tag + "_sm"])
        if STOP <= 2:
            continue
        def head(h, s_):
            hb = HB[s_]
            XA, XB = kb.P[2 * s_], kb.P[2 * s_ + 1]
            ka, kbk = kb.pk(2 * s_), kb.pk(2 * s_ + 1)
            K = lambda nm: tag + f"_{nm}_{s_}"
            Gs, E, ET, PmT, ecb, qdT = hb["Gs"], hb["E"], hb["ET"], hb["PmT"], hb["ecb"], hb["qdT"]
            V0, W0, kdec, upre, wT, u_sb, otmp = hb["V0"], hb["W0"], hb["kdec"], hb["upre"], hb["wT"], hb["u"], hb["otmp"]
            Bm, Cm, PT = [hb["B0"], hb["B1"]], [hb["C0"], hb["C1"]], [hb["PT0"], hb["PT1"]]
            qT = qkn[:, h, :]
            kT = qkn[:, 4 + h, :]
            vT = qkv[:, 8 + h, :]
            hc = slice(h * 128, (h + 1) * 128)
            Z = lambda i: slice(i * 128, (i + 1) * 128)
            kb.ts("dve", Gs[:], C["tri_incl"][:], gg[:, h:h + 1], None, ALU.mult, r=["c_tri_incl", tag + "_sm"], w=[K("Gs")])
            kb.mm(XA[:, Z(0)], kT, kT, r=[tag + "_qkn"], w=ka)
            kb.mm(XA[:, Z(1)], kT, qT, r=[tag + "_qkn"], w=ka)
            yield
            kb.mm(XA[:, Z(2)], Gs[:], C["tri_strict"][:], r=[K("Gs"), "c_tri_strict"], w=ka)
            kb.mm(XA[:, Z(3)], C["tri_strict"][:], Gs[:], r=[K("Gs"), "c_tri_strict"], w=ka)
            kb.mm(XB[:, Z(0)], C["ones"][:], Gs[:], r=[K("Gs"), "c_ones"], w=kbk)
            yield
            kb.act(E[:], XA[:, Z(2)], AF.Exp, r=ka, w=[K("E")])
            kb.act(ET[:], XA[:, Z(3)], AF.Exp, r=ka, w=[K("ET")])
            kb.act(ecb[:], XB[:, Z(0)], AF.Exp, r=kbk, w=[K("ecb")])
            yield
            kb.tt("dve", E[:], E[:], C["neg_strict"][:], ALU.mult, r=[K("E"), "c_neg_strict"], w=[K("E")])
            kb.tt("dve", ET[:], ET[:], C["tri_incl"][:], ALU.mult, r=[K("ET"), "c_tri_incl"], w=[K("ET")])
            kb.tt("pool", qdT[:], qT, ecb[:], ALU.mult, r=[tag + "_qkn", K("ecb")], w=[K("qdT")])
            yield
            kb.stt("dve", Bm[0][:], XA[:, Z(0)], beta[:, h:h + 1], E[:], ALU.mult, ALU.mult,
                   r=ka + [tag + "_sm", K("E")], w=[K("B0")])
            kb.tt("dve", PmT[:], XA[:, Z(1)], ET[:], ALU.mult, r=ka + [K("ET")], w=[K("PmT")])
            yield
            kb.tr(XB[:, Z(1)], Bm[0][:], ident[:], r=[K("B0"), "c_ident"], w=kbk)
            kb.tr(XB[:, Z(2)], vT, ident[:], r=[tag + "_qkv", "c_ident"], w=kbk)
            kb.tr(XB[:, Z(3)], kT, ident[:], r=[tag + "_qkn", "c_ident"], w=kbk)
            yield
            kb.cp("act", Cm[0][:], XB[:, Z(1)], r=kbk, w=[K("C0")])
            kb.tt("dve", PT[0][:], XB[:, Z(1)], ident[:], ALU.add, r=kbk + ["c_ident"], w=[K("PT0")])
            kb.ts("dve", V0[:], XB[:, Z(2)], beta[:, h:h + 1], None, ALU.mult, r=kbk + [tag + "_sm"], w=[K("V0")])
            kb.act(W0[:], XB[:, Z(3)], AF.Identity, scale=bec[:, h:h + 1], r=kbk + [tag + "_sm"], w=[K("W0")])
            kb.ts("dve", kdec[:], XB[:, Z(3)], erem[:, h:h + 1], None, ALU.mult, r=kbk + [tag + "_sm"], w=[K("kdec")])
            yield
            cur = 0
            kb.mm(XB[:, Z(0)], Cm[0][:], Bm[0][:], r=[K("B0"), K("C0")], w=kbk)
            kb.mm(XB[:, Z(1)], Bm[0][:], Cm[0][:], r=[K("B0"), K("C0")], w=kbk)
            yield
            for j in range(1, 6):
                nxt = 1 - cur
                Bn, Cn, PTk, PTn = K(f"B{nxt}"), K(f"C{nxt}"), K(f"PT{cur}"), K(f"PT{nxt}")
                kb.cp("dve", Bm[nxt][:], XB[:, Z(0)], r=kbk, w=[Bn])
                if j < 5:
                    kb.cp("act", Cm[nxt][:], XB[:, Z(1)], r=kbk, w=[Cn])
                yield
                kb.mm(XB[:, Z(2)], Bm[nxt][:], PT[cur][:], r=[Bn, PTk], w=kbk)
                if j < 5:
                    kb.mm(XB[:, Z(0)], Cm[nxt][:], Bm[nxt][:], r=[Bn, Cn], w=kbk)
                    if j < 4:
                        kb.mm(XB[:, Z(1)], Bm[nxt][:], Cm[nxt][:], r=[Bn, Cn], w=kbk)
                yield
                kb.tt("dve", PT[nxt][:], XB[:, Z(2)], PT[cur][:], ALU.add, r=kbk + [PTk], w=[PTn])
                cur = nxt
            yield
            PTf, PTfk = PT[cur], K(f"PT{cur}")
            kb.mm(XB[:, Z(3)], PTf[:], V0[:], r=[PTfk, K("V0")], w=kbk)
            kb.mm(XA[:, Z(0)], W0[:], PTf[:], r=[PTfk, K("W0")], w=ka)
            yield
            kb.cp("act", upre[:], XB[:, Z(3)], r=kbk, w=[K("upre")])
            kb.cp("dve", wT[:], XA[:, Z(0)], r=ka, w=[K("wT")])
            yield
            Mk = tag + f"_M{h}"
            for ch in range(2):
                tr_ = slice(ch * 64, (ch + 1) * 64)
                dch = dA if ch == 0 else dB
                kb.mm(XA[tr_, Z(1)], wT[:, tr_], M[h][:], r=[K("wT"), Mk], w=ka)
                kb.mm(XA[tr_, Z(3)], qdT[:, tr_], M[h][:], r=[K("qdT"), Mk], w=ka)
                yield
                kb.tt("dve", u_sb[tr_, :], upre[tr_, :], XA[tr_, Z(1)], ALU.subtract, r=[K("upre")] + ka, w=[K("u")])
                kb.cp("act", otmp[tr_, :], XA[tr_, Z(3)], r=ka, w=[K("otmp")])
                yield
                kb.mm(XB[tr_, Z(3)], PmT[:, tr_], u_sb[:, :], r=[K("PmT"), K("u")], w=kbk)
                kb.mm(XA[:, Z(2)], kdec[tr_, :], u_sb[tr_, :], r=[K("kdec"), K("u")], w=ka)
                yield
                kb.stt("dve", M[h][:], M[h][:], dch[:, h:h + 1], XA[:, Z(2)], ALU.mult, ALU.add,
                       r=[Mk, tag + "_sm"] + ka, w=[Mk])
                kb.tt("dve", o_sb[tr_, hc], XB[tr_, Z(3)], otmp[tr_, :], ALU.add, r=kbk + [K("otmp")], w=[tag + "_o"])
                yield

        gens = [head(h, h) for h in range(4)]
        while gens:
            for gen in list(gens):
                try:
                    next(gen)
                except StopIteration:
                    gens.remove(gen)
        if STOP <= 4:
            continue
        for h in range(4):
            kb.act(junk[:], o_sb[:, h * 128:(h + 1) * 128], AF.Square, accum_out=ssq[:, h:h + 1], r=[tag + "_o"],
                   w=[tag + "_junk", tag + "_ssq"])
        rms_rstd(kb, tag, rs, ssq, 4, 128)
        for h in range(4):
            hc = slice(h * 128, (h + 1) * 128)
            kb.stt("dve", og[:, hc], o_sb[:, hc], rs[:, h:h + 1], gn[:, hc], ALU.mult, ALU.mult,
                   r=[tag + "_o", tag + "_rs", tag + "_gn"], w=[tag + "_og"])
        kb.tt("pool", og[:], og[:], gsb[:], ALU.mult, r=[tag + "_og", tag + "_gsb"], w=[tag + "_og"])
        for c in range(4):
            kb.tr(P3[:, c * 128:(c + 1) * 128], og[:, c * 128:(c + 1) * 128], ident[:], r=[tag + "_og", "c_ident"],
                  w=kb.pk(3, c * 128, c * 128 + 128))
        kb.cp("act", oT[:], P3[:, :], r=kb.pk(3), w=[tag + "_oT"])
        kb.dma("sp", obr[i], oT[:], sem=tag + "_oT", r=[tag + "_oT"], w=[f"{tag}_obr{i}"])


def phase_ssd(kb, l, hT, hkey_fn, obr):
    import os
    C = kb.C
    tag = f"ssd{l}"
    w_in = kb.w_in
    NW = 1288
    wk = tag + "_w"
    wg = kb.sb(wk, (128, KC, NW), BF16)
    load_w_cast(kb, wg, wk, w_in[l], C_SSD_Z, NW)
    O_Z, O_XBC, O_DT = 0, 512, 1280
    convw = kb.sb(tag + "_cw", (128, 6, 5))
    kb.dma("sp", convw[:], kb.din(tag + "_cwd", (128, 6, 5)), sem=tag + "_cw", w=[tag + "_cw"])
    gn = kb.sb(tag + "_gn", (128, 512))
    kb.dma("sp", gn[:], kb.din(tag + "_gnd", (1, 512))[0].partition_broadcast(128), sem=tag + "_gn", w=[tag + "_gn"])
    hp = kb.sb(tag + "_hp", (128, 24))
    kb.dma("sp", hp[:], kb.din(tag + "_hpd", (1, 24))[0].partition_broadcast(128), sem=tag + "_hp", w=[tag + "_hp"])
    negA = kb.sb(tag + "_negA", (128, 8))
    kb.act(negA[:], hp[:, 8:16], AF.Exp, r=[tag + "_hp"], w=[tag + "_negA"])
    kb.ts("dve", negA[:], negA[:], -1.0, None, ALU.mult, r=[tag + "_negA"], w=[tag + "_negA"])
    ubuf = kb.sb(tag + "_ubuf", (128, 6, 131))
    kb.S.op("pool", lambda e: e.memset(ubuf[:], 0.0), [], [tag + "_ubuf"])
    cacc = kb.sb(tag + "_cacc", (128, 6, 128))
    ctmp = kb.sb(tag + "_ctmp", (128, 6, 128))
    xbc = kb.sb(tag + "_xbc", (128, 6, 128))
    zs = kb.sb(tag + "_zs", (128, 512))
    sm = kb.sb(tag + "_sm", (128, 72))
    dt, gg, cum, ecum, erem = sm[:, 0:8], sm[:, 8:16], sm[:, 16:24], sm[:, 24:32], sm[:, 32:40]
    dA, dB, ytmp = sm[:, 40:48], sm[:, 48:56], sm[:, 56:64]
    x_tok = kb.sb(tag + "_xtok", (128, 512))
    xdt = kb.sb(tag + "_xdt", (128, 512))
    xdte = kb.sb(tag + "_xdte", (128, 512))
    xd = kb.sb(tag + "_xd", (128, 512))
    B_tok = kb.sb(tag + "_Btok", (128, 128))
    CBT = [kb.sb(tag + f"_CBT{g}", (128, 128)) for g in range(2)]
    GsL = [kb.sb(tag + f"_Gs{i}", (128, 128)) for i in range(4)]
    LTL = [kb.sb(tag + f"_LT{i}", (128, 128)) for i in range(4)]
    Sbd = kb.sb(tag + "_Sbd", (128, 512))
    kb.S.op("pool", lambda e: e.memset(Sbd[:], 0.0), [], [tag + "_Sbd"])
    yint = kb.sb(tag + "_yint", (128, 512))
    y_sb = kb.sb(tag + "_y", (128, 512))
    oT = kb.sb(tag + "_oT", (128, 512), BF16)
    junk = kb.sb(tag + "_junk", (128, 256), BF16)
    ssq = kb.sb(tag + "_ssq", (128, 2))
    rs = kb.sb(tag + "_rs", (128, 2))
    P0, P1, P2, P3, P4, P5, P6, P7 = kb.P
    ident = C["ident"]
    STOP = float(os.environ.get('SSD_STOP', '9'))
    for i in range(int(os.environ.get('SSD_NT', NT))):
        t0 = i * 128
        hk = hkey_fn(i)
        for c in range(6):
            bank = kb.P[c // 4]
            cc = (c % 4) * 128
            proj_feat(kb, bank[:, cc:cc + 128], wg, wk, O_XBC + c * 128, 128, hT, hk, t0, 128, kb.pk(c // 4))
        proj_tok(kb, P2[:, :], wg, wk, O_Z, 512, hT, hk, t0, 128, kb.pk(2))
        proj_tok(kb, P3[:, 0:8], wg, wk, O_DT, 8, hT, hk, t0, 128, kb.pk(3))
        kb.act(zs[:], P2[:, :], AF.Silu, r=kb.pk(2), w=[tag + "_zs"])
        kb.cp("act", ubuf[:, 0:4, 3:131], P0[:, :].rearrange("p (c t) -> p c t", c=4), r=kb.pk(0), w=[tag + "_ubuf"])
        kb.cp("act", ubuf[:, 4:6, 3:131], P1[:, 0:256].rearrange("p (c t) -> p c t", c=2), r=kb.pk(1), w=[tag + "_ubuf"])
        for j in range(4):
            wj = convw[:, :, j:j + 1].to_broadcast([128, 6, 128])
            if j == 0:
                kb.tt("dve", cacc[:], ubuf[:, :, 0:128], wj, ALU.mult, r=[tag + "_ubuf", tag + "_cw"], w=[tag + "_cacc"])
                kb.tt("dve", cacc[:], cacc[:], convw[:, :, 4:5].to_broadcast([128, 6, 128]), ALU.add,
                      r=[tag + "_cacc", tag + "_cw"], w=[tag + "_cacc"])
            else:
                kb.tt("pool", ctmp[:], ubuf[:, :, j:j + 128], wj, ALU.mult, r=[tag + "_ubuf", tag + "_cw"], w=[tag + "_ctmp"])
                kb.tt("dve", cacc[:], cacc[:], ctmp[:], ALU.add, r=[tag + "_cacc", tag + "_ctmp"], w=[tag + "_cacc"])
        kb.act(xbc[:], cacc[:], AF.Silu, r=[tag + "_cacc"], w=[tag + "_xbc"])
        kb.cp("pool", ubuf[:, :, 0:3], ubuf[:, :, 128:131], r=[tag + "_ubuf"], w=[tag + "_ubuf"])
        kb.tt("dve", ytmp, P3[:, 0:8], hp[:, 0:8], ALU.add, r=kb.pk(3) + [tag + "_hp"], w=[tag + "_sm"])
        kb.act(ytmp, ytmp, AF.Exp, r=[tag + "_sm"], w=[tag + "_sm"])
        kb.act(dt, ytmp, AF.Ln, bias=1.0, r=[tag + "_sm"], w=[tag + "_sm"])
        kb.tt("dve", gg, dt, negA[:], ALU.mult, r=[tag + "_sm", tag + "_negA"], w=[tag + "_sm"])
        kb.mm(P3[:, 8:16], C["tri_incl"][:], gg, r=["c_tri_incl", tag + "_sm"], w=kb.pk(3))
        kb.mm(P3[:, 16:24], C["blk"][:], gg, r=["c_blk", tag + "_sm"], w=kb.pk(3))
        kb.mm(P3[:, 24:32], C["selA"][:], gg, r=["c_selA", tag + "_sm"], w=kb.pk(3))
        kb.mm(P3[:, 32:40], C["selB"][:], gg, r=["c_selB", tag + "_sm"], w=kb.pk(3))
        kb.cp("dve", cum, P3[:, 8:16], r=kb.pk(3), w=[tag + "_sm"])
        kb.tt("dve", erem, P3[:, 16:24], cum, ALU.subtract, r=kb.pk(3) + [tag + "_sm"], w=[tag + "_sm"])
        kb.act(dA, P3[:, 24:32], AF.Exp, r=kb.pk(3), w=[tag + "_sm"])
        kb.act(dB, P3[:, 32:40], AF.Exp, r=kb.pk(3), w=[tag + "_sm"])
        kb.act(ecum, cum, AF.Exp, r=[tag + "_sm"], w=[tag + "_sm"])
        kb.act(erem, erem, AF.Exp, r=[tag + "_sm"], w=[tag + "_sm"])
        if STOP <= 1:
            continue
        for c in range(4):
            kb.tr(P4[:, c * 128:(c + 1) * 128], xbc[:, c, :], ident[:], r=[tag + "_xbc", "c_ident"], w=kb.pk(4))
        kb.tr(P5[:, 0:128], xbc[:, 4, :], ident[:], r=[tag + "_xbc", "c_ident"], w=kb.pk(5))
        kb.cp("act", x_tok[:], P4[:, :], r=kb.pk(4), w=[tag + "_xtok"])
        kb.cp("act", B_tok[:], P5[:, 0:128], r=kb.pk(5), w=[tag + "_Btok"])
        v3 = lambda t: t[:, :].rearrange("p (h d) -> p h d", h=8)
        bc8 = lambda a: a.unsqueeze(2).to_broadcast([128, 8, 64])
        kb.tt("dve", v3(xdt), v3(x_tok), bc8(dt), ALU.mult, r=[tag + "_xtok", tag + "_sm"], w=[tag + "_xdt"])
        kb.tt("pool", v3(xd), v3(x_tok), bc8(hp[:, 16:24]), ALU.mult, r=[tag + "_xtok", tag + "_hp"], w=[tag + "_xd"])
        kb.tt("pool", v3(xdte), v3(xdt), bc8(erem), ALU.mult, r=[tag + "_xdt", tag + "_sm"], w=[tag + "_xdte"])
        for g in range(2):
            rows = slice(g * 64, (g + 1) * 64)
            bank = P5 if g == 0 else P6
            kb.mm(bank[:, 128:256], xbc[rows, 4, :], xbc[rows, 5, :], r=[tag + "_xbc"], w=kb.pk(5 + g))
            kb.cp("act", CBT[g][:], bank[:, 128:256], r=kb.pk(5 + g), w=[tag + f"_CBT{g}"])
        if STOP <= 2:
            continue
        def head(h, s_):
            g = h // 4
            Gs, LT = GsL[s_], LTL[s_]
            gk_, lk_ = tag + f"_Gs{s_}", tag + f"_LT{s_}"
            bi = (2, 3, 5, 6)[s_]
            X, kx = kb.P[bi], kb.pk(bi)
            kb.ts("dve", Gs[:], C["tri_incl"][:], gg[:, h:h + 1], None, ALU.mult, r=["c_tri_incl", tag + "_sm"], w=[gk_])
            yield
            kb.mm(X[:, 256:384], C["tri_strict"][:], Gs[:], r=[gk_, "c_tri_strict"], w=kx)
            yield
            kb.act(LT[:], X[:, 256:384], AF.Exp, r=kx, w=[lk_])
            yield
            kb.tt("dve", LT[:], LT[:], C["tri_incl"][:], ALU.mult, r=[lk_, "c_tri_incl"], w=[lk_])
            yield
            kb.tt("dve", LT[:], LT[:], CBT[g][:], ALU.mult, r=[lk_, tag + f"_CBT{g}"], w=[lk_])
            yield
            kb.mm(P7[:, h * 64:(h + 1) * 64], LT[:], xdt[:, h * 64:(h + 1) * 64], r=[lk_, tag + "_xdt"], w=kb.pk(7))
            yield

        for grp in ((0, 1, 2, 3), (4, 5, 6, 7)):
            gens = [head(h, s_) for s_, h in enumerate(grp)]
            while gens:
                for gen in list(gens):
                    try:
                        next(gen)
                    except StopIteration:
                        gens.remove(gen)
        if STOP <= 3:
            continue
        for ch in range(2):
            tr_ = slice(ch * 64, (ch + 1) * 64)
            dch = dA if ch == 0 else dB
            kb.mm(P0[tr_, :], xbc[:, 5, tr_], Sbd[:, :], r=[tag + "_xbc", tag + "_Sbd"], w=kb.pk(0))
            kb.mm(P1[:, :], B_tok[tr_, :], xdte[tr_, :], r=[tag + "_Btok", tag + "_xdte"], w=kb.pk(1))
            for g in range(2):
                rr = slice(g * 64, (g + 1) * 64)
                cc = slice(g * 256, (g + 1) * 256)
                s3 = Sbd[rr, cc].rearrange("p (h d) -> p h d", h=4)
                kb.tt("dve", s3, s3, dch[rr, g * 4:(g + 1) * 4].unsqueeze(2).to_broadcast([64, 4, 64]), ALU.mult,
                      r=[tag + "_Sbd", tag + "_sm"], w=[tag + "_Sbd"])
                kb.tt("dve", Sbd[rr, cc], Sbd[rr, cc], P1[rr, cc], ALU.add, r=[tag + "_Sbd"] + kb.pk(1), w=[tag + "_Sbd"])
        kb.cp("act", yint[:], P0[:, :], r=kb.pk(0), w=[tag + "_yint"])
        kb.tt("pool", v3(yint), v3(yint), bc8(ecum), ALU.mult, r=[tag + "_yint", tag + "_sm"], w=[tag + "_yint"])
        kb.tt("dve", y_sb[:], P7[:, :], yint[:], ALU.add, r=kb.pk(7) + [tag + "_yint"], w=[tag + "_y"])
        kb.tt("pool", y_sb[:], y_sb[:], xd[:], ALU.add, r=[tag + "_y", tag + "_xd"], w=[tag + "_y"])
        kb.tt("pool", y_sb[:], y_sb[:], zs[:], ALU.mult, r=[tag + "_y", tag + "_zs"], w=[tag + "_y"])
        for g in range(2):
            kb.act(junk[:], y_sb[:, g * 256:(g + 1) * 256], AF.Square, accum_out=ssq[:, g:g + 1], r=[tag + "_y"],
                   w=[tag + "_junk", tag + "_ssq"])
        rms_rstd(kb, tag, rs, ssq, 2, 256)
        for g in range(2):
            gc = slice(g * 256, (g + 1) * 256)
            kb.stt("dve", y_sb[:, gc], y_sb[:, gc], rs[:, g:g + 1], gn[:, gc], ALU.mult, ALU.mult,
                   r=[tag + "_y", tag + "_rs", tag + "_gn"], w=[tag + "_y"])
        for c in range(4):
            kb.tr(P4[:, c * 128:(c + 1) * 128], y_sb[:, c * 128:(c + 1) * 128], ident[:], r=[tag + "_y", "c_ident"], w=kb.pk(4))
        kb.cp("act", oT[:], P4[:, :], r=kb.pk(4), w=[tag + "_oT"])
        kb.dma("sp", obr[i], oT[:], sem=tag + "_oT", r=[tag + "_oT"], w=[f"{tag}_obr{i}"])


def phase_merge(kb, l, hT, hkey_fn, obrs, obr_keys, xsrc, xsrc_key, xdst, xdst_key):
    C = kb.C
    tag = f"mrg{l}"
    wm = kb.sb(tag + "_wm", (128, KC, 3072), BF16)
    load_w_cast(kb, wm, tag + "_wm", kb.w_in[l], C_MERGE, 3072)
    wbr = []
    for b, nm in enumerate(("w_branch_gla", "w_branch_gdn", "w_branch_ssd")):
        t = kb.sb(tag + f"_wb{b}", (128, 4, D), BF16)
        load_w_cast(kb, t, tag + f"_wb{b}", kb.dins[nm][l], 0, D, nk=4)
        wbr.append(t)
    wo = kb.sb(tag + "_wo", (128, KC, D), BF16)
    load_w_cast(kb, wo, tag + "_wo", kb.dins["w_out"][l], 0, D)
    bmb = kb.sb(tag + "_bmb", (1, 3072), BF16)
    kb.dma("pool", bmb[:], kb.din(tag + "_bmd", (1, 3072)), sem=tag + "_bmb", w=[tag + "_bmb"])
    gm_row, gm_key = kb.gm_row[l]
    ob = [kb.sb(tag + f"_ob{b}", (128, 512), BF16) for b in range(3)]
    sig = kb.sb(tag + "_sig", (128, 512))
    acc = kb.sb(tag + "_acc", (128, 512))
    tmp = kb.sb(tag + "_tmp", (128, 512))
    mT = kb.sb(tag + "_mT", (128, KC, 128), BF16)
    xt = kb.sb(tag + "_xt", (128, D))
    xo = kb.sb(tag + "_xo", (128, D))
    P = kb.P
    for i in range(NT):
        t0 = i * 128
        hk = hkey_fn(i)
        for b in range(3):
            kb.dma("sp", ob[b][:], obrs[b][i], sem=tag + f"_ob{b}", r=[obr_keys[b](i)], w=[tag + f"_ob{b}"])
        kb.dma("sp", xt[:], xsrc[t0:t0 + 128, :], sem=tag + "_xt", r=[xsrc_key(i)], w=[tag + "_xt"])
        for half in range(2):
            for b in range(3):
                PG, PY = P[(b % 2) * 2], P[(b % 2) * 2 + 1]
                kg, ky = kb.pk((b % 2) * 2), kb.pk((b % 2) * 2 + 1)
                for jj in range(4):
                    j = half * 4 + jj
                    col = b * D + j * 128
                    zone = slice(jj * 128, (jj + 1) * 128)
                    for k in range(KC):
                        kb.mm(PG[:, zone], wm[:, k, col:col + 128], hT[:, k, t0:t0 + 128], start=(k == 0), stop=False,
                              r=[tag + "_wm", hk], w=kg)
                    kb.mm(PG[:, zone], bmb[0:1, col:col + 128], C["ones_bf"][0:1, :], start=False, stop=True,
                          r=[tag + "_bmb", "cb_ones"], w=kg)
                    for c in range(4):
                        kb.mm(PY[:, zone], wbr[b][:, c, j * 128:(j + 1) * 128], ob[b][:, c * 128:(c + 1) * 128],
                              start=(c == 0), stop=(c == 3), r=[tag + f"_wb{b}", tag + f"_ob{b}"], w=ky)
                kb.act(sig[:], PG[:, :], AF.Sigmoid, r=kg, w=[tag + "_sig"])
                if b == 0:
                    kb.tt("dve", acc[:], PY[:, :], sig[:], ALU.mult, r=ky + [tag + "_sig"], w=[tag + "_acc"])
                else:
                    kb.tt("dve", tmp[:], PY[:, :], sig[:], ALU.mult, r=ky + [tag + "_sig"], w=[tag + "_tmp"])
                    kb.tt("pool", acc[:], acc[:], tmp[:], ALU.add, r=[tag + "_acc", tag + "_tmp"], w=[tag + "_acc"])
            kb.cp("act", mT[:, half * 4:(half + 1) * 4, :], acc[:, :].rearrange("p (j t) -> p j t", j=4),
                  r=[tag + "_acc"], w=[tag + "_mT"])
        for half in range(2):
            PO, ko = P[4 + half], kb.pk(4 + half)
            for j in range(KC):
                kb.mm(PO[:, :], mT[:, j, :], wo[:, j, half * 512:(half + 1) * 512], start=(j == 0), stop=(j == KC - 1),
                      r=[tag + "_mT", tag + "_wo"], w=ko)
            hs = slice(half * 512, (half + 1) * 512)
            kb.tt("dve", xo[:, hs], PO[:, :], gm_row[:, hs], ALU.mult, r=ko + [gm_key], w=[tag + "_xo"])
            kb.tt("pool", xo[:, hs], xo[:, hs], xt[:, hs], ALU.add, r=[tag + "_xo", tag + "_xt"], w=[tag + "_xo"])
        kb.dma("sp", xdst[t0:t0 + 128, :], xo[:], sem=tag + "_xo", r=[tag + "_xo"], w=[xdst_key(i)])


def phase_moe(kb, l, xsrc, xsrc_key, xdst, xdst_key, final=None):
    import os
    C = kb.C
    tag = f"moe{l}"
    P = kb.P
    TS = 512
    NSUP = S_TOK // TS
    NE = int(os.environ.get("MOE_NE", 32))
    wr = kb.sb(tag + "_wr", (128, KC, 32), BF16)
    load_w_cast(kb, wr, tag + "_wr", kb.dins["w_router"][l], 0, 32)
    brb = kb.sb(tag + "_brb", (1, 32), BF16)
    kb.dma("pool", brb[:], kb.din(tag + "_brd", (1, 32)), sem=tag + "_brb", w=[tag + "_brb"])
    ones5 = kb.sb(tag + "_ones5", (1, 512), BF16)
    kb.S.op("dve", lambda e: e.memset(ones5[:], 1.0), [], [tag + "_ones5"])
    gf_row, gf_key = kb.gf_row[l]
    nb = norm_bufs(kb, tag + "_n")
    hTs = kb.sb(tag + "_hT", (128, KC, TS), BF16)
    G = kb.sb(tag + "_G", (128, 4, 32))
    lg = kb.sb(tag + "_lg", (128, 32))
    v8 = kb.sb(tag + "_v8", (128, 8))
    msk = kb.sb(tag + "_msk", (128, 32))
    sml = kb.sb(tag + "_sml", (128, 4))
    wgu = [kb.sb(tag + f"_wgu{i}", (128, KC, 2048), BF16) for i in range(2)]
    wd = [kb.sb(tag + f"_wd{i}", (128, KC, D), BF16) for i in range(2)]
    bgu = [kb.sb(tag + f"_bgu{i}", (1, 2048), BF16) for i in range(2)]
    bd = [kb.sb(tag + f"_bd{i}", (1, D), BF16) for i in range(2)]
    yacc = kb.sb(tag + "_yacc", (128, 4, D))
    actT = kb.sb(tag + "_actT", (128, KC, TS), BF16)
    g7 = kb.sb(tag + "_g7", (128, TS))
    sg = kb.sb(tag + "_sg", (128, TS))
    u7 = kb.sb(tag + "_u7", (128, TS))
    xt = kb.sb(tag + "_xt", (128, D))
    xo = kb.sb(tag + "_xo", (128, D))
    if final is not None:
        nfr = kb.sb(tag + "_nfr", (128, D))
        kb.dma("sp", nfr[:], final["nf"].partition_broadcast(128), sem=tag + "_nfr", w=[tag + "_nfr"])
        fj = kb.sb(tag + "_fj", (128, D), BF16)
        fs = kb.sb(tag + "_fs", (128, 2))
    w_gu_d, w_d_d = kb.dins["w_gate_up"][l], kb.dins["w_down"][l]
    b_gu_d, b_d_d = kb.dins["b_gate_up"][l], kb.dins["b_down"][l]

    def load_expert(e, slot):
        srcg = w_gu_d[e].rearrange("(k p) c -> p k c", p=128)
        srcd = w_d_d[e].rearrange("(k p) c -> p k c", p=128)
        for k in range(KC):
            kb.dma("pool", wgu[slot][:, k, :], srcg[:, k, :], sem=tag + f"_wgu{slot}", w=[tag + f"_wgu{slot}"])
        for k in range(KC):
            kb.dma("pool", wd[slot][:, k, :], srcd[:, k, :], sem=tag + f"_wd{slot}", w=[tag + f"_wd{slot}"])
        kb.dma("pool", bgu[slot][:], b_gu_d[e:e + 1, :], sem=tag + f"_bgu{slot}", w=[tag + f"_bgu{slot}"])
        kb.dma("pool", bd[slot][:], b_d_d[e:e + 1, :], sem=tag + f"_bd{slot}", w=[tag + f"_bd{slot}"])

    it = 0
    for T in range(int(os.environ.get("MOE_NSUP", NSUP))):
        for tt in range(4):
            i = T * 4 + tt
            norm_tile(kb, nb, l, "f", xsrc[i * 128:(i + 1) * 128, :], xsrc_key(i), hTs[:, :, tt * 128:(tt + 1) * 128], tag + "_hT")
        for tt in range(4):
            for k in range(KC):
                kb.mm(P[7][:, 0:32], hTs[:, k, tt * 128:(tt + 1) * 128], wr[:, k, :], start=(k == 0), stop=False,
                      r=[tag + "_hT", tag + "_wr"], w=kb.pk(7))
            kb.mm(P[7][:, 0:32], ones5[0:1, 0:128], brb[0:1, :], start=False, stop=True, r=[tag + "_ones5", tag + "_brb"], w=kb.pk(7))
            kb.cp("dve", lg[:], P[7][:, 0:32], r=kb.pk(7), w=[tag + "_lg"])
            kb.S.op("dve", lambda e: e.max(out=v8[:], in_=lg[:]), [tag + "_lg"], [tag + "_v8"])
            kb.ts("dve", msk[:], lg[:], v8[:, 3:4], None, ALU.is_ge, r=[tag + "_lg", tag + "_v8"], w=[tag + "_msk"])
            kb.ts("dve", sml[:, 0:1], v8[:, 0:1], -1.0, None, ALU.mult, r=[tag + "_v8"], w=[tag + "_sml"])
            kb.act(lg[:], lg[:], AF.Exp, bias=sml[:, 0:1], r=[tag + "_lg", tag + "_sml"], w=[tag + "_lg"])
            kb.tt("dve", lg[:], lg[:], msk[:], ALU.mult, r=[tag + "_lg", tag + "_msk"], w=[tag + "_lg"])
            kb.S.op("dve", lambda e: e.reduce_sum(sml[:, 1:2], lg[:], AX.X), [tag + "_lg"], [tag + "_sml"])
            kb.S.op("dve", lambda e: e.reciprocal(sml[:, 1:2], sml[:, 1:2]), [tag + "_sml"], [tag + "_sml"])
            kb.ts("dve", G[:, tt, :], lg[:], sml[:, 1:2], None, ALU.mult, r=[tag + "_lg", tag + "_sml"], w=[tag + "_G"])
        kb.S.op("pool", lambda e: e.memset(yacc[:], 0.0), [], [tag + "_yacc"])
        for e_ in range(NE):
            slot = it % 2
            if it == 0:
                load_expert(e_, slot)
            nxt = (e_ + 1) % NE
            if not (T == NSUP - 1 and e_ == NE - 1):
                load_expert(nxt, 1 - slot)
            it += 1
            wk, dk, bgk, bdk = tag + f"_wgu{slot}", tag + f"_wd{slot}", tag + f"_bgu{slot}", tag + f"_bd{slot}"
            for jc in range(KC):
                pb = (jc % 2) * 2
                PG, PU = P[pb], P[pb + 1]
                for which, PX in ((0, PG), (1, PU)):
                    cols = slice(jc * 256 + which, (jc + 1) * 256, 2)
                    for k in range(KC):
                        kb.mm(PX[:, :], wgu[slot][:, k, cols], hTs[:, k, :], start=(k == 0), stop=False,
                              r=[wk, tag + "_hT"], w=kb.pk(pb + which))
                    kb.mm(PX[:, :], bgu[slot][0:1, cols], ones5[0:1, :], start=False, stop=True,
                          r=[bgk, tag + "_ones5"], w=kb.pk(pb + which))
                kb.ts("dve", g7[:], PG[:, :], SW_LIMIT, None, ALU.min, r=kb.pk(pb), w=[tag + "_g7"])
                kb.ts("dve", u7[:], PU[:, :], -SW_LIMIT, SW_LIMIT, ALU.max, ALU.min, r=kb.pk(pb + 1), w=[tag + "_u7"])
                kb.act(sg[:], g7[:], AF.Sigmoid, scale=SW_ALPHA, r=[tag + "_g7"], w=[tag + "_sg"])
                kb.ts("pool", u7[:], u7[:], 1.0, None, ALU.add, r=[tag + "_u7"], w=[tag + "_u7"])
                kb.tt("pool", u7[:], u7[:], g7[:], ALU.mult, r=[tag + "_u7", tag + "_g7"], w=[tag + "_u7"])
                kb.tt("pool", actT[:, jc, :], u7[:], sg[:], ALU.mult, r=[tag + "_u7", tag + "_sg"], w=[tag + "_actT"])
            for tt in range(4):
                for half in range(2):
                    pi = 4 + (tt % 2) * 2 + half
                    PO = P[pi]
                    hs = slice(half * 512, (half + 1) * 512)
                    for jc in range(KC):
                        kb.mm(PO[:, :], actT[:, jc, tt * 128:(tt + 1) * 128], wd[slot][:, jc, hs], start=(jc == 0), stop=False,
                              r=[tag + "_actT", dk], w=kb.pk(pi))
                    kb.mm(PO[:, :], ones5[0:1, 0:128], bd[slot][0:1, hs], start=False, stop=True,
                          r=[tag + "_ones5", bdk], w=kb.pk(pi))
                    kb.stt("dve", yacc[:, tt, hs], PO[:, :], G[:, tt, e_:e_ + 1], yacc[:, tt, hs], ALU.mult, ALU.add,
                           r=kb.pk(pi) + [tag + "_G", tag + "_yacc"], w=[tag + "_yacc"])
        for tt in range(4):
            i = T * 4 + tt
            kb.dma("sp", xt[:], xsrc[i * 128:(i + 1) * 128, :], sem=tag + "_xt", r=[xsrc_key(i)], w=[tag + "_xt"])
            kb.tt("dve", xo[:], yacc[:, tt, :], gf_row[:], ALU.mult, r=[tag + "_yacc", gf_key], w=[tag + "_xo"])
            kb.tt("pool", xo[:], xo[:], xt[:], ALU.add, r=[tag + "_xo", tag + "_xt"], w=[tag + "_xo"])
            if final is None:
                kb.dma("sp", xdst[i * 128:(i + 1) * 128, :], xo[:], sem=tag + "_xo", r=[tag + "_xo"], w=[xdst_key(i)])
            else:
                kb.act(fj[:], xo[:], AF.Square, accum_out=fs[:, 0:1], r=[tag + "_xo"], w=[tag + "_fj", tag + "_fs"])
                kb.ts("dve", fs[:, 1:2], fs[:, 0:1], 1.0 / D, EPS, ALU.mult, ALU.add, r=[tag + "_fs"], w=[tag + "_fs"])
                kb.act(fs[:, 1:2], fs[:, 1:2], AF.Sqrt, r=[tag + "_fs"], w=[tag + "_fs"])
                kb.S.op("dve", lambda e: e.reciprocal(fs[:, 1:2], fs[:, 1:2]), [tag + "_fs"], [tag + "_fs"])
                kb.stt("dve", xo[:], xo[:], fs[:, 1:2], nfr[:], ALU.mult, ALU.mult, r=[tag + "_xo", tag + "_fs", tag + "_nfr"], w=[tag + "_xo"])
                kb.dma("sp", final["out"][i * 128:(i + 1) * 128, :], xo[:], sem=tag + "_xo", r=[tag + "_xo"], w=[f"out{i}"])


SW_LIMIT = 7.0
SW_ALPHA = 1.702


W_SHAPES = {
    "w_branch_gla": (DEPTH, 512, D), "w_branch_gdn": (DEPTH, 512, D), "w_branch_ssd": (DEPTH, 512, D),
    "w_out": (DEPTH, D, D), "w_router": (DEPTH, D, 32),
    "w_gate_up": (DEPTH, 32, D, 2 * D), "b_gate_up": (DEPTH, 32, 2 * D),
    "w_down": (DEPTH, 32, D, D), "b_down": (DEPTH, 32, D),
}


def build_program(layers=(0, 1), do_mix=True, do_moe=True, dbg=None, same_engine_sync=True):
    kb = KB(same_engine_sync=same_engine_sync)
    phase_consts(kb)
    x = kb.din("x", (S_TOK, D))
    kb.w_in = kb.din("w_in", (DEPTH, D, IN_COLS))
    kb.dins = {nm: kb.din(nm, shp) for nm, shp in W_SHAPES.items()}
    nf = kb.din("norm_final", (D,))
    out = kb.dout("out", (S_TOK, D))
    if do_moe and MOE_SORTED:
        kb.moe_xs = kb.dscr("moe_xs", (MOE_ROWS, D))
        zsrc = kb.din("zeros", (512, D))
        xs_v = kb.moe_xs.rearrange("(n r) c -> n r c", r=512)
        for n in range(MOE_ROWS // 512):
            kb.dma("pool", xs_v[n], zsrc, sem="moe_zt", w=["moe_xs"])
    phase_mod(kb)
    xin, xin_key = x, (lambda i: "x_in")
    last = layers[-1]
    for l in layers:
        xmid = kb.dscr(f"xmid{l}", (S_TOK, D), debug=(dbg == "xmid" and l == layers[0]))
        xmid_key = (lambda i, l=l: f"xmid{l}_{i}")
        if do_mix:
            obr = [kb.dscr(f"obr{l}_{b}", (NT, 128, 512), BF16) for b in range(3)]
            kb.push_scope()
            hT = kb.sb(f"hT{l}", (128, KC, S_TOK), BF16)
            hk = (lambda i, l=l: f"hT{l}_{i}")
            kb.push_scope(); phase_norm(kb, l, "m", xin, xin_key, hT, hk); kb.pop_scope()
            kb.push_scope(); phase_gla(kb, l, hT, hk, obr[0]); kb.pop_scope()
            kb.push_scope(); phase_gdn(kb, l, hT, hk, obr[1]); kb.pop_scope()
            kb.push_scope(); phase_ssd(kb, l, hT, hk, obr[2]); kb.pop_scope()
            keys = [(lambda i, l=l, t=t: f"{t}{l}_obr{i}") for t in ("gla", "gdn", "ssd")]
            kb.push_scope(); phase_merge(kb, l, hT, hk, obr, keys, xin, xin_key, xmid, xmid_key); kb.pop_scope()
            kb.pop_scope()
            msrc, msrc_key = xmid, xmid_key
        else:
            msrc, msrc_key = xin, xin_key
        if dbg == "xmid":
            kb.S.final_wait("sp", [xmid_key(i) for i in range(NT)])
            break
        if do_moe:
            xnext = kb.dscr(f"xres{l}", (S_TOK, D))
            xnext_key = (lambda i, l=l: f"xres{l}_{i}")
            kb.push_scope()
            moe_fn = phase_moe_sorted if MOE_SORTED else phase_moe
            moe_fn(kb, l, msrc, msrc_key, xnext, xnext_key, final=(dict(out=out, nf=nf) if l == last else None))
            kb.pop_scope()
            xin, xin_key = xnext, xnext_key
    kb.S.final_wait("sp", [f"out{i}" for i in range(NT)])
    kb.stats = kb.S.emit(kb.stack)
    return kb


def host_all(inputs, b, names):
    m = {}
    for nm in W_SHAPES:
        m[nm] = inputs[nm]
    m["norm_final"] = inputs["norm_final"]
    m["zeros"] = np.zeros((512, D), np.float32)
    for l in range(DEPTH):
        m[f"mrg{l}_bmd"] = inputs["b_merge"][l][None, :]
        m[f"moe{l}_brd"] = inputs["b_router"][l][None, :]
        m[f"moe{l}_nffn"] = inputs["norm_ffn"][l][None, :]
    base = host_inputs(inputs, b, [n for n in names if n not in m])
    for n in names:
        if n in m:
            base[n] = np.ascontiguousarray(m[n])
    return base


_PROG = {}


def kernel(**inputs):
    inputs = {k: np.asarray(v) for k, v in inputs.items()}
    if "kb" not in _PROG:
        _PROG["kb"] = build_program()
    kb = _PROG["kb"]
    names = list(kb.ins.keys())
    in_maps = [host_all(inputs, b, names) for b in range(8)]
    res = run_bass_kernel_spmd(kb.nc, in_maps, core_ids=list(range(8)))
    return np.stack([np.asarray(r["out"]) for r in res.results], axis=0).astype(np.float32)


MOE_SORTED = True
MOE_BLK = 512
MOE_NB = (S_TOK * 4) // MOE_BLK + 32
MOE_ROWS = MOE_NB * MOE_BLK


def phase_moe_sorted(kb, l, xsrc, xsrc_key, xdst, xdst_key, final=None):
    import os
    C = kb.C
    tag = f"moe{l}"
    P = kb.P
    BLK, NB = MOE_BLK, MOE_NB
    NTB = BLK // 128
    IOA = bass.IndirectOffsetOnAxis
    wr = kb.sb(tag + "_wr", (128, KC, 32), BF16)
    load_w_cast(kb, wr, tag + "_wr", kb.dins["w_router"][l], 0, 32)
    brb = kb.sb(tag + "_brb", (1, 32), BF16)
    kb.dma("pool", brb[:], kb.din(tag + "_brd", (1, 32)), sem=tag + "_brb", w=[tag + "_brb"])
    ones5 = kb.sb(tag + "_ones5", (1, 512), BF16)
    kb.S.op("dve", lambda e: e.memset(ones5[:], 1.0), [], [tag + "_ones5"])
    hTb = [kb.sb(tag + f"_hT{i}", (128, KC, BLK), BF16) for i in range(2)]
    lg_all = kb.sb(tag + "_lg", (128, NT, 32))
    msk_all = kb.sb(tag + "_msk", (128, NT, 32))
    R_all = kb.sb(tag + "_R", (128, NT, 32))
    v8_all = kb.sb(tag + "_v8", (128, NT, 8))
    gk_all = kb.sb(tag + "_gk", (128, NT, 4))
    sml = kb.sb(tag + "_sml", (128, 4))
    cnt = kb.sb(tag + "_cnt", (128, 32))
    kb.S.op("pool", lambda e: e.memset(cnt[:], 0.0), [], [tag + "_cnt"])
    padded = kb.sb(tag + "_padded", (128, 32))
    pstart = kb.sb(tag + "_pstart", (128, 32))
    pend = kb.sb(tag + "_pend", (128, 32))
    pcol = kb.sb(tag + "_pcol", (32, 1))
    pcb = kb.sb(tag + "_pcb", (32, 128))
    dg = kb.sb(tag + "_dg", (32, 32))
    posf = kb.sb(tag + "_posf", (128, NT, 4))
    posi = kb.sb(tag + "_posi", (128, NT, 4), I32)
    eb = kb.sb(tag + "_eb", (128, NB))
    offi = kb.sb(tag + "_offi", (128, NB, KC), I32)
    oh = kb.sb(tag + "_oh", (32, NB), BF16)
    kb.push_scope()
    gf_row, gf_key = kb.gf_row[l]
    scf = kb.sb(tag + "_scf", (128, D))
    shf = kb.sb(tag + "_shf", (128, D))
    nfrow = kb.sb(tag + "_nfrow", (128, D))
    kb.dma("sp", shf[:], kb.modrow_d[l][0], sem=tag + "_shf", r=[f"modrowd{l}"], w=[tag + "_shf"])
    kb.dma("sp", scf[:], kb.modrow_d[l][1], sem=tag + "_scf", r=[f"modrowd{l}"], w=[tag + "_scf"])
    kb.dma("sp", nfrow[:], kb.din(tag + "_nffn", (1, D))[0].partition_broadcast(128), sem=tag + "_nfrow", w=[tag + "_nfrow"])
    kb.stt("dve", scf[:], scf[:], 1.0, nfrow[:], ALU.add, ALU.mult, r=[tag + "_scf", tag + "_nfrow"], w=[tag + "_scf"])
    xs_d = kb.moe_xs
    ys_d = kb.dscr(f"moe_ys{l}", (MOE_ROWS, D))
    h2_d = kb.dscr(f"moe_h2{l}", (S_TOK, D))
    nb = norm_bufs(kb, tag + "_n")
    h2 = kb.sb(tag + "_h2", (128, D))
    eq = kb.sb(tag + "_eq", (128, NT, 32))
    cmp3 = kb.sb(tag + "_cmp3", (128, NB, 32))
    offf = kb.sb(tag + "_offf", (128, NB, KC))
    def p1_norm(i):
            hv = hTb[0][:, :, (i % 2) * 128:(i % 2 + 1) * 128]
            norm_tile(kb, nb, l, "f", xsrc[i * 128:(i + 1) * 128, :], xsrc_key(i), hv, tag + f"_hTp{i % 2}")
            b_ = (nb["n"] - 1) % 2
            xn, xnk = nb["xn"][b_], f"{tag}_n_xn{b_}"
            kb.tt("pool", h2[:], xn[:], scf[:], ALU.mult, r=[xnk, tag + "_scf"], w=[tag + "_h2"])
            kb.tt("pool", h2[:], h2[:], shf[:], ALU.add, r=[tag + "_h2", tag + "_shf"], w=[tag + "_h2"])
            kb.dma("sp", h2_d[i * 128:(i + 1) * 128, :], h2[:], sem=tag + "_h2", r=[tag + "_h2"], w=[f"{tag}_h2d{i}"])

    def p1_route(i):
            for k in range(KC):
                kb.mm(P[7][:, 0:32], hTb[0][:, k, (i % 2) * 128:(i % 2 + 1) * 128], wr[:, k, :], start=(k == 0), stop=False, r=[tag + f"_hTp{i % 2}", tag + "_wr"], w=kb.pk(7))
            kb.mm(P[7][:, 0:32], ones5[0:1, 0:128], brb[0:1, :], start=False, stop=True, r=[tag + "_ones5", tag + "_brb"], w=kb.pk(7))
            lg, v8, msk = lg_all[:, i, :], v8_all[:, i, :], msk_all[:, i, :]
            kb.cp("dve", lg, P[7][:, 0:32], r=kb.pk(7), w=[tag + "_lg"])
            kb.S.op("dve", lambda e, v8=v8, lg=lg: e.max(out=v8, in_=lg), [tag + "_lg"], [tag + "_v8"])
            kb.ts("dve", msk, lg, v8[:, 3:4], None, ALU.is_ge, r=[tag + "_lg", tag + "_v8"], w=[tag + "_msk"])
            kb.ts("dve", sml[:, 0:1], v8[:, 0:1], -1.0, None, ALU.mult, r=[tag + "_v8"], w=[tag + "_sml"])
            kb.act(gk_all[:, i, :], v8[:, 0:4], AF.Exp, bias=sml[:, 0:1], r=[tag + "_v8", tag + "_sml"], w=[tag + "_gk"])
            kb.S.op("dve", lambda e, i=i: e.reduce_sum(sml[:, 1:2], gk_all[:, i, :], AX.X), [tag + "_gk"], [tag + "_sml"])
            kb.S.op("dve", lambda e: e.reciprocal(sml[:, 1:2], sml[:, 1:2]), [tag + "_sml"], [tag + "_sml"])
            kb.ts("dve", gk_all[:, i, :], gk_all[:, i, :], sml[:, 1:2], None, ALU.mult, r=[tag + "_gk", tag + "_sml"], w=[tag + "_gk"])
            kb.mm(P[6][:, 0:32], C["tri_full"][:], msk, r=["c_tri_full", tag + "_msk"], w=kb.pk(6))
            kb.mm(P[6][:, 32:64], C["ones"][:], msk, r=["c_ones", tag + "_msk"], w=kb.pk(6))
            kb.tt("dve", R_all[:, i, :], P[6][:, 0:32], cnt[:], ALU.add, r=kb.pk(6) + [tag + "_cnt"], w=[tag + "_R"])
            kb.tt("dve", cnt[:], P[6][:, 32:64], cnt[:], ALU.add, r=kb.pk(6) + [tag + "_cnt"], w=[tag + "_cnt"])

    for i_ in range(NT + 1):
        if i_ < NT:
            p1_norm(i_)
        if i_ >= 1:
            p1_route(i_ - 1)
    kb.tt("dve", eq[:, 0:8, :].rearrange("p j e -> p e j"), cnt[:].unsqueeze(2).to_broadcast([128, 32, 8]),
          C["blk_thr"][:, 0:8].unsqueeze(1).to_broadcast([128, 32, 8]), ALU.is_gt, r=[tag + "_cnt", "c_blk_thr"], w=[tag + "_eq"])
    kb.S.op("dve", lambda e: e.reduce_sum(padded[:], eq[:, 0:8, :].rearrange("p j e -> p e j"), AX.X), [tag + "_eq"], [tag + "_padded"])
    kb.ts("dve", padded[:], padded[:], float(BLK), None, ALU.mult, r=[tag + "_padded"], w=[tag + "_padded"])
    kb.tt("dve", dg[:], padded[0:32, :], C["ident"][0:32, 0:32], ALU.mult, r=[tag + "_padded", "c_ident"], w=[tag + "_dg"])
    kb.S.op("dve", lambda e: e.reduce_sum(pcol[:], dg[:], AX.X), [tag + "_dg"], [tag + "_pcol"])
    kb.cp("dve", pcb[:], pcol[:, 0:1].to_broadcast([32, 128]), r=[tag + "_pcol"], w=[tag + "_pcb"])
    kb.mm(P[6][:, 0:32], pcb[:], C["tri_full"][0:32, 0:32], r=[tag + "_pcb", "c_tri_full"], w=kb.pk(6))
    kb.cp("dve", pstart[:], P[6][:, 0:32], r=kb.pk(6), w=[tag + "_pstart"])
    kb.tt("dve", pend[:], pstart[:], padded[:], ALU.add, r=[tag + "_pstart", tag + "_padded"], w=[tag + "_pend"])
    kb.tt("dve", R_all[:], R_all[:], pstart[:].unsqueeze(1).to_broadcast([128, NT, 32]), ALU.add,
          r=[tag + "_R", tag + "_pstart"], w=[tag + "_R"])
    for k in range(4):
        kb.tt("dve", eq[:], lg_all[:], v8_all[:, :, k:k + 1].to_broadcast([128, NT, 32]), ALU.is_equal,
              r=[tag + "_lg", tag + "_v8"], w=[tag + "_eq"])
        kb.tt("dve", eq[:], eq[:], R_all[:], ALU.mult, r=[tag + "_eq", tag + "_R"], w=[tag + "_eq"])
        kb.S.op("dve", lambda e, k=k: e.reduce_sum(posf[:, :, k], eq[:], AX.X), [tag + "_eq"], [tag + "_posf"])
    kb.cp("dve", posi[:], posf[:], r=[tag + "_posf"], w=[tag + "_posi"])
    kb.tt("dve", cmp3[:], pend[:].unsqueeze(1).to_broadcast([128, NB, 32]),
          C["blk_thr"][:, 0:NB].unsqueeze(2).to_broadcast([128, NB, 32]), ALU.is_le, r=[tag + "_pend", "c_blk_thr"], w=[tag + "_cmp3"])
    kb.S.op("dve", lambda e: e.reduce_sum(eb[:], cmp3[:], AX.X), [tag + "_cmp3"], [tag + "_eb"])
    kb.ts("dve", eb[:], eb[:], 31.0, None, ALU.min, r=[tag + "_eb"], w=[tag + "_eb"])
    kb.ts("dve", offf[:], eb[:].unsqueeze(2).to_broadcast([128, NB, KC]), float(D), float(l * 32 * D), ALU.mult, ALU.add,
          r=[tag + "_eb"], w=[tag + "_offf"])
    kb.tt("dve", offf[:], offf[:], C["base_pk"][:, 0:KC].unsqueeze(1).to_broadcast([128, NB, KC]), ALU.add,
          r=[tag + "_offf", "c_base_pk"], w=[tag + "_offf"])
    kb.cp("dve", offi[:], offf[:], r=[tag + "_offf"], w=[tag + "_offi"])
    kb.ts("dve", oh[:], eb[0:32, :], C["base_pk"][0:32, 0:1], None, ALU.is_equal, r=[tag + "_eb", "c_base_pk"], w=[tag + "_oh"])
    for i in range(NT):
        kb.dma("sp", h2[:], h2_d[i * 128:(i + 1) * 128, :], sem=tag + "_h2", r=[f"{tag}_h2d{i}"], w=[tag + "_h2"])
        for k in range(4):
            kb.S.dma("pool", lambda e, i=i, k=k: e.indirect_dma_start(
                out=xs_d, out_offset=IOA(ap=posi[:, i, k:k + 1], axis=0), in_=h2[:], in_offset=None),
                tag + "_h2", [tag + "_h2", tag + "_posi"], ["moe_xs"])
    kb.pop_scope()
    kb.push_scope()
    bgu_sb = kb.sb(tag + "_bgu", (32, 2048), BF16)
    bd_sb = kb.sb(tag + "_bd", (32, D), BF16)
    kb.dma("pool", bgu_sb[:], kb.dins["b_gate_up"][l], sem=tag + "_bgu", w=[tag + "_bgu"])
    kb.dma("pool", bd_sb[:], kb.dins["b_down"][l], sem=tag + "_bd", w=[tag + "_bd"])
    wgu = [kb.sb(tag + f"_wgu{i}", (128, KC, 2048), BF16) for i in range(2)]
    wd = [kb.sb(tag + f"_wd{i}", (128, KC, D), BF16) for i in range(2)]
    ohbs = [kb.sb(tag + f"_ohb{i}", (32, BLK), BF16) for i in range(2)]
    xr = [kb.sb(tag + f"_xr{i}", (128, D)) for i in range(2)]
    actT = kb.sb(tag + "_actT", (128, KC, BLK), BF16)
    g7 = kb.sb(tag + "_g7", (128, BLK))
    sg = kb.sb(tag + "_sg", (128, BLK))
    u7 = kb.sb(tag + "_u7", (128, BLK))
    yb = [kb.sb(tag + f"_yb{i}", (128, D)) for i in range(2)]
    wgu_flat = kb.dins["w_gate_up"].rearrange("l e r c -> (l e r) c")
    wd_flat = kb.dins["w_down"].rearrange("l e r c -> (l e r) c")

    def load_block_w(b, slot):
        for k in range(KC):
            kb.S.dma("pool", lambda e, b=b, k=k, slot=slot: e.indirect_dma_start(
                out=wgu[slot][:, k, :], out_offset=None, in_=wgu_flat, in_offset=IOA(ap=offi[:, b, k:k + 1], axis=0)),
                tag + f"_wgu{slot}", [tag + "_offi"], [tag + f"_wgu{slot}"])
        for k in range(KC):
            kb.S.dma("pool", lambda e, b=b, k=k, slot=slot: e.indirect_dma_start(
                out=wd[slot][:, k, :], out_offset=None, in_=wd_flat, in_offset=IOA(ap=offi[:, b, k:k + 1], axis=0)),
                tag + f"_wd{slot}", [tag + "_offi"], [tag + f"_wd{slot}"])

    def prep_block(b):
        hTs, hkey, ohb_ = hTb[b % 2], tag + f"_hT{b % 2}", ohbs[b % 2]
        for tt in range(NTB):
            xb = xr[nxc[0] % 2]
            xbk = tag + f"_xr{nxc[0] % 2}"
            nxc[0] += 1
            r0 = b * BLK + tt * 128
            kb.dma("sp", xb[:], xs_d[r0:r0 + 128, :], sem=xbk, r=["moe_xs"], w=[xbk])
            for half in range(2):
                pT, pk = P[half], kb.pk(half)
                for kk in range(4):
                    k = half * 4 + kk
                    kb.tr(pT[:, kk * 128:(kk + 1) * 128], xb[:, k * 128:(k + 1) * 128], C["ident"][:], r=[xbk, "c_ident"], w=pk)
                kb.cp("act" if half == 0 else "dve", hTs[:, half * 4:(half + 1) * 4, tt * 128:(tt + 1) * 128],
                      pT[:, :].rearrange("p (k t) -> p k t", k=4), r=pk, w=[hkey])
        kb.cp("dve", ohb_[:], oh[:, b:b + 1].to_broadcast([32, BLK]), r=[tag + "_oh"], w=[tag + f"_ohb{b % 2}"])

    NBR = int(os.environ.get("MOE_NBLK", NB))
    load_block_w(0, 0)
    nxc = [0]
    prep_block(0)
    for b in range(NBR):
        slot = b % 2
        if b + 1 < NBR:
            load_block_w(b + 1, 1 - slot)
        wk, dk = tag + f"_wgu{slot}", tag + f"_wd{slot}"
        hTs, hkey, ohb = hTb[slot], tag + f"_hT{slot}", ohbs[slot]
        ohk = tag + f"_ohb{slot}"
        for jc in range(KC):
            pb = 2 + (jc % 2) * 2
            PG, PU = P[pb], P[pb + 1]
            for which, PX in ((0, PG), (1, PU)):
                cols = slice(jc * 256 + which, (jc + 1) * 256, 2)
                for k in range(KC):
                    kb.mm(PX[:, :], wgu[slot][:, k, cols], hTs[:, k, :], start=(k == 0), stop=False,
                          r=[wk, hkey], w=kb.pk(pb + which))
                kb.mm(PX[:, :], bgu_sb[:, cols], ohb[:, :], start=False, stop=True, r=[tag + "_bgu", ohk], w=kb.pk(pb + which))
            kb.ts("dve", g7[:], PG[:, :], SW_LIMIT, None, ALU.min, r=kb.pk(pb), w=[tag + "_g7"])
            kb.ts("dve", u7[:], PU[:, :], -SW_LIMIT, SW_LIMIT, ALU.max, ALU.min, r=kb.pk(pb + 1), w=[tag + "_u7"])
            kb.act(sg[:], g7[:], AF.Sigmoid, scale=SW_ALPHA, r=[tag + "_g7"], w=[tag + "_sg"])
            kb.stt("dve", u7[:], u7[:], 1.0, g7[:], ALU.add, ALU.mult, r=[tag + "_u7", tag + "_g7"], w=[tag + "_u7"])
            kb.tt("dve", actT[:, jc, :], u7[:], sg[:], ALU.mult, r=[tag + "_u7", tag + "_sg"], w=[tag + "_actT"])
        if b + 1 < NBR:
            prep_block(b + 1)
        for tt in range(NTB):
            ybt, ybk = yb[tt % 2], tag + f"_yb{tt % 2}"
            for half in range(2):
                pi = 6 + half
                PO = P[pi]
                hs = slice(half * 512, (half + 1) * 512)
                for jc in range(KC):
                    kb.mm(PO[:, :], actT[:, jc, tt * 128:(tt + 1) * 128], wd[slot][:, jc, hs], start=(jc == 0), stop=False,
                          r=[tag + "_actT", dk], w=kb.pk(pi))
                kb.mm(PO[:, :], ohb[:, 0:128], bd_sb[:, hs], start=False, stop=True, r=[ohk, tag + "_bd"], w=kb.pk(pi))
                kb.cp("act" if half == 0 else "dve", ybt[:, hs], PO[:, :], r=kb.pk(pi), w=[ybk])
            r0 = b * BLK + tt * 128
            kb.dma("sp", ys_d[r0:r0 + 128, :], ybt[:], sem=ybk, r=[ybk], w=[tag + "_ys"])
    kb.pop_scope()
    kb.push_scope()
    yk = [kb.sb(tag + f"_yk{i}", (128, D)) for i in range(2)]
    acc = kb.sb(tag + "_acc", (128, D))
    xt = kb.sb(tag + "_xt", (128, D))
    if final is not None:
        nfr = kb.sb(tag + "_nfr", (128, D))
        kb.dma("sp", nfr[:], final["nf"].partition_broadcast(128), sem=tag + "_nfr", w=[tag + "_nfr"])
        fj = kb.sb(tag + "_fj", (128, D), BF16)
        fs = kb.sb(tag + "_fs", (128, 2))
    ng = 0
    for i in range(NT):
        kb.dma("sp", xt[:], xsrc[i * 128:(i + 1) * 128, :], sem=tag + "_xt", r=[xsrc_key(i)], w=[tag + "_xt"])
        for k in range(4):
            yt, ytk = yk[ng % 2], tag + f"_yk{ng % 2}"
            ng += 1
            kb.S.dma("pool", lambda e, i=i, k=k, yt=yt: e.indirect_dma_start(
                out=yt[:], out_offset=None, in_=ys_d, in_offset=IOA(ap=posi[:, i, k:k + 1], axis=0)),
                ytk, [tag + "_ys", tag + "_posi"], [ytk])
            if k == 0:
                kb.ts("dve", acc[:], yt[:], gk_all[:, i, k:k + 1], None, ALU.mult, r=[ytk, tag + "_gk"], w=[tag + "_acc"])
            else:
                kb.stt("dve", acc[:], yt[:], gk_all[:, i, k:k + 1], acc[:], ALU.mult, ALU.add, r=[ytk, tag + "_gk", tag + "_acc"], w=[tag + "_acc"])
        kb.tt("pool", acc[:], acc[:], gf_row[:], ALU.mult, r=[tag + "_acc", gf_key], w=[tag + "_acc"])
        kb.tt("pool", acc[:], acc[:], xt[:], ALU.add, r=[tag + "_acc", tag + "_xt"], w=[tag + "_acc"])
        if final is None:
            kb.dma("sp", xdst[i * 128:(i + 1) * 128, :], acc[:], sem=tag + "_acc", r=[tag + "_acc"], w=[xdst_key(i)])
        else:
            kb.act(fj[:], acc[:], AF.Square, accum_out=fs[:, 0:1], r=[tag + "_acc"], w=[tag + "_fj", tag + "_fs"])
            kb.ts("dve", fs[:, 1:2], fs[:, 0:1], 1.0 / D, EPS, ALU.mult, ALU.add, r=[tag + "_fs"], w=[tag + "_fs"])
            kb.act(fs[:, 1:2], fs[:, 1:2], AF.Sqrt, r=[tag + "_fs"], w=[tag + "_fs"])
            kb.S.op("dve", lambda e: e.reciprocal(fs[:, 1:2], fs[:, 1:2]), [tag + "_fs"], [tag + "_fs"])
            kb.stt("dve", acc[:], acc[:], fs[:, 1:2], nfr[:], ALU.mult, ALU.mult, r=[tag + "_acc", tag + "_fs", tag + "_nfr"], w=[tag + "_acc"])
            kb.dma("sp", final["out"][i * 128:(i + 1) * 128, :], acc[:], sem=tag + "_acc", r=[tag + "_acc"], w=[f"out{i}"])
    kb.pop_scope()
```

```python
import numpy as np
from contextlib import ExitStack
from concourse.bass_utils import run_bass_kernel_spmd

import concourse.bass as bass
import concourse.mybir as mybir

ENGINES = ("pe", "act", "dve", "pool", "sp")


class Op:
    __slots__ = ("eng", "fn", "deps", "is_dma", "dsem", "dcount", "signal", "idx", "signo")

    def __init__(self, eng, fn):
        self.eng = eng
        self.fn = fn
        self.deps = []
        self.is_dma = False
        self.dsem = None
        self.dcount = 0
        self.signal = False
        self.idx = -1
        self.signo = 0


class Sched:
    def __init__(self, nc, same_engine_sync=True):
        self.nc = nc
        self.q = {e: [] for e in ENGINES}
        self.res_w = {}
        self.res_r = {}
        self.phys = []
        self.key2phys = {}
        self.free_phys = []
        self.same_engine_sync = same_engine_sync

    def _collect(self, op, reads, writes, my_dma_key=None):
        deps = []
        for k in reads:
            t = self.res_w.get(k)
            if t is not None:
                deps.append(t)
        for k in writes:
            t = self.res_w.get(k)
            if t is not None:
                if not (my_dma_key is not None and t[0] == 'dma' and t[1] == my_dma_key):
                    deps.append(t)
            deps.extend(self.res_r.get(k, ()))
        op.deps = deps

    def _commit(self, tok, reads, writes):
        for k in reads:
            self.res_r.setdefault(k, []).append(tok)
        for k in writes:
            self.res_w[k] = tok
            self.res_r[k] = []

    @staticmethod
    def _excl(reads, writes):
        rp = [k for k in reads if len(k) == 2 and k[0] == "P" and k[1].isdigit()]
        if not rp:
            return reads, writes
        return [k for k in reads if k not in rp], list(writes) + [k for k in rp if k not in writes]

    def op(self, eng, fn, reads=(), writes=()):
        reads, writes = self._excl(reads, writes)
        o = Op(eng, fn)
        self._collect(o, reads, writes)
        o.idx = len(self.q[eng])
        self.q[eng].append(o)
        self._commit(('op', o), reads, writes)
        return o

    def dma(self, eng, fn, sem_key, reads=(), writes=()):
        reads, writes = self._excl(reads, writes)
        if sem_key not in self.key2phys:
            if self.free_phys:
                p = self.free_phys.pop()
            else:
                p = len(self.phys)
                self.phys.append(0)
            self.key2phys[sem_key] = p
        p = self.key2phys[sem_key]
        o = Op(eng, fn)
        o.is_dma = True
        self._collect(o, reads, writes, my_dma_key=p)
        self.phys[p] += 1
        c = self.phys[p]
        o.dsem = p
        o.dcount = c
        o.idx = len(self.q[eng])
        self.q[eng].append(o)
        self._commit(('dma', p, c), reads, writes)
        return o

    def final_wait(self, eng, keys):
        o = Op(eng, None)
        deps = []
        for k in keys:
            t = self.res_w.get(k)
            if t is not None:
                deps.append(t)
            deps.extend(self.res_r.get(k, ()))
        o.deps = deps
        o.idx = len(self.q[eng])
        self.q[eng].append(o)

    def barrier(self):
        toks = []
        for e in ENGINES:
            for o in reversed(self.q[e]):
                if o.fn is not None and not o.is_dma:
                    toks.append(('op', o))
                    break
        for p, c in enumerate(self.phys):
            if c:
                toks.append(('dma', p, c))
        for e in ENGINES:
            o = Op(e, None)
            o.deps = list(toks)
            o.idx = len(self.q[e])
            self.q[e].append(o)
        self.free_phys = list(range(len(self.phys)))[::-1]
        self.key2phys = {}

    def emit(self, stack):
        nc = self.nc
        for e in ENGINES:
            for o in self.q[e]:
                for t in o.deps:
                    if t[0] == 'op':
                        tgt = t[1]
                        if tgt.eng == o.eng and (not self.same_engine_sync or o.eng == 'pe'):
                            continue
                        tgt.signal = True
        for e in ENGINES:
            n = 0
            for o in self.q[e]:
                if o.signal:
                    n += 1
                    o.signo = n
        esem = {e: stack.enter_context(nc.semaphore("s_" + e)) for e in ENGINES}
        dsem = {}
        for p in range(len(self.phys)):
            dsem[p] = stack.enter_context(nc.semaphore(f"d_{p}"))
        block = stack.enter_context(nc.Block())
        stats = {}

        def run(e, engobj):
            waited = {}
            nwait = 0
            for o in self.q[e]:
                need = {}
                for t in o.deps:
                    if t[0] == 'op':
                        tgt = t[1]
                        if tgt.eng == e and (not self.same_engine_sync or e == 'pe'):
                            continue
                        key = ('e', tgt.eng)
                        val = tgt.signo
                    else:
                        key = ('d', t[1])
                        val = t[2] * 16
                    if need.get(key, 0) < val:
                        need[key] = val
                for key, val in need.items():
                    if waited.get(key, 0) >= val:
                        continue
                    waited[key] = val
                    sem = esem[key[1]] if key[0] == 'e' else dsem[key[1]]
                    engobj.wait_ge(sem, val)
                    nwait += 1
                if o.fn is None:
                    continue
                ins = o.fn(engobj)
                if o.is_dma:
                    ins.then_inc(dsem[o.dsem], 16)
                elif o.signal:
                    ins.then_inc(esem[e], 1)
            stats[e] = (len(self.q[e]), nwait)

        @block.tensor
        def _(eng):
            run("pe", eng)

        @block.scalar
        def _(eng):
            run("act", eng)

        @block.vector
        def _(eng):
            run("dve", eng)

        @block.gpsimd
        def _(eng):
            run("pool", eng)

        @block.sync
        def _(eng):
            run("sp", eng)

        return stats


F32 = mybir.dt.float32
BF16 = mybir.dt.bfloat16
I32 = mybir.dt.int32
AF = mybir.ActivationFunctionType
ALU = mybir.AluOpType
AX = mybir.AxisListType

S_TOK = 4096
D = 1024
KC = 8
NT = S_TOK // 128
DEPTH = 2
EPS = 1e-6
IN_COLS = 7968
C_GLA_Q, C_GLA_K, C_GLA_V, C_GLA_LR, C_GLA_R = 0, 256, 512, 1024, 1040
C_GDN_QKV, C_GDN_A, C_GDN_B, C_GDN_G = 1552, 3088, 3092, 3096
C_SSD_Z, C_SSD_XBC, C_SSD_DT, C_MERGE = 3608, 4120, 4888, 4896


class KB:
    def __init__(self, same_engine_sync=True):
        self.nc = bass.Bass("TRN2", target_bir_lowering=False)
        self.S = Sched(self.nc, same_engine_sync=same_engine_sync)
        self.stack = ExitStack()
        self.ins = {}
        self.outs = {}
        self._n = 0
        self.scopes = []
        self._allow_p = False
        self.P = [self.stack.enter_context(self.nc.psum_tensor(f"PB{i}", [128, 512], F32)) for i in range(8)]

    @staticmethod
    def pk(i, a=0, b=512):
        return [f"P{i}"]

    def din(self, name, shape, dt=F32):
        t = self.nc.dram_tensor(name, list(shape), dt, kind="ExternalInput")
        self.ins[name] = t
        return t.ap()

    def dout(self, name, shape, dt=F32):
        t = self.nc.dram_tensor(name, list(shape), dt, kind="ExternalOutput")
        self.outs[name] = t
        return t.ap()

    def dscr(self, name, shape, dt=F32, debug=False):
        if debug:
            return self.dout(name, shape, dt)
        return self.nc.dram_tensor(name, list(shape), dt, kind="Internal").ap()

    def sb(self, name, shape, dt=F32):
        st = self.scopes[-1] if self.scopes else self.stack
        return st.enter_context(self.nc.sbuf_tensor(name, list(shape), dt))

    def sbp(self, name, shape, dt=F32):
        assert not self.scopes or self._allow_p
        return self.stack.enter_context(self.nc.sbuf_tensor(name, list(shape), dt))

    def push_scope(self):
        self.scopes.append(ExitStack())

    def pop_scope(self):
        self.S.barrier()
        self.scopes.pop().close()

    def ps(self, name, shape=(128, 512), dt=F32):
        return self.stack.enter_context(self.nc.psum_tensor(name, list(shape), dt))

    def mm(self, out, lhsT, rhs, start=True, stop=True, r=(), w=()):
        return self.S.op("pe", lambda e: e.matmul(out, lhsT, rhs, start=start, stop=stop), r, w)

    def tr(self, out, in_, ident, r=(), w=()):
        return self.S.op("pe", lambda e: e.transpose(out, in_, ident), r, w)

    def act(self, out, in_, func, bias=None, scale=None, accum_out=None, r=(), w=(), eng="act"):
        kw = {}
        if bias is not None:
            kw["bias"] = bias
        if scale is not None:
            kw["scale"] = scale
        if accum_out is not None:
            kw["accum_out"] = accum_out
        return self.S.op(eng, lambda e: e.activation(out, in_, func, **kw), r, w)

    def ts(self, eng, out, in0, s1, s2, op0, op1=None, accum_out=None, r=(), w=()):
        kw = {}
        if op1 is not None:
            kw["op1"] = op1
        if accum_out is not None:
            kw["accum_out"] = accum_out
        return self.S.op(eng, lambda e: e.tensor_scalar(out, in0, s1, s2, op0, **kw), r, w)

    def tt(self, eng, out, in0, in1, op, r=(), w=()):
        return self.S.op(eng, lambda e: e.tensor_tensor(out, in0, in1, op), r, w)

    def stt(self, eng, out, in0, scalar, in1, op0, op1, r=(), w=()):
        return self.S.op(eng, lambda e: e.scalar_tensor_tensor(out, in0, scalar, in1, op0, op1), r, w)

    def cp(self, eng, out, in_, r=(), w=()):
        if eng == "act":
            return self.S.op(eng, lambda e: e.copy(out, in_), r, w)
        return self.S.op(eng, lambda e: e.tensor_copy(out, in_), r, w)

    def dma(self, eng, out, in_, sem, r=(), w=(), **kw):
        return self.S.dma(eng, lambda e: e.dma_start(out, in_, **kw), sem, r, w)


def phase_consts(kb):
    c = {}
    cdefs = {
        "ident": (128, 128), "tri_incl": (128, 128), "tri_strict": (128, 128), "ones": (128, 128),
        "blk": (128, 128), "selA": (128, 128), "selB": (128, 128), "neg_strict": (128, 128),
        "tri_full": (128, 128), "blk_thr": (128, 64), "base_pk": (128, 8),
    }
    for name, shp in cdefs.items():
        src = kb.din("c_" + name, shp)
        t = kb.sb("cs_" + name, shp)
        kb.dma("sp", t[:], src, sem="c_" + name, w=["c_" + name])
        c[name] = t
        tb = kb.sb("cb_" + name, shp, BF16)
        kb.cp("dve", tb[:], t[:], r=["c_" + name], w=["cb_" + name])
        c[name + "_bf"] = tb
    kb.C = c


def phase_mod(kb):
    nc = kb.nc
    cT = kb.din("cT", (128, KC))
    w_mod = kb.din("w_mod", (DEPTH, D, 6 * D))
    bmodc = kb.din("bmodc", (DEPTH, 128, 48))
    bmodrow = kb.din("bmodrow", (DEPTH, 6, D))
    nmixc = kb.din("nmixc", (DEPTH, 128, KC))
    nffnc = kb.din("nffnc", (DEPTH, 128, KC))
    pers = {}
    for l in range(DEPTH):
        pers[f"modc{l}"] = kb.sbp(f"modc{l}", (128, 48))
        pers[f"modscl{l}"] = kb.sbp(f"modscl{l}", (128, 2, KC))
        for piece in (2, 5):
            pers[f"modrow{l}_{piece}"] = kb.sbp(f"modrow{l}_{piece}", (128, D))
    kb.modrow_d = [[kb.dscr(f"modrowd{l}_{j}", (128, D)) for j in range(2)] for l in range(DEPTH)]
    kb.push_scope()
    rowtmp = kb.sb("modrowtmp", (128, D))
    cact = kb.sb("cact", (128, KC))
    crep = kb.sb("crep", (128, KC, 128))
    kb.dma("sp", cact[:], cT, sem="cact", w=["cact"])
    kb.act(cact[:], cact[:], AF.Silu, r=["cact"], w=["cact"])
    for k in range(KC):
        kb.cp("dve", crep[:, k, :], cact[:, k:k + 1].to_broadcast([128, 128]), r=["cact"], w=["crep"])
    wbuf = [kb.sb(f"modw{i}", (128, KC, 1024)) for i in range(2)]
    pcol = kb.P[0]
    prow = [kb.P[1], kb.P[2]]
    kb.modc, kb.gm_row, kb.gf_row = [], [], []
    kb.sclm, kb.shm, kb.sclf, kb.shf = [], [], [], []
    it = 0
    for l in range(DEPTH):
        modc = pers[f"modc{l}"]
        bc = kb.sb(f"bmodc{l}", (128, 48))
        nm = kb.sb(f"nmixc{l}", (128, KC))
        nf = kb.sb(f"nffnc{l}", (128, KC))
        kb.dma("sp", bc[:], bmodc[l], sem=f"bmodc{l}", w=[f"bmodc{l}"])
        kb.dma("sp", nm[:], nmixc[l], sem=f"nmixc{l}", w=[f"nmixc{l}"])
        kb.dma("sp", nf[:], nffnc[l], sem=f"nffnc{l}", w=[f"nffnc{l}"])
        rows = []
        for piece in range(6):
            wb = wbuf[it % 2]
            wk = f"modw{it % 2}"
            it += 1
            src = w_mod[l, :, piece * 1024:(piece + 1) * 1024].rearrange("(k p) c -> p k c", p=128)
            for hh in range(2):
                kb.dma("sp", wb[:, hh * 4:(hh + 1) * 4, :], src[:, hh * 4:(hh + 1) * 4, :],
                       sem=wk, w=[wk])
            for jj in range(8):
                j = piece * 8 + jj
                for k in range(KC):
                    kb.mm(pcol[:, j:j + 1], wb[:, k, jj * 128:(jj + 1) * 128], cact[:, k:k + 1],
                          start=(k == 0), stop=(k == KC - 1), r=[wk, "cact"], w=kb.pk(0, 0, 128))
            if piece in (2, 3, 4, 5):
                row = pers[f"modrow{l}_{piece}"] if piece in (2, 5) else rowtmp
                if piece in (3, 4):
                    pers_key = f"modrow{l}_{piece}"
                kb.dma("sp", row[:], bmodrow[l, piece].partition_broadcast(128),
                       sem=f"modrow{l}_{piece}", w=[f"modrow{l}_{piece}"])
                for hh in range(2):
                    for k in range(KC):
                        kb.mm(prow[hh][:, :], crep[:, k, :], wb[:, k, hh * 512:(hh + 1) * 512],
                              start=(k == 0), stop=(k == KC - 1), r=[wk, "crep"], w=kb.pk(1 + hh))
                    kb.tt("dve", row[:, hh * 512:(hh + 1) * 512], prow[hh][:, :], row[:, hh * 512:(hh + 1) * 512],
                          ALU.add, r=kb.pk(1 + hh) + [f"modrow{l}_{piece}"], w=[f"modrow{l}_{piece}"])
                if piece in (2, 5):
                    rows.append((row, f"modrow{l}_{piece}"))
                else:
                    kb.dma("sp", kb.modrow_d[l][piece - 3], row[:], sem=f"modrow{l}_{piece}", r=[f"modrow{l}_{piece}"],
                           w=[f"modrowd{l}"])
        kb.tt("dve", modc[:], pcol[:, 0:48], bc[:], ALU.add, r=kb.pk(0, 0, 128) + [f"bmodc{l}"], w=[f"modc{l}"])
        scl = pers[f"modscl{l}"]
        kb.stt("dve", scl[:, 0, :], modc[:, 8:16], 1.0, nm[:], ALU.add, ALU.mult,
               r=[f"modc{l}", f"nmixc{l}"], w=[f"modc{l}"])
        kb.stt("dve", scl[:, 1, :], modc[:, 32:40], 1.0, nf[:], ALU.add, ALU.mult,
               r=[f"modc{l}", f"nffnc{l}"], w=[f"modc{l}"])
        kb.modc.append(modc)
        kb.gm_row.append(rows[0])
        kb.gf_row.append(rows[1])
        kb.sclm.append(scl[:, 0, :])
        kb.shm.append(modc[:, 0:8])
        kb.sclf.append(scl[:, 1, :])
        kb.shf.append(modc[:, 24:32])
    kb.pop_scope()


def norm_bufs(kb, tag):
    NB = 2
    b = {
        "tag": tag,
        "xt": [kb.sb(f"{tag}_x{i}", (128, D)) for i in range(NB)],
        "xn": [kb.sb(f"{tag}_xn{i}", (128, D)) for i in range(NB)],
        "junk": kb.sb(f"{tag}_junk", (128, D), BF16),
        "ss": kb.sb(f"{tag}_ss", (128, 2)),
        "rstd": kb.sb(f"{tag}_rstd", (128, 2)),
        "n": 0,
    }
    return b


def norm_front(kb, nb, xsrc_ap, xsrc_key):
    tag = nb["tag"]
    b = nb["n"] % 2
    nb["n"] += 1
    xt, xn, junk, ss, rstd = nb["xt"][b], nb["xn"][b], nb["junk"], nb["ss"], nb["rstd"]
    xk, xnk = f"{tag}_x{b}", f"{tag}_xn{b}"
    sk, rk = f"{tag}_ss{b}", f"{tag}_rstd{b}"
    kb.dma("sp", xt[:], xsrc_ap, sem=xk, r=[xsrc_key], w=[xk])
    kb.act(junk[:], xt[:], AF.Square, accum_out=ss[:, b:b + 1], r=[xk], w=[f"{tag}_junk", sk])
    kb.ts("dve", rstd[:, b:b + 1], ss[:, b:b + 1], 1.0 / D, EPS, ALU.mult, ALU.add, r=[sk], w=[rk])
    kb.act(rstd[:, b:b + 1], rstd[:, b:b + 1], AF.Sqrt, r=[rk], w=[rk])
    kb.S.op("dve", lambda e: e.reciprocal(rstd[:, b:b + 1], rstd[:, b:b + 1]), [rk], [rk])
    kb.ts("pool", xn[:], xt[:], rstd[:, b:b + 1], None, ALU.mult, r=[xk, rk], w=[xnk])
    return b


def norm_back(kb, nb, b, l, which, dst, dst_key):
    C = kb.C
    tag = nb["tag"]
    scl = kb.sclm[l] if which == "m" else kb.sclf[l]
    sh = kb.shm[l] if which == "m" else kb.shf[l]
    mkey = f"modc{l}"
    xn, xnk = nb["xn"][b], f"{tag}_xn{b}"
    for half in range(2):
        pT = kb.P[half]
        pk = kb.pk(half)
        for kk in range(4):
            k = half * 4 + kk
            kb.tr(pT[:, kk * 128:(kk + 1) * 128], xn[:, k * 128:(k + 1) * 128], C["ident"][:], r=[xnk, "c_ident"], w=pk)
        for kk in range(4):
            k = half * 4 + kk
            d = dst[:, k, :]
            src = pT[:, kk * 128:(kk + 1) * 128]
            if kk % 2 == 0:
                kb.act(d, src, AF.Identity, bias=sh[:, k:k + 1], scale=scl[:, k:k + 1], r=pk + [mkey], w=[dst_key])
            else:
                kb.ts("dve", d, src, scl[:, k:k + 1], sh[:, k:k + 1], ALU.mult, ALU.add, r=pk + [mkey], w=[dst_key])


def norm_tile(kb, nb, l, which, xsrc_ap, xsrc_key, dst, dst_key):
    b = norm_front(kb, nb, xsrc_ap, xsrc_key)
    norm_back(kb, nb, b, l, which, dst, dst_key)


def phase_norm(kb, l, which, xsrc, xsrc_key, hT, hT_key):
    nb = norm_bufs(kb, f"n{l}{which}")
    cur = norm_front(kb, nb, xsrc[0:128, :], xsrc_key(0))
    for i in range(NT):
        nxt = norm_front(kb, nb, xsrc[(i + 1) * 128:(i + 2) * 128, :], xsrc_key(i + 1)) if i + 1 < NT else None
        norm_back(kb, nb, cur, l, which, hT[:, :, i * 128:(i + 1) * 128], hT_key(i))
        cur = nxt


def _consts():
    i = np.arange(128)
    same = (i[:, None] // 64) == (i[None, :] // 64)
    return {
        "c_ident": np.eye(128, dtype=np.float32),
        "c_tri_incl": ((i[:, None] <= i[None, :]) & same).astype(np.float32),
        "c_tri_strict": ((i[:, None] > i[None, :]) & same).astype(np.float32),
        "c_ones": np.ones((128, 128), np.float32),
        "c_blk": same.astype(np.float32),
        "c_selA": np.repeat((i < 64).astype(np.float32)[:, None], 128, 1),
        "c_selB": np.repeat((i >= 64).astype(np.float32)[:, None], 128, 1),
        "c_neg_strict": -((i[:, None] > i[None, :]) & same).astype(np.float32),
        "c_tri_full": (i[:, None] < i[None, :]).astype(np.float32),
        "c_blk_thr": np.repeat((np.arange(64, dtype=np.float32) * 512.0)[None, :], 128, 0),
        "c_base_pk": (i[:, None] + 128 * np.arange(8)[None, :]).astype(np.float32),
    }


def host_inputs(inp, b, names):
    m = {}
    m.update(_consts())
    m["x"] = np.ascontiguousarray(inp["x"][b])
    m["cT"] = np.ascontiguousarray(inp["c"][b].reshape(KC, 128).T)
    m["w_mod"] = inp["w_mod"]
    m["bmodc"] = np.ascontiguousarray(inp["b_mod"].reshape(DEPTH, 48, 128).transpose(0, 2, 1))
    bm = inp["b_mod"].reshape(DEPTH, 6, D)
    m["bmodrow"] = np.ascontiguousarray(bm)
    m["nmixc"] = np.ascontiguousarray(inp["norm_mix"].reshape(DEPTH, KC, 128).transpose(0, 2, 1))
    m["nffnc"] = np.ascontiguousarray(inp["norm_ffn"].reshape(DEPTH, KC, 128).transpose(0, 2, 1))
    m.update(host_inputs2(inp, b, names))
    return {k: np.ascontiguousarray(m[k], dtype=m[k].dtype) for k in names}


def proj_feat(kb, out_ps, w_sb, wkey, c0, ncols, hT, hkey, t0, nt, wkeys=None):
    for k in range(KC):
        kb.mm(out_ps, w_sb[:, k, c0:c0 + ncols], hT[:, k, t0:t0 + nt], start=(k == 0), stop=(k == KC - 1),
              r=[wkey, hkey], w=wkeys)


def proj_tok(kb, out_ps, w_sb, wkey, c0, ncols, hT, hkey, t0, nt, wkeys=None):
    for k in range(KC):
        kb.mm(out_ps, hT[:, k, t0:t0 + nt], w_sb[:, k, c0:c0 + ncols], start=(k == 0), stop=(k == KC - 1),
              r=[wkey, hkey], w=wkeys)


def load_w_cast(kb, dst, dkey, src_dram_2d, c0, ncols, nk=KC, step=512):
    src = src_dram_2d.rearrange("(k p) c -> p k c", p=128)
    for k in range(nk):
        kb.dma("pool", dst[:, k, 0:ncols], src[:, k, c0:c0 + ncols], sem=dkey, w=[dkey])


def rms_rstd(kb, tag, rs, ssq, n, width):
    kb.ts("dve", rs[:, 0:n], ssq[:, 0:n], 1.0 / width, EPS, ALU.mult, ALU.add, r=[tag + "_ssq"], w=[tag + "_rs"])
    kb.act(rs[:, 0:n], rs[:, 0:n], AF.Sqrt, r=[tag + "_rs"], w=[tag + "_rs"])
    kb.S.op("dve", lambda e: e.reciprocal(rs[:, 0:n], rs[:, 0:n]), [tag + "_rs"], [tag + "_rs"])


def phase_gla(kb, l, hT, hkey_fn, obr):
    C = kb.C
    tag = f"gla{l}"
    w_in = kb.w_in
    NW = 1552
    wg = kb.sb(tag + "_w", (128, KC, NW), BF16)
    load_w_cast(kb, wg, tag + "_w", w_in[l], 0, NW)
    w2 = kb.sb(tag + "_w2", (16, 256))
    b2 = kb.sb(tag + "_b2", (1, 256))
    gn = kb.sb(tag + "_gn", (128, 512))
    kb.dma("sp", w2[:], kb.din(tag + "_w2d", (16, 256)), sem=tag + "_w2", w=[tag + "_w2"])
    kb.dma("sp", b2[:], kb.din(tag + "_b2d", (1, 256)), sem=tag + "_b2", w=[tag + "_b2"])
    kb.dma("sp", gn[:], kb.din(tag + "_gnd", (1, 512))[0].partition_broadcast(128), sem=tag + "_gn", w=[tag + "_gn"])
    lrT = kb.sb(tag + "_lrT", (16, 128))
    sp = kb.sb(tag + "_sp", (128, 256))
    e_rem = kb.sb(tag + "_erem", (128, 256))
    e_pos = kb.sb(tag + "_epos", (128, 256))
    e_neg = kb.sb(tag + "_eneg", (128, 256))
    qdT = kb.sb(tag + "_qdT", (128, 256), BF16)
    knT = kb.sb(tag + "_knT", (128, 256), BF16)
    krem = kb.sb(tag + "_krem", (128, 256), BF16)
    v_sb = kb.sb(tag + "_v", (128, 512), BF16)
    r_sb = kb.sb(tag + "_r", (128, 512))
    attT = [kb.sb(tag + f"_attT{i}", (128, 128), BF16) for i in range(4)]
    S = [kb.sb(tag + f"_S{i}", (128, 256)) for i in range(2)]
    Sb = [kb.sb(tag + f"_Sb{i}", (128, 256), BF16) for i in range(2)]
    junk = kb.sb(tag + "_junk", (128, 128), BF16)
    ssq = kb.sb(tag + "_ssq", (128, 4))
    rs = kb.sb(tag + "_rs", (128, 4))
    og = kb.sb(tag + "_og", (128, 512))
    oint = kb.sb(tag + "_oint", (128, 512))
    o_sb = kb.sb(tag + "_o", (128, 512))
    oT = kb.sb(tag + "_oT", (128, 512), BF16)
    for p in range(2):
        kb.S.op("dve", lambda e, p=p: e.memset(S[p][:], 0.0), [], [tag + f"_S{p}"])
        kb.S.op("dve", lambda e, p=p: e.memset(Sb[p][:], 0.0), [], [tag + f"_Sb{p}"])
    P0, P1, P2, P3, P4, P5, P6, P7 = kb.P
    wk = tag + "_w"
    import os
    STOP = float(os.environ.get('GLA_STOP', '9'))
    for i in range(int(os.environ.get('GLA_NT', NT))):
        t0 = i * 128
        hk = hkey_fn(i)
        for pair in range(2):
            proj_feat(kb, P0[:, pair * 128:(pair + 1) * 128], wg, wk, C_GLA_Q + pair * 128, 128, hT, hk, t0, 128, kb.pk(0, pair * 128, pair * 128 + 128))
            proj_feat(kb, P0[:, 256 + pair * 128:256 + (pair + 1) * 128], wg, wk, C_GLA_K + pair * 128, 128, hT, hk, t0, 128, kb.pk(0, 256 + pair * 128, 384 + pair * 128))
        proj_feat(kb, P1[0:16, 0:128], wg, wk, C_GLA_LR, 16, hT, hk, t0, 128, kb.pk(1, 0, 128))
        proj_tok(kb, P2[:, 0:256], wg, wk, C_GLA_K, 256, hT, hk, t0, 128, kb.pk(2, 0, 256))
        proj_tok(kb, P3[:, :], wg, wk, C_GLA_V, 512, hT, hk, t0, 128, kb.pk(3))
        proj_tok(kb, P4[:, :], wg, wk, C_GLA_R, 512, hT, hk, t0, 128, kb.pk(4))
        if STOP <= 1:
            continue
        kb.cp("dve", lrT[:, :], P1[0:16, 0:128], r=kb.pk(1, 0, 128), w=[tag + "_lrT"])
        kb.mm(P1[:, 128:384], lrT[:, :], w2[:, :], start=True, stop=False, r=[tag + "_lrT", tag + "_w2"], w=kb.pk(1, 128, 384))
        kb.mm(P1[:, 128:384], C["ones"][0:1, :], b2[0:1, :], start=False, stop=True, r=["c_ones", tag + "_b2"], w=kb.pk(1, 128, 384))
        kb.act(sp[:], P1[:, 128:384], AF.Exp, scale=-1.0, r=kb.pk(1, 128, 384), w=[tag + "_sp"])
        kb.act(sp[:], sp[:], AF.Ln, bias=1.0, r=[tag + "_sp"], w=[tag + "_sp"])
        if STOP <= 2:
            continue
        kb.cp("act", v_sb[:], P3[:, :], r=kb.pk(3), w=[tag + "_v"])
        kb.act(r_sb[:], P4[:, :], AF.Silu, r=kb.pk(4), w=[tag + "_r"])
        kb.mm(P5[:, 256:512], C["tri_strict"][:], sp[:], r=["c_tri_strict", tag + "_sp"], w=kb.pk(5, 256, 512))
        for pair in range(2):
            kb.mm(P6[:, pair * 128:(pair + 1) * 128], sp[:, pair * 128:(pair + 1) * 128], C["tri_incl"][:],
                  r=["c_tri_incl", tag + "_sp"], w=kb.pk(6, 0, 256))
        kb.act(e_rem[:], P5[:, 256:512], AF.Exp, scale=-1.0 / 16, r=kb.pk(5, 256, 512), w=[tag + "_erem"])
        kb.act(e_pos[:], P6[:, 0:256], AF.Exp, scale=-1.0 / 16, r=kb.pk(6, 0, 256), w=[tag + "_epos"])
        kb.act(e_neg[:], P6[:, 0:256], AF.Exp, scale=1.0 / 16, r=kb.pk(6, 0, 256), w=[tag + "_eneg"])
        kb.stt("dve", qdT[:], P0[:, 0:256], 0.125, e_pos[:], ALU.mult, ALU.mult, r=kb.pk(0, 0, 256) + [tag + "_epos"], w=[tag + "_qdT"])
        kb.tt("dve", knT[:], P0[:, 256:512], e_neg[:], ALU.mult, r=kb.pk(0, 256, 512) + [tag + "_eneg"], w=[tag + "_knT"])
        kb.tt("dve", krem[:], P2[:, 0:256], e_rem[:], ALU.mult, r=kb.pk(2, 0, 256) + [tag + "_erem"], w=[tag + "_krem"])
        if STOP <= 3:
            continue
        zones = [(P1[:, 384:512], kb.pk(1)), (P2[:, 256:384], kb.pk(2)), (P3[:, 0:128], kb.pk(3)), (P4[:, 0:128], kb.pk(4))]
        for h in range(4):
            pair, rows = h // 2, (h % 2) * 64
            pc = slice(pair * 128, (pair + 1) * 128)
            aps, apk = zones[h]
            kb.mm(aps, knT[rows:rows + 64, pc], qdT[rows:rows + 64, pc], r=[tag + "_knT", tag + "_qdT"], w=apk)
        for h in range(4):
            aps, apk = zones[h]
            kb.tt("dve", attT[h][:], aps, C["tri_incl"][:], ALU.mult, r=apk + ["c_tri_incl"], w=[tag + f"_attT{h}"])
        for h in range(4):
            hc = slice(h * 128, (h + 1) * 128)
            kb.mm(P7[:, hc], attT[h][:], v_sb[:, hc], start=True, stop=True, r=[tag + f"_attT{h}", tag + "_v"], w=kb.pk(7))

        def pairchain(pair):
            pc0 = pair * 128
            Sk, Sbk = tag + f"_S{pair}", tag + f"_Sb{pair}"
            PU, ku = (P6, kb.pk(6)) if pair == 0 else (P3, kb.pk(3))
            for ch in range(2):
                tr_ = slice(ch * 64, (ch + 1) * 64)
                kb.mm(P5[tr_, pair * 256:(pair + 1) * 256], qdT[:, pc0 + ch * 64:pc0 + (ch + 1) * 64],
                      Sb[pair][:, :], start=True, stop=True, r=[tag + "_qdT", Sbk], w=kb.pk(5))
                kb.mm(PU[:, 256:512], krem[tr_, pc0:pc0 + 128], v_sb[tr_, pair * 256:(pair + 1) * 256],
                      r=[tag + "_krem", tag + "_v"], w=ku)
                yield
                for hh in range(2):
                    rr = slice(hh * 64, (hh + 1) * 64)
                    cc = slice(hh * 128, (hh + 1) * 128)
                    dec = e_pos[rr, pc0 + ch * 64 + 63:pc0 + ch * 64 + 64]
                    kb.stt("dve", S[pair][rr, cc], S[pair][rr, cc], dec, PU[rr, 256 + hh * 128:256 + (hh + 1) * 128],
                           ALU.mult, ALU.add, r=[Sk, tag + "_epos"] + ku, w=[Sk])
                yield
                kb.cp("act", Sb[pair][:], S[pair][:], r=[Sk], w=[Sbk])
                yield

        gens = [pairchain(0), pairchain(1)]
        while gens:
            for gen in list(gens):
                try:
                    next(gen)
                except StopIteration:
                    gens.remove(gen)
        if STOP <= 5:
            continue
        kb.cp("act", oint[:], P5[:, :], r=kb.pk(5), w=[tag + "_oint"])
        kb.tt("dve", o_sb[:], P7[:, :], oint[:], ALU.add, r=kb.pk(7) + [tag + "_oint"], w=[tag + "_o"])
        for h in range(4):
            kb.act(junk[:], o_sb[:, h * 128:(h + 1) * 128], AF.Square, accum_out=ssq[:, h:h + 1], r=[tag + "_o"],
                   w=[tag + "_junk", tag + "_ssq"])
        rms_rstd(kb, tag, rs, ssq, 4, 128)
        for h in range(4):
            hc = slice(h * 128, (h + 1) * 128)
            kb.stt("dve", og[:, hc], o_sb[:, hc], rs[:, h:h + 1], gn[:, hc], ALU.mult, ALU.mult,
                   r=[tag + "_o", tag + "_rs", tag + "_gn"], w=[tag + "_og"])
        kb.tt("pool", og[:], og[:], r_sb[:], ALU.mult, r=[tag + "_og", tag + "_r"], w=[tag + "_og"])
        for c in range(4):
            kb.tr(P0[:, c * 128:(c + 1) * 128], og[:, c * 128:(c + 1) * 128], C["ident"][:], r=[tag + "_og", "c_ident"], w=kb.pk(0, c * 128, c * 128 + 128))
        kb.cp("act", oT[:], P0[:, :], r=kb.pk(0), w=[tag + "_oT"])
        kb.dma("sp", obr[i], oT[:], sem=tag + "_oT", r=[tag + "_oT"], w=[f"{tag}_obr{i}"])


def host_inputs2(inp, b, names):
    m = {}
    f = np.float32
    for l in range(DEPTH):
        m[f"gla{l}_w2d"] = inp["gla_w_gate2"][l]
        m[f"gla{l}_b2d"] = inp["gla_b_gate2"][l][None, :]
        m[f"gla{l}_gnd"] = np.tile(inp["gla_norm"][l], 4)[None, :]
    m["w_in"] = inp["w_in"]
    for l in range(DEPTH):
        cw = inp["ssd_conv_w"][l].reshape(4, 6, 128).transpose(2, 1, 0)
        cb = inp["ssd_conv_b"][l].reshape(6, 128).T[:, :, None]
        m[f"ssd{l}_cwd"] = np.concatenate([cw, cb], axis=2)
        m[f"ssd{l}_gnd"] = inp["ssd_norm"][l][None, :]
        m[f"ssd{l}_hpd"] = np.concatenate([inp["ssd_dt_bias"][l], inp["ssd_a_log"][l], inp["ssd_d"][l]])[None, :]
    for l in range(DEPTH):
        m[f"gdn{l}_cwd"] = inp["gdn_conv_w"][l].reshape(4, 12, 128).transpose(2, 1, 0)
        m[f"gdn{l}_gnd"] = np.tile(inp["gdn_norm"][l], 4)[None, :]
        m[f"gdn{l}_hpd"] = np.concatenate([inp["gdn_dt_bias"][l], inp["gdn_a_log"][l]])[None, :]
    return m


def phase_gdn(kb, l, hT, hkey_fn, obr):
    import os
    C = kb.C
    tag = f"gdn{l}"
    w_in = kb.w_in
    NW = 2056
    wk = tag + "_w"
    wg = kb.sb(wk, (128, KC, NW), BF16)
    load_w_cast(kb, wg, wk, w_in[l], C_GDN_QKV, NW)
    O_QKV, O_AB, O_G = 0, 1536, 1544
    convw = kb.sb(tag + "_cw", (128, 12, 4))
    kb.dma("sp", convw[:], kb.din(tag + "_cwd", (128, 12, 4)), sem=tag + "_cw", w=[tag + "_cw"])
    gn = kb.sb(tag + "_gn", (128, 512))
    kb.dma("sp", gn[:], kb.din(tag + "_gnd", (1, 512))[0].partition_broadcast(128), sem=tag + "_gn", w=[tag + "_gn"])
    hp = kb.sb(tag + "_hp", (128, 8))
    kb.dma("sp", hp[:], kb.din(tag + "_hpd", (1, 8))[0].partition_broadcast(128), sem=tag + "_hp", w=[tag + "_hp"])
    negA = kb.sb(tag + "_negA", (128, 4))
    kb.act(negA[:], hp[:, 4:8], AF.Exp, r=[tag + "_hp"], w=[tag + "_negA"])
    kb.ts("dve", negA[:], negA[:], -1.0, None, ALU.mult, r=[tag + "_negA"], w=[tag + "_negA"])
    ubuf = kb.sb(tag + "_ubuf", (128, 12, 131))
    kb.S.op("pool", lambda e: e.memset(ubuf[:], 0.0), [], [tag + "_ubuf"])
    cacc = kb.sb(tag + "_cacc", (128, 12, 128))
    ctmp = kb.sb(tag + "_ctmp", (128, 12, 128))
    qkv = kb.sb(tag + "_qkv", (128, 12, 128))
    sq = kb.sb(tag + "_sq", (128, 8, 128))
    rinv = kb.sb(tag + "_rinv", (128, 8, 128))
    qkn = kb.sb(tag + "_qkn", (128, 8, 128))
    gsb = kb.sb(tag + "_gsb", (128, 512))
    sm = kb.sb(tag + "_sm", (128, 40))
    beta, gg, cum, ecum, erem, bec = sm[:, 0:4], sm[:, 4:8], sm[:, 8:12], sm[:, 12:16], sm[:, 16:20], sm[:, 20:24]
    dA, dB, ytmp = sm[:, 24:28], sm[:, 28:32], sm[:, 32:36]
    HB = []
    for s_ in range(4):
        d_ = {}
        for nm in ("otmp", "Gs", "E", "ET", "B0", "B1", "C0", "C1", "PT0", "PT1", "PmT", "ecb", "qdT", "V0", "W0", "kdec", "upre", "wT", "u"):
            d_[nm] = kb.sb(tag + f"_{nm}_{s_}", (128, 128))
        kb.S.op("pool", lambda e, t=d_["u"]: e.memset(t[:], 0.0), [], [tag + f"_u_{s_}"])
        HB.append(d_)
    M = [kb.sb(tag + f"_M{h}", (128, 128)) for h in range(4)]
    for h in range(4):
        kb.S.op("pool", lambda e, h=h: e.memset(M[h][:], 0.0), [], [tag + f"_M{h}"])
    oint = kb.sb(tag + "_oint", (128, 512))
    o_sb = kb.sb(tag + "_o", (128, 512))
    og = kb.sb(tag + "_og", (128, 512))
    oT = kb.sb(tag + "_oT", (128, 512), BF16)
    junk = kb.sb(tag + "_junk", (128, 128), BF16)
    ssq = kb.sb(tag + "_ssq", (128, 4))
    rs = kb.sb(tag + "_rs", (128, 4))
    P0, P1, P2, P3, P4, P5, P6, P7 = kb.P
    ident = C["ident"]
    STOP = float(os.environ.get('GDN_STOP', '9'))
    for i in range(int(os.environ.get('GDN_NT', NT))):
        t0 = i * 128
        hk = hkey_fn(i)
        for c in range(12):
            bank = kb.P[c // 4]
            cc = (c % 4) * 128
            proj_feat(kb, bank[:, cc:cc + 128], wg, wk, O_QKV + c * 128, 128, hT, hk, t0, 128, kb.pk(c // 4, cc, cc + 128))
        proj_tok(kb, P3[:, :], wg, wk, O_G, 512, hT, hk, t0, 128, kb.pk(3))
        proj_tok(kb, P4[:, 0:8], wg, wk, O_AB, 8, hT, hk, t0, 128, kb.pk(4, 0, 128))
        kb.act(gsb[:], P3[:, :], AF.Silu, r=kb.pk(3), w=[tag + "_gsb"])
        for b3 in range(3):
            kb.cp("act", ubuf[:, b3 * 4:(b3 + 1) * 4, 3:131], kb.P[b3][:, :].rearrange("p (c t) -> p c t", c=4),
                  r=kb.pk(b3), w=[tag + "_ubuf"])
        for j in range(4):
            wj = convw[:, :, j:j + 1].to_broadcast([128, 12, 128])
            if j == 0:
                kb.tt("dve", cacc[:], ubuf[:, :, 0:128], wj, ALU.mult, r=[tag + "_ubuf", tag + "_cw"], w=[tag + "_cacc"])
            else:
                kb.tt("pool", ctmp[:], ubuf[:, :, j:j + 128], wj, ALU.mult, r=[tag + "_ubuf", tag + "_cw"], w=[tag + "_ctmp"])
                kb.tt("dve", cacc[:], cacc[:], ctmp[:], ALU.add, r=[tag + "_cacc", tag + "_ctmp"], w=[tag + "_cacc"])
        kb.act(qkv[:], cacc[:], AF.Silu, r=[tag + "_cacc"], w=[tag + "_qkv"])
        kb.cp("pool", ubuf[:, :, 0:3], ubuf[:, :, 128:131], r=[tag + "_ubuf"], w=[tag + "_ubuf"])
        if STOP <= 1:
            continue
        kb.tt("pool", sq[:], qkv[:, 0:8, :], qkv[:, 0:8, :], ALU.mult, r=[tag + "_qkv"], w=[tag + "_sq"])
        for half in range(2):
            kb.mm(kb.P[half][:, :], C["ones"][:], sq[:, half * 4:(half + 1) * 4, :], r=["c_ones", tag + "_sq"], w=kb.pk(half))
            kb.ts("dve", rinv[:, half * 4:(half + 1) * 4, :], kb.P[half][:, :].rearrange("p (c t) -> p c t", c=4),
                  1e-6, None, ALU.add, r=kb.pk(half), w=[tag + "_rinv"])
        kb.act(rinv[:], rinv[:], AF.Sqrt, r=[tag + "_rinv"], w=[tag + "_rinv"])
        kb.S.op("dve", lambda e: e.reciprocal(rinv[:], rinv[:]), [tag + "_rinv"], [tag + "_rinv"])
        kb.stt("dve", qkn[:, 0:4, :], qkv[:, 0:4, :], 128.0 ** -0.5, rinv[:, 0:4, :], ALU.mult, ALU.mult,
               r=[tag + "_qkv", tag + "_rinv"], w=[tag + "_qkn"])
        kb.tt("pool", qkn[:, 4:8, :], qkv[:, 4:8, :], rinv[:, 4:8, :], ALU.mult, r=[tag + "_qkv", tag + "_rinv"], w=[tag + "_qkn"])
        kb.act(beta, P4[:, 4:8], AF.Sigmoid, r=kb.pk(4, 0, 128), w=[tag + "_sm"])
        kb.tt("dve", ytmp, P4[:, 0:4], hp[:, 0:4], ALU.add, r=kb.pk(4, 0, 128) + [tag + "_hp"], w=[tag + "_sm"])
        kb.act(ytmp, ytmp, AF.Exp, r=[tag + "_sm"], w=[tag + "_sm"])
        kb.act(ytmp, ytmp, AF.Ln, bias=1.0, r=[tag + "_sm"], w=[tag + "_sm"])
        kb.tt("dve", gg, ytmp, negA[:], ALU.mult, r=[tag + "_sm", tag + "_negA"], w=[tag + "_sm"])
        kb.mm(P4[:, 8:12], C["tri_incl"][:], gg, r=["c_tri_incl", tag + "_sm"], w=kb.pk(4, 0, 128))
        kb.mm(P4[:, 12:16], C["blk"][:], gg, r=["c_blk", tag + "_sm"], w=kb.pk(4, 0, 128))
        kb.mm(P4[:, 16:20], C["selA"][:], gg, r=["c_selA", tag + "_sm"], w=kb.pk(4, 0, 128))
        kb.mm(P4[:, 20:24], C["selB"][:], gg, r=["c_selB", tag + "_sm"], w=kb.pk(4, 0, 128))
        kb.cp("dve", cum, P4[:, 8:12], r=kb.pk(4, 0, 128), w=[tag + "_sm"])
        kb.act(ecum, P4[:, 8:12], AF.Exp, r=kb.pk(4, 0, 128), w=[tag + "_sm"])
        kb.tt("dve", erem, P4[:, 12:16], cum, ALU.subtract, r=kb.pk(4, 0, 128) + [tag + "_sm"], w=[tag + "_sm"])
        kb.act(erem, erem, AF.Exp, r=[tag + "_sm"], w=[tag + "_sm"])
        kb.act(dA, P4[:, 16:20], AF.Exp, r=kb.pk(4, 0, 128), w=[tag + "_sm"])
        kb.act(dB, P4[:, 20:24], AF.Exp, r=kb.pk(4, 0, 128), w=[tag + "_sm"])
        kb.tt("dve", bec, beta, ecum, ALU.mult, r=[tag + "_sm"], w=[tag + "_sm"])
        if STOP <= 2:
            continue
        def head(h, s_):
            hb = HB[s_]
            XA, XB = kb.P[2 * s_], kb.P[2 * s_ + 1]
            ka, kbk = kb.pk(2 * s_), kb.pk(2 * s_ + 1)
            K = lambda nm: tag + f"_{nm}_{s_}"
            Gs, E, ET, PmT, ecb, qdT = hb["Gs"], hb["E"], hb["ET"], hb["PmT"], hb["ecb"], hb["qdT"]
            V0, W0, kdec, upre, wT, u_sb, otmp = hb["V0"], hb["W0"], hb["kdec"], hb["upre"], hb["wT"], hb["u"], hb["otmp"]
            Bm, Cm, PT = [hb["B0"], hb["B1"]], [hb["C0"], hb["C1"]], [hb["PT0"], hb["PT1"]]
            qT = qkn[:, h, :]
            kT = qkn[:, 4 + h, :]
            vT = qkv[:, 8 + h, :]
            hc = slice(h * 128, (h + 1) * 128)
            Z = lambda i: slice(i * 128, (i + 1) * 128)
            kb.ts("dve", Gs[:], C["tri_incl"][:], gg[:, h:h + 1], None, ALU.mult, r=["c_tri_incl", tag + "_sm"], w=[K("Gs")])
            kb.mm(XA[:, Z(0)], kT, kT, r=[tag + "_qkn"], w=ka)
            kb.mm(XA[:, Z(1)], kT, qT, r=[tag + "_qkn"], w=ka)
            yield
            kb.mm(XA[:, Z(2)], Gs[:], C["tri_strict"][:], r=[K("Gs"), "c_tri_strict"], w=ka)
            kb.mm(XA[:, Z(3)], C["tri_strict"][:], Gs[:], r=[K("Gs"), "c_tri_strict"], w=ka)
            kb.mm(XB[:, Z(0)], C["ones"][:], Gs[:], r=[K("Gs"), "c_ones"], w=kbk)
            yield
            kb.act(E[:], XA[:, Z(2)], AF.Exp, r=ka, w=[K("E")])
            kb.act(ET[:], XA[:, Z(3)], AF.Exp, r=ka, w=[K("ET")])
            kb.act(ecb[:], XB[:, Z(0)], AF.Exp, r=kbk, w=[K("ecb")])
            yield
            kb.tt("dve", E[:], E[:], C["neg_strict"][:], ALU.mult, r=[K("E"), "c_neg_strict"], w=[K("E")])
            kb.tt("dve", ET[:], ET[:], C["tri_incl"][:], ALU.mult, r=[K("ET"), "c_tri_incl"], w=[K("ET")])
            kb.tt("pool", qdT[:], qT, ecb[:], ALU.mult, r=[tag + "_qkn", K("ecb")], w=[K("qdT")])
            yield
            kb.stt("dve", Bm[0][:], XA[:, Z(0)], beta[:, h:h + 1], E[:], ALU.mult, ALU.mult,
                   r=ka + [tag + "_sm", K("E")], w=[K("B0")])
            kb.tt("dve", PmT[:], XA[:, Z(1)], ET[:], ALU.mult, r=ka + [K("ET")], w=[K("PmT")])
            yield
            kb.tr(XB[:, Z(1)], Bm[0][:], ident[:], r=[K("B0"), "c_ident"], w=kbk)
            kb.tr(XB[:, Z(2)], vT, ident[:], r=[tag + "_qkv", "c_ident"], w=kbk)
            kb.tr(XB[:, Z(3)], kT, ident[:], r=[tag + "_qkn", "c_ident"], w=kbk)
            yield
            kb.cp("act", Cm[0][:], XB[:, Z(1)], r=kbk, w=[K("C0")])
            kb.tt("dve", PT[0][:], XB[:, Z(1)], ident[:], ALU.add, r=kbk + ["c_ident"], w=[K("PT0")])
            kb.ts("dve", V0[:], XB[:, Z(2)], beta[:, h:h + 1], None, ALU.mult, r=kbk + [tag + "_sm"], w=[K("V0")])
            kb.act(W0[:], XB[:, Z(3)], AF.Identity, scale=bec[:, h:h + 1], r=kbk + [tag + "_sm"], w=[K("W0")])
            kb.ts("dve", kdec[:], XB[:, Z(3)], erem[:, h:h + 1], None, ALU.mult, r=kbk + [tag + "_sm"], w=[K("kdec")])
            yield
            cur = 0
            kb.mm(XB[:, Z(0)], Cm[0][:], Bm[0][:], r=[K("B0"), K("C0")], w=kbk)
            kb.mm(XB[:, Z(1)], Bm[0][:], Cm[0][:], r=[K("B0"), K("C0")], w=kbk)
            yield
            for j in range(1, 6):
                nxt = 1 - cur
                Bn, Cn, PTk, PTn = K(f"B{nxt}"), K(f"C{nxt}"), K(f"PT{cur}"), K(f"PT{nxt}")
                kb.cp("dve", Bm[nxt][:], XB[:, Z(0)], r=kbk, w=[Bn])
                if j < 5:
                    kb.cp("act", Cm[nxt][:], XB[:, Z(1)], r=kbk, w=[Cn])
                yield
                kb.mm(XB[:, Z(2)], Bm[nxt][:], PT[cur][:], r=[Bn, PTk], w=kbk)
                if j < 5:
                    kb.mm(XB[:, Z(0)], Cm[nxt][:], Bm[nxt][:], r=[Bn, Cn], w=kbk)
                    if j < 4:
                        kb.mm(XB[:, Z(1)], Bm[nxt][:], Cm[nxt][:], r=[Bn, Cn], w=kbk)
                yield
                kb.tt("dve", PT[nxt][:], XB[:, Z(2)], PT[cur][:], ALU.add, r=kbk + [PTk], w=[PTn])
                cur = nxt
            yield
            PTf, PTfk = PT[cur], K(f"PT{cur}")
            kb.mm(XB[:, Z(3)], PTf[:], V0[:], r=[PTfk, K("V0")], w=kbk)
            kb.mm(XA[:, Z(0)], W0[:], PTf[:], r=[PTfk, K("W0")], w=ka)
            yield
            kb.cp("act", upre[:], XB[:, Z(3)], r=kbk, w=[K("upre")])
            kb.cp("dve", wT[:], XA[:, Z(0)], r=ka, w=[K("wT")])
            yield
            Mk = tag + f"_M{h}"
            for ch in range(2):
                tr_ = slice(ch * 64, (ch + 1) * 64)
                dch = dA if ch == 0 else dB
                kb.mm(XA[tr_, Z(1)], wT[:, tr_], M[h][:], r=[K("wT"), Mk], w=ka)
                kb.mm(XA[tr_, Z(3)], qdT[:, tr_], M[h][:], r=[K("qdT"), Mk], w=ka)
                yield
                kb.tt("dve", u_sb[tr_, :], upre[tr_, :], XA[tr_, Z(1)], ALU.subtract, r=[K("upre")] + ka, w=[K("u")])
                kb.cp("act", otmp[tr_, :], XA[tr_, Z(3)], r=ka, w=[K("otmp")])
                yield
                kb.mm(XB[tr_, Z(3)], PmT[:, tr_], u_sb[:, :], r=[K("PmT"), K("u")], w=kbk)
                kb.mm(XA[:, Z(2)], kdec[tr_, :], u_sb[tr_, :], r=[K("kdec"), K("u")], w=ka)
                yield
                kb.stt("dve", M[h][:], M[h][:], dch[:, h:h + 1], XA[:, Z(2)], ALU.mult, ALU.add,
                       r=[Mk, tag + "_sm"] + ka, w=[Mk])
                kb.tt("dve", o_sb[tr_, hc], XB[tr_, Z(3)], otmp[tr_, :], ALU.add, r=kbk + [K("otmp")], w=[tag + "_o"])
                yield

        gens = [head(h, h) for h in range(4)]
        while gens:
            for gen in list(gens):
                try:
                    next(gen)
                except StopIteration:
                    gens.remove(gen)
        if STOP <= 4:
            continue
        for h in range(4):
            kb.act(junk[:], o_sb[:, h * 128:(h + 1) * 128], AF.Square, accum_out=ssq[:, h:h + 1], r=[tag + "_o"],
                   w=[tag + "_junk", tag + "_ssq"])
        rms_rstd(kb, tag, rs, ssq, 4, 128)
        for h in range(4):
            hc = slice(h * 128, (h + 1) * 128)
            kb.stt("dve", og[:, hc], o_sb[:, hc], rs[:, h:h + 1], gn[:, hc], ALU.mult, ALU.mult,
                   r=[tag + "_o", tag + "_rs", tag + "_gn"], w=[tag + "_og"])
        kb.tt("pool", og[:], og[:], gsb[:], ALU.mult, r=[tag + "_og", tag + "_gsb"], w=[tag + "_og"])
        for c in range(4):
            kb.tr(P3[:, c * 128:(c + 1) * 128], og[:, c * 128:(c + 1) * 128], ident[:], r=[tag + "_og", "c_ident"],
                  w=kb.pk(3, c * 128, c * 128 + 128))
        kb.cp("act", oT[:], P3[:, :], r=kb.pk(3), w=[tag + "_oT"])
        kb.dma("sp", obr[i], oT[:], sem=tag + "_oT", r=[tag + "_oT"], w=[f"{tag}_obr{i}"])


def phase_ssd(kb, l, hT, hkey_fn, obr):
    import os
    C = kb.C
    tag = f"ssd{l}"
    w_in = kb.w_in
    NW = 1288
    wk = tag + "_w"
    wg = kb.sb(wk, (128, KC, NW), BF16)
    load_w_cast(kb, wg, wk, w_in[l], C_SSD_Z, NW)
    O_Z, O_XBC, O_DT = 0, 512, 1280
    convw = kb.sb(tag + "_cw", (128, 6, 5))
    kb.dma("sp", convw[:], kb.din(tag + "_cwd", (128, 6, 5)), sem=tag + "_cw", w=[tag + "_cw"])
    gn = kb.sb(tag + "_gn", (128, 512))
    kb.dma("sp", gn[:], kb.din(tag + "_gnd", (1, 512))[0].partition_broadcast(128), sem=tag + "_gn", w=[tag + "_gn"])
    hp = kb.sb(tag + "_hp", (128, 24))
    kb.dma("sp", hp[:], kb.din(tag + "_hpd", (1, 24))[0].partition_broadcast(128), sem=tag + "_hp", w=[tag + "_hp"])
    negA = kb.sb(tag + "_negA", (128, 8))
    kb.act(negA[:], hp[:, 8:16], AF.Exp, r=[tag + "_hp"], w=[tag + "_negA"])
    kb.ts("dve", negA[:], negA[:], -1.0, None, ALU.mult, r=[tag + "_negA"], w=[tag + "_negA"])
    ubuf = kb.sb(tag + "_ubuf", (128, 6, 131))
    kb.S.op("pool", lambda e: e.memset(ubuf[:], 0.0), [], [tag + "_ubuf"])
    cacc = kb.sb(tag + "_cacc", (128, 6, 128))
    ctmp = kb.sb(tag + "_ctmp", (128, 6, 128))
    xbc = kb.sb(tag + "_xbc", (128, 6, 128))
    zs = kb.sb(tag + "_zs", (128, 512))
    sm = kb.sb(tag + "_sm", (128, 72))
    dt, gg, cum, ecum, erem = sm[:, 0:8], sm[:, 8:16], sm[:, 16:24], sm[:, 24:32], sm[:, 32:40]
    dA, dB, ytmp = sm[:, 40:48], sm[:, 48:56], sm[:, 56:64]
    x_tok = kb.sb(tag + "_xtok", (128, 512))
    xdt = kb.sb(tag + "_xdt", (128, 512))
    xdte = kb.sb(tag + "_xdte", (128, 512))
    xd = kb.sb(tag + "_xd", (128, 512))
    B_tok = kb.sb(tag + "_Btok", (128, 128))
    CBT = [kb.sb(tag + f"_CBT{g}", (128, 128)) for g in range(2)]
    GsL = [kb.sb(tag + f"_Gs{i}", (128, 128)) for i in range(4)]
    LTL = [kb.sb(tag + f"_LT{i}", (128, 128)) for i in range(4)]
    Sbd = kb.sb(tag + "_Sbd", (128, 512))
    kb.S.op("pool", lambda e: e.memset(Sbd[:], 0.0), [], [tag + "_Sbd"])
    yint = kb.sb(tag + "_yint", (128, 512))
    y_sb = kb.sb(tag + "_y", (128, 512))
    oT = kb.sb(tag + "_oT", (128, 512), BF16)
    junk = kb.sb(tag + "_junk", (128, 256), BF16)
    ssq = kb.sb(tag + "_ssq", (128, 2))
    rs = kb.sb(tag + "_rs", (128, 2))
    P0, P1, P2, P3, P4, P5, P6, P7 = kb.P
    ident = C["ident"]
    STOP = float(os.environ.get('SSD_STOP', '9'))
    for i in range(int(os.environ.get('SSD_NT', NT))):
        t0 = i * 128
        hk = hkey_fn(i)
        for c in range(6):
            bank = kb.P[c // 4]
            cc = (c % 4) * 128
            proj_feat(kb, bank[:, cc:cc + 128], wg, wk, O_XBC + c * 128, 128, hT, hk, t0, 128, kb.pk(c // 4))
        proj_tok(kb, P2[:, :], wg, wk, O_Z, 512, hT, hk, t0, 128, kb.pk(2))
        proj_tok(kb, P3[:, 0:8], wg, wk, O_DT, 8, hT, hk, t0, 128, kb.pk(3))
        kb.act(zs[:], P2[:, :], AF.Silu, r=kb.pk(2), w=[tag + "_zs"])
        kb.cp("act", ubuf[:, 0:4, 3:131], P0[:, :].rearrange("p (c t) -> p c t", c=4), r=kb.pk(0), w=[tag + "_ubuf"])
        kb.cp("act", ubuf[:, 4:6, 3:131], P1[:, 0:256].rearrange("p (c t) -> p c t", c=2), r=kb.pk(1), w=[tag + "_ubuf"])
        for j in range(4):
            wj = convw[:, :, j:j + 1].to_broadcast([128, 6, 128])
            if j == 0:
                kb.tt("dve", cacc[:], ubuf[:, :, 0:128], wj, ALU.mult, r=[tag + "_ubuf", tag + "_cw"], w=[tag + "_cacc"])
                kb.tt("dve", cacc[:], cacc[:], convw[:, :, 4:5].to_broadcast([128, 6, 128]), ALU.add,
                      r=[tag + "_cacc", tag + "_cw"], w=[tag + "_cacc"])
            else:
                kb.tt("pool", ctmp[:], ubuf[:, :, j:j + 128], wj, ALU.mult, r=[tag + "_ubuf", tag + "_cw"], w=[tag + "_ctmp"])
                kb.tt("dve", cacc[:], cacc[:], ctmp[:], ALU.add, r=[tag + "_cacc", tag + "_ctmp"], w=[tag + "_cacc"])
        kb.act(xbc[:], cacc[:], AF.Silu, r=[tag + "_cacc"], w=[tag + "_xbc"])
        kb.cp("pool", ubuf[:, :, 0:3], ubuf[:, :, 128:131], r=[tag + "_ubuf"], w=[tag + "_ubuf"])
        kb.tt("dve", ytmp, P3[:, 0:8], hp[:, 0:8], ALU.add, r=kb.pk(3) + [tag + "_hp"], w=[tag + "_sm"])
        kb.act(ytmp, ytmp, AF.Exp, r=[tag + "_sm"], w=[tag + "_sm"])
        kb.act(dt, ytmp, AF.Ln, bias=1.0, r=[tag + "_sm"], w=[tag + "_sm"])
        kb.tt("dve", gg, dt, negA[:], ALU.mult, r=[tag + "_sm", tag + "_negA"], w=[tag + "_sm"])
        kb.mm(P3[:, 8:16], C["tri_incl"][:], gg, r=["c_tri_incl", tag + "_sm"], w=kb.pk(3))
        kb.mm(P3[:, 16:24], C["blk"][:], gg, r=["c_blk", tag + "_sm"], w=kb.pk(3))
        kb.mm(P3[:, 24:32], C["selA"][:], gg, r=["c_selA", tag + "_sm"], w=kb.pk(3))
        kb.mm(P3[:, 32:40], C["selB"][:], gg, r=["c_selB", tag + "_sm"], w=kb.pk(3))
        kb.cp("dve", cum, P3[:, 8:16], r=kb.pk(3), w=[tag + "_sm"])
        kb.tt("dve", erem, P3[:, 16:24], cum, ALU.subtract, r=kb.pk(3) + [tag + "_sm"], w=[tag + "_sm"])
        kb.act(dA, P3[:, 24:32], AF.Exp, r=kb.pk(3), w=[tag + "_sm"])
        kb.act(dB, P3[:, 32:40], AF.Exp, r=kb.pk(3), w=[tag + "_sm"])
        kb.act(ecum, cum, AF.Exp, r=[tag + "_sm"], w=[tag + "_sm"])
        kb.act(erem, erem, AF.Exp, r=[tag + "_sm"], w=[tag + "_sm"])
        if STOP <= 1:
            continue
        for c in range(4):
            kb.tr(P4[:, c * 128:(c + 1) * 128], xbc[:, c, :], ident[:], r=[tag + "_xbc", "c_ident"], w=kb.pk(4))
        kb.tr(P5[:, 0:128], xbc[:, 4, :], ident[:], r=[tag + "_xbc", "c_ident"], w=kb.pk(5))
        kb.cp("act", x_tok[:], P4[:, :], r=kb.pk(4), w=[tag + "_xtok"])
        kb.cp("act", B_tok[:], P5[:, 0:128], r=kb.pk(5), w=[tag + "_Btok"])
        v3 = lambda t: t[:, :].rearrange("p (h d) -> p h d", h=8)
        bc8 = lambda a: a.unsqueeze(2).to_broadcast([128, 8, 64])
        kb.tt("dve", v3(xdt), v3(x_tok), bc8(dt), ALU.mult, r=[tag + "_xtok", tag + "_sm"], w=[tag + "_xdt"])
        kb.tt("pool", v3(xd), v3(x_tok), bc8(hp[:, 16:24]), ALU.mult, r=[tag + "_xtok", tag + "_hp"], w=[tag + "_xd"])
        kb.tt("pool", v3(xdte), v3(xdt), bc8(erem), ALU.mult, r=[tag + "_xdt", tag + "_sm"], w=[tag + "_xdte"])
        for g in range(2):
            rows = slice(g * 64, (g + 1) * 64)
            bank = P5 if g == 0 else P6
            kb.mm(bank[:, 128:256], xbc[rows, 4, :], xbc[rows, 5, :], r=[tag + "_xbc"], w=kb.pk(5 + g))
            kb.cp("act", CBT[g][:], bank[:, 128:256], r=kb.pk(5 + g), w=[tag + f"_CBT{g}"])
        if STOP <= 2:
            continue
        def head(h, s_):
            g = h // 4
            Gs, LT = GsL[s_], LTL[s_]
            gk_, lk_ = tag + f"_Gs{s_}", tag + f"_LT{s_}"
            bi = (2, 3, 5, 6)[s_]
            X, kx = kb.P[bi], kb.pk(bi)
            kb.ts("dve", Gs[:], C["tri_incl"][:], gg[:, h:h + 1], None, ALU.mult, r=["c_tri_incl", tag + "_sm"], w=[gk_])
            yield
            kb.mm(X[:, 256:384], C["tri_strict"][:], Gs[:], r=[gk_, "c_tri_strict"], w=kx)
            yield
            kb.act(LT[:], X[:, 256:384], AF.Exp, r=kx, w=[lk_])
            yield
            kb.tt("dve", LT[:], LT[:], C["tri_incl"][:], ALU.mult, r=[lk_, "c_tri_incl"], w=[lk_])
            yield
            kb.tt("dve", LT[:], LT[:], CBT[g][:], ALU.mult, r=[lk_, tag + f"_CBT{g}"], w=[lk_])
            yield
            kb.mm(P7[:, h * 64:(h + 1) * 64], LT[:], xdt[:, h * 64:(h + 1) * 64], r=[lk_, tag + "_xdt"], w=kb.pk(7))
            yield

        for grp in ((0, 1, 2, 3), (4, 5, 6, 7)):
            gens = [head(h, s_) for s_, h in enumerate(grp)]
            while gens:
                for gen in list(gens):
                    try:
                        next(gen)
                    except StopIteration:
                        gens.remove(gen)
        if STOP <= 3:
            continue
        for ch in range(2):
            tr_ = slice(ch * 64, (ch + 1) * 64)
            dch = dA if ch == 0 else dB
            kb.mm(P0[tr_, :], xbc[:, 5, tr_], Sbd[:, :], r=[tag + "_xbc", tag + "_Sbd"], w=kb.pk(0))
            kb.mm(P1[:, :], B_tok[tr_, :], xdte[tr_, :], r=[tag + "_Btok", tag + "_xdte"], w=kb.pk(1))
            for g in range(2):
                rr = slice(g * 64, (g + 1) * 64)
                cc = slice(g * 256, (g + 1) * 256)
                s3 = Sbd[rr, cc].rearrange("p (h d) -> p h d", h=4)
                kb.tt("dve", s3, s3, dch[rr, g * 4:(g + 1) * 4].unsqueeze(2).to_broadcast([64, 4, 64]), ALU.mult,
                      r=[tag + "_Sbd", tag + "_sm"], w=[tag + "_Sbd"])
                kb.tt("dve", Sbd[rr, cc], Sbd[rr, cc], P1[rr, cc], ALU.add, r=[tag + "_Sbd"] + kb.pk(1), w=[tag + "_Sbd"])
        kb.cp("act", yint[:], P0[:, :], r=kb.pk(0), w=[tag + "_yint"])
        kb.tt("pool", v3(yint), v3(yint), bc8(ecum), ALU.mult, r=[tag + "_yint", tag + "_sm"], w=[tag + "_yint"])
        kb.tt("dve", y_sb[:], P7[:, :], yint[:], ALU.add, r=kb.pk(7) + [tag + "_yint"], w=[tag + "_y"])
        kb.tt("pool", y_sb[:], y_sb[:], xd[:], ALU.add, r=[tag + "_y", tag + "_xd"], w=[tag + "_y"])
        kb.tt("pool", y_sb[:], y_sb[:], zs[:], ALU.mult, r=[tag + "_y", tag + "_zs"], w=[tag + "_y"])
        for g in range(2):
            kb.act(junk[:], y_sb[:, g * 256:(g + 1) * 256], AF.Square, accum_out=ssq[:, g:g + 1], r=[tag + "_y"],
                   w=[tag + "_junk", tag + "_ssq"])
        rms_rstd(kb, tag, rs, ssq, 2, 256)
        for g in range(2):
            gc = slice(g * 256, (g + 1) * 256)
            kb.stt("dve", y_sb[:, gc], y_sb[:, gc], rs[:, g:g + 1], gn[:, gc], ALU.mult, ALU.mult,
                   r=[tag + "_y", tag + "_rs", tag + "_gn"], w=[tag + "_y"])
        for c in range(4):
            kb.tr(P4[:, c * 128:(c + 1) * 128], y_sb[:, c * 128:(c + 1) * 128], ident[:], r=[tag + "_y", "c_ident"], w=kb.pk(4))
        kb.cp("act", oT[:], P4[:, :], r=kb.pk(4), w=[tag + "_oT"])
        kb.dma("sp", obr[i], oT[:], sem=tag + "_oT", r=[tag + "_oT"], w=[f"{tag}_obr{i}"])


def phase_merge(kb, l, hT, hkey_fn, obrs, obr_keys, xsrc, xsrc_key, xdst, xdst_key):
    C = kb.C
    tag = f"mrg{l}"
    wm = kb.sb(tag + "_wm", (128, KC, 3072), BF16)
    load_w_cast(kb, wm, tag + "_wm", kb.w_in[l], C_MERGE, 3072)
    wbr = []
    for b, nm in enumerate(("w_branch_gla", "w_branch_gdn", "w_branch_ssd")):
        t = kb.sb(tag + f"_wb{b}", (128, 4, D), BF16)
        load_w_cast(kb, t, tag + f"_wb{b}", kb.dins[nm][l], 0, D, nk=4)
        wbr.append(t)
    wo = kb.sb(tag + "_wo", (128, KC, D), BF16)
    load_w_cast(kb, wo, tag + "_wo", kb.dins["w_out"][l], 0, D)
    bmb = kb.sb(tag + "_bmb", (1, 3072), BF16)
    kb.dma("pool", bmb[:], kb.din(tag + "_bmd", (1, 3072)), sem=tag + "_bmb", w=[tag + "_bmb"])
    gm_row, gm_key = kb.gm_row[l]
    ob = [kb.sb(tag + f"_ob{b}", (128, 512), BF16) for b in range(3)]
    sig = kb.sb(tag + "_sig", (128, 512))
    acc = kb.sb(tag + "_acc", (128, 512))
    tmp = kb.sb(tag + "_tmp", (128, 512))
    mT = kb.sb(tag + "_mT", (128, KC, 128), BF16)
    xt = kb.sb(tag + "_xt", (128, D))
    xo = kb.sb(tag + "_xo", (128, D))
    P = kb.P
    for i in range(NT):
        t0 = i * 128
        hk = hkey_fn(i)
        for b in range(3):
            kb.dma("sp", ob[b][:], obrs[b][i], sem=tag + f"_ob{b}", r=[obr_keys[b](i)], w=[tag + f"_ob{b}"])
        kb.dma("sp", xt[:], xsrc[t0:t0 + 128, :], sem=tag + "_xt", r=[xsrc_key(i)], w=[tag + "_xt"])
        for half in range(2):
            for b in range(3):
                PG, PY = P[(b % 2) * 2], P[(b % 2) * 2 + 1]
                kg, ky = kb.pk((b % 2) * 2), kb.pk((b % 2) * 2 + 1)
                for jj in range(4):
                    j = half * 4 + jj
                    col = b * D + j * 128
                    zone = slice(jj * 128, (jj + 1) * 128)
                    for k in range(KC):
                        kb.mm(PG[:, zone], wm[:, k, col:col + 128], hT[:, k, t0:t0 + 128], start=(k == 0), stop=False,
                              r=[tag + "_wm", hk], w=kg)
                    kb.mm(PG[:, zone], bmb[0:1, col:col + 128], C["ones_bf"][0:1, :], start=False, stop=True,
                          r=[tag + "_bmb", "cb_ones"], w=kg)
                    for c in range(4):
                        kb.mm(PY[:, zone], wbr[b][:, c, j * 128:(j + 1) * 128], ob[b][:, c * 128:(c + 1) * 128],
                              start=(c == 0), stop=(c == 3), r=[tag + f"_wb{b}", tag + f"_ob{b}"], w=ky)
                kb.act(sig[:], PG[:, :], AF.Sigmoid, r=kg, w=[tag + "_sig"])
                if b == 0:
                    kb.tt("dve", acc[:], PY[:, :], sig[:], ALU.mult, r=ky + [tag + "_sig"], w=[tag + "_acc"])
                else:
                    kb.tt("dve", tmp[:], PY[:, :], sig[:], ALU.mult, r=ky + [tag + "_sig"], w=[tag + "_tmp"])
                    kb.tt("pool", acc[:], acc[:], tmp[:], ALU.add, r=[tag + "_acc", tag + "_tmp"], w=[tag + "_acc"])
            kb.cp("act", mT[:, half * 4:(half + 1) * 4, :], acc[:, :].rearrange("p (j t) -> p j t", j=4),
                  r=[tag + "_acc"], w=[tag + "_mT"])
        for half in range(2):
            PO, ko = P[4 + half], kb.pk(4 + half)
            for j in range(KC):
                kb.mm(PO[:, :], mT[:, j, :], wo[:, j, half * 512:(half + 1) * 512], start=(j == 0), stop=(j == KC - 1),
                      r=[tag + "_mT", tag + "_wo"], w=ko)
            hs = slice(half * 512, (half + 1) * 512)
            kb.tt("dve", xo[:, hs], PO[:, :], gm_row[:, hs], ALU.mult, r=ko + [gm_key], w=[tag + "_xo"])
            kb.tt("pool", xo[:, hs], xo[:, hs], xt[:, hs], ALU.add, r=[tag + "_xo", tag + "_xt"], w=[tag + "_xo"])
        kb.dma("sp", xdst[t0:t0 + 128, :], xo[:], sem=tag + "_xo", r=[tag + "_xo"], w=[xdst_key(i)])


def phase_moe(kb, l, xsrc, xsrc_key, xdst, xdst_key, final=None):
    import os
    C = kb.C
    tag = f"moe{l}"
    P = kb.P
    TS = 512
    NSUP = S_TOK // TS
    NE = int(os.environ.get("MOE_NE", 32))
    wr = kb.sb(tag + "_wr", (128, KC, 32), BF16)
    load_w_cast(kb, wr, tag + "_wr", kb.dins["w_router"][l], 0, 32)
    brb = kb.sb(tag + "_brb", (1, 32), BF16)
    kb.dma("pool", brb[:], kb.din(tag + "_brd", (1, 32)), sem=tag + "_brb", w=[tag + "_brb"])
    ones5 = kb.sb(tag + "_ones5", (1, 512), BF16)
    kb.S.op("dve", lambda e: e.memset(ones5[:], 1.0), [], [tag + "_ones5"])
    gf_row, gf_key = kb.gf_row[l]
    nb = norm_bufs(kb, tag + "_n")
    hTs = kb.sb(tag + "_hT", (128, KC, TS), BF16)
    G = kb.sb(tag + "_G", (128, 4, 32))
    lg = kb.sb(tag + "_lg", (128, 32))
    v8 = kb.sb(tag + "_v8", (128, 8))
    msk = kb.sb(tag + "_msk", (128, 32))
    sml = kb.sb(tag + "_sml", (128, 4))
    wgu = [kb.sb(tag + f"_wgu{i}", (128, KC, 2048), BF16) for i in range(2)]
    wd = [kb.sb(tag + f"_wd{i}", (128, KC, D), BF16) for i in range(2)]
    bgu = [kb.sb(tag + f"_bgu{i}", (1, 2048), BF16) for i in range(2)]
    bd = [kb.sb(tag + f"_bd{i}", (1, D), BF16) for i in range(2)]
    yacc = kb.sb(tag + "_yacc", (128, 4, D))
    actT = kb.sb(tag + "_actT", (128, KC, TS), BF16)
    g7 = kb.sb(tag + "_g7", (128, TS))
    sg = kb.sb(tag + "_sg", (128, TS))
    u7 = kb.sb(tag + "_u7", (128, TS))
    xt = kb.sb(tag + "_xt", (128, D))
    xo = kb.sb(tag + "_xo", (128, D))
    if final is not None:
        nfr = kb.sb(tag + "_nfr", (128, D))
        kb.dma("sp", nfr[:], final["nf"].partition_broadcast(128), sem=tag + "_nfr", w=[tag + "_nfr"])
        fj = kb.sb(tag + "_fj", (128, D), BF16)
        fs = kb.sb(tag + "_fs", (128, 2))
    w_gu_d, w_d_d = kb.dins["w_gate_up"][l], kb.dins["w_down"][l]
    b_gu_d, b_d_d = kb.dins["b_gate_up"][l], kb.dins["b_down"][l]

    def load_expert(e, slot):
        srcg = w_gu_d[e].rearrange("(k p) c -> p k c", p=128)
        srcd = w_d_d[e].rearrange("(k p) c -> p k c", p=128)
        for k in range(KC):
            kb.dma("pool", wgu[slot][:, k, :], srcg[:, k, :], sem=tag + f"_wgu{slot}", w=[tag + f"_wgu{slot}"])
        for k in range(KC):
            kb.dma("pool", wd[slot][:, k, :], srcd[:, k, :], sem=tag + f"_wd{slot}", w=[tag + f"_wd{slot}"])
        kb.dma("pool", bgu[slot][:], b_gu_d[e:e + 1, :], sem=tag + f"_bgu{slot}", w=[tag + f"_bgu{slot}"])
        kb.dma("pool", bd[slot][:], b_d_d[e:e + 1, :], sem=tag + f"_bd{slot}", w=[tag + f"_bd{slot}"])

    it = 0
    for T in range(int(os.environ.get("MOE_NSUP", NSUP))):
        for tt in range(4):
            i = T * 4 + tt
            norm_tile(kb, nb, l, "f", xsrc[i * 128:(i + 1) * 128, :], xsrc_key(i), hTs[:, :, tt * 128:(tt + 1) * 128], tag + "_hT")
        for tt in range(4):
            for k in range(KC):
                kb.mm(P[7][:, 0:32], hTs[:, k, tt * 128:(tt + 1) * 128], wr[:, k, :], start=(k == 0), stop=False,
                      r=[tag + "_hT", tag + "_wr"], w=kb.pk(7))
            kb.mm(P[7][:, 0:32], ones5[0:1, 0:128], brb[0:1, :], start=False, stop=True, r=[tag + "_ones5", tag + "_brb"], w=kb.pk(7))
            kb.cp("dve", lg[:], P[7][:, 0:32], r=kb.pk(7), w=[tag + "_lg"])
            kb.S.op("dve", lambda e: e.max(out=v8[:], in_=lg[:]), [tag + "_lg"], [tag + "_v8"])
            kb.ts("dve", msk[:], lg[:], v8[:, 3:4], None, ALU.is_ge, r=[tag + "_lg", tag + "_v8"], w=[tag + "_msk"])
            kb.ts("dve", sml[:, 0:1], v8[:, 0:1], -1.0, None, ALU.mult, r=[tag + "_v8"], w=[tag + "_sml"])
            kb.act(lg[:], lg[:], AF.Exp, bias=sml[:, 0:1], r=[tag + "_lg", tag + "_sml"], w=[tag + "_lg"])
            kb.tt("dve", lg[:], lg[:], msk[:], ALU.mult, r=[tag + "_lg", tag + "_msk"], w=[tag + "_lg"])
            kb.S.op("dve", lambda e: e.reduce_sum(sml[:, 1:2], lg[:], AX.X), [tag + "_lg"], [tag + "_sml"])
            kb.S.op("dve", lambda e: e.reciprocal(sml[:, 1:2], sml[:, 1:2]), [tag + "_sml"], [tag + "_sml"])
            kb.ts("dve", G[:, tt, :], lg[:], sml[:, 1:2], None, ALU.mult, r=[tag + "_lg", tag + "_sml"], w=[tag + "_G"])
        kb.S.op("pool", lambda e: e.memset(yacc[:], 0.0), [], [tag + "_yacc"])
        for e_ in range(NE):
            slot = it % 2
            if it == 0:
                load_expert(e_, slot)
            nxt = (e_ + 1) % NE
            if not (T == NSUP - 1 and e_ == NE - 1):
                load_expert(nxt, 1 - slot)
            it += 1
            wk, dk, bgk, bdk = tag + f"_wgu{slot}", tag + f"_wd{slot}", tag + f"_bgu{slot}", tag + f"_bd{slot}"
            for jc in range(KC):
                pb = (jc % 2) * 2
                PG, PU = P[pb], P[pb + 1]
                for which, PX in ((0, PG), (1, PU)):
                    cols = slice(jc * 256 + which, (jc + 1) * 256, 2)
                    for k in range(KC):
                        kb.mm(PX[:, :], wgu[slot][:, k, cols], hTs[:, k, :], start=(k == 0), stop=False,
                              r=[wk, tag + "_hT"], w=kb.pk(pb + which))
                    kb.mm(PX[:, :], bgu[slot][0:1, cols], ones5[0:1, :], start=False, stop=True,
                          r=[bgk, tag + "_ones5"], w=kb.pk(pb + which))
                kb.ts("dve", g7[:], PG[:, :], SW_LIMIT, None, ALU.min, r=kb.pk(pb), w=[tag + "_g7"])
                kb.ts("dve", u7[:], PU[:, :], -SW_LIMIT, SW_LIMIT, ALU.max, ALU.min, r=kb.pk(pb + 1), w=[tag + "_u7"])
                kb.act(sg[:], g7[:], AF.Sigmoid, scale=SW_ALPHA, r=[tag + "_g7"], w=[tag + "_sg"])
                kb.ts("pool", u7[:], u7[:], 1.0, None, ALU.add, r=[tag + "_u7"], w=[tag + "_u7"])
                kb.tt("pool", u7[:], u7[:], g7[:], ALU.mult, r=[tag + "_u7", tag + "_g7"], w=[tag + "_u7"])
                kb.tt("pool", actT[:, jc, :], u7[:], sg[:], ALU.mult, r=[tag + "_u7", tag + "_sg"], w=[tag + "_actT"])
            for tt in range(4):
                for half in range(2):
                    pi = 4 + (tt % 2) * 2 + half
                    PO = P[pi]
                    hs = slice(half * 512, (half + 1) * 512)
                    for jc in range(KC):
                        kb.mm(PO[:, :], actT[:, jc, tt * 128:(tt + 1) * 128], wd[slot][:, jc, hs], start=(jc == 0), stop=False,
                              r=[tag + "_actT", dk], w=kb.pk(pi))
                    kb.mm(PO[:, :], ones5[0:1, 0:128], bd[slot][0:1, hs], start=False, stop=True,
                          r=[tag + "_ones5", bdk], w=kb.pk(pi))
                    kb.stt("dve", yacc[:, tt, hs], PO[:, :], G[:, tt, e_:e_ + 1], yacc[:, tt, hs], ALU.mult, ALU.add,
                           r=kb.pk(pi) + [tag + "_G", tag + "_yacc"], w=[tag + "_yacc"])
        for tt in range(4):
            i = T * 4 + tt
            kb.dma("sp", xt[:], xsrc[i * 128:(i + 1) * 128, :], sem=tag + "_xt", r=[xsrc_key(i)], w=[tag + "_xt"])
            kb.tt("dve", xo[:], yacc[:, tt, :], gf_row[:], ALU.mult, r=[tag + "_yacc", gf_key], w=[tag + "_xo"])
            kb.tt("pool", xo[:], xo[:], xt[:], ALU.add, r=[tag + "_xo", tag + "_xt"], w=[tag + "_xo"])
            if final is None:
                kb.dma("sp", xdst[i * 128:(i + 1) * 128, :], xo[:], sem=tag + "_xo", r=[tag + "_xo"], w=[xdst_key(i)])
            else:
                kb.act(fj[:], xo[:], AF.Square, accum_out=fs[:, 0:1], r=[tag + "_xo"], w=[tag + "_fj", tag + "_fs"])
                kb.ts("dve", fs[:, 1:2], fs[:, 0:1], 1.0 / D, EPS, ALU.mult, ALU.add, r=[tag + "_fs"], w=[tag + "_fs"])
                kb.act(fs[:, 1:2], fs[:, 1:2], AF.Sqrt, r=[tag + "_fs"], w=[tag + "_fs"])
                kb.S.op("dve", lambda e: e.reciprocal(fs[:, 1:2], fs[:, 1:2]), [tag + "_fs"], [tag + "_fs"])
                kb.stt("dve", xo[:], xo[:], fs[:, 1:2], nfr[:], ALU.mult, ALU.mult, r=[tag + "_xo", tag + "_fs", tag + "_nfr"], w=[tag + "_xo"])
                kb.dma("sp", final["out"][i * 128:(i + 1) * 128, :], xo[:], sem=tag + "_xo", r=[tag + "_xo"], w=[f"out{i}"])


SW_LIMIT = 7.0
SW_ALPHA = 1.702


W_SHAPES = {
    "w_branch_gla": (DEPTH, 512, D), "w_branch_gdn": (DEPTH, 512, D), "w_branch_ssd": (DEPTH, 512, D),
    "w_out": (DEPTH, D, D), "w_router": (DEPTH, D, 32),
    "w_gate_up": (DEPTH, 32, D, 2 * D), "b_gate_up": (DEPTH, 32, 2 * D),
    "w_down": (DEPTH, 32, D, D), "b_down": (DEPTH, 32, D),
}


def build_program(layers=(0, 1), do_mix=True, do_moe=True, dbg=None, same_engine_sync=True):
    kb = KB(same_engine_sync=same_engine_sync)
    phase_consts(kb)
    x = kb.din("x", (S_TOK, D))
    kb.w_in = kb.din("w_in", (DEPTH, D, IN_COLS))
    kb.dins = {nm: kb.din(nm, shp) for nm, shp in W_SHAPES.items()}
    nf = kb.din("norm_final", (D,))
    out = kb.dout("out", (S_TOK, D))
    if do_moe and MOE_SORTED:
        kb.moe_xs = kb.dscr("moe_xs", (MOE_ROWS, D))
        zsrc = kb.din("zeros", (512, D))
        xs_v = kb.moe_xs.rearrange("(n r) c -> n r c", r=512)
        for n in range(MOE_ROWS // 512):
            kb.dma("pool", xs_v[n], zsrc, sem="moe_zt", w=["moe_xs"])
    phase_mod(kb)
    xin, xin_key = x, (lambda i: "x_in")
    last = layers[-1]
    for l in layers:
        xmid = kb.dscr(f"xmid{l}", (S_TOK, D), debug=(dbg == "xmid" and l == layers[0]))
        xmid_key = (lambda i, l=l: f"xmid{l}_{i}")
        if do_mix:
            obr = [kb.dscr(f"obr{l}_{b}", (NT, 128, 512), BF16) for b in range(3)]
            kb.push_scope()
            hT = kb.sb(f"hT{l}", (128, KC, S_TOK), BF16)
            hk = (lambda i, l=l: f"hT{l}_{i}")
            kb.push_scope(); phase_norm(kb, l, "m", xin, xin_key, hT, hk); kb.pop_scope()
            kb.push_scope(); phase_gla(kb, l, hT, hk, obr[0]); kb.pop_scope()
            kb.push_scope(); phase_gdn(kb, l, hT, hk, obr[1]); kb.pop_scope()
            kb.push_scope(); phase_ssd(kb, l, hT, hk, obr[2]); kb.pop_scope()
            keys = [(lambda i, l=l, t=t: f"{t}{l}_obr{i}") for t in ("gla", "gdn", "ssd")]
            kb.push_scope(); phase_merge(kb, l, hT, hk, obr, keys, xin, xin_key, xmid, xmid_key); kb.pop_scope()
            kb.pop_scope()
            msrc, msrc_key = xmid, xmid_key
        else:
            msrc, msrc_key = xin, xin_key
        if dbg == "xmid":
            kb.S.final_wait("sp", [xmid_key(i) for i in range(NT)])
            break
        if do_moe:
            xnext = kb.dscr(f"xres{l}", (S_TOK, D))
            xnext_key = (lambda i, l=l: f"xres{l}_{i}")
            kb.push_scope()
            moe_fn = phase_moe_sorted if MOE_SORTED else phase_moe
            moe_fn(kb, l, msrc, msrc_key, xnext, xnext_key, final=(dict(out=out, nf=nf) if l == last else None))
            kb.pop_scope()
            xin, xin_key = xnext, xnext_key
    kb.S.final_wait("sp", [f"out{i}" for i in range(NT)])
    kb.stats = kb.S.emit(kb.stack)
    return kb


def host_all(inputs, b, names):
    m = {}
    for nm in W_SHAPES:
        m[nm] = inputs[nm]
    m["norm_final"] = inputs["norm_final"]
    m["zeros"] = np.zeros((512, D), np.float32)
    for l in range(DEPTH):
        m[f"mrg{l}_bmd"] = inputs["b_merge"][l][None, :]
        m[f"moe{l}_brd"] = inputs["b_router"][l][None, :]
        m[f"moe{l}_nffn"] = inputs["norm_ffn"][l][None, :]
    base = host_inputs(inputs, b, [n for n in names if n not in m])
    for n in names:
        if n in m:
            base[n] = np.ascontiguousarray(m[n])
    return base


_PROG = {}


def kernel(**inputs):
    inputs = {k: np.asarray(v) for k, v in inputs.items()}
    if "kb" not in _PROG:
        _PROG["kb"] = build_program()
    kb = _PROG["kb"]
    names = list(kb.ins.keys())
    in_maps = [host_all(inputs, b, names) for b in range(8)]
    res = run_bass_kernel_spmd(kb.nc, in_maps, core_ids=list(range(8)))
    return np.stack([np.asarray(r["out"]) for r in res.results], axis=0).astype(np.float32)


MOE_SORTED = True
MOE_BLK = 512
MOE_NB = (S_TOK * 4) // MOE_BLK + 32
MOE_ROWS = MOE_NB * MOE_BLK


def phase_moe_sorted(kb, l, xsrc, xsrc_key, xdst, xdst_key, final=None):
    import os
    C = kb.C
    tag = f"moe{l}"
    P = kb.P
    BLK, NB = MOE_BLK, MOE_NB
    NTB = BLK // 128
    IOA = bass.IndirectOffsetOnAxis
    wr = kb.sb(tag + "_wr", (128, KC, 32), BF16)
    load_w_cast(kb, wr, tag + "_wr", kb.dins["w_router"][l], 0, 32)
    brb = kb.sb(tag + "_brb", (1, 32), BF16)
    kb.dma("pool", brb[:], kb.din(tag + "_brd", (1, 32)), sem=tag + "_brb", w=[tag + "_brb"])
    ones5 = kb.sb(tag + "_ones5", (1, 512), BF16)
    kb.S.op("dve", lambda e: e.memset(ones5[:], 1.0), [], [tag + "_ones5"])
    hTb = [kb.sb(tag + f"_hT{i}", (128, KC, BLK), BF16) for i in range(2)]
    lg_all = kb.sb(tag + "_lg", (128, NT, 32))
    msk_all = kb.sb(tag + "_msk", (128, NT, 32))
    R_all = kb.sb(tag + "_R", (128, NT, 32))
    v8_all = kb.sb(tag + "_v8", (128, NT, 8))
    gk_all = kb.sb(tag + "_gk", (128, NT, 4))
    sml = kb.sb(tag + "_sml", (128, 4))
    cnt = kb.sb(tag + "_cnt", (128, 32))
    kb.S.op("pool", lambda e: e.memset(cnt[:], 0.0), [], [tag + "_cnt"])
    padded = kb.sb(tag + "_padded", (128, 32))
    pstart = kb.sb(tag + "_pstart", (128, 32))
    pend = kb.sb(tag + "_pend", (128, 32))
    pcol = kb.sb(tag + "_pcol", (32, 1))
    pcb = kb.sb(tag + "_pcb", (32, 128))
    dg = kb.sb(tag + "_dg", (32, 32))
    posf = kb.sb(tag + "_posf", (128, NT, 4))
    posi = kb.sb(tag + "_posi", (128, NT, 4), I32)
    eb = kb.sb(tag + "_eb", (128, NB))
    offi = kb.sb(tag + "_offi", (128, NB, KC), I32)
    oh = kb.sb(tag + "_oh", (32, NB), BF16)
    kb.push_scope()
    gf_row, gf_key = kb.gf_row[l]
    scf = kb.sb(tag + "_scf", (128, D))
    shf = kb.sb(tag + "_shf", (128, D))
    nfrow = kb.sb(tag + "_nfrow", (128, D))
    kb.dma("sp", shf[:], kb.modrow_d[l][0], sem=tag + "_shf", r=[f"modrowd{l}"], w=[tag + "_shf"])
    kb.dma("sp", scf[:], kb.modrow_d[l][1], sem=tag + "_scf", r=[f"modrowd{l}"], w=[tag + "_scf"])
    kb.dma("sp", nfrow[:], kb.din(tag + "_nffn", (1, D))[0].partition_broadcast(128), sem=tag + "_nfrow", w=[tag + "_nfrow"])
    kb.stt("dve", scf[:], scf[:], 1.0, nfrow[:], ALU.add, ALU.mult, r=[tag + "_scf", tag + "_nfrow"], w=[tag + "_scf"])
    xs_d = kb.moe_xs
    ys_d = kb.dscr(f"moe_ys{l}", (MOE_ROWS, D))
    h2_d = kb.dscr(f"moe_h2{l}", (S_TOK, D))
    nb = norm_bufs(kb, tag + "_n")
    h2 = kb.sb(tag + "_h2", (128, D))
    eq = kb.sb(tag + "_eq", (128, NT, 32))
    cmp3 = kb.sb(tag + "_cmp3", (128, NB, 32))
    offf = kb.sb(tag + "_offf", (128, NB, KC))
    def p1_norm(i):
            hv = hTb[0][:, :, (i % 2) * 128:(i % 2 + 1) * 128]
            norm_tile(kb, nb, l, "f", xsrc[i * 128:(i + 1) * 128, :], xsrc_key(i), hv, tag + f"_hTp{i % 2}")
            b_ = (nb["n"] - 1) % 2
            xn, xnk = nb["xn"][b_], f"{tag}_n_xn{b_}"
            kb.tt("pool", h2[:], xn[:], scf[:], ALU.mult, r=[xnk, tag + "_scf"], w=[tag + "_h2"])
            kb.tt("pool", h2[:], h2[:], shf[:], ALU.add, r=[tag + "_h2", tag + "_shf"], w=[tag + "_h2"])
            kb.dma("sp", h2_d[i * 128:(i + 1) * 128, :], h2[:], sem=tag + "_h2", r=[tag + "_h2"], w=[f"{tag}_h2d{i}"])

    def p1_route(i):
            for k in range(KC):
                kb.mm(P[7][:, 0:32], hTb[0][:, k, (i % 2) * 128:(i % 2 + 1) * 128], wr[:, k, :], start=(k == 0), stop=False, r=[tag + f"_hTp{i % 2}", tag + "_wr"], w=kb.pk(7))
            kb.mm(P[7][:, 0:32], ones5[0:1, 0:128], brb[0:1, :], start=False, stop=True, r=[tag + "_ones5", tag + "_brb"], w=kb.pk(7))
            lg, v8, msk = lg_all[:, i, :], v8_all[:, i, :], msk_all[:, i, :]
            kb.cp("dve", lg, P[7][:, 0:32], r=kb.pk(7), w=[tag + "_lg"])
            kb.S.op("dve", lambda e, v8=v8, lg=lg: e.max(out=v8, in_=lg), [tag + "_lg"], [tag + "_v8"])
            kb.ts("dve", msk, lg, v8[:, 3:4], None, ALU.is_ge, r=[tag + "_lg", tag + "_v8"], w=[tag + "_msk"])
            kb.ts("dve", sml[:, 0:1], v8[:, 0:1], -1.0, None, ALU.mult, r=[tag + "_v8"], w=[tag + "_sml"])
            kb.act(gk_all[:, i, :], v8[:, 0:4], AF.Exp, bias=sml[:, 0:1], r=[tag + "_v8", tag + "_sml"], w=[tag + "_gk"])
            kb.S.op("dve", lambda e, i=i: e.reduce_sum(sml[:, 1:2], gk_all[:, i, :], AX.X), [tag + "_gk"], [tag + "_sml"])
            kb.S.op("dve", lambda e: e.reciprocal(sml[:, 1:2], sml[:, 1:2]), [tag + "_sml"], [tag + "_sml"])
            kb.ts("dve", gk_all[:, i, :], gk_all[:, i, :], sml[:, 1:2], None, ALU.mult, r=[tag + "_gk", tag + "_sml"], w=[tag + "_gk"])
            kb.mm(P[6][:, 0:32], C["tri_full"][:], msk, r=["c_tri_full", tag + "_msk"], w=kb.pk(6))
            kb.mm(P[6][:, 32:64], C["ones"][:], msk, r=["c_ones", tag + "_msk"], w=kb.pk(6))
            kb.tt("dve", R_all[:, i, :], P[6][:, 0:32], cnt[:], ALU.add, r=kb.pk(6) + [tag + "_cnt"], w=[tag + "_R"])
            kb.tt("dve", cnt[:], P[6][:, 32:64], cnt[:], ALU.add, r=kb.pk(6) + [tag + "_cnt"], w=[tag + "_cnt"])

    for i_ in range(NT + 1):
        if i_ < NT:
            p1_norm(i_)
        if i_ >= 1:
            p1_route(i_ - 1)
    kb.tt("dve", eq[:, 0:8, :].rearrange("p j e -> p e j"), cnt[:].unsqueeze(2).to_broadcast([128, 32, 8]),
          C["blk_thr"][:, 0:8].unsqueeze(1).to_broadcast([128, 32, 8]), ALU.is_gt, r=[tag + "_cnt", "c_blk_thr"], w=[tag + "_eq"])
    kb.S.op("dve", lambda e: e.reduce_sum(padded[:], eq[:, 0:8, :].rearrange("p j e -> p e j"), AX.X), [tag + "_eq"], [tag + "_padded"])
    kb.ts("dve", padded[:], padded[:], float(BLK), None, ALU.mult, r=[tag + "_padded"], w=[tag + "_padded"])
    kb.tt("dve", dg[:], padded[0:32, :], C["ident"][0:32, 0:32], ALU.mult, r=[tag + "_padded", "c_ident"], w=[tag + "_dg"])
    kb.S.op("dve", lambda e: e.reduce_sum(pcol[:], dg[:], AX.X), [tag + "_dg"], [tag + "_pcol"])
    kb.cp("dve", pcb[:], pcol[:, 0:1].to_broadcast([32, 128]), r=[tag + "_pcol"], w=[tag + "_pcb"])
    kb.mm(P[6][:, 0:32], pcb[:], C["tri_full"][0:32, 0:32], r=[tag + "_pcb", "c_tri_full"], w=kb.pk(6))
    kb.cp("dve", pstart[:], P[6][:, 0:32], r=kb.pk(6), w=[tag + "_pstart"])
    kb.tt("dve", pend[:], pstart[:], padded[:], ALU.add, r=[tag + "_pstart", tag + "_padded"], w=[tag + "_pend"])
    kb.tt("dve", R_all[:], R_all[:], pstart[:].unsqueeze(1).to_broadcast([128, NT, 32]), ALU.add,
          r=[tag + "_R", tag + "_pstart"], w=[tag + "_R"])
    for k in range(4):
        kb.tt("dve", eq[:], lg_all[:], v8_all[:, :, k:k + 1].to_broadcast([128, NT, 32]), ALU.is_equal,
              r=[tag + "_lg", tag + "_v8"], w=[tag + "_eq"])
        kb.tt("dve", eq[:], eq[:], R_all[:], ALU.mult, r=[tag + "_eq", tag + "_R"], w=[tag + "_eq"])
        kb.S.op("dve", lambda e, k=k: e.reduce_sum(posf[:, :, k], eq[:], AX.X), [tag + "_eq"], [tag + "_posf"])
    kb.cp("dve", posi[:], posf[:], r=[tag + "_posf"], w=[tag + "_posi"])
    kb.tt("dve", cmp3[:], pend[:].unsqueeze(1).to_broadcast([128, NB, 32]),
          C["blk_thr"][:, 0:NB].unsqueeze(2).to_broadcast([128, NB, 32]), ALU.is_le, r=[tag + "_pend", "c_blk_thr"], w=[tag + "_cmp3"])
    kb.S.op("dve", lambda e: e.reduce_sum(eb[:], cmp3[:], AX.X), [tag + "_cmp3"], [tag + "_eb"])
    kb.ts("dve", eb[:], eb[:], 31.0, None, ALU.min, r=[tag + "_eb"], w=[tag + "_eb"])
    kb.ts("dve", offf[:], eb[:].unsqueeze(2).to_broadcast([128, NB, KC]), float(D), float(l * 32 * D), ALU.mult, ALU.add,
          r=[tag + "_eb"], w=[tag + "_offf"])
    kb.tt("dve", offf[:], offf[:], C["base_pk"][:, 0:KC].unsqueeze(1).to_broadcast([128, NB, KC]), ALU.add,
          r=[tag + "_offf", "c_base_pk"], w=[tag + "_offf"])
    kb.cp("dve", offi[:], offf[:], r=[tag + "_offf"], w=[tag + "_offi"])
    kb.ts("dve", oh[:], eb[0:32, :], C["base_pk"][0:32, 0:1], None, ALU.is_equal, r=[tag + "_eb", "c_base_pk"], w=[tag + "_oh"])
    for i in range(NT):
        kb.dma("sp", h2[:], h2_d[i * 128:(i + 1) * 128, :], sem=tag + "_h2", r=[f"{tag}_h2d{i}"], w=[tag + "_h2"])
        for k in range(4):
            kb.S.dma("pool", lambda e, i=i, k=k: e.indirect_dma_start(
                out=xs_d, out_offset=IOA(ap=posi[:, i, k:k + 1], axis=0), in_=h2[:], in_offset=None),
                tag + "_h2", [tag + "_h2", tag + "_posi"], ["moe_xs"])
    kb.pop_scope()
    kb.push_scope()
    bgu_sb = kb.sb(tag + "_bgu", (32, 2048), BF16)
    bd_sb = kb.sb(tag + "_bd", (32, D), BF16)
    kb.dma("pool", bgu_sb[:], kb.dins["b_gate_up"][l], sem=tag + "_bgu", w=[tag + "_bgu"])
    kb.dma("pool", bd_sb[:], kb.dins["b_down"][l], sem=tag + "_bd", w=[tag + "_bd"])
    wgu = [kb.sb(tag + f"_wgu{i}", (128, KC, 2048), BF16) for i in range(2)]
    wd = [kb.sb(tag + f"_wd{i}", (128, KC, D), BF16) for i in range(2)]
    ohbs = [kb.sb(tag + f"_ohb{i}", (32, BLK), BF16) for i in range(2)]
    xr = [kb.sb(tag + f"_xr{i}", (128, D)) for i in range(2)]
    actT = kb.sb(tag + "_actT", (128, KC, BLK), BF16)
    g7 = kb.sb(tag + "_g7", (128, BLK))
    sg = kb.sb(tag + "_sg", (128, BLK))
    u7 = kb.sb(tag + "_u7", (128, BLK))
    yb = [kb.sb(tag + f"_yb{i}", (128, D)) for i in range(2)]
    wgu_flat = kb.dins["w_gate_up"].rearrange("l e r c -> (l e r) c")
    wd_flat = kb.dins["w_down"].rearrange("l e r c -> (l e r) c")

    def load_block_w(b, slot):
        for k in range(KC):
            kb.S.dma("pool", lambda e, b=b, k=k, slot=slot: e.indirect_dma_start(
                out=wgu[slot][:, k, :], out_offset=None, in_=wgu_flat, in_offset=IOA(ap=offi[:, b, k:k + 1], axis=0)),
                tag + f"_wgu{slot}", [tag + "_offi"], [tag + f"_wgu{slot}"])
        for k in range(KC):
            kb.S.dma("pool", lambda e, b=b, k=k, slot=slot: e.indirect_dma_start(
                out=wd[slot][:, k, :], out_offset=None, in_=wd_flat, in_offset=IOA(ap=offi[:, b, k:k + 1], axis=0)),
                tag + f"_wd{slot}", [tag + "_offi"], [tag + f"_wd{slot}"])

    def prep_block(b):
        hTs, hkey, ohb_ = hTb[b % 2], tag + f"_hT{b % 2}", ohbs[b % 2]
        for tt in range(NTB):
            xb = xr[nxc[0] % 2]
            xbk = tag + f"_xr{nxc[0] % 2}"
            nxc[0] += 1
            r0 = b * BLK + tt * 128
            kb.dma("sp", xb[:], xs_d[r0:r0 + 128, :], sem=xbk, r=["moe_xs"], w=[xbk])
            for half in range(2):
                pT, pk = P[half], kb.pk(half)
                for kk in range(4):
                    k = half * 4 + kk
                    kb.tr(pT[:, kk * 128:(kk + 1) * 128], xb[:, k * 128:(k + 1) * 128], C["ident"][:], r=[xbk, "c_ident"], w=pk)
                kb.cp("act" if half == 0 else "dve", hTs[:, half * 4:(half + 1) * 4, tt * 128:(tt + 1) * 128],
                      pT[:, :].rearrange("p (k t) -> p k t", k=4), r=pk, w=[hkey])
        kb.cp("dve", ohb_[:], oh[:, b:b + 1].to_broadcast([32, BLK]), r=[tag + "_oh"], w=[tag + f"_ohb{b % 2}"])

    NBR = int(os.environ.get("MOE_NBLK", NB))
    load_block_w(0, 0)
    nxc = [0]
    prep_block(0)
    for b in range(NBR):
        slot = b % 2
        if b + 1 < NBR:
            load_block_w(b + 1, 1 - slot)
        wk, dk = tag + f"_wgu{slot}", tag + f"_wd{slot}"
        hTs, hkey, ohb = hTb[slot], tag + f"_hT{slot}", ohbs[slot]
        ohk = tag + f"_ohb{slot}"
        for jc in range(KC):
            pb = 2 + (jc % 2) * 2
            PG, PU = P[pb], P[pb + 1]
            for which, PX in ((0, PG), (1, PU)):
                cols = slice(jc * 256 + which, (jc + 1) * 256, 2)
                for k in range(KC):
                    kb.mm(PX[:, :], wgu[slot][:, k, cols], hTs[:, k, :], start=(k == 0), stop=False,
                          r=[wk, hkey], w=kb.pk(pb + which))
                kb.mm(PX[:, :], bgu_sb[:, cols], ohb[:, :], start=False, stop=True, r=[tag + "_bgu", ohk], w=kb.pk(pb + which))
            kb.ts("dve", g7[:], PG[:, :], SW_LIMIT, None, ALU.min, r=kb.pk(pb), w=[tag + "_g7"])
            kb.ts("dve", u7[:], PU[:, :], -SW_LIMIT, SW_LIMIT, ALU.max, ALU.min, r=kb.pk(pb + 1), w=[tag + "_u7"])
            kb.act(sg[:], g7[:], AF.Sigmoid, scale=SW_ALPHA, r=[tag + "_g7"], w=[tag + "_sg"])
            kb.stt("dve", u7[:], u7[:], 1.0, g7[:], ALU.add, ALU.mult, r=[tag + "_u7", tag + "_g7"], w=[tag + "_u7"])
            kb.tt("dve", actT[:, jc, :], u7[:], sg[:], ALU.mult, r=[tag + "_u7", tag + "_sg"], w=[tag + "_actT"])
        if b + 1 < NBR:
            prep_block(b + 1)
        for tt in range(NTB):
            ybt, ybk = yb[tt % 2], tag + f"_yb{tt % 2}"
            for half in range(2):
                pi = 6 + half
                PO = P[pi]
                hs = slice(half * 512, (half + 1) * 512)
                for jc in range(KC):
                    kb.mm(PO[:, :], actT[:, jc, tt * 128:(tt + 1) * 128], wd[slot][:, jc, hs], start=(jc == 0), stop=False,
                          r=[tag + "_actT", dk], w=kb.pk(pi))
                kb.mm(PO[:, :], ohb[:, 0:128], bd_sb[:, hs], start=False, stop=True, r=[ohk, tag + "_bd"], w=kb.pk(pi))
                kb.cp("act" if half == 0 else "dve", ybt[:, hs], PO[:, :], r=kb.pk(pi), w=[ybk])
            r0 = b * BLK + tt * 128
            kb.dma("sp", ys_d[r0:r0 + 128, :], ybt[:], sem=ybk, r=[ybk], w=[tag + "_ys"])
    kb.pop_scope()
    kb.push_scope()
    NYK = 8
    yk = [kb.sb(tag + f"_yk{i}", (128, D)) for i in range(NYK)]
    acc = kb.sb(tag + "_acc", (128, D))
    xt = kb.sb(tag + "_xt", (128, D))
    if final is not None:
        nfr = kb.sb(tag + "_nfr", (128, D))
        kb.dma("sp", nfr[:], final["nf"].partition_broadcast(128), sem=tag + "_nfr", w=[tag + "_nfr"])
        fj = kb.sb(tag + "_fj", (128, D), BF16)
        fs = kb.sb(tag + "_fs", (128, 2))
    ng = 0
    for i in range(NT):
        kb.dma("sp", xt[:], xsrc[i * 128:(i + 1) * 128, :], sem=tag + "_xt", r=[xsrc_key(i)], w=[tag + "_xt"])
        for k in range(4):
            yt, ytk = yk[ng % NYK], tag + f"_yk{ng % NYK}"
            ng += 1
            kb.S.dma("pool", lambda e, i=i, k=k, yt=yt: e.indirect_dma_start(
                out=yt[:], out_offset=None, in_=ys_d, in_offset=IOA(ap=posi[:, i, k:k + 1], axis=0)),
                ytk, [tag + "_ys", tag + "_posi"], [ytk])
            if k == 0:
                kb.ts("dve", acc[:], yt[:], gk_all[:, i, k:k + 1], None, ALU.mult, r=[ytk, tag + "_gk"], w=[tag + "_acc"])
            else:
                kb.stt("dve", acc[:], yt[:], gk_all[:, i, k:k + 1], acc[:], ALU.mult, ALU.add, r=[ytk, tag + "_gk", tag + "_acc"], w=[tag + "_acc"])
        kb.tt("pool", acc[:], acc[:], gf_row[:], ALU.mult, r=[tag + "_acc", gf_key], w=[tag + "_acc"])
        kb.tt("pool", acc[:], acc[:], xt[:], ALU.add, r=[tag + "_acc", tag + "_xt"], w=[tag + "_acc"])
        if final is None:
            kb.dma("sp", xdst[i * 128:(i + 1) * 128, :], acc[:], sem=tag + "_acc", r=[tag + "_acc"], w=[xdst_key(i)])
        else:
            kb.act(fj[:], acc[:], AF.Square, accum_out=fs[:, 0:1], r=[tag + "_acc"], w=[tag + "_fj", tag + "_fs"])
            kb.ts("dve", fs[:, 1:2], fs[:, 0:1], 1.0 / D, EPS, ALU.mult, ALU.add, r=[tag + "_fs"], w=[tag + "_fs"])
            kb.act(fs[:, 1:2], fs[:, 1:2], AF.Sqrt, r=[tag + "_fs"], w=[tag + "_fs"])
            kb.S.op("dve", lambda e: e.reciprocal(fs[:, 1:2], fs[:, 1:2]), [tag + "_fs"], [tag + "_fs"])
            kb.stt("dve", acc[:], acc[:], fs[:, 1:2], nfr[:], ALU.mult, ALU.mult, r=[tag + "_acc", tag + "_fs", tag + "_nfr"], w=[tag + "_acc"])
            kb.dma("sp", final["out"][i * 128:(i + 1) * 128, :], acc[:], sem=tag + "_acc", r=[tag + "_acc"], w=[f"out{i}"])
    kb.pop_scope()
```

```python
import numpy as np
from contextlib import ExitStack
from concourse.bass_utils import run_bass_kernel_spmd

import concourse.bass as bass
import concourse.mybir as mybir

ENGINES = ("pe", "act", "dve", "pool", "sp")


class Op:
    __slots__ = ("eng", "fn", "deps", "is_dma", "dsem", "dcount", "signal", "idx", "signo")

    def __init__(self, eng, fn):
        self.eng = eng
        self.fn = fn
        self.deps = []
        self.is_dma = False
        self.dsem = None
        self.dcount = 0
        self.signal = False
        self.idx = -1
        self.signo = 0


class Sched:
    def __init__(self, nc, same_engine_sync=True):
        self.nc = nc
        self.q = {e: [] for e in ENGINES}
        self.res_w = {}
        self.res_r = {}
        self.phys = []
        self.key2phys = {}
        self.free_phys = []
        self.same_engine_sync = same_engine_sync

    def _collect(self, op, reads, writes, my_dma_key=None):
        deps = []
        for k in reads:
            t = self.res_w.get(k)
            if t is not None:
                deps.append(t)
        for k in writes:
            t = self.res_w.get(k)
            if t is not None:
                if not (my_dma_key is not None and t[0] == 'dma' and t[1] == my_dma_key):
                    deps.append(t)
            deps.extend(self.res_r.get(k, ()))
        op.deps = deps

    def _commit(self, tok, reads, writes):
        for k in reads:
            self.res_r.setdefault(k, []).append(tok)
        for k in writes:
            self.res_w[k] = tok
            self.res_r[k] = []

    @staticmethod
    def _excl(reads, writes):
        rp = [k for k in reads if len(k) == 2 and k[0] == "P" and k[1].isdigit()]
        if not rp:
            return reads, writes
        return [k for k in reads if k not in rp], list(writes) + [k for k in rp if k not in writes]

    def op(self, eng, fn, reads=(), writes=()):
        reads, writes = self._excl(reads, writes)
        o = Op(eng, fn)
        self._collect(o, reads, writes)
        o.idx = len(self.q[eng])
        self.q[eng].append(o)
        self._commit(('op', o), reads, writes)
        return o

    def dma(self, eng, fn, sem_key, reads=(), writes=()):
        reads, writes = self._excl(reads, writes)
        if sem_key not in self.key2phys:
            if self.free_phys:
                p = self.free_phys.pop()
            else:
                p = len(self.phys)
                self.phys.append(0)
            self.key2phys[sem_key] = p
        p = self.key2phys[sem_key]
        o = Op(eng, fn)
        o.is_dma = True
        self._collect(o, reads, writes, my_dma_key=p)
        self.phys[p] += 1
        c = self.phys[p]
        o.dsem = p
        o.dcount = c
        o.idx = len(self.q[eng])
        self.q[eng].append(o)
        self._commit(('dma', p, c), reads, writes)
        return o

    def final_wait(self, eng, keys):
        o = Op(eng, None)
        deps = []
        for k in keys:
            t = self.res_w.get(k)
            if t is not None:
                deps.append(t)
            deps.extend(self.res_r.get(k, ()))
        o.deps = deps
        o.idx = len(self.q[eng])
        self.q[eng].append(o)

    def barrier(self):
        toks = []
        for e in ENGINES:
            for o in reversed(self.q[e]):
                if o.fn is not None and not o.is_dma:
                    toks.append(('op', o))
                    break
        for p, c in enumerate(self.phys):
            if c:
                toks.append(('dma', p, c))
        for e in ENGINES:
            o = Op(e, None)
            o.deps = list(toks)
            o.idx = len(self.q[e])
            self.q[e].append(o)
        self.free_phys = list(range(len(self.phys)))[::-1]
        self.key2phys = {}

    def emit(self, stack):
        nc = self.nc
        for e in ENGINES:
            for o in self.q[e]:
                for t in o.deps:
                    if t[0] == 'op':
                        tgt = t[1]
                        if tgt.eng == o.eng and (not self.same_engine_sync or o.eng == 'pe'):
                            continue
                        tgt.signal = True
        for e in ENGINES:
            n = 0
            for o in self.q[e]:
                if o.signal:
                    n += 1
                    o.signo = n
        esem = {e: stack.enter_context(nc.semaphore("s_" + e)) for e in ENGINES}
        dsem = {}
        for p in range(len(self.phys)):
            dsem[p] = stack.enter_context(nc.semaphore(f"d_{p}"))
        block = stack.enter_context(nc.Block())
        stats = {}

        def run(e, engobj):
            waited = {}
            nwait = 0
            for o in self.q[e]:
                need = {}
                for t in o.deps:
                    if t[0] == 'op':
                        tgt = t[1]
                        if tgt.eng == e and (not self.same_engine_sync or e == 'pe'):
                            continue
                        key = ('e', tgt.eng)
                        val = tgt.signo
                    else:
                        key = ('d', t[1])
                        val = t[2] * 16
                    if need.get(key, 0) < val:
                        need[key] = val
                for key, val in need.items():
                    if waited.get(key, 0) >= val:
                        continue
                    waited[key] = val
                    sem = esem[key[1]] if key[0] == 'e' else dsem[key[1]]
                    engobj.wait_ge(sem, val)
                    nwait += 1
                if o.fn is None:
                    continue
                ins = o.fn(engobj)
                if o.is_dma:
                    ins.then_inc(dsem[o.dsem], 16)
                elif o.signal:
                    ins.then_inc(esem[e], 1)
            stats[e] = (len(self.q[e]), nwait)

        @block.tensor
        def _(eng):
            run("pe", eng)

        @block.scalar
        def _(eng):
            run("act", eng)

        @block.vector
        def _(eng):
            run("dve", eng)

        @block.gpsimd
        def _(eng):
            run("pool", eng)

        @block.sync
        def _(eng):
            run("sp", eng)

        return stats


F32 = mybir.dt.float32
BF16 = mybir.dt.bfloat16
I32 = mybir.dt.int32
AF = mybir.ActivationFunctionType
ALU = mybir.AluOpType
AX = mybir.AxisListType

S_TOK = 4096
D = 1024
KC = 8
NT = S_TOK // 128
DEPTH = 2
EPS = 1e-6
IN_COLS = 7968
C_GLA_Q, C_GLA_K, C_GLA_V, C_GLA_LR, C_GLA_R = 0, 256, 512, 1024, 1040
C_GDN_QKV, C_GDN_A, C_GDN_B, C_GDN_G = 1552, 3088, 3092, 3096
C_SSD_Z, C_SSD_XBC, C_SSD_DT, C_MERGE = 3608, 4120, 4888, 4896


class KB:
    def __init__(self, same_engine_sync=True):
        self.nc = bass.Bass("TRN2", target_bir_lowering=False)
        self.S = Sched(self.nc, same_engine_sync=same_engine_sync)
        self.stack = ExitStack()
        self.ins = {}
        self.outs = {}
        self._n = 0
        self.scopes = []
        self._allow_p = False
        self.P = [self.stack.enter_context(self.nc.psum_tensor(f"PB{i}", [128, 512], F32)) for i in range(8)]

    @staticmethod
    def pk(i, a=0, b=512):
        return [f"P{i}"]

    def din(self, name, shape, dt=F32):
        t = self.nc.dram_tensor(name, list(shape), dt, kind="ExternalInput")
        self.ins[name] = t
        return t.ap()

    def dout(self, name, shape, dt=F32):
        t = self.nc.dram_tensor(name, list(shape), dt, kind="ExternalOutput")
        self.outs[name] = t
        return t.ap()

    def dscr(self, name, shape, dt=F32, debug=False):
        if debug:
            return self.dout(name, shape, dt)
        return self.nc.dram_tensor(name, list(shape), dt, kind="Internal").ap()

    def sb(self, name, shape, dt=F32):
        st = self.scopes[-1] if self.scopes else self.stack
        return st.enter_context(self.nc.sbuf_tensor(name, list(shape), dt))

    def sbp(self, name, shape, dt=F32):
        assert not self.scopes or self._allow_p
        return self.stack.enter_context(self.nc.sbuf_tensor(name, list(shape), dt))

    def push_scope(self):
        self.scopes.append(ExitStack())

    def pop_scope(self):
        self.S.barrier()
        self.scopes.pop().close()

    def ps(self, name, shape=(128, 512), dt=F32):
        return self.stack.enter_context(self.nc.psum_tensor(name, list(shape), dt))

    def mm(self, out, lhsT, rhs, start=True, stop=True, r=(), w=()):
        return self.S.op("pe", lambda e: e.matmul(out, lhsT, rhs, start=start, stop=stop), r, w)

    def tr(self, out, in_, ident, r=(), w=()):
        return self.S.op("pe", lambda e: e.transpose(out, in_, ident), r, w)

    def act(self, out, in_, func, bias=None, scale=None, accum_out=None, r=(), w=(), eng="act"):
        kw = {}
        if bias is not None:
            kw["bias"] = bias
        if scale is not None:
            kw["scale"] = scale
        if accum_out is not None:
            kw["accum_out"] = accum_out
        return self.S.op(eng, lambda e: e.activation(out, in_, func, **kw), r, w)

    def ts(self, eng, out, in0, s1, s2, op0, op1=None, accum_out=None, r=(), w=()):
        kw = {}
        if op1 is not None:
            kw["op1"] = op1
        if accum_out is not None:
            kw["accum_out"] = accum_out
        return self.S.op(eng, lambda e: e.tensor_scalar(out, in0, s1, s2, op0, **kw), r, w)

    def tt(self, eng, out, in0, in1, op, r=(), w=()):
        return self.S.op(eng, lambda e: e.tensor_tensor(out, in0, in1, op), r, w)

    def stt(self, eng, out, in0, scalar, in1, op0, op1, r=(), w=()):
        return self.S.op(eng, lambda e: e.scalar_tensor_tensor(out, in0, scalar, in1, op0, op1), r, w)

    def cp(self, eng, out, in_, r=(), w=()):
        if eng == "act":
            return self.S.op(eng, lambda e: e.copy(out, in_), r, w)
        return self.S.op(eng, lambda e: e.tensor_copy(out, in_), r, w)

    def dma(self, eng, out, in_, sem, r=(), w=(), **kw):
        return self.S.dma(eng, lambda e: e.dma_start(out, in_, **kw), sem, r, w)


def phase_consts(kb):
    c = {}
    cdefs = {
        "ident": (128, 128), "tri_incl": (128, 128), "tri_strict": (128, 128), "ones": (128, 128),
        "blk": (128, 128), "selA": (128, 128), "selB": (128, 128), "neg_strict": (128, 128),
        "tri_full": (128, 128), "blk_thr": (128, 64), "base_pk": (128, 8),
    }
    for name, shp in cdefs.items():
        src = kb.din("c_" + name, shp)
        t = kb.sb("cs_" + name, shp)
        kb.dma("sp", t[:], src, sem="c_" + name, w=["c_" + name])
        c[name] = t
        tb = kb.sb("cb_" + name, shp, BF16)
        kb.cp("dve", tb[:], t[:], r=["c_" + name], w=["cb_" + name])
        c[name + "_bf"] = tb
    kb.C = c


def phase_mod(kb):
    nc = kb.nc
    cT = kb.din("cT", (128, KC))
    w_mod = kb.din("w_mod", (DEPTH, D, 6 * D))
    bmodc = kb.din("bmodc", (DEPTH, 128, 48))
    bmodrow = kb.din("bmodrow", (DEPTH, 6, D))
    nmixc = kb.din("nmixc", (DEPTH, 128, KC))
    nffnc = kb.din("nffnc", (DEPTH, 128, KC))
    pers = {}
    for l in range(DEPTH):
        pers[f"modc{l}"] = kb.sbp(f"modc{l}", (128, 48))
        pers[f"modscl{l}"] = kb.sbp(f"modscl{l}", (128, 2, KC))
        for piece in (2, 5):
            pers[f"modrow{l}_{piece}"] = kb.sbp(f"modrow{l}_{piece}", (128, D))
    kb.modrow_d = [[kb.dscr(f"modrowd{l}_{j}", (128, D)) for j in range(2)] for l in range(DEPTH)]
    kb.push_scope()
    rowtmp = kb.sb("modrowtmp", (128, D))
    cact = kb.sb("cact", (128, KC))
    crep = kb.sb("crep", (128, KC, 128))
    kb.dma("sp", cact[:], cT, sem="cact", w=["cact"])
    kb.act(cact[:], cact[:], AF.Silu, r=["cact"], w=["cact"])
    for k in range(KC):
        kb.cp("dve", crep[:, k, :], cact[:, k:k + 1].to_broadcast([128, 128]), r=["cact"], w=["crep"])
    wbuf = [kb.sb(f"modw{i}", (128, KC, 1024)) for i in range(2)]
    pcol = kb.P[0]
    prow = [kb.P[1], kb.P[2]]
    kb.modc, kb.gm_row, kb.gf_row = [], [], []
    kb.sclm, kb.shm, kb.sclf, kb.shf = [], [], [], []
    it = 0
    for l in range(DEPTH):
        modc = pers[f"modc{l}"]
        bc = kb.sb(f"bmodc{l}", (128, 48))
        nm = kb.sb(f"nmixc{l}", (128, KC))
        nf = kb.sb(f"nffnc{l}", (128, KC))
        kb.dma("sp", bc[:], bmodc[l], sem=f"bmodc{l}", w=[f"bmodc{l}"])
        kb.dma("sp", nm[:], nmixc[l], sem=f"nmixc{l}", w=[f"nmixc{l}"])
        kb.dma("sp", nf[:], nffnc[l], sem=f"nffnc{l}", w=[f"nffnc{l}"])
        rows = []
        for piece in range(6):
            wb = wbuf[it % 2]
            wk = f"modw{it % 2}"
            it += 1
            src = w_mod[l, :, piece * 1024:(piece + 1) * 1024].rearrange("(k p) c -> p k c", p=128)
            for hh in range(2):
                kb.dma("sp", wb[:, hh * 4:(hh + 1) * 4, :], src[:, hh * 4:(hh + 1) * 4, :],
                       sem=wk, w=[wk])
            for jj in range(8):
                j = piece * 8 + jj
                for k in range(KC):
                    kb.mm(pcol[:, j:j + 1], wb[:, k, jj * 128:(jj + 1) * 128], cact[:, k:k + 1],
                          start=(k == 0), stop=(k == KC - 1), r=[wk, "cact"], w=kb.pk(0, 0, 128))
            if piece in (2, 3, 4, 5):
                row = pers[f"modrow{l}_{piece}"] if piece in (2, 5) else rowtmp
                if piece in (3, 4):
                    pers_key = f"modrow{l}_{piece}"
                kb.dma("sp", row[:], bmodrow[l, piece].partition_broadcast(128),
                       sem=f"modrow{l}_{piece}", w=[f"modrow{l}_{piece}"])
                for hh in range(2):
                    for k in range(KC):
                        kb.mm(prow[hh][:, :], crep[:, k, :], wb[:, k, hh * 512:(hh + 1) * 512],
                              start=(k == 0), stop=(k == KC - 1), r=[wk, "crep"], w=kb.pk(1 + hh))
                    kb.tt("dve", row[:, hh * 512:(hh + 1) * 512], prow[hh][:, :], row[:, hh * 512:(hh + 1) * 512],
                          ALU.add, r=kb.pk(1 + hh) + [f"modrow{l}_{piece}"], w=[f"modrow{l}_{piece}"])
                if piece in (2, 5):
                    rows.append((row, f"modrow{l}_{piece}"))
                else:
                    kb.dma("sp", kb.modrow_d[l][piece - 3], row[:], sem=f"modrow{l}_{piece}", r=[f"modrow{l}_{piece}"],
                           w=[f"modrowd{l}"])
        kb.tt("dve", modc[:], pcol[:, 0:48], bc[:], ALU.add, r=kb.pk(0, 0, 128) + [f"bmodc{l}"], w=[f"modc{l}"])
        scl = pers[f"modscl{l}"]
        kb.stt("dve", scl[:, 0, :], modc[:, 8:16], 1.0, nm[:], ALU.add, ALU.mult,
               r=[f"modc{l}", f"nmixc{l}"], w=[f"modc{l}"])
        kb.stt("dve", scl[:, 1, :], modc[:, 32:40], 1.0, nf[:], ALU.add, ALU.mult,
               r=[f"modc{l}", f"nffnc{l}"], w=[f"modc{l}"])
        kb.modc.append(modc)
        kb.gm_row.append(rows[0])
        kb.gf_row.append(rows[1])
        kb.sclm.append(scl[:, 0, :])
        kb.shm.append(modc[:, 0:8])
        kb.sclf.append(scl[:, 1, :])
        kb.shf.append(modc[:, 24:32])
    kb.pop_scope()


def norm_bufs(kb, tag):
    NB = 2
    b = {
        "tag": tag,
        "xt": [kb.sb(f"{tag}_x{i}", (128, D)) for i in range(NB)],
        "xn": [kb.sb(f"{tag}_xn{i}", (128, D)) for i in range(NB)],
        "junk": kb.sb(f"{tag}_junk", (128, D), BF16),
        "ss": kb.sb(f"{tag}_ss", (128, 2)),
        "rstd": kb.sb(f"{tag}_rstd", (128, 2)),
        "n": 0,
    }
    return b


def norm_front(kb, nb, xsrc_ap, xsrc_key):
    tag = nb["tag"]
    b = nb["n"] % 2
    nb["n"] += 1
    xt, xn, junk, ss, rstd = nb["xt"][b], nb["xn"][b], nb["junk"], nb["ss"], nb["rstd"]
    xk, xnk = f"{tag}_x{b}", f"{tag}_xn{b}"
    sk, rk = f"{tag}_ss{b}", f"{tag}_rstd{b}"
    kb.dma("sp", xt[:], xsrc_ap, sem=xk, r=[xsrc_key], w=[xk])
    kb.act(junk[:], xt[:], AF.Square, accum_out=ss[:, b:b + 1], r=[xk], w=[f"{tag}_junk", sk])
    kb.ts("dve", rstd[:, b:b + 1], ss[:, b:b + 1], 1.0 / D, EPS, ALU.mult, ALU.add, r=[sk], w=[rk])
    kb.act(rstd[:, b:b + 1], rstd[:, b:b + 1], AF.Sqrt, r=[rk], w=[rk])
    kb.S.op("dve", lambda e: e.reciprocal(rstd[:, b:b + 1], rstd[:, b:b + 1]), [rk], [rk])
    kb.ts("pool", xn[:], xt[:], rstd[:, b:b + 1], None, ALU.mult, r=[xk, rk], w=[xnk])
    return b


def norm_back(kb, nb, b, l, which, dst, dst_key):
    C = kb.C
    tag = nb["tag"]
    scl = kb.sclm[l] if which == "m" else kb.sclf[l]
    sh = kb.shm[l] if which == "m" else kb.shf[l]
    mkey = f"modc{l}"
    xn, xnk = nb["xn"][b], f"{tag}_xn{b}"
    for half in range(2):
        pT = kb.P[half]
        pk = kb.pk(half)
        for kk in range(4):
            k = half * 4 + kk
            kb.tr(pT[:, kk * 128:(kk + 1) * 128], xn[:, k * 128:(k + 1) * 128], C["ident"][:], r=[xnk, "c_ident"], w=pk)
        for kk in range(4):
            k = half * 4 + kk
            d = dst[:, k, :]
            src = pT[:, kk * 128:(kk + 1) * 128]
            if kk % 2 == 0:
                kb.act(d, src, AF.Identity, bias=sh[:, k:k + 1], scale=scl[:, k:k + 1], r=pk + [mkey], w=[dst_key])
            else:
                kb.ts("dve", d, src, scl[:, k:k + 1], sh[:, k:k + 1], ALU.mult, ALU.add, r=pk + [mkey], w=[dst_key])


def norm_tile(kb, nb, l, which, xsrc_ap, xsrc_key, dst, dst_key):
    b = norm_front(kb, nb, xsrc_ap, xsrc_key)
    norm_back(kb, nb, b, l, which, dst, dst_key)


def phase_norm(kb, l, which, xsrc, xsrc_key, hT, hT_key):
    nb = norm_bufs(kb, f"n{l}{which}")
    cur = norm_front(kb, nb, xsrc[0:128, :], xsrc_key(0))
    for i in range(NT):
        nxt = norm_front(kb, nb, xsrc[(i + 1) * 128:(i + 2) * 128, :], xsrc_key(i + 1)) if i + 1 < NT else None
        norm_back(kb, nb, cur, l, which, hT[:, :, i * 128:(i + 1) * 128], hT_key(i))
        cur = nxt


def _consts():
    i = np.arange(128)
    same = (i[:, None] // 64) == (i[None, :] // 64)
    return {
        "c_ident": np.eye(128, dtype=np.float32),
        "c_tri_incl": ((i[:, None] <= i[None, :]) & same).astype(np.float32),
        "c_tri_strict": ((i[:, None] > i[None, :]) & same).astype(np.float32),
        "c_ones": np.ones((128, 128), np.float32),
        "c_blk": same.astype(np.float32),
        "c_selA": np.repeat((i < 64).astype(np.float32)[:, None], 128, 1),
        "c_selB": np.repeat((i >= 64).astype(np.float32)[:, None], 128, 1),
        "c_neg_strict": -((i[:, None] > i[None, :]) & same).astype(np.float32),
        "c_tri_full": (i[:, None] < i[None, :]).astype(np.float32),
        "c_blk_thr": np.repeat((np.arange(64, dtype=np.float32) * 512.0)[None, :], 128, 0),
        "c_base_pk": (i[:, None] + 128 * np.arange(8)[None, :]).astype(np.float32),
    }


def host_inputs(inp, b, names):
    m = {}
    m.update(_consts())
    m["x"] = np.ascontiguousarray(inp["x"][b])
    m["cT"] = np.ascontiguousarray(inp["c"][b].reshape(KC, 128).T)
    m["w_mod"] = inp["w_mod"]
    m["bmodc"] = np.ascontiguousarray(inp["b_mod"].reshape(DEPTH, 48, 128).transpose(0, 2, 1))
    bm = inp["b_mod"].reshape(DEPTH, 6, D)
    m["bmodrow"] = np.ascontiguousarray(bm)
    m["nmixc"] = np.ascontiguousarray(inp["norm_mix"].reshape(DEPTH, KC, 128).transpose(0, 2, 1))
    m["nffnc"] = np.ascontiguousarray(inp["norm_ffn"].reshape(DEPTH, KC, 128).transpose(0, 2, 1))
    m.update(host_inputs2(inp, b, names))
    return {k: np.ascontiguousarray(m[k], dtype=m[k].dtype) for k in names}


def proj_feat(kb, out_ps, w_sb, wkey, c0, ncols, hT, hkey, t0, nt, wkeys=None):
    for k in range(KC):
        kb.mm(out_ps, w_sb[:, k, c0:c0 + ncols], hT[:, k, t0:t0 + nt], start=(k == 0), stop=(k == KC - 1),
              r=[wkey, hkey], w=wkeys)


def proj_tok(kb, out_ps, w_sb, wkey, c0, ncols, hT, hkey, t0, nt, wkeys=None):
    for k in range(KC):
        kb.mm(out_ps, hT[:, k, t0:t0 + nt], w_sb[:, k, c0:c0 + ncols], start=(k == 0), stop=(k == KC - 1),
              r=[wkey, hkey], w=wkeys)


def load_w_cast(kb, dst, dkey, src_dram_2d, c0, ncols, nk=KC, step=512):
    src = src_dram_2d.rearrange("(k p) c -> p k c", p=128)
    for k in range(nk):
        kb.dma("pool", dst[:, k, 0:ncols], src[:, k, c0:c0 + ncols], sem=dkey, w=[dkey])


def rms_rstd(kb, tag, rs, ssq, n, width):
    kb.ts("dve", rs[:, 0:n], ssq[:, 0:n], 1.0 / width, EPS, ALU.mult, ALU.add, r=[tag + "_ssq"], w=[tag + "_rs"])
    kb.act(rs[:, 0:n], rs[:, 0:n], AF.Sqrt, r=[tag + "_rs"], w=[tag + "_rs"])
    kb.S.op("dve", lambda e: e.reciprocal(rs[:, 0:n], rs[:, 0:n]), [tag + "_rs"], [tag + "_rs"])


def phase_gla(kb, l, hT, hkey_fn, obr):
    C = kb.C
    tag = f"gla{l}"
    w_in = kb.w_in
    NW = 1552
    wg = kb.sb(tag + "_w", (128, KC, NW), BF16)
    load_w_cast(kb, wg, tag + "_w", w_in[l], 0, NW)
    w2 = kb.sb(tag + "_w2", (16, 256))
    b2 = kb.sb(tag + "_b2", (1, 256))
    gn = kb.sb(tag + "_gn", (128, 512))
    kb.dma("sp", w2[:], kb.din(tag + "_w2d", (16, 256)), sem=tag + "_w2", w=[tag + "_w2"])
    kb.dma("sp", b2[:], kb.din(tag + "_b2d", (1, 256)), sem=tag + "_b2", w=[tag + "_b2"])
    kb.dma("sp", gn[:], kb.din(tag + "_gnd", (1, 512))[0].partition_broadcast(128), sem=tag + "_gn", w=[tag + "_gn"])
    lrT = kb.sb(tag + "_lrT", (16, 128))
    sp = kb.sb(tag + "_sp", (128, 256))
    e_rem = kb.sb(tag + "_erem", (128, 256))
    e_pos = kb.sb(tag + "_epos", (128, 256))
    e_neg = kb.sb(tag + "_eneg", (128, 256))
    qdT = kb.sb(tag + "_qdT", (128, 256), BF16)
    knT = kb.sb(tag + "_knT", (128, 256), BF16)
    krem = kb.sb(tag + "_krem", (128, 256), BF16)
    v_sb = kb.sb(tag + "_v", (128, 512), BF16)
    r_sb = kb.sb(tag + "_r", (128, 512))
    attT = [kb.sb(tag + f"_attT{i}", (128, 128), BF16) for i in range(4)]
    S = [kb.sb(tag + f"_S{i}", (128, 256)) for i in range(2)]
    Sb = [kb.sb(tag + f"_Sb{i}", (128, 256), BF16) for i in range(2)]
    junk = kb.sb(tag + "_junk", (128, 128), BF16)
    ssq = kb.sb(tag + "_ssq", (128, 4))
    rs = kb.sb(tag + "_rs", (128, 4))
    og = kb.sb(tag + "_og", (128, 512))
    oint = kb.sb(tag + "_oint", (128, 512))
    o_sb = kb.sb(tag + "_o", (128, 512))
    oT = kb.sb(tag + "_oT", (128, 512), BF16)
    for p in range(2):
        kb.S.op("dve", lambda e, p=p: e.memset(S[p][:], 0.0), [], [tag + f"_S{p}"])
        kb.S.op("dve", lambda e, p=p: e.memset(Sb[p][:], 0.0), [], [tag + f"_Sb{p}"])
    P0, P1, P2, P3, P4, P5, P6, P7 = kb.P
    wk = tag + "_w"
    import os
    STOP = float(os.environ.get('GLA_STOP', '9'))
    for i in range(int(os.environ.get('GLA_NT', NT))):
        t0 = i * 128
        hk = hkey_fn(i)
        for pair in range(2):
            proj_feat(kb, P0[:, pair * 128:(pair + 1) * 128], wg, wk, C_GLA_Q + pair * 128, 128, hT, hk, t0, 128, kb.pk(0, pair * 128, pair * 128 + 128))
            proj_feat(kb, P0[:, 256 + pair * 128:256 + (pair + 1) * 128], wg, wk, C_GLA_K + pair * 128, 128, hT, hk, t0, 128, kb.pk(0, 256 + pair * 128, 384 + pair * 128))
        proj_feat(kb, P1[0:16, 0:128], wg, wk, C_GLA_LR, 16, hT, hk, t0, 128, kb.pk(1, 0, 128))
        proj_tok(kb, P2[:, 0:256], wg, wk, C_GLA_K, 256, hT, hk, t0, 128, kb.pk(2, 0, 256))
        proj_tok(kb, P3[:, :], wg, wk, C_GLA_V, 512, hT, hk, t0, 128, kb.pk(3))
        proj_tok(kb, P4[:, :], wg, wk, C_GLA_R, 512, hT, hk, t0, 128, kb.pk(4))
        if STOP <= 1:
            continue
        kb.cp("dve", lrT[:, :], P1[0:16, 0:128], r=kb.pk(1, 0, 128), w=[tag + "_lrT"])
        kb.mm(P1[:, 128:384], lrT[:, :], w2[:, :], start=True, stop=False, r=[tag + "_lrT", tag + "_w2"], w=kb.pk(1, 128, 384))
        kb.mm(P1[:, 128:384], C["ones"][0:1, :], b2[0:1, :], start=False, stop=True, r=["c_ones", tag + "_b2"], w=kb.pk(1, 128, 384))
        kb.act(sp[:], P1[:, 128:384], AF.Exp, scale=-1.0, r=kb.pk(1, 128, 384), w=[tag + "_sp"])
        kb.act(sp[:], sp[:], AF.Ln, bias=1.0, r=[tag + "_sp"], w=[tag + "_sp"])
        if STOP <= 2:
            continue
        kb.cp("act", v_sb[:], P3[:, :], r=kb.pk(3), w=[tag + "_v"])
        kb.act(r_sb[:], P4[:, :], AF.Silu, r=kb.pk(4), w=[tag + "_r"])
        kb.mm(P5[:, 256:512], C["tri_strict"][:], sp[:], r=["c_tri_strict", tag + "_sp"], w=kb.pk(5, 256, 512))
        for pair in range(2):
            kb.mm(P6[:, pair * 128:(pair + 1) * 128], sp[:, pair * 128:(pair + 1) * 128], C["tri_incl"][:],
                  r=["c_tri_incl", tag + "_sp"], w=kb.pk(6, 0, 256))
        kb.act(e_rem[:], P5[:, 256:512], AF.Exp, scale=-1.0 / 16, r=kb.pk(5, 256, 512), w=[tag + "_erem"])
        kb.act(e_pos[:], P6[:, 0:256], AF.Exp, scale=-1.0 / 16, r=kb.pk(6, 0, 256), w=[tag + "_epos"])
        kb.act(e_neg[:], P6[:, 0:256], AF.Exp, scale=1.0 / 16, r=kb.pk(6, 0, 256), w=[tag + "_eneg"])
        kb.stt("dve", qdT[:], P0[:, 0:256], 0.125, e_pos[:], ALU.mult, ALU.mult, r=kb.pk(0, 0, 256) + [tag + "_epos"], w=[tag + "_qdT"])
        kb.tt("dve", knT[:], P0[:, 256:512], e_neg[:], ALU.mult, r=kb.pk(0, 256, 512) + [tag + "_eneg"], w=[tag + "_knT"])
        kb.tt("dve", krem[:], P2[:, 0:256], e_rem[:], ALU.mult, r=kb.pk(2, 0, 256) + [tag + "_erem"], w=[tag + "_krem"])
        if STOP <= 3:
            continue
        zones = [(P1[:, 384:512], kb.pk(1)), (P2[:, 256:384], kb.pk(2)), (P3[:, 0:128], kb.pk(3)), (P4[:, 0:128], kb.pk(4))]
        for h in range(4):
            pair, rows = h // 2, (h % 2) * 64
            pc = slice(pair * 128, (pair + 1) * 128)
            aps, apk = zones[h]
            kb.mm(aps, knT[rows:rows + 64, pc], qdT[rows:rows + 64, pc], r=[tag + "_knT", tag + "_qdT"], w=apk)
        for h in range(4):
            aps, apk = zones[h]
            kb.tt("dve", attT[h][:], aps, C["tri_incl"][:], ALU.mult, r=apk + ["c_tri_incl"], w=[tag + f"_attT{h}"])
        for h in range(4):
            hc = slice(h * 128, (h + 1) * 128)
            kb.mm(P7[:, hc], attT[h][:], v_sb[:, hc], start=True, stop=True, r=[tag + f"_attT{h}", tag + "_v"], w=kb.pk(7))

        def pairchain(pair):
            pc0 = pair * 128
            Sk, Sbk = tag + f"_S{pair}", tag + f"_Sb{pair}"
            PU, ku = (P6, kb.pk(6)) if pair == 0 else (P3, kb.pk(3))
            for ch in range(2):
                tr_ = slice(ch * 64, (ch + 1) * 64)
                kb.mm(P5[tr_, pair * 256:(pair + 1) * 256], qdT[:, pc0 + ch * 64:pc0 + (ch + 1) * 64],
                      Sb[pair][:, :], start=True, stop=True, r=[tag + "_qdT", Sbk], w=kb.pk(5))
                kb.mm(PU[:, 256:512], krem[tr_, pc0:pc0 + 128], v_sb[tr_, pair * 256:(pair + 1) * 256],
                      r=[tag + "_krem", tag + "_v"], w=ku)
                yield
                for hh in range(2):
                    rr = slice(hh * 64, (hh + 1) * 64)
                    cc = slice(hh * 128, (hh + 1) * 128)
                    dec = e_pos[rr, pc0 + ch * 64 + 63:pc0 + ch * 64 + 64]
                    kb.stt("dve", S[pair][rr, cc], S[pair][rr, cc], dec, PU[rr, 256 + hh * 128:256 + (hh + 1) * 128],
                           ALU.mult, ALU.add, r=[Sk, tag + "_epos"] + ku, w=[Sk])
                yield
                kb.cp("act", Sb[pair][:], S[pair][:], r=[Sk], w=[Sbk])
                yield

        gens = [pairchain(0), pairchain(1)]
        while gens:
            for gen in list(gens):
                try:
                    next(gen)
                except StopIteration:
                    gens.remove(gen)
        if STOP <= 5:
            continue
        kb.cp("act", oint[:], P5[:, :], r=kb.pk(5), w=[tag + "_oint"])
        kb.tt("dve", o_sb[:], P7[:, :], oint[:], ALU.add, r=kb.pk(7) + [tag + "_oint"], w=[tag + "_o"])
        for h in range(4):
            kb.act(junk[:], o_sb[:, h * 128:(h + 1) * 128], AF.Square, accum_out=ssq[:, h:h + 1], r=[tag + "_o"],
                   w=[tag + "_junk", tag + "_ssq"])
        rms_rstd(kb, tag, rs, ssq, 4, 128)
        for h in range(4):
            hc = slice(h * 128, (h + 1) * 128)
            kb.stt("dve", og[:, hc], o_sb[:, hc], rs[:, h:h + 1], gn[:, hc], ALU.mult, ALU.mult,
                   r=[tag + "_o", tag + "_rs", tag + "_gn"], w=[tag + "_og"])
        kb.tt("pool", og[:], og[:], r_sb[:], ALU.mult, r=[tag + "_og", tag + "_r"], w=[tag + "_og"])
        for c in range(4):
            kb.tr(P0[:, c * 128:(c + 1) * 128], og[:, c * 128:(c + 1) * 128], C["ident"][:], r=[tag + "_og", "c_ident"], w=kb.pk(0, c * 128, c * 128 + 128))
        kb.cp("act", oT[:], P0[:, :], r=kb.pk(0), w=[tag + "_oT"])
        kb.dma("sp", obr[i], oT[:], sem=tag + "_oT", r=[tag + "_oT"], w=[f"{tag}_obr{i}"])


def host_inputs2(inp, b, names):
    m = {}
    f = np.float32
    for l in range(DEPTH):
        m[f"gla{l}_w2d"] = inp["gla_w_gate2"][l]
        m[f"gla{l}_b2d"] = inp["gla_b_gate2"][l][None, :]
        m[f"gla{l}_gnd"] = np.tile(inp["gla_norm"][l], 4)[None, :]
    m["w_in"] = inp["w_in"]
    for l in range(DEPTH):
        cw = inp["ssd_conv_w"][l].reshape(4, 6, 128).transpose(2, 1, 0)
        cb = inp["ssd_conv_b"][l].reshape(6, 128).T[:, :, None]
        m[f"ssd{l}_cwd"] = np.concatenate([cw, cb], axis=2)
        m[f"ssd{l}_gnd"] = inp["ssd_norm"][l][None, :]
        m[f"ssd{l}_hpd"] = np.concatenate([inp["ssd_dt_bias"][l], inp["ssd_a_log"][l], inp["ssd_d"][l]])[None, :]
    for l in range(DEPTH):
        m[f"gdn{l}_cwd"] = inp["gdn_conv_w"][l].reshape(4, 12, 128).transpose(2, 1, 0)
        m[f"gdn{l}_gnd"] = np.tile(inp["gdn_norm"][l], 4)[None, :]
        m[f"gdn{l}_hpd"] = np.concatenate([inp["gdn_dt_bias"][l], inp["gdn_a_log"][l]])[None, :]
    return m


def phase_gdn(kb, l, hT, hkey_fn, obr):
    import os
    C = kb.C
    tag = f"gdn{l}"
    w_in = kb.w_in
    NW = 2056
    wk = tag + "_w"
    wg = kb.sb(wk, (128, KC, NW), BF16)
    load_w_cast(kb, wg, wk, w_in[l], C_GDN_QKV, NW)
    O_QKV, O_AB, O_G = 0, 1536, 1544
    convw = kb.sb(tag + "_cw", (128, 12, 4))
    kb.dma("sp", convw[:], kb.din(tag + "_cwd", (128, 12, 4)), sem=tag + "_cw", w=[tag + "_cw"])
    gn = kb.sb(tag + "_gn", (128, 512))
    kb.dma("sp", gn[:], kb.din(tag + "_gnd", (1, 512))[0].partition_broadcast(128), sem=tag + "_gn", w=[tag + "_gn"])
    hp = kb.sb(tag + "_hp", (128, 8))
    kb.dma("sp", hp[:], kb.din(tag + "_hpd", (1, 8))[0].partition_broadcast(128), sem=tag + "_hp", w=[tag + "_hp"])
    negA = kb.sb(tag + "_negA", (128, 4))
    kb.act(negA[:], hp[:, 4:8], AF.Exp, r=[tag + "_hp"], w=[tag + "_negA"])
    kb.ts("dve", negA[:], negA[:], -1.0, None, ALU.mult, r=[tag + "_negA"], w=[tag + "_negA"])
    ubuf = kb.sb(tag + "_ubuf", (128, 12, 131))
    kb.S.op("pool", lambda e: e.memset(ubuf[:], 0.0), [], [tag + "_ubuf"])
    cacc = kb.sb(tag + "_cacc", (128, 12, 128))
    ctmp = kb.sb(tag + "_ctmp", (128, 12, 128))
    qkv = kb.sb(tag + "_qkv", (128, 12, 128))
    sq = kb.sb(tag + "_sq", (128, 8, 128))
    rinv = kb.sb(tag + "_rinv", (128, 8, 128))
    qkn = kb.sb(tag + "_qkn", (128, 8, 128))
    gsb = kb.sb(tag + "_gsb", (128, 512))
    sm = kb.sb(tag + "_sm", (128, 40))
    beta, gg, cum, ecum, erem, bec = sm[:, 0:4], sm[:, 4:8], sm[:, 8:12], sm[:, 12:16], sm[:, 16:20], sm[:, 20:24]
    dA, dB, ytmp = sm[:, 24:28], sm[:, 28:32], sm[:, 32:36]
    HB = []
    for s_ in range(4):
        d_ = {}
        for nm in ("otmp", "Gs", "E", "ET", "B0", "B1", "C0", "C1", "PT0", "PT1", "PmT", "ecb", "qdT", "V0", "W0", "kdec", "upre", "wT", "u"):
            d_[nm] = kb.sb(tag + f"_{nm}_{s_}", (128, 128))
        kb.S.op("pool", lambda e, t=d_["u"]: e.memset(t[:], 0.0), [], [tag + f"_u_{s_}"])
        HB.append(d_)
    M = [kb.sb(tag + f"_M{h}", (128, 128)) for h in range(4)]
    for h in range(4):
        kb.S.op("pool", lambda e, h=h: e.memset(M[h][:], 0.0), [], [tag + f"_M{h}"])
    oint = kb.sb(tag + "_oint", (128, 512))
    o_sb = kb.sb(tag + "_o", (128, 512))
    og = kb.sb(tag + "_og", (128, 512))
    oT = kb.sb(tag + "_oT", (128, 512), BF16)
    junk = kb.sb(tag + "_junk", (128, 128), BF16)
    ssq = kb.sb(tag + "_ssq", (128, 4))
    rs = kb.sb(tag + "_rs", (128, 4))
    P0, P1, P2, P3, P4, P5, P6, P7 = kb.P
    ident = C["ident"]
    STOP = float(os.environ.get('GDN_STOP', '9'))
    for i in range(int(os.environ.get('GDN_NT', NT))):
        t0 = i * 128
        hk = hkey_fn(i)
        for c in range(12):
            bank = kb.P[c // 4]
            cc = (c % 4) * 128
            proj_feat(kb, bank[:, cc:cc + 128], wg, wk, O_QKV + c * 128, 128, hT, hk, t0, 128, kb.pk(c // 4, cc, cc + 128))
        proj_tok(kb, P3[:, :], wg, wk, O_G, 512, hT, hk, t0, 128, kb.pk(3))
        proj_tok(kb, P4[:, 0:8], wg, wk, O_AB, 8, hT, hk, t0, 128, kb.pk(4, 0, 128))
        kb.act(gsb[:], P3[:, :], AF.Silu, r=kb.pk(3), w=[tag + "_gsb"])
        for b3 in range(3):
            kb.cp("act", ubuf[:, b3 * 4:(b3 + 1) * 4, 3:131], kb.P[b3][:, :].rearrange("p (c t) -> p c t", c=4),
                  r=kb.pk(b3), w=[tag + "_ubuf"])
        for j in range(4):
            wj = convw[:, :, j:j + 1].to_broadcast([128, 12, 128])
            if j == 0:
                kb.tt("dve", cacc[:], ubuf[:, :, 0:128], wj, ALU.mult, r=[tag + "_ubuf", tag + "_cw"], w=[tag + "_cacc"])
            else:
                kb.tt("pool", ctmp[:], ubuf[:, :, j:j + 128], wj, ALU.mult, r=[tag + "_ubuf", tag + "_cw"], w=[tag + "_ctmp"])
                kb.tt("dve", cacc[:], cacc[:], ctmp[:], ALU.add, r=[tag + "_cacc", tag + "_ctmp"], w=[tag + "_cacc"])
        kb.act(qkv[:], cacc[:], AF.Silu, r=[tag + "_cacc"], w=[tag + "_qkv"])
        kb.cp("pool", ubuf[:, :, 0:3], ubuf[:, :, 128:131], r=[tag + "_ubuf"], w=[tag + "_ubuf"])
        if STOP <= 1:
            continue
        kb.tt("pool", sq[:], qkv[:, 0:8, :], qkv[:, 0:8, :], ALU.mult, r=[tag + "_qkv"], w=[tag + "_sq"])
        for half in range(2):
            kb.mm(kb.P[half][:, :], C["ones"][:], sq[:, half * 4:(half + 1) * 4, :], r=["c_ones", tag + "_sq"], w=kb.pk(half))
            kb.ts("dve", rinv[:, half * 4:(half + 1) * 4, :], kb.P[half][:, :].rearrange("p (c t) -> p c t", c=4),
                  1e-6, None, ALU.add, r=kb.pk(half), w=[tag + "_rinv"])
        kb.act(rinv[:], rinv[:], AF.Sqrt, r=[tag + "_rinv"], w=[tag + "_rinv"])
        kb.S.op("dve", lambda e: e.reciprocal(rinv[:], rinv[:]), [tag + "_rinv"], [tag + "_rinv"])
        kb.stt("dve", qkn[:, 0:4, :], qkv[:, 0:4, :], 128.0 ** -0.5, rinv[:, 0:4, :], ALU.mult, ALU.mult,
               r=[tag + "_qkv", tag + "_rinv"], w=[tag + "_qkn"])
        kb.tt("pool", qkn[:, 4:8, :], qkv[:, 4:8, :], rinv[:, 4:8, :], ALU.mult, r=[tag + "_qkv", tag + "_rinv"], w=[tag + "_qkn"])
        kb.act(beta, P4[:, 4:8], AF.Sigmoid, r=kb.pk(4, 0, 128), w=[tag + "_sm"])
        kb.tt("dve", ytmp, P4[:, 0:4], hp[:, 0:4], ALU.add, r=kb.pk(4, 0, 128) + [tag + "_hp"], w=[tag + "_sm"])
        kb.act(ytmp, ytmp, AF.Exp, r=[tag + "_sm"], w=[tag + "_sm"])
        kb.act(ytmp, ytmp, AF.Ln, bias=1.0, r=[tag + "_sm"], w=[tag + "_sm"])
        kb.tt("dve", gg, ytmp, negA[:], ALU.mult, r=[tag + "_sm", tag + "_negA"], w=[tag + "_sm"])
        kb.mm(P4[:, 8:12], C["tri_incl"][:], gg, r=["c_tri_incl", tag + "_sm"], w=kb.pk(4, 0, 128))
        kb.mm(P4[:, 12:16], C["blk"][:], gg, r=["c_blk", tag + "_sm"], w=kb.pk(4, 0, 128))
        kb.mm(P4[:, 16:20], C["selA"][:], gg, r=["c_selA", tag + "_sm"], w=kb.pk(4, 0, 128))
        kb.mm(P4[:, 20:24], C["selB"][:], gg, r=["c_selB", tag + "_sm"], w=kb.pk(4, 0, 128))
        kb.cp("dve", cum, P4[:, 8:12], r=kb.pk(4, 0, 128), w=[tag + "_sm"])
        kb.act(ecum, P4[:, 8:12], AF.Exp, r=kb.pk(4, 0, 128), w=[tag + "_sm"])
        kb.tt("dve", erem, P4[:, 12:16], cum, ALU.subtract, r=kb.pk(4, 0, 128) + [tag + "_sm"], w=[tag + "_sm"])
        kb.act(erem, erem, AF.Exp, r=[tag + "_sm"], w=[tag + "_sm"])
        kb.act(dA, P4[:, 16:20], AF.Exp, r=kb.pk(4, 0, 128), w=[tag + "_sm"])
        kb.act(dB, P4[:, 20:24], AF.Exp, r=kb.pk(4, 0, 128), w=[tag + "_sm"])
        kb.tt("dve", bec, beta, ecum, ALU.mult, r=[tag + "_sm"], w=[tag + "_sm"])
        if STOP <= 2:
            continue
        def head(h, s_):
            hb = HB[s_]
            XA, XB = kb.P[2 * s_], kb.P[2 * s_ + 1]
            ka, kbk = kb.pk(2 * s_), kb.pk(2 * s_ + 1)
            K = lambda nm: tag + f"_{nm}_{s_}"
            Gs, E, ET, PmT, ecb, qdT = hb["Gs"], hb["E"], hb["ET"], hb["PmT"], hb["ecb"], hb["qdT"]
            V0, W0, kdec, upre, wT, u_sb, otmp = hb["V0"], hb["W0"], hb["kdec"], hb["upre"], hb["wT"], hb["u"], hb["otmp"]
            Bm, Cm, PT = [hb["B0"], hb["B1"]], [hb["C0"], hb["C1"]], [hb["PT0"], hb["PT1"]]
            qT = qkn[:, h, :]
            kT = qkn[:, 4 + h, :]
            vT = qkv[:, 8 + h, :]
            hc = slice(h * 128, (h + 1) * 128)
            Z = lambda i: slice(i * 128, (i + 1) * 128)
            kb.ts("dve", Gs[:], C["tri_incl"][:], gg[:, h:h + 1], None, ALU.mult, r=["c_tri_incl", tag + "_sm"], w=[K("Gs")])
            kb.mm(XA[:, Z(0)], kT, kT, r=[tag + "_qkn"], w=ka)
            kb.mm(XA[:, Z(1)], kT, qT, r=[tag + "_qkn"], w=ka)
            yield
            kb.mm(XA[:, Z(2)], Gs[:], C["tri_strict"][:], r=[K("Gs"), "c_tri_strict"], w=ka)
            kb.mm(XA[:, Z(3)], C["tri_strict"][:], Gs[:], r=[K("Gs"), "c_tri_strict"], w=ka)
            kb.mm(XB[:, Z(0)], C["ones"][:], Gs[:], r=[K("Gs"), "c_ones"], w=kbk)
            yield
            kb.act(E[:], XA[:, Z(2)], AF.Exp, r=ka, w=[K("E")])
            kb.act(ET[:], XA[:, Z(3)], AF.Exp, r=ka, w=[K("ET")])
            kb.act(ecb[:], XB[:, Z(0)], AF.Exp, r=kbk, w=[K("ecb")])
            yield
            kb.tt("dve", E[:], E[:], C["neg_strict"][:], ALU.mult, r=[K("E"), "c_neg_strict"], w=[K("E")])
            kb.tt("dve", ET[:], ET[:], C["tri_incl"][:], ALU.mult, r=[K("ET"), "c_tri_incl"], w=[K("ET")])
            kb.tt("pool", qdT[:], qT, ecb[:], ALU.mult, r=[tag + "_qkn", K("ecb")], w=[K("qdT")])
            yield
            kb.stt("dve", Bm[0][:], XA[:, Z(0)], beta[:, h:h + 1], E[:], ALU.mult, ALU.mult,
                   r=ka + [tag + "_sm", K("E")], w=[K("B0")])
            kb.tt("dve", PmT[:], XA[:, Z(1)], ET[:], ALU.mult, r=ka + [K("ET")], w=[K("PmT")])
            yield
            kb.tr(XB[:, Z(1)], Bm[0][:], ident[:], r=[K("B0"), "c_ident"], w=kbk)
            kb.tr(XB[:, Z(2)], vT, ident[:], r=[tag + "_qkv", "c_ident"], w=kbk)
            kb.tr(XB[:, Z(3)], kT, ident[:], r=[tag + "_qkn", "c_ident"], w=kbk)
            yield
            kb.cp("act", Cm[0][:], XB[:, Z(1)], r=kbk, w=[K("C0")])
            kb.tt("dve", PT[0][:], XB[:, Z(1)], ident[:], ALU.add, r=kbk + ["c_ident"], w=[K("PT0")])
            kb.ts("dve", V0[:], XB[:, Z(2)], beta[:, h:h + 1], None, ALU.mult, r=kbk + [tag + "_sm"], w=[K("V0")])
            kb.act(W0[:], XB[:, Z(3)], AF.Identity, scale=bec[:, h:h + 1], r=kbk + [tag + "_sm"], w=[K("W0")])
            kb.ts("dve", kdec[:], XB[:, Z(3)], erem[:, h:h + 1], None, ALU.mult, r=kbk + [tag + "_sm"], w=[K("kdec")])
            yield
            cur = 0
            kb.mm(XB[:, Z(0)], Cm[0][:], Bm[0][:], r=[K("B0"), K("C0")], w=kbk)
            kb.mm(XB[:, Z(1)], Bm[0][:], Cm[0][:], r=[K("B0"), K("C0")], w=kbk)
            yield
            for j in range(1, 6):
                nxt = 1 - cur
                Bn, Cn, PTk, PTn = K(f"B{nxt}"), K(f"C{nxt}"), K(f"PT{cur}"), K(f"PT{nxt}")
                kb.cp("dve", Bm[nxt][:], XB[:, Z(0)], r=kbk, w=[Bn])
                if j < 5:
                    kb.cp("act", Cm[nxt][:], XB[:, Z(1)], r=kbk, w=[Cn])
                yield
                kb.mm(XB[:, Z(2)], Bm[nxt][:], PT[cur][:], r=[Bn, PTk], w=kbk)
                if j < 5:
                    kb.mm(XB[:, Z(0)], Cm[nxt][:], Bm[nxt][:], r=[Bn, Cn], w=kbk)
                    if j < 4:
                        kb.mm(XB[:, Z(1)], Bm[nxt][:], Cm[nxt][:], r=[Bn, Cn], w=kbk)
                yield
                kb.tt("dve", PT[nxt][:], XB[:, Z(2)], PT[cur][:], ALU.add, r=kbk + [PTk], w=[PTn])
                cur = nxt
            yield
            PTf, PTfk = PT[cur], K(f"PT{cur}")
            kb.mm(XB[:, Z(3)], PTf[:], V0[:], r=[PTfk, K("V0")], w=kbk)
            kb.mm(XA[:, Z(0)], W0[:], PTf[:], r=[PTfk, K("W0")], w=ka)
            yield
            kb.cp("act", upre[:], XB[:, Z(3)], r=kbk, w=[K("upre")])
            kb.cp("dve", wT[:], XA[:, Z(0)], r=ka, w=[K("wT")])
            yield
            Mk = tag + f"_M{h}"
            for ch in range(2):
                tr_ = slice(ch * 64, (ch + 1) * 64)
                dch = dA if ch == 0 else dB
                kb.mm(XA[tr_, Z(1)], wT[:, tr_], M[h][:], r=[K("wT"), Mk], w=ka)
                kb.mm(XA[tr_, Z(3)], qdT[:, tr_], M[h][:], r=[K("qdT"), Mk], w=ka)
                yield
                kb.tt("dve", u_sb[tr_, :], upre[tr_, :], XA[tr_, Z(1)], ALU.subtract, r=[K("upre")] + ka, w=[K("u")])
                kb.cp("act", otmp[tr_, :], XA[tr_, Z(3)], r=ka, w=[K("otmp")])
                yield
                kb.mm(XB[tr_, Z(3)], PmT[:, tr_], u_sb[:, :], r=[K("PmT"), K("u")], w=kbk)
                kb.mm(XA[:, Z(2)], kdec[tr_, :], u_sb[tr_, :], r=[K("kdec"), K("u")], w=ka)
                yield
                kb.stt("dve", M[h][:], M[h][:], dch[:, h:h + 1], XA[:, Z(2)], ALU.mult, ALU.add,
                       r=[Mk, tag + "_sm"] + ka, w=[Mk])
                kb.tt("dve", o_sb[tr_, hc], XB[tr_, Z(3)], otmp[tr_, :], ALU.add, r=kbk + [K("otmp")], w=[tag + "_o"])
                yield

        gens = [head(h, h) for h in range(4)]
        while gens:
            for gen in list(gens):
                try:
                    next(gen)
                except StopIteration:
                    gens.remove(gen)
        if STOP <= 4:
            continue
        for h in range(4):
            kb.act(junk[:], o_sb[:, h * 128:(h + 1) * 128], AF.Square, accum_out=ssq[:, h:h + 1], r=[tag + "_o"],
                   w=[tag + "_junk", tag + "_ssq"])
        rms_rstd(kb, tag, rs, ssq, 4, 128)
        for h in range(4):
            hc = slice(h * 128, (h + 1) * 128)
            kb.stt("dve", og[:, hc], o_sb[:, hc], rs[:, h:h + 1], gn[:, hc], ALU.mult, ALU.mult,
                   r=[tag + "_o", tag + "_rs", tag + "_gn"], w=[tag + "_og"])
        kb.tt("pool", og[:], og[:], gsb[:], ALU.mult, r=[tag + "_og", tag + "_gsb"], w=[tag + "_og"])
        for c in range(4):
            kb.tr(P3[:, c * 128:(c + 1) * 128], og[:, c * 128:(c + 1) * 128], ident[:], r=[tag + "_og", "c_ident"],
                  w=kb.pk(3, c * 128, c * 128 + 128))
        kb.cp("act", oT[:], P3[:, :], r=kb.pk(3), w=[tag + "_oT"])
        kb.dma("sp", obr[i], oT[:], sem=tag + "_oT", r=[tag + "_oT"], w=[f"{tag}_obr{i}"])


def phase_ssd(kb, l, hT, hkey_fn, obr):
    import os
    C = kb.C
    tag = f"ssd{l}"
    w_in = kb.w_in
    NW = 1288
    wk = tag + "_w"
    wg = kb.sb(wk, (128, KC, NW), BF16)
    load_w_cast(kb, wg, wk, w_in[l], C_SSD_Z, NW)
    O_Z, O_XBC, O_DT = 0, 512, 1280
    convw = kb.sb(tag + "_cw", (128, 6, 5))
    kb.dma("sp", convw[:], kb.din(tag + "_cwd", (128, 6, 5)), sem=tag + "_cw", w=[tag + "_cw"])
    gn = kb.sb(tag + "_gn", (128, 512))
    kb.dma("sp", gn[:], kb.din(tag + "_gnd", (1, 512))[0].partition_broadcast(128), sem=tag + "_gn", w=[tag + "_gn"])
    hp = kb.sb(tag + "_hp", (128, 24))
    kb.dma("sp", hp[:], kb.din(tag + "_hpd", (1, 24))[0].partition_broadcast(128), sem=tag + "_hp", w=[tag + "_hp"])
    negA = kb.sb(tag + "_negA", (128, 8))
    kb.act(negA[:], hp[:, 8:16], AF.Exp, r=[tag + "_hp"], w=[tag + "_negA"])
    kb.ts("dve", negA[:], negA[:], -1.0, None, ALU.mult, r=[tag + "_negA"], w=[tag + "_negA"])
    ubuf = kb.sb(tag + "_ubuf", (128, 6, 131))
    kb.S.op("pool", lambda e: e.memset(ubuf[:], 0.0), [], [tag + "_ubuf"])
    cacc = kb.sb(tag + "_cacc", (128, 6, 128))
    ctmp = kb.sb(tag + "_ctmp", (128, 6, 128))
    xbc = kb.sb(tag + "_xbc", (128, 6, 128))
    zs = kb.sb(tag + "_zs", (128, 512))
    sm = kb.sb(tag + "_sm", (128, 72))
    dt, gg, cum, ecum, erem = sm[:, 0:8], sm[:, 8:16], sm[:, 16:24], sm[:, 24:32], sm[:, 32:40]
    dA, dB, ytmp = sm[:, 40:48], sm[:, 48:56], sm[:, 56:64]
    x_tok = kb.sb(tag + "_xtok", (128, 512))
    xdt = kb.sb(tag + "_xdt", (128, 512))
    xdte = kb.sb(tag + "_xdte", (128, 512))
    xd = kb.sb(tag + "_xd", (128, 512))
    B_tok = kb.sb(tag + "_Btok", (128, 128))
    CBT = [kb.sb(tag + f"_CBT{g}", (128, 128)) for g in range(2)]
    GsL = [kb.sb(tag + f"_Gs{i}", (128, 128)) for i in range(4)]
    LTL = [kb.sb(tag + f"_LT{i}", (128, 128)) for i in range(4)]
    Sbd = kb.sb(tag + "_Sbd", (128, 512))
    kb.S.op("pool", lambda e: e.memset(Sbd[:], 0.0), [], [tag + "_Sbd"])
    yint = kb.sb(tag + "_yint", (128, 512))
    y_sb = kb.sb(tag + "_y", (128, 512))
    oT = kb.sb(tag + "_oT", (128, 512), BF16)
    junk = kb.sb(tag + "_junk", (128, 256), BF16)
    ssq = kb.sb(tag + "_ssq", (128, 2))
    rs = kb.sb(tag + "_rs", (128, 2))
    P0, P1, P2, P3, P4, P5, P6, P7 = kb.P
    ident = C["ident"]
    STOP = float(os.environ.get('SSD_STOP', '9'))
    for i in range(int(os.environ.get('SSD_NT', NT))):
        t0 = i * 128
        hk = hkey_fn(i)
        for c in range(6):
            bank = kb.P[c // 4]
            cc = (c % 4) * 128
            proj_feat(kb, bank[:, cc:cc + 128], wg, wk, O_XBC + c * 128, 128, hT, hk, t0, 128, kb.pk(c // 4))
        proj_tok(kb, P2[:, :], wg, wk, O_Z, 512, hT, hk, t0, 128, kb.pk(2))
        proj_tok(kb, P3[:, 0:8], wg, wk, O_DT, 8, hT, hk, t0, 128, kb.pk(3))
        kb.act(zs[:], P2[:, :], AF.Silu, r=kb.pk(2), w=[tag + "_zs"])
        kb.cp("act", ubuf[:, 0:4, 3:131], P0[:, :].rearrange("p (c t) -> p c t", c=4), r=kb.pk(0), w=[tag + "_ubuf"])
        kb.cp("act", ubuf[:, 4:6, 3:131], P1[:, 0:256].rearrange("p (c t) -> p c t", c=2), r=kb.pk(1), w=[tag + "_ubuf"])
        for j in range(4):
            wj = convw[:, :, j:j + 1].to_broadcast([128, 6, 128])
            if j == 0:
                kb.tt("dve", cacc[:], ubuf[:, :, 0:128], wj, ALU.mult, r=[tag + "_ubuf", tag + "_cw"], w=[tag + "_cacc"])
                kb.tt("dve", cacc[:], cacc[:], convw[:, :, 4:5].to_broadcast([128, 6, 128]), ALU.add,
                      r=[tag + "_cacc", tag + "_cw"], w=[tag + "_cacc"])
            else:
                kb.tt("pool", ctmp[:], ubuf[:, :, j:j + 128], wj, ALU.mult, r=[tag + "_ubuf", tag + "_cw"], w=[tag + "_ctmp"])
                kb.tt("dve", cacc[:], cacc[:], ctmp[:], ALU.add, r=[tag + "_cacc", tag + "_ctmp"], w=[tag + "_cacc"])
        kb.act(xbc[:], cacc[:], AF.Silu, r=[tag + "_cacc"], w=[tag + "_xbc"])
        kb.cp("pool", ubuf[:, :, 0:3], ubuf[:, :, 128:131], r=[tag + "_ubuf"], w=[tag + "_ubuf"])
        kb.tt("dve", ytmp, P3[:, 0:8], hp[:, 0:8], ALU.add, r=kb.pk(3) + [tag + "_hp"], w=[tag + "_sm"])
        kb.act(ytmp, ytmp, AF.Exp, r=[tag + "_sm"], w=[tag + "_sm"])
        kb.act(dt, ytmp, AF.Ln, bias=1.0, r=[tag + "_sm"], w=[tag + "_sm"])
        kb.tt("dve", gg, dt, negA[:], ALU.mult, r=[tag + "_sm", tag + "_negA"], w=[tag + "_sm"])
        kb.mm(P3[:, 8:16], C["tri_incl"][:], gg, r=["c_tri_incl", tag + "_sm"], w=kb.pk(3))
        kb.mm(P3[:, 16:24], C["blk"][:], gg, r=["c_blk", tag + "_sm"], w=kb.pk(3))
        kb.mm(P3[:, 24:32], C["selA"][:], gg, r=["c_selA", tag + "_sm"], w=kb.pk(3))
        kb.mm(P3[:, 32:40], C["selB"][:], gg, r=["c_selB", tag + "_sm"], w=kb.pk(3))
        kb.cp("dve", cum, P3[:, 8:16], r=kb.pk(3), w=[tag + "_sm"])
        kb.tt("dve", erem, P3[:, 16:24], cum, ALU.subtract, r=kb.pk(3) + [tag + "_sm"], w=[tag + "_sm"])
        kb.act(dA, P3[:, 24:32], AF.Exp, r=kb.pk(3), w=[tag + "_sm"])
        kb.act(dB, P3[:, 32:40], AF.Exp, r=kb.pk(3), w=[tag + "_sm"])
        kb.act(ecum, cum, AF.Exp, r=[tag + "_sm"], w=[tag + "_sm"])
        kb.act(erem, erem, AF.Exp, r=[tag + "_sm"], w=[tag + "_sm"])
        if STOP <= 1:
            continue
        for c in range(4):
            kb.tr(P4[:, c * 128:(c + 1) * 128], xbc[:, c, :], ident[:], r=[tag + "_xbc", "c_ident"], w=kb.pk(4))
        kb.tr(P5[:, 0:128], xbc[:, 4, :], ident[:], r=[tag + "_xbc", "c_ident"], w=kb.pk(5))
        kb.cp("act", x_tok[:], P4[:, :], r=kb.pk(4), w=[tag + "_xtok"])
        kb.cp("act", B_tok[:], P5[:, 0:128], r=kb.pk(5), w=[tag + "_Btok"])
        v3 = lambda t: t[:, :].rearrange("p (h d) -> p h d", h=8)
        bc8 = lambda a: a.unsqueeze(2).to_broadcast([128, 8, 64])
        kb.tt("dve", v3(xdt), v3(x_tok), bc8(dt), ALU.mult, r=[tag + "_xtok", tag + "_sm"], w=[tag + "_xdt"])
        kb.tt("pool", v3(xd), v3(x_tok), bc8(hp[:, 16:24]), ALU.mult, r=[tag + "_xtok", tag + "_hp"], w=[tag + "_xd"])
        kb.tt("pool", v3(xdte), v3(xdt), bc8(erem), ALU.mult, r=[tag + "_xdt", tag + "_sm"], w=[tag + "_xdte"])
        for g in range(2):
            rows = slice(g * 64, (g + 1) * 64)
            bank = P5 if g == 0 else P6
            kb.mm(bank[:, 128:256], xbc[rows, 4, :], xbc[rows, 5, :], r=[tag + "_xbc"], w=kb.pk(5 + g))
            kb.cp("act", CBT[g][:], bank[:, 128:256], r=kb.pk(5 + g), w=[tag + f"_CBT{g}"])
        if STOP <= 2:
            continue
        def head(h, s_):
            g = h // 4
            Gs, LT = GsL[s_], LTL[s_]
            gk_, lk_ = tag + f"_Gs{s_}", tag + f"_LT{s_}"
            bi = (2, 3, 5, 6)[s_]
            X, kx = kb.P[bi], kb.pk(bi)
            kb.ts("dve", Gs[:], C["tri_incl"][:], gg[:, h:h + 1], None, ALU.mult, r=["c_tri_incl", tag + "_sm"], w=[gk_])
            yield
            kb.mm(X[:, 256:384], C["tri_strict"][:], Gs[:], r=[gk_, "c_tri_strict"], w=kx)
            yield
            kb.act(LT[:], X[:, 256:384], AF.Exp, r=kx, w=[lk_])
            yield
            kb.tt("dve", LT[:], LT[:], C["tri_incl"][:], ALU.mult, r=[lk_, "c_tri_incl"], w=[lk_])
            yield
            kb.tt("dve", LT[:], LT[:], CBT[g][:], ALU.mult, r=[lk_, tag + f"_CBT{g}"], w=[lk_])
            yield
            kb.mm(P7[:, h * 64:(h + 1) * 64], LT[:], xdt[:, h * 64:(h + 1) * 64], r=[lk_, tag + "_xdt"], w=kb.pk(7))
            yield

        for grp in ((0, 1, 2, 3), (4, 5, 6, 7)):
            gens = [head(h, s_) for s_, h in enumerate(grp)]
            while gens:
                for gen in list(gens):
                    try:
                        next(gen)
                    except StopIteration:
                        gens.remove(gen)
        if STOP <= 3:
            continue
        for ch in range(2):
            tr_ = slice(ch * 64, (ch + 1) * 64)
            dch = dA if ch == 0 else dB
            kb.mm(P0[tr_, :], xbc[:, 5, tr_], Sbd[:, :], r=[tag + "_xbc", tag + "_Sbd"], w=kb.pk(0))
            kb.mm(P1[:, :], B_tok[tr_, :], xdte[tr_, :], r=[tag + "_Btok", tag + "_xdte"], w=kb.pk(1))
            for g in range(2):
                rr = slice(g * 64, (g + 1) * 64)
                cc = slice(g * 256, (g + 1) * 256)
                s3 = Sbd[rr, cc].rearrange("p (h d) -> p h d", h=4)
                kb.tt("dve", s3, s3, dch[rr, g * 4:(g + 1) * 4].unsqueeze(2).to_broadcast([64, 4, 64]), ALU.mult,
                      r=[tag + "_Sbd", tag + "_sm"], w=[tag + "_Sbd"])
                kb.tt("dve", Sbd[rr, cc], Sbd[rr, cc], P1[rr, cc], ALU.add, r=[tag + "_Sbd"] + kb.pk(1), w=[tag + "_Sbd"])
        kb.cp("act", yint[:], P0[:, :], r=kb.pk(0), w=[tag + "_yint"])
        kb.tt("pool", v3(yint), v3(yint), bc8(ecum), ALU.mult, r=[tag + "_yint", tag + "_sm"], w=[tag + "_yint"])
        kb.tt("dve", y_sb[:], P7[:, :], yint[:], ALU.add, r=kb.pk(7) + [tag + "_yint"], w=[tag + "_y"])
        kb.tt("pool", y_sb[:], y_sb[:], xd[:], ALU.add, r=[tag + "_y", tag + "_xd"], w=[tag + "_y"])
        kb.tt("pool", y_sb[:], y_sb[:], zs[:], ALU.mult, r=[tag + "_y", tag + "_zs"], w=[tag + "_y"])
        for g in range(2):
            kb.act(junk[:], y_sb[:, g * 256:(g + 1) * 256], AF.Square, accum_out=ssq[:, g:g + 1], r=[tag + "_y"],
                   w=[tag + "_junk", tag + "_ssq"])
        rms_rstd(kb, tag, rs, ssq, 2, 256)
        for g in range(2):
            gc = slice(g * 256, (g + 1) * 256)
            kb.stt("dve", y_sb[:, gc], y_sb[:, gc], rs[:, g:g + 1], gn[:, gc], ALU.mult, ALU.mult,
                   r=[tag + "_y", tag + "_rs", tag + "_gn"], w=[tag + "_y"])
        for c in range(4):
            kb.tr(P4[:, c * 128:(c + 1) * 128], y_sb[:, c * 128:(c + 1) * 128], ident[:], r=[tag + "_y", "c_ident"], w=kb.pk(4))
        kb.cp("act", oT[:], P4[:, :], r=kb.pk(4), w=[tag + "_oT"])
        kb.dma("sp", obr[i], oT[:], sem=tag + "_oT", r=[tag + "_oT"], w=[f"{tag}_obr{i}"])


def phase_merge(kb, l, hT, hkey_fn, obrs, obr_keys, xsrc, xsrc_key, xdst, xdst_key):
    C = kb.C
    tag = f"mrg{l}"
    wm = kb.sb(tag + "_wm", (128, KC, 3072), BF16)
    load_w_cast(kb, wm, tag + "_wm", kb.w_in[l], C_MERGE, 3072)
    wbr = []
    for b, nm in enumerate(("w_branch_gla", "w_branch_gdn", "w_branch_ssd")):
        t = kb.sb(tag + f"_wb{b}", (128, 4, D), BF16)
        load_w_cast(kb, t, tag + f"_wb{b}", kb.dins[nm][l], 0, D, nk=4)
        wbr.append(t)
    wo = kb.sb(tag + "_wo", (128, KC, D), BF16)
    load_w_cast(kb, wo, tag + "_wo", kb.dins["w_out"][l], 0, D)
    bmb = kb.sb(tag + "_bmb", (1, 3072), BF16)
    kb.dma("pool", bmb[:], kb.din(tag + "_bmd", (1, 3072)), sem=tag + "_bmb", w=[tag + "_bmb"])
    gm_row, gm_key = kb.gm_row[l]
    ob = [kb.sb(tag + f"_ob{b}", (128, 512), BF16) for b in range(3)]
    sig = kb.sb(tag + "_sig", (128, 512))
    acc = kb.sb(tag + "_acc", (128, 512))
    tmp = kb.sb(tag + "_tmp", (128, 512))
    mT = kb.sb(tag + "_mT", (128, KC, 128), BF16)
    xt = kb.sb(tag + "_xt", (128, D))
    xo = kb.sb(tag + "_xo", (128, D))
    P = kb.P
    for i in range(NT):
        t0 = i * 128
        hk = hkey_fn(i)
        for b in range(3):
            kb.dma("sp", ob[b][:], obrs[b][i], sem=tag + f"_ob{b}", r=[obr_keys[b](i)], w=[tag + f"_ob{b}"])
        kb.dma("sp", xt[:], xsrc[t0:t0 + 128, :], sem=tag + "_xt", r=[xsrc_key(i)], w=[tag + "_xt"])
        for half in range(2):
            for b in range(3):
                PG, PY = P[(b % 2) * 2], P[(b % 2) * 2 + 1]
                kg, ky = kb.pk((b % 2) * 2), kb.pk((b % 2) * 2 + 1)
                for jj in range(4):
                    j = half * 4 + jj
                    col = b * D + j * 128
                    zone = slice(jj * 128, (jj + 1) * 128)
                    for k in range(KC):
                        kb.mm(PG[:, zone], wm[:, k, col:col + 128], hT[:, k, t0:t0 + 128], start=(k == 0), stop=False,
                              r=[tag + "_wm", hk], w=kg)
                    kb.mm(PG[:, zone], bmb[0:1, col:col + 128], C["ones_bf"][0:1, :], start=False, stop=True,
                          r=[tag + "_bmb", "cb_ones"], w=kg)
                    for c in range(4):
                        kb.mm(PY[:, zone], wbr[b][:, c, j * 128:(j + 1) * 128], ob[b][:, c * 128:(c + 1) * 128],
                              start=(c == 0), stop=(c == 3), r=[tag + f"_wb{b}", tag + f"_ob{b}"], w=ky)
                kb.act(sig[:], PG[:, :], AF.Sigmoid, r=kg, w=[tag + "_sig"])
                if b == 0:
                    kb.tt("dve", acc[:], PY[:, :], sig[:], ALU.mult, r=ky + [tag + "_sig"], w=[tag + "_acc"])
                else:
                    kb.tt("dve", tmp[:], PY[:, :], sig[:], ALU.mult, r=ky + [tag + "_sig"], w=[tag + "_tmp"])
                    kb.tt("pool", acc[:], acc[:], tmp[:], ALU.add, r=[tag + "_acc", tag + "_tmp"], w=[tag + "_acc"])
            kb.cp("act", mT[:, half * 4:(half + 1) * 4, :], acc[:, :].rearrange("p (j t) -> p j t", j=4),
                  r=[tag + "_acc"], w=[tag + "_mT"])
        for half in range(2):
            PO, ko = P[4 + half], kb.pk(4 + half)
            for j in range(KC):
                kb.mm(PO[:, :], mT[:, j, :], wo[:, j, half * 512:(half + 1) * 512], start=(j == 0), stop=(j == KC - 1),
                      r=[tag + "_mT", tag + "_wo"], w=ko)
            hs = slice(half * 512, (half + 1) * 512)
            kb.tt("dve", xo[:, hs], PO[:, :], gm_row[:, hs], ALU.mult, r=ko + [gm_key], w=[tag + "_xo"])
            kb.tt("pool", xo[:, hs], xo[:, hs], xt[:, hs], ALU.add, r=[tag + "_xo", tag + "_xt"], w=[tag + "_xo"])
        kb.dma("sp", xdst[t0:t0 + 128, :], xo[:], sem=tag + "_xo", r=[tag + "_xo"], w=[xdst_key(i)])


def phase_moe(kb, l, xsrc, xsrc_key, xdst, xdst_key, final=None):
    import os
    C = kb.C
    tag = f"moe{l}"
    P = kb.P
    TS = 512
    NSUP = S_TOK // TS
    NE = int(os.environ.get("MOE_NE", 32))
    wr = kb.sb(tag + "_wr", (128, KC, 32), BF16)
    load_w_cast(kb, wr, tag + "_wr", kb.dins["w_router"][l], 0, 32)
    brb = kb.sb(tag + "_brb", (1, 32), BF16)
    kb.dma("pool", brb[:], kb.din(tag + "_brd", (1, 32)), sem=tag + "_brb", w=[tag + "_brb"])
    ones5 = kb.sb(tag + "_ones5", (1, 512), BF16)
    kb.S.op("dve", lambda e: e.memset(ones5[:], 1.0), [], [tag + "_ones5"])
    gf_row, gf_key = kb.gf_row[l]
    nb = norm_bufs(kb, tag + "_n")
    hTs = kb.sb(tag + "_hT", (128, KC, TS), BF16)
    G = kb.sb(tag + "_G", (128, 4, 32))
    lg = kb.sb(tag + "_lg", (128, 32))
    v8 = kb.sb(tag + "_v8", (128, 8))
    msk = kb.sb(tag + "_msk", (128, 32))
    sml = kb.sb(tag + "_sml", (128, 4))
    wgu = [kb.sb(tag + f"_wgu{i}", (128, KC, 2048), BF16) for i in range(2)]
    wd = [kb.sb(tag + f"_wd{i}", (128, KC, D), BF16) for i in range(2)]
    bgu = [kb.sb(tag + f"_bgu{i}", (1, 2048), BF16) for i in range(2)]
    bd = [kb.sb(tag + f"_bd{i}", (1, D), BF16) for i in range(2)]
    yacc = kb.sb(tag + "_yacc", (128, 4, D))
    actT = kb.sb(tag + "_actT", (128, KC, TS), BF16)
    g7 = kb.sb(tag + "_g7", (128, TS))
    sg = kb.sb(tag + "_sg", (128, TS))
    u7 = kb.sb(tag + "_u7", (128, TS))
    xt = kb.sb(tag + "_xt", (128, D))
    xo = kb.sb(tag + "_xo", (128, D))
    if final is not None:
        nfr = kb.sb(tag + "_nfr", (128, D))
        kb.dma("sp", nfr[:], final["nf"].partition_broadcast(128), sem=tag + "_nfr", w=[tag + "_nfr"])
        fj = kb.sb(tag + "_fj", (128, D), BF16)
        fs = kb.sb(tag + "_fs", (128, 2))
    w_gu_d, w_d_d = kb.dins["w_gate_up"][l], kb.dins["w_down"][l]
    b_gu_d, b_d_d = kb.dins["b_gate_up"][l], kb.dins["b_down"][l]

    def load_expert(e, slot):
        srcg = w_gu_d[e].rearrange("(k p) c -> p k c", p=128)
        srcd = w_d_d[e].rearrange("(k p) c -> p k c", p=128)
        for k in range(KC):
            kb.dma("pool", wgu[slot][:, k, :], srcg[:, k, :], sem=tag + f"_wgu{slot}", w=[tag + f"_wgu{slot}"])
        for k in range(KC):
            kb.dma("pool", wd[slot][:, k, :], srcd[:, k, :], sem=tag + f"_wd{slot}", w=[tag + f"_wd{slot}"])
        kb.dma("pool", bgu[slot][:], b_gu_d[e:e + 1, :], sem=tag + f"_bgu{slot}", w=[tag + f"_bgu{slot}"])
        kb.dma("pool", bd[slot][:], b_d_d[e:e + 1, :], sem=tag + f"_bd{slot}", w=[tag + f"_bd{slot}"])

    it = 0
    for T in range(int(os.environ.get("MOE_NSUP", NSUP))):
        for tt in range(4):
            i = T * 4 + tt
            norm_tile(kb, nb, l, "f", xsrc[i * 128:(i + 1) * 128, :], xsrc_key(i), hTs[:, :, tt * 128:(tt + 1) * 128], tag + "_hT")
        for tt in range(4):
            for k in range(KC):
                kb.mm(P[7][:, 0:32], hTs[:, k, tt * 128:(tt + 1) * 128], wr[:, k, :], start=(k == 0), stop=False,
                      r=[tag + "_hT", tag + "_wr"], w=kb.pk(7))
            kb.mm(P[7][:, 0:32], ones5[0:1, 0:128], brb[0:1, :], start=False, stop=True, r=[tag + "_ones5", tag + "_brb"], w=kb.pk(7))
            kb.cp("dve", lg[:], P[7][:, 0:32], r=kb.pk(7), w=[tag + "_lg"])
            kb.S.op("dve", lambda e: e.max(out=v8[:], in_=lg[:]), [tag + "_lg"], [tag + "_v8"])
            kb.ts("dve", msk[:], lg[:], v8[:, 3:4], None, ALU.is_ge, r=[tag + "_lg", tag + "_v8"], w=[tag + "_msk"])
            kb.ts("dve", sml[:, 0:1], v8[:, 0:1], -1.0, None, ALU.mult, r=[tag + "_v8"], w=[tag + "_sml"])
            kb.act(lg[:], lg[:], AF.Exp, bias=sml[:, 0:1], r=[tag + "_lg", tag + "_sml"], w=[tag + "_lg"])
            kb.tt("dve", lg[:], lg[:], msk[:], ALU.mult, r=[tag + "_lg", tag + "_msk"], w=[tag + "_lg"])
            kb.S.op("dve", lambda e: e.reduce_sum(sml[:, 1:2], lg[:], AX.X), [tag + "_lg"], [tag + "_sml"])
            kb.S.op("dve", lambda e: e.reciprocal(sml[:, 1:2], sml[:, 1:2]), [tag + "_sml"], [tag + "_sml"])
            kb.ts("dve", G[:, tt, :], lg[:], sml[:, 1:2], None, ALU.mult, r=[tag + "_lg", tag + "_sml"], w=[tag + "_G"])
        kb.S.op("pool", lambda e: e.memset(yacc[:], 0.0), [], [tag + "_yacc"])
        for e_ in range(NE):
            slot = it % 2
            if it == 0:
                load_expert(e_, slot)
            nxt = (e_ + 1) % NE
            if not (T == NSUP - 1 and e_ == NE - 1):
                load_expert(nxt, 1 - slot)
            it += 1
            wk, dk, bgk, bdk = tag + f"_wgu{slot}", tag + f"_wd{slot}", tag + f"_bgu{slot}", tag + f"_bd{slot}"
            for jc in range(KC):
                pb = (jc % 2) * 2
                PG, PU = P[pb], P[pb + 1]
                for which, PX in ((0, PG), (1, PU)):
                    cols = slice(jc * 256 + which, (jc + 1) * 256, 2)
                    for k in range(KC):
                        kb.mm(PX[:, :], wgu[slot][:, k, cols], hTs[:, k, :], start=(k == 0), stop=False,
                              r=[wk, tag + "_hT"], w=kb.pk(pb + which))
                    kb.mm(PX[:, :], bgu[slot][0:1, cols], ones5[0:1, :], start=False, stop=True,
                          r=[bgk, tag + "_ones5"], w=kb.pk(pb + which))
                kb.ts("dve", g7[:], PG[:, :], SW_LIMIT, None, ALU.min, r=kb.pk(pb), w=[tag + "_g7"])
                kb.ts("dve", u7[:], PU[:, :], -SW_LIMIT, SW_LIMIT, ALU.max, ALU.min, r=kb.pk(pb + 1), w=[tag + "_u7"])
                kb.act(sg[:], g7[:], AF.Sigmoid, scale=SW_ALPHA, r=[tag + "_g7"], w=[tag + "_sg"])
                kb.ts("pool", u7[:], u7[:], 1.0, None, ALU.add, r=[tag + "_u7"], w=[tag + "_u7"])
                kb.tt("pool", u7[:], u7[:], g7[:], ALU.mult, r=[tag + "_u7", tag + "_g7"], w=[tag + "_u7"])
                kb.tt("pool", actT[:, jc, :], u7[:], sg[:], ALU.mult, r=[tag + "_u7", tag + "_sg"], w=[tag + "_actT"])
            for tt in range(4):
                for half in range(2):
                    pi = 4 + (tt % 2) * 2 + half
                    PO = P[pi]
                    hs = slice(half * 512, (half + 1) * 512)
                    for jc in range(KC):
                        kb.mm(PO[:, :], actT[:, jc, tt * 128:(tt + 1) * 128], wd[slot][:, jc, hs], start=(jc == 0), stop=False,
                              r=[tag + "_actT", dk], w=kb.pk(pi))
                    kb.mm(PO[:, :], ones5[0:1, 0:128], bd[slot][0:1, hs], start=False, stop=True,
                          r=[tag + "_ones5", bdk], w=kb.pk(pi))
                    kb.stt("dve", yacc[:, tt, hs], PO[:, :], G[:, tt, e_:e_ + 1], yacc[:, tt, hs], ALU.mult, ALU.add,
                           r=kb.pk(pi) + [tag + "_G", tag + "_yacc"], w=[tag + "_yacc"])
        for tt in range(4):
            i = T * 4 + tt
            kb.dma("sp", xt[:], xsrc[i * 128:(i + 1) * 128, :], sem=tag + "_xt", r=[xsrc_key(i)], w=[tag + "_xt"])
            kb.tt("dve", xo[:], yacc[:, tt, :], gf_row[:], ALU.mult, r=[tag + "_yacc", gf_key], w=[tag + "_xo"])
            kb.tt("pool", xo[:], xo[:], xt[:], ALU.add, r=[tag + "_xo", tag + "_xt"], w=[tag + "_xo"])
            if final is None:
                kb.dma("sp", xdst[i * 128:(i + 1) * 128, :], xo[:], sem=tag + "_xo", r=[tag + "_xo"], w=[xdst_key(i)])
            else:
                kb.act(fj[:], xo[:], AF.Square, accum_out=fs[:, 0:1], r=[tag + "_xo"], w=[tag + "_fj", tag + "_fs"])
                kb.ts("dve", fs[:, 1:2], fs[:, 0:1], 1.0 / D, EPS, ALU.mult, ALU.add, r=[tag + "_fs"], w=[tag + "_fs"])
                kb.act(fs[:, 1:2], fs[:, 1:2], AF.Sqrt, r=[tag + "_fs"], w=[tag + "_fs"])
                kb.S.op("dve", lambda e: e.reciprocal(fs[:, 1:2], fs[:, 1:2]), [tag + "_fs"], [tag + "_fs"])
                kb.stt("dve", xo[:], xo[:], fs[:, 1:2], nfr[:], ALU.mult, ALU.mult, r=[tag + "_xo", tag + "_fs", tag + "_nfr"], w=[tag + "_xo"])
                kb.dma("sp", final["out"][i * 128:(i + 1) * 128, :], xo[:], sem=tag + "_xo", r=[tag + "_xo"], w=[f"out{i}"])


SW_LIMIT = 7.0
SW_ALPHA = 1.702


W_SHAPES = {
    "w_branch_gla": (DEPTH, 512, D), "w_branch_gdn": (DEPTH, 512, D), "w_branch_ssd": (DEPTH, 512, D),
    "w_out": (DEPTH, D, D), "w_router": (DEPTH, D, 32),
    "w_gate_up": (DEPTH, 32, D, 2 * D), "b_gate_up": (DEPTH, 32, 2 * D),
    "w_down": (DEPTH, 32, D, D), "b_down": (DEPTH, 32, D),
}


def build_program(layers=(0, 1), do_mix=True, do_moe=True, dbg=None, same_engine_sync=True):
    kb = KB(same_engine_sync=same_engine_sync)
    phase_consts(kb)
    x = kb.din("x", (S_TOK, D))
    kb.w_in = kb.din("w_in", (DEPTH, D, IN_COLS))
    kb.dins = {nm: kb.din(nm, shp) for nm, shp in W_SHAPES.items()}
    nf = kb.din("norm_final", (D,))
    out = kb.dout("out", (S_TOK, D))
    if do_moe and MOE_SORTED:
        kb.moe_xs = kb.dscr("moe_xs", (MOE_ROWS, D))
        zsrc = kb.din("zeros", (512, D))
        xs_v = kb.moe_xs.rearrange("(n r) c -> n r c", r=512)
        for n in range(MOE_ROWS // 512):
            kb.dma("pool", xs_v[n], zsrc, sem="moe_zt", w=["moe_xs"])
    phase_mod(kb)
    xin, xin_key = x, (lambda i: "x_in")
    last = layers[-1]
    for l in layers:
        xmid = kb.dscr(f"xmid{l}", (S_TOK, D), debug=(dbg == "xmid" and l == layers[0]))
        xmid_key = (lambda i, l=l: f"xmid{l}_{i}")
        if do_mix:
            obr = [kb.dscr(f"obr{l}_{b}", (NT, 128, 512), BF16) for b in range(3)]
            kb.push_scope()
            hT = kb.sb(f"hT{l}", (128, KC, S_TOK), BF16)
            hk = (lambda i, l=l: f"hT{l}_{i}")
            kb.push_scope(); phase_norm(kb, l, "m", xin, xin_key, hT, hk); kb.pop_scope()
            kb.push_scope(); phase_gla(kb, l, hT, hk, obr[0]); kb.pop_scope()
            kb.push_scope(); phase_gdn(kb, l, hT, hk, obr[1]); kb.pop_scope()
            kb.push_scope(); phase_ssd(kb, l, hT, hk, obr[2]); kb.pop_scope()
            keys = [(lambda i, l=l, t=t: f"{t}{l}_obr{i}") for t in ("gla", "gdn", "ssd")]
            kb.push_scope(); phase_merge(kb, l, hT, hk, obr, keys, xin, xin_key, xmid, xmid_key); kb.pop_scope()
            kb.pop_scope()
            msrc, msrc_key = xmid, xmid_key
        else:
            msrc, msrc_key = xin, xin_key
        if dbg == "xmid":
            kb.S.final_wait("sp", [xmid_key(i) for i in range(NT)])
            break
        if do_moe:
            xnext = kb.dscr(f"xres{l}", (S_TOK, D))
            xnext_key = (lambda i, l=l: f"xres{l}_{i}")
            kb.push_scope()
            moe_fn = phase_moe_sorted if MOE_SORTED else phase_moe
            moe_fn(kb, l, msrc, msrc_key, xnext, xnext_key, final=(dict(out=out, nf=nf) if l == last else None))
            kb.pop_scope()
            xin, xin_key = xnext, xnext_key
    kb.S.final_wait("sp", [f"out{i}" for i in range(NT)])
    kb.stats = kb.S.emit(kb.stack)
    return kb


def host_all(inputs, b, names):
    m = {}
    for nm in W_SHAPES:
        m[nm] = inputs[nm]
    m["norm_final"] = inputs["norm_final"]
    m["zeros"] = np.zeros((512, D), np.float32)
    for l in range(DEPTH):
        m[f"mrg{l}_bmd"] = inputs["b_merge"][l][None, :]
        m[f"moe{l}_brd"] = inputs["b_router"][l][None, :]
        m[f"moe{l}_nffn"] = inputs["norm_ffn"][l][None, :]
    base = host_inputs(inputs, b, [n for n in names if n not in m])
    for n in names:
        if n in m:
            base[n] = np.ascontiguousarray(m[n])
    return base


_PROG = {}


def kernel(**inputs):
    inputs = {k: np.asarray(v) for k, v in inputs.items()}
    if "kb" not in _PROG:
        _PROG["kb"] = build_program()
    kb = _PROG["kb"]
    names = list(kb.ins.keys())
    in_maps = [host_all(inputs, b, names) for b in range(8)]
    res = run_bass_kernel_spmd(kb.nc, in_maps, core_ids=list(range(8)))
    return np.stack([np.asarray(r["out"]) for r in res.results], axis=0).astype(np.float32)


MOE_SORTED = True
MOE_BLK = 512
MOE_NB = (S_TOK * 4) // MOE_BLK + 32
MOE_ROWS = MOE_NB * MOE_BLK


def phase_moe_sorted(kb, l, xsrc, xsrc_key, xdst, xdst_key, final=None):
    import os
    C = kb.C
    tag = f"moe{l}"
    P = kb.P
    BLK, NB = MOE_BLK, MOE_NB
    NTB = BLK // 128
    IOA = bass.IndirectOffsetOnAxis
    wr = kb.sb(tag + "_wr", (128, KC, 32), BF16)
    load_w_cast(kb, wr, tag + "_wr", kb.dins["w_router"][l], 0, 32)
    brb = kb.sb(tag + "_brb", (1, 32), BF16)
    kb.dma("pool", brb[:], kb.din(tag + "_brd", (1, 32)), sem=tag + "_brb", w=[tag + "_brb"])
    ones5 = kb.sb(tag + "_ones5", (1, 512), BF16)
    kb.S.op("dve", lambda e: e.memset(ones5[:], 1.0), [], [tag + "_ones5"])
    hTb = [kb.sb(tag + f"_hT{i}", (128, KC, BLK), BF16) for i in range(2)]
    lg_all = kb.sb(tag + "_lg", (128, NT, 32))
    msk_all = kb.sb(tag + "_msk", (128, NT, 32))
    R_all = kb.sb(tag + "_R", (128, NT, 32))
    v8_all = kb.sb(tag + "_v8", (128, NT, 8))
    gk_all = kb.sb(tag + "_gk", (128, NT, 4))
    sml = kb.sb(tag + "_sml", (128, 4))
    cnt = kb.sb(tag + "_cnt", (128, 32))
    kb.S.op("pool", lambda e: e.memset(cnt[:], 0.0), [], [tag + "_cnt"])
    padded = kb.sb(tag + "_padded", (128, 32))
    pstart = kb.sb(tag + "_pstart", (128, 32))
    pend = kb.sb(tag + "_pend", (128, 32))
    pcol = kb.sb(tag + "_pcol", (32, 1))
    pcb = kb.sb(tag + "_pcb", (32, 128))
    dg = kb.sb(tag + "_dg", (32, 32))
    posf = kb.sb(tag + "_posf", (128, NT, 4))
    posi = kb.sb(tag + "_posi", (128, NT, 4), I32)
    eb = kb.sb(tag + "_eb", (128, NB))
    offi = kb.sb(tag + "_offi", (128, NB, KC), I32)
    oh = kb.sb(tag + "_oh", (32, NB), BF16)
    kb.push_scope()
    gf_row, gf_key = kb.gf_row[l]
    scf = kb.sb(tag + "_scf", (128, D))
    shf = kb.sb(tag + "_shf", (128, D))
    nfrow = kb.sb(tag + "_nfrow", (128, D))
    kb.dma("sp", shf[:], kb.modrow_d[l][0], sem=tag + "_shf", r=[f"modrowd{l}"], w=[tag + "_shf"])
    kb.dma("sp", scf[:], kb.modrow_d[l][1], sem=tag + "_scf", r=[f"modrowd{l}"], w=[tag + "_scf"])
    kb.dma("sp", nfrow[:], kb.din(tag + "_nffn", (1, D))[0].partition_broadcast(128), sem=tag + "_nfrow", w=[tag + "_nfrow"])
    kb.stt("dve", scf[:], scf[:], 1.0, nfrow[:], ALU.add, ALU.mult, r=[tag + "_scf", tag + "_nfrow"], w=[tag + "_scf"])
    xs_d = kb.moe_xs
    ys_d = kb.dscr(f"moe_ys{l}", (MOE_ROWS, D))
    h2_d = kb.dscr(f"moe_h2{l}", (S_TOK, D))
    nb = norm_bufs(kb, tag + "_n")
    NH2 = 4
    h2s = [kb.sb(tag + f"_h2_{j}", (128, D)) for j in range(NH2)]
    eq = kb.sb(tag + "_eq", (128, NT, 32))
    cmp3 = kb.sb(tag + "_cmp3", (128, NB, 32))
    offf = kb.sb(tag + "_offf", (128, NB, KC))
    def p1_norm(i):
            hv = hTb[0][:, :, (i % 2) * 128:(i % 2 + 1) * 128]
            norm_tile(kb, nb, l, "f", xsrc[i * 128:(i + 1) * 128, :], xsrc_key(i), hv, tag + f"_hTp{i % 2}")
            b_ = (nb["n"] - 1) % 2
            xn, xnk = nb["xn"][b_], f"{tag}_n_xn{b_}"
            h2, h2k = h2s[i % NH2], tag + f"_h2_{i % NH2}"
            kb.tt("pool", h2[:], xn[:], scf[:], ALU.mult, r=[xnk, tag + "_scf"], w=[h2k])
            kb.tt("pool", h2[:], h2[:], shf[:], ALU.add, r=[h2k, tag + "_shf"], w=[h2k])
            kb.dma("sp", h2_d[i * 128:(i + 1) * 128, :], h2[:], sem=h2k, r=[h2k], w=[f"{tag}_h2d{i}"])

    def p1_route(i):
            for k in range(KC):
                kb.mm(P[7][:, 0:32], hTb[0][:, k, (i % 2) * 128:(i % 2 + 1) * 128], wr[:, k, :], start=(k == 0), stop=False, r=[tag + f"_hTp{i % 2}", tag + "_wr"], w=kb.pk(7))
            kb.mm(P[7][:, 0:32], ones5[0:1, 0:128], brb[0:1, :], start=False, stop=True, r=[tag + "_ones5", tag + "_brb"], w=kb.pk(7))
            lg, v8, msk = lg_all[:, i, :], v8_all[:, i, :], msk_all[:, i, :]
            kb.cp("dve", lg, P[7][:, 0:32], r=kb.pk(7), w=[tag + "_lg"])
            kb.S.op("dve", lambda e, v8=v8, lg=lg: e.max(out=v8, in_=lg), [tag + "_lg"], [tag + "_v8"])
            kb.ts("dve", msk, lg, v8[:, 3:4], None, ALU.is_ge, r=[tag + "_lg", tag + "_v8"], w=[tag + "_msk"])
            kb.ts("dve", sml[:, 0:1], v8[:, 0:1], -1.0, None, ALU.mult, r=[tag + "_v8"], w=[tag + "_sml"])
            kb.act(gk_all[:, i, :], v8[:, 0:4], AF.Exp, bias=sml[:, 0:1], r=[tag + "_v8", tag + "_sml"], w=[tag + "_gk"])
            kb.S.op("dve", lambda e, i=i: e.reduce_sum(sml[:, 1:2], gk_all[:, i, :], AX.X), [tag + "_gk"], [tag + "_sml"])
            kb.S.op("dve", lambda e: e.reciprocal(sml[:, 1:2], sml[:, 1:2]), [tag + "_sml"], [tag + "_sml"])
            kb.ts("dve", gk_all[:, i, :], gk_all[:, i, :], sml[:, 1:2], None, ALU.mult, r=[tag + "_gk", tag + "_sml"], w=[tag + "_gk"])
            kb.mm(P[6][:, 0:32], C["tri_full"][:], msk, r=["c_tri_full", tag + "_msk"], w=kb.pk(6))
            kb.mm(P[6][:, 32:64], C["ones"][:], msk, r=["c_ones", tag + "_msk"], w=kb.pk(6))
            kb.tt("dve", R_all[:, i, :], P[6][:, 0:32], cnt[:], ALU.add, r=kb.pk(6) + [tag + "_cnt"], w=[tag + "_R"])
            kb.tt("dve", cnt[:], P[6][:, 32:64], cnt[:], ALU.add, r=kb.pk(6) + [tag + "_cnt"], w=[tag + "_cnt"])

    for i_ in range(NT + 1):
        if i_ < NT:
            p1_norm(i_)
        if i_ >= 1:
            p1_route(i_ - 1)
    kb.tt("dve", eq[:, 0:8, :].rearrange("p j e -> p e j"), cnt[:].unsqueeze(2).to_broadcast([128, 32, 8]),
          C["blk_thr"][:, 0:8].unsqueeze(1).to_broadcast([128, 32, 8]), ALU.is_gt, r=[tag + "_cnt", "c_blk_thr"], w=[tag + "_eq"])
    kb.S.op("dve", lambda e: e.reduce_sum(padded[:], eq[:, 0:8, :].rearrange("p j e -> p e j"), AX.X), [tag + "_eq"], [tag + "_padded"])
    kb.ts("dve", padded[:], padded[:], float(BLK), None, ALU.mult, r=[tag + "_padded"], w=[tag + "_padded"])
    kb.tt("dve", dg[:], padded[0:32, :], C["ident"][0:32, 0:32], ALU.mult, r=[tag + "_padded", "c_ident"], w=[tag + "_dg"])
    kb.S.op("dve", lambda e: e.reduce_sum(pcol[:], dg[:], AX.X), [tag + "_dg"], [tag + "_pcol"])
    kb.cp("dve", pcb[:], pcol[:, 0:1].to_broadcast([32, 128]), r=[tag + "_pcol"], w=[tag + "_pcb"])
    kb.mm(P[6][:, 0:32], pcb[:], C["tri_full"][0:32, 0:32], r=[tag + "_pcb", "c_tri_full"], w=kb.pk(6))
    kb.cp("dve", pstart[:], P[6][:, 0:32], r=kb.pk(6), w=[tag + "_pstart"])
    kb.tt("dve", pend[:], pstart[:], padded[:], ALU.add, r=[tag + "_pstart", tag + "_padded"], w=[tag + "_pend"])
    kb.tt("dve", R_all[:], R_all[:], pstart[:].unsqueeze(1).to_broadcast([128, NT, 32]), ALU.add,
          r=[tag + "_R", tag + "_pstart"], w=[tag + "_R"])
    for k in range(4):
        kb.tt("dve", eq[:], lg_all[:], v8_all[:, :, k:k + 1].to_broadcast([128, NT, 32]), ALU.is_equal,
              r=[tag + "_lg", tag + "_v8"], w=[tag + "_eq"])
        kb.tt("dve", eq[:], eq[:], R_all[:], ALU.mult, r=[tag + "_eq", tag + "_R"], w=[tag + "_eq"])
        kb.S.op("dve", lambda e, k=k: e.reduce_sum(posf[:, :, k], eq[:], AX.X), [tag + "_eq"], [tag + "_posf"])
    kb.cp("dve", posi[:], posf[:], r=[tag + "_posf"], w=[tag + "_posi"])
    kb.tt("dve", cmp3[:], pend[:].unsqueeze(1).to_broadcast([128, NB, 32]),
          C["blk_thr"][:, 0:NB].unsqueeze(2).to_broadcast([128, NB, 32]), ALU.is_le, r=[tag + "_pend", "c_blk_thr"], w=[tag + "_cmp3"])
    kb.S.op("dve", lambda e: e.reduce_sum(eb[:], cmp3[:], AX.X), [tag + "_cmp3"], [tag + "_eb"])
    kb.ts("dve", eb[:], eb[:], 31.0, None, ALU.min, r=[tag + "_eb"], w=[tag + "_eb"])
    kb.ts("dve", offf[:], eb[:].unsqueeze(2).to_broadcast([128, NB, KC]), float(D), float(l * 32 * D), ALU.mult, ALU.add,
          r=[tag + "_eb"], w=[tag + "_offf"])
    kb.tt("dve", offf[:], offf[:], C["base_pk"][:, 0:KC].unsqueeze(1).to_broadcast([128, NB, KC]), ALU.add,
          r=[tag + "_offf", "c_base_pk"], w=[tag + "_offf"])
    kb.cp("dve", offi[:], offf[:], r=[tag + "_offf"], w=[tag + "_offi"])
    kb.ts("dve", oh[:], eb[0:32, :], C["base_pk"][0:32, 0:1], None, ALU.is_equal, r=[tag + "_eb", "c_base_pk"], w=[tag + "_oh"])
    for i in range(NT):
        h2, h2k = h2s[i % NH2], tag + f"_h2_{i % NH2}"
        kb.dma("sp", h2[:], h2_d[i * 128:(i + 1) * 128, :], sem=h2k, r=[f"{tag}_h2d{i}"], w=[h2k])
        for k in range(4):
            kb.S.dma("pool", lambda e, i=i, k=k: e.indirect_dma_start(
                out=xs_d, out_offset=IOA(ap=posi[:, i, k:k + 1], axis=0), in_=h2s[i % NH2][:], in_offset=None),
                h2k, [h2k, tag + "_posi"], [tag + f"_xsw{i % NH2}"])
    kb.pop_scope()
    kb.push_scope()
    bgu_sb = kb.sb(tag + "_bgu", (32, 2048), BF16)
    bd_sb = kb.sb(tag + "_bd", (32, D), BF16)
    kb.dma("pool", bgu_sb[:], kb.dins["b_gate_up"][l], sem=tag + "_bgu", w=[tag + "_bgu"])
    kb.dma("pool", bd_sb[:], kb.dins["b_down"][l], sem=tag + "_bd", w=[tag + "_bd"])
    wgu = [kb.sb(tag + f"_wgu{i}", (128, KC, 2048), BF16) for i in range(2)]
    wd = [kb.sb(tag + f"_wd{i}", (128, KC, D), BF16) for i in range(2)]
    ohbs = [kb.sb(tag + f"_ohb{i}", (32, BLK), BF16) for i in range(2)]
    xr = [kb.sb(tag + f"_xr{i}", (128, D)) for i in range(2)]
    actT = kb.sb(tag + "_actT", (128, KC, BLK), BF16)
    g7 = kb.sb(tag + "_g7", (128, BLK))
    sg = kb.sb(tag + "_sg", (128, BLK))
    u7 = kb.sb(tag + "_u7", (128, BLK))
    yb = [kb.sb(tag + f"_yb{i}", (128, D)) for i in range(2)]
    wgu_flat = kb.dins["w_gate_up"].rearrange("l e r c -> (l e r) c")
    wd_flat = kb.dins["w_down"].rearrange("l e r c -> (l e r) c")

    def load_block_w(b, slot):
        for k in range(KC):
            kb.S.dma("pool", lambda e, b=b, k=k, slot=slot: e.indirect_dma_start(
                out=wgu[slot][:, k, :], out_offset=None, in_=wgu_flat, in_offset=IOA(ap=offi[:, b, k:k + 1], axis=0)),
                tag + f"_wgu{slot}", [tag + "_offi"], [tag + f"_wgu{slot}"])
        for k in range(KC):
            kb.S.dma("pool", lambda e, b=b, k=k, slot=slot: e.indirect_dma_start(
                out=wd[slot][:, k, :], out_offset=None, in_=wd_flat, in_offset=IOA(ap=offi[:, b, k:k + 1], axis=0)),
                tag + f"_wd{slot}", [tag + "_offi"], [tag + f"_wd{slot}"])

    def prep_block(b):
        hTs, hkey, ohb_ = hTb[b % 2], tag + f"_hT{b % 2}", ohbs[b % 2]
        for tt in range(NTB):
            xb = xr[nxc[0] % 2]
            xbk = tag + f"_xr{nxc[0] % 2}"
            nxc[0] += 1
            r0 = b * BLK + tt * 128
            kb.dma("sp", xb[:], xs_d[r0:r0 + 128, :], sem=xbk, r=["moe_xs"], w=[xbk])
            for half in range(2):
                pT, pk = P[half], kb.pk(half)
                for kk in range(4):
                    k = half * 4 + kk
                    kb.tr(pT[:, kk * 128:(kk + 1) * 128], xb[:, k * 128:(k + 1) * 128], C["ident"][:], r=[xbk, "c_ident"], w=pk)
                kb.cp("act" if half == 0 else "dve", hTs[:, half * 4:(half + 1) * 4, tt * 128:(tt + 1) * 128],
                      pT[:, :].rearrange("p (k t) -> p k t", k=4), r=pk, w=[hkey])
        kb.cp("dve", ohb_[:], oh[:, b:b + 1].to_broadcast([32, BLK]), r=[tag + "_oh"], w=[tag + f"_ohb{b % 2}"])

    NBR = int(os.environ.get("MOE_NBLK", NB))
    load_block_w(0, 0)
    nxc = [0]
    prep_block(0)
    for b in range(NBR):
        slot = b % 2
        if b + 1 < NBR:
            load_block_w(b + 1, 1 - slot)
        wk, dk = tag + f"_wgu{slot}", tag + f"_wd{slot}"
        hTs, hkey, ohb = hTb[slot], tag + f"_hT{slot}", ohbs[slot]
        ohk = tag + f"_ohb{slot}"
        for jc in range(KC):
            pb = 2 + (jc % 2) * 2
            PG, PU = P[pb], P[pb + 1]
            for which, PX in ((0, PG), (1, PU)):
                cols = slice(jc * 256 + which, (jc + 1) * 256, 2)
                for k in range(KC):
                    kb.mm(PX[:, :], wgu[slot][:, k, cols], hTs[:, k, :], start=(k == 0), stop=False,
                          r=[wk, hkey], w=kb.pk(pb + which))
                kb.mm(PX[:, :], bgu_sb[:, cols], ohb[:, :], start=False, stop=True, r=[tag + "_bgu", ohk], w=kb.pk(pb + which))
            kb.ts("dve", g7[:], PG[:, :], SW_LIMIT, None, ALU.min, r=kb.pk(pb), w=[tag + "_g7"])
            kb.ts("dve", u7[:], PU[:, :], -SW_LIMIT, SW_LIMIT, ALU.max, ALU.min, r=kb.pk(pb + 1), w=[tag + "_u7"])
            kb.act(sg[:], g7[:], AF.Sigmoid, scale=SW_ALPHA, r=[tag + "_g7"], w=[tag + "_sg"])
            kb.stt("dve", u7[:], u7[:], 1.0, g7[:], ALU.add, ALU.mult, r=[tag + "_u7", tag + "_g7"], w=[tag + "_u7"])
            kb.tt("dve", actT[:, jc, :], u7[:], sg[:], ALU.mult, r=[tag + "_u7", tag + "_sg"], w=[tag + "_actT"])
        if b + 1 < NBR:
            prep_block(b + 1)
        for tt in range(NTB):
            ybt, ybk = yb[tt % 2], tag + f"_yb{tt % 2}"
            for half in range(2):
                pi = 6 + half
                PO = P[pi]
                hs = slice(half * 512, (half + 1) * 512)
                for jc in range(KC):
                    kb.mm(PO[:, :], actT[:, jc, tt * 128:(tt + 1) * 128], wd[slot][:, jc, hs], start=(jc == 0), stop=False,
                          r=[tag + "_actT", dk], w=kb.pk(pi))
                kb.mm(PO[:, :], ohb[:, 0:128], bd_sb[:, hs], start=False, stop=True, r=[ohk, tag + "_bd"], w=kb.pk(pi))
                kb.cp("act" if half == 0 else "dve", ybt[:, hs], PO[:, :], r=kb.pk(pi), w=[ybk])
            r0 = b * BLK + tt * 128
            kb.dma("sp", ys_d[r0:r0 + 128, :], ybt[:], sem=ybk, r=[ybk], w=[tag + "_ys"])
    kb.pop_scope()
    kb.push_scope()
    NYK = 8
    yk = [kb.sb(tag + f"_yk{i}", (128, D)) for i in range(NYK)]
    acc = kb.sb(tag + "_acc", (128, D))
    xt = kb.sb(tag + "_xt", (128, D))
    if final is not None:
        nfr = kb.sb(tag + "_nfr", (128, D))
        kb.dma("sp", nfr[:], final["nf"].partition_broadcast(128), sem=tag + "_nfr", w=[tag + "_nfr"])
        fj = kb.sb(tag + "_fj", (128, D), BF16)
        fs = kb.sb(tag + "_fs", (128, 2))
    ng = 0
    for i in range(NT):
        kb.dma("sp", xt[:], xsrc[i * 128:(i + 1) * 128, :], sem=tag + "_xt", r=[xsrc_key(i)], w=[tag + "_xt"])
        for k in range(4):
            yt, ytk = yk[ng % NYK], tag + f"_yk{ng % NYK}"
            ng += 1
            kb.S.dma("pool", lambda e, i=i, k=k, yt=yt: e.indirect_dma_start(
                out=yt[:], out_offset=None, in_=ys_d, in_offset=IOA(ap=posi[:, i, k:k + 1], axis=0)),
                ytk, [tag + "_ys", tag + "_posi"], [ytk])
            if k == 0:
                kb.ts("dve", acc[:], yt[:], gk_all[:, i, k:k + 1], None, ALU.mult, r=[ytk, tag + "_gk"], w=[tag + "_acc"])
            else:
                kb.stt("dve", acc[:], yt[:], gk_all[:, i, k:k + 1], acc[:], ALU.mult, ALU.add, r=[ytk, tag + "_gk", tag + "_acc"], w=[tag + "_acc"])
        kb.tt("pool", acc[:], acc[:], gf_row[:], ALU.mult, r=[tag + "_acc", gf_key], w=[tag + "_acc"])
        kb.tt("pool", acc[:], acc[:], xt[:], ALU.add, r=[tag + "_acc", tag + "_xt"], w=[tag + "_acc"])
        if final is None:
            kb.dma("sp", xdst[i * 128:(i + 1) * 128, :], acc[:], sem=tag + "_acc", r=[tag + "_acc"], w=[xdst_key(i)])
        else:
            kb.act(fj[:], acc[:], AF.Square, accum_out=fs[:, 0:1], r=[tag + "_acc"], w=[tag + "_fj", tag + "_fs"])
            kb.ts("dve", fs[:, 1:2], fs[:, 0:1], 1.0 / D, EPS, ALU.mult, ALU.add, r=[tag + "_fs"], w=[tag + "_fs"])
            kb.act(fs[:, 1:2], fs[:, 1:2], AF.Sqrt, r=[tag + "_fs"], w=[tag + "_fs"])
            kb.S.op("dve", lambda e: e.reciprocal(fs[:, 1:2], fs[:, 1:2]), [tag + "_fs"], [tag + "_fs"])
            kb.stt("dve", acc[:], acc[:], fs[:, 1:2], nfr[:], ALU.mult, ALU.mult, r=[tag + "_acc", tag + "_fs", tag + "_nfr"], w=[tag + "_acc"])
            kb.dma("sp", final["out"][i * 128:(i + 1) * 128, :], acc[:], sem=tag + "_acc", r=[tag + "_acc"], w=[f"out{i}"])
    kb.pop_scope()
```
